# Optimizing a Trainium2 kernel written in Bass

```python
import jax
import jax.numpy as jnp
from jax import lax
import numpy as np

D_MODEL = 1024
BATCH = 16
SEQ = 2048
DEPTH = 2

CTX_LEN = 256
GRID_W = 64
ROPE_THETA = 10000.0
NORM_EPS = 1e-6

A_HEADS = 8
A_KV_HEADS = 2
A_HEAD_DIM = 64
A_WIDTH = A_HEADS * A_HEAD_DIM
WINDOW = 128
WBLK = 128
B_WIDTH = 512
CONV_W = 3
C_HEADS = 8
C_NOPE = 64
C_ROPE = 32
C_VDIM = 64
C_KV_LORA = 256
C_Q_LORA = 768
C_WIDTH = C_HEADS * C_VDIM
QBLK = 128
M_HEADS = 4
M_QK = 64
M_V = 128
M_WIDTH = M_HEADS * M_V
M_CHUNK = 64

E_GATE = A_WIDTH + B_WIDTH
O_GATE = C_WIDTH + M_WIDTH
E_COLS = (A_KV_HEADS * A_HEAD_DIM, A_KV_HEADS * A_HEAD_DIM,
          A_WIDTH,
          B_WIDTH, B_WIDTH, B_WIDTH,
          E_GATE)
E_CTX_COLS = sum(E_COLS[:2])
E_IN = sum(E_COLS)
O_COLS = (C_KV_LORA, C_ROPE,
          M_HEADS * M_QK, M_WIDTH, 4 * M_HEADS,
          C_Q_LORA,
          M_HEADS * M_QK, M_WIDTH,
          O_GATE)
O_CTX_COLS = sum(O_COLS[:5])
O_IN = sum(O_COLS)
N_EVEN = (DEPTH + 1) // 2
N_ODD = DEPTH // 2

kernel_name = 'hybrid_swa_conv_mla_mlstm_dit'


def split_cols(y, sizes):
    idx = np.cumsum(sizes)[:-1].tolist()
    return jnp.split(y, idx, axis=-1)


def rms_norm(x, w):
    xf = x.astype(jnp.float32)
    y = xf * lax.rsqrt(jnp.mean(xf * xf, axis=-1, keepdims=True) + NORM_EPS)
    return (y * w.astype(jnp.float32)).astype(x.dtype)


def axial_rope(rows, rot_dim):
    row = jnp.repeat(jnp.arange(rows), GRID_W).astype(jnp.float32)
    col = jnp.tile(jnp.arange(GRID_W), rows).astype(jnp.float32)
    n_freq = rot_dim // 4
    inv = ROPE_THETA ** (-jnp.arange(n_freq, dtype=jnp.float32) / n_freq)
    ang = jnp.concatenate([row[:, None] * inv, col[:, None] * inv], axis=-1)
    return jnp.cos(ang), jnp.sin(ang)


def apply_rope(x, cos, sin):
    x1, x2 = jnp.split(x.astype(jnp.float32), 2, axis=-1)
    cs, sn = cos[:, None, :], sin[:, None, :]
    return jnp.concatenate([x1 * cs - x2 * sn, x1 * sn + x2 * cs], axis=-1).astype(x.dtype)


def window_attention_latent(q, k, v, k_c, v_c, sink):
    bsz, n_lat = q.shape[:2]
    n_ctx = k_c.shape[1]
    nb = n_lat // WBLK
    grp = A_HEADS // A_KV_HEADS
    span = WBLK + 2 * WINDOW
    scale = A_HEAD_DIM ** -0.5
    qb = q.reshape(bsz, nb, WBLK, A_KV_HEADS, grp, A_HEAD_DIM)
    start = jnp.arange(nb) * WBLK
    idx = start[:, None] + jnp.arange(span)[None, :]
    pad = ((0, 0), (WINDOW, WINDOW), (0, 0), (0, 0))
    kb = jnp.pad(k, pad)[:, idx]
    vb = jnp.pad(v, pad)[:, idx]
    qpos = start[:, None] + jnp.arange(WBLK)[None, :]
    kpos = (idx - WINDOW)[:, None, :]
    valid = (jnp.abs(kpos - qpos[:, :, None]) <= WINDOW) & (kpos >= 0) & (kpos < n_lat)
    s_win = jnp.einsum('bnqhgd,bnkhd->bnhgqk', qb, kb).astype(jnp.float32) * scale
    s_win = jnp.where(valid[None, :, None, None], s_win, -jnp.inf)
    s_ctx = jnp.einsum('bnqhgd,bkhd->bnhgqk', qb, k_c).astype(jnp.float32) * scale
    s_sink = jnp.broadcast_to(sink.astype(jnp.float32).reshape(A_KV_HEADS, grp, 1, 1), s_ctx.shape[:-1] + (1,))
    p = jax.nn.softmax(jnp.concatenate([s_sink, s_ctx, s_win], axis=-1), axis=-1)
    p_ctx = p[..., 1:1 + n_ctx].astype(v.dtype)
    p_win = p[..., 1 + n_ctx:].astype(v.dtype)
    o = jnp.einsum('bnhgqk,bkhd->bnqhgd', p_ctx, v_c) + jnp.einsum('bnhgqk,bnkhd->bnqhgd', p_win, vb)
    return o.reshape(bsz, n_lat, A_WIDTH)


def context_attention_sink(q_c, k_c, v_c, sink):
    bsz, n_ctx = q_c.shape[:2]
    grp = A_HEADS // A_KV_HEADS
    qg = q_c.reshape(bsz, n_ctx, A_KV_HEADS, grp, A_HEAD_DIM)
    s = jnp.einsum('bqhgd,bkhd->bhgqk', qg, k_c).astype(jnp.float32) * (A_HEAD_DIM ** -0.5)
    s_sink = jnp.broadcast_to(sink.astype(jnp.float32).reshape(A_KV_HEADS, grp, 1, 1), s.shape[:-1] + (1,))
    p = jax.nn.softmax(jnp.concatenate([s_sink, s], axis=-1), axis=-1)[..., 1:].astype(v_c.dtype)
    return jnp.einsum('bhgqk,bkhd->bqhgd', p, v_c).reshape(bsz, n_ctx, A_WIDTH)


def short_conv(u, w):
    n = u.shape[1]
    half = CONV_W // 2
    up = jnp.pad(u, ((0, 0), (half, half), (0, 0)))
    return sum(up[:, j:j + n] * w[j] for j in range(CONV_W))


def even_mixer(h, h_c, w_in, sink, conv_w, w_out, rope, ctx_out):
    bsz, n_lat = h.shape[:2]
    n_ctx = h_c.shape[1]
    ak, av, aq, bb, bc, bx, z = split_cols(h @ w_in, E_COLS)
    if ctx_out:
        parts_c = split_cols(h_c @ w_in, E_COLS)
    else:
        parts_c = split_cols(h_c @ w_in[:, :E_CTX_COLS], E_COLS[:2])
    k_c = parts_c[0].reshape(bsz, n_ctx, A_KV_HEADS, A_HEAD_DIM)
    v_c = parts_c[1].reshape(bsz, n_ctx, A_KV_HEADS, A_HEAD_DIM)
    q = apply_rope(aq.reshape(bsz, n_lat, A_HEADS, A_HEAD_DIM), *rope)
    k = apply_rope(ak.reshape(bsz, n_lat, A_KV_HEADS, A_HEAD_DIM), *rope)
    v = av.reshape(bsz, n_lat, A_KV_HEADS, A_HEAD_DIM)
    a_out = window_attention_latent(q, k, v, k_c, v_c, sink)
    b_out = bb * short_conv(bc * bx, conv_w)
    out = (jnp.concatenate([a_out, b_out], axis=-1) * jax.nn.silu(z)) @ w_out
    if not ctx_out:
        return out, None
    q_c = parts_c[2].reshape(bsz, n_ctx, A_HEADS, A_HEAD_DIM)
    a_c = context_attention_sink(q_c, k_c, v_c, sink)
    b_c = parts_c[3] * short_conv(parts_c[4] * parts_c[5], conv_w)
    out_c = (jnp.concatenate([a_c, b_c], axis=-1) * jax.nn.silu(parts_c[6])) @ w_out
    return out, out_c


def mla_keys_values(ckv, kr, kv_norm_w, w_ukv, rope):
    bsz, n = ckv.shape[:2]
    kv = (rms_norm(ckv, kv_norm_w) @ w_ukv).reshape(bsz, n, C_HEADS, C_NOPE + C_VDIM)
    k_nope, v = jnp.split(kv, [C_NOPE], axis=-1)
    kr = kr.reshape(bsz, n, 1, C_ROPE)
    if rope is not None:
        kr = apply_rope(kr, *rope)
    k = jnp.concatenate([k_nope, jnp.broadcast_to(kr, (bsz, n, C_HEADS, C_ROPE))], axis=-1)
    return k, v


def mla_queries(cq, q_norm_w, w_uq, rope):
    bsz, n = cq.shape[:2]
    q = (rms_norm(cq, q_norm_w) @ w_uq).reshape(bsz, n, C_HEADS, C_NOPE + C_ROPE)
    q_nope, q_rope = jnp.split(q, [C_NOPE], axis=-1)
    if rope is not None:
        q_rope = apply_rope(q_rope, *rope)
    return jnp.concatenate([q_nope, q_rope], axis=-1)


def dense_attention_blocks(q, k, v):
    bsz, n_q, nh, dqk = q.shape
    nb = n_q // QBLK
    scale = dqk ** -0.5
    qb = jnp.moveaxis(q.reshape(bsz, nb, QBLK, nh, dqk), 1, 0)

    def one_block(qi):
        s = jnp.einsum('bqhd,bkhd->bhqk', qi, k).astype(jnp.float32) * scale
        p = jax.nn.softmax(s, axis=-1).astype(v.dtype)
        return jnp.einsum('bhqk,bkhd->bqhd', p, v)

    o = lax.map(one_block, qb)
    return jnp.moveaxis(o, 0, 1).reshape(bsz, n_q, nh * v.shape[-1])


def mlstm_chunkwise(q, k, v, log_i, log_f, state, need_h):
    bsz, nh, n, dk = k.shape
    dv = v.shape[-1]
    nc = n // M_CHUNK
    kc = k.reshape(bsz, nh, nc, M_CHUNK, dk)
    vc = v.reshape(bsz, nh, nc, M_CHUNK, dv)
    ic = log_i.reshape(bsz, nh, nc, M_CHUNK)
    b = jnp.cumsum(log_f.reshape(bsz, nh, nc, M_CHUNK), axis=-1)
    g = b[..., -1]
    a = g[..., None] - b + ic
    m_loc = jnp.max(a, axis=-1)
    w = jnp.exp(a - m_loc[..., None])
    c_loc = jnp.einsum('bhcs,bhcsv,bhcsk->bhcvk', w, vc, kc)
    n_loc = jnp.einsum('bhcs,bhcsk->bhck', w, kc)

    def step(carry, inp):
        c_prev, n_prev, m_prev = carry
        g_c, m_l, c_l, n_l = inp
        m_new = jnp.maximum(g_c + m_prev, m_l)
        a_prev = jnp.exp(g_c + m_prev - m_new)
        a_loc = jnp.exp(m_l - m_new)
        c_new = a_prev[..., None, None] * c_prev + a_loc[..., None, None] * c_l
        n_new = a_prev[..., None] * n_prev + a_loc[..., None] * n_l
        return (c_new, n_new, m_new), (c_prev, n_prev, m_prev)

    xs = tuple(jnp.moveaxis(t, 2, 0) for t in (g, m_loc, c_loc, n_loc))
    final, entering = lax.scan(step, state, xs)
    if not need_h:
        return None, final
    c_in, n_in, m_in = (jnp.moveaxis(t, 0, 2) for t in entering)
    qc = q.reshape(bsz, nh, nc, M_CHUNK, dk)
    causal = jnp.tril(jnp.ones((M_CHUNK, M_CHUNK), dtype=bool))
    dmat = jnp.where(causal, b[..., :, None] - b[..., None, :] + ic[..., None, :], -jnp.inf)
    inter = b + m_in[..., None]
    m_t = jnp.maximum(inter, jnp.max(dmat, axis=-1))
    sc = jnp.einsum('bhctk,bhcsk->bhcts', qc, kc) * jnp.exp(dmat - m_t[..., None])
    w_inter = jnp.exp(inter - m_t)
    num = w_inter[..., None] * jnp.einsum('bhcvk,bhctk->bhctv', c_in, qc) + jnp.einsum('bhcts,bhcsv->bhctv', sc, vc)
    den = w_inter * jnp.einsum('bhck,bhctk->bhct', n_in, qc) + jnp.sum(sc, axis=-1)
    h = num / jnp.maximum(jnp.abs(den), jnp.exp(-m_t))[..., None]
    return h.reshape(bsz, nh, n, dv), final


def mlstm_direction(q, k, v, gate_pre, i_bias, f_bias, state, need_h, reverse):
    heads_first = lambda a: jnp.moveaxis(a, 2, 1).astype(jnp.float32)
    kh = heads_first(k) * (M_QK ** -0.5)
    vh = heads_first(v)
    log_i = heads_first(gate_pre[:, :, 0] + i_bias)
    log_f = jax.nn.log_sigmoid(heads_first(gate_pre[:, :, 1] + f_bias))
    qh = heads_first(q) if q is not None else None
    if reverse:
        kh, vh, log_i, log_f = (jnp.flip(t, 2) for t in (kh, vh, log_i, log_f))
        qh = jnp.flip(qh, 2) if qh is not None else None
    h, final = mlstm_chunkwise(qh, kh, vh, log_i, log_f, state, need_h)
    if h is not None and reverse:
        h = jnp.flip(h, 2)
    return h, final


def mlstm_readout(h_sum, o_pre, head_norm_w):
    bsz, nh, n, dv = h_sum.shape
    hs = rms_norm(jnp.moveaxis(h_sum, 1, 2), head_norm_w.reshape(M_HEADS, M_V)).reshape(bsz, n, M_WIDTH)
    return (jax.nn.sigmoid(o_pre.astype(jnp.float32)) * hs).astype(o_pre.dtype)


def odd_mixer(h, h_c, w_in, q_norm_w, kv_norm_w, w_uq, w_ukv, i_bias, f_bias, head_norm_w, w_out, rope, ctx_out):
    bsz, n_lat = h.shape[:2]
    n_ctx = h_c.shape[1]
    ckv, kr, mk, mv, mg, cq, mq, mo, z = split_cols(h @ w_in, O_COLS)
    if ctx_out:
        parts_c = split_cols(h_c @ w_in, O_COLS)
    else:
        parts_c = split_cols(h_c @ w_in[:, :O_CTX_COLS], O_COLS[:5])
    ckv_c, kr_c, mk_c, mv_c, mg_c = parts_c[:5]
    k_c, v_c = mla_keys_values(ckv_c, kr_c, kv_norm_w, w_ukv, None)
    k_x, v_x = mla_keys_values(ckv, kr, kv_norm_w, w_ukv, rope)
    q_x = mla_queries(cq, q_norm_w, w_uq, rope)
    c_out = dense_attention_blocks(q_x, jnp.concatenate([k_c, k_x], axis=1), jnp.concatenate([v_c, v_x], axis=1))
    heads = lambda a, n, dim: a.reshape(bsz, n, M_HEADS, dim)
    gates = lambda a, n: a.reshape(bsz, n, 2, 2, M_HEADS)
    qm, km, vm, gm = heads(mq, n_lat, M_QK), heads(mk, n_lat, M_QK), heads(mv, n_lat, M_V), gates(mg, n_lat)
    qm_c = heads(parts_c[6], n_ctx, M_QK) if ctx_out else None
    km_c, vm_c, gm_c = heads(mk_c, n_ctx, M_QK), heads(mv_c, n_ctx, M_V), gates(mg_c, n_ctx)
    zero = (jnp.zeros((bsz, M_HEADS, M_V, M_QK), jnp.float32),
            jnp.zeros((bsz, M_HEADS, M_QK), jnp.float32),
            jnp.zeros((bsz, M_HEADS), jnp.float32))
    h_lat, h_ctx = [], []
    for d in range(2):
        hc_d, st_d = mlstm_direction(qm_c, km_c, vm_c, gm_c[:, :, d], i_bias[d], f_bias[d], zero, ctx_out, d == 1)
        hx_d, _ = mlstm_direction(qm, km, vm, gm[:, :, d], i_bias[d], f_bias[d], st_d, True, d == 1)
        h_lat.append(hx_d)
        h_ctx.append(hc_d)
    m_out = mlstm_readout(h_lat[0] + h_lat[1], mo, head_norm_w)
    out = (jnp.concatenate([c_out, m_out], axis=-1) * jax.nn.silu(z)) @ w_out
    if not ctx_out:
        return out, None
    q_c = mla_queries(parts_c[5], q_norm_w, w_uq, None)
    c_out_c = dense_attention_blocks(q_c, k_c, v_c)
    m_out_c = mlstm_readout(h_ctx[0] + h_ctx[1], parts_c[7], head_norm_w)
    out_c = (jnp.concatenate([c_out_c, m_out_c], axis=-1) * jax.nn.silu(parts_c[8])) @ w_out
    return out, out_c


def setup_inputs(seed: int = 0) -> dict:
    key = jax.random.key(seed)
    ks = jax.random.split(key, 24)
    nrm = lambda k, shape, s: jax.random.normal(k, shape, jnp.float32) * s
    d = D_MODEL
    return {
        'x': nrm(ks[0], (BATCH, SEQ, d), 1.0),
        'c': nrm(ks[1], (BATCH, d), 1.0),
        'ctx': nrm(ks[2], (BATCH, CTX_LEN, d), 1.0),
        'c_ctx': nrm(ks[3], (d,), 1.0),
        'mod_w': nrm(ks[4], (DEPTH, d, 3 * d), 0.5 * d ** -0.5),
        'mod_b': nrm(ks[5], (DEPTH, 3 * d), 0.02),
        'pre_norm_w': 1.0 + nrm(ks[6], (DEPTH, d), 0.05),
        'post_norm_w': 1.0 + nrm(ks[7], (DEPTH, d), 0.05),
        'e_w_in': nrm(ks[8], (N_EVEN, d, E_IN), d ** -0.5),
        'e_sink': nrm(ks[9], (N_EVEN, A_HEADS), 0.5),
        'e_conv_w': nrm(ks[10], (N_EVEN, CONV_W, B_WIDTH), CONV_W ** -0.5),
        'e_w_out': nrm(ks[11], (N_EVEN, E_GATE, d), E_GATE ** -0.5),
        'o_w_in': nrm(ks[12], (N_ODD, d, O_IN), d ** -0.5),
        'o_q_norm_w': 1.0 + nrm(ks[13], (N_ODD, C_Q_LORA), 0.05),
        'o_kv_norm_w': 1.0 + nrm(ks[14], (N_ODD, C_KV_LORA), 0.05),
        'o_w_uq': nrm(ks[15], (N_ODD, C_Q_LORA, C_HEADS * (C_NOPE + C_ROPE)), C_Q_LORA ** -0.5),
        'o_w_ukv': nrm(ks[16], (N_ODD, C_KV_LORA, C_HEADS * (C_NOPE + C_VDIM)), C_KV_LORA ** -0.5),
        'o_i_bias': nrm(ks[17], (N_ODD, 2, M_HEADS), 0.1),
        'o_f_bias': jnp.linspace(3.0, 6.0, M_HEADS, dtype=jnp.float32) + nrm(ks[18], (N_ODD, 2, M_HEADS), 0.1),
        'o_head_norm_w': 1.0 + nrm(ks[19], (N_ODD, M_WIDTH), 0.05),
        'o_w_out': nrm(ks[20], (N_ODD, O_GATE, d), O_GATE ** -0.5),
    }


def reference(x, c, ctx, c_ctx, mod_w, mod_b, pre_norm_w, post_norm_w, e_w_in, e_sink, e_conv_w, e_w_out,
              o_w_in, o_q_norm_w, o_kv_norm_w, o_w_uq, o_w_ukv, o_i_bias, o_f_bias, o_head_norm_w, o_w_out):
    n_lat = x.shape[1]
    ROWS = n_lat // GRID_W
    rope_a = axial_rope(ROWS, A_HEAD_DIM)
    rope_c = axial_rope(ROWS, C_ROPE)
    for layer in range(DEPTH):
        ctx_out = layer < DEPTH - 1
        mod_x = jax.nn.silu(c) @ mod_w[layer] + mod_b[layer]
        mod_c = jax.nn.silu(c_ctx) @ mod_w[layer] + mod_b[layer]
        sh_x, sc_x, g_x = (m[:, None, :] for m in jnp.split(mod_x, 3, axis=-1))
        sh_c, sc_c, g_c = jnp.split(mod_c, 3, axis=-1)
        h = rms_norm(x, pre_norm_w[layer]) * (1.0 + sc_x) + sh_x
        h_c = rms_norm(ctx, pre_norm_w[layer]) * (1.0 + sc_c) + sh_c
        j = layer // 2
        if layer % 2 == 0:
            y, y_c = even_mixer(h, h_c, e_w_in[j], e_sink[j], e_conv_w[j], e_w_out[j], rope_a, ctx_out)
        else:
            y, y_c = odd_mixer(h, h_c, o_w_in[j], o_q_norm_w[j], o_kv_norm_w[j], o_w_uq[j], o_w_ukv[j],
                               o_i_bias[j], o_f_bias[j], o_head_norm_w[j], o_w_out[j], rope_c, ctx_out)
        x = x + g_x * rms_norm(y, post_norm_w[layer])
        if ctx_out:
            ctx = ctx + g_c * rms_norm(y_c, post_norm_w[layer])
    return x
```

```python
import numpy as np
from contextlib import ExitStack
import concourse.bass as bass
import concourse.mybir as mybir
from concourse.bass_utils import run_bass_kernel_spmd

F32 = mybir.dt.float32
BF16 = mybir.dt.bfloat16
AF = mybir.ActivationFunctionType
ALU = mybir.AluOpType

D = 1024
S = 2048
LC = 256
T = S + LC
NT = T // 128
NB = 2
NCORES = 8
EPS = 1e-6
NEG = -30000.0
E_IN = 3328
O_IN = 3632
CHUNKS = [(0, 256), (256, 512), (768, 512), (1280, 512), (1792, 512)]

C_ID, C_ONES, C_TRIF, C_TRIR, C_BLK, C_SEL0, C_SEL1, C_MBF, C_MBR, C_MPREV, C_MNEXT = range(11)


def _nruns(ap):
    pat = [list(x) for x in ap.ap]
    tot = 1
    for st, n in pat:
        tot *= n
    run = 1
    for st, n in sorted(pat, key=lambda x: abs(x[0]) if x[0] != 0 else 1 << 60):
        if st == run:
            run *= n
        elif n > 1:
            break
    return max(1, tot // run)


def _nbytes(ap):
    tot = 1
    for st, n in ap.ap:
        tot *= n
    return tot


def _os_env(k):
    import os
    return os.environ.get(k)


class FW:
    ROT = 30000

    def __init__(self, nc, es):
        self.nc = nc
        self.es = es
        self.eng = {"pe": nc.tensor, "act": nc.scalar, "dve": nc.vector, "pool": nc.gpsimd, "sp": nc.sync}
        self.comp = ["pe", "act", "dve", "pool"]
        self.epoch = {k: 0 for k in self.comp}
        self.sem = {k: es.enter_context(nc.semaphore(f"s_{k}_0")) for k in self.comp}
        self.cnt = {k: 0 for k in self.comp}
        self.seen = {e: {} for e in self.eng}
        self.NQ = int(_os_env("KNQ") or 8)
        self.DBUDGET = int(_os_env("KDB") or 1024)
        self.dq = {}
        for q in ["sp", "act", "pool"]:
            sems = [es.enter_context(nc.semaphore(f"d_{q}{i}")) for i in range(self.NQ)]
            self.dq[q] = {"sems": sems, "n": 0}
        self.lastw = {}
        self.reads = {}
        self.ninst = 0
        self.psum_keys = set()

    def _wait(self, e, tok):
        key, sem, val = tok
        if self.seen[e].get(key, 0) >= val:
            return
        self.eng[e].wait_ge(sem, val)
        self.seen[e][key] = val
        self.ninst += 1

    def _deps(self, e, reads, writes):
        toks = []
        for b in list(reads) + list(writes):
            t = self.lastw.get(b)
            if t is not None:
                toks.append(t)
        for b in writes:
            toks.extend(self.reads.get(b, []))
        for b in reads:
            if b in self.psum_keys:
                toks.extend(t for t in self.reads.get(b, []) if not t[0].startswith(e + "#"))
        for t in toks:
            if e == "pe" and t[0].startswith("pe#"):
                continue
            self._wait(e, t)

    def _commit(self, tok, reads, writes):
        for b in writes:
            self.lastw[b] = tok
            self.reads[b] = []
        for b in reads:
            lst = self.reads.setdefault(b, [])
            lst[:] = [t for t in lst if t[0] != tok[0]]
            lst.append(tok)

    def op(self, e, fn, reads=(), writes=()):
        self._deps(e, reads, writes)
        if self.cnt[e] >= self.ROT:
            self.epoch[e] += 1
            self.sem[e] = self.es.enter_context(self.nc.semaphore(f"s_{e}_{self.epoch[e]}"))
            self.cnt[e] = 0
        ins = fn()
        self.cnt[e] += 1
        ins.then_inc(self.sem[e], 1)
        tok = (f"{e}#{self.epoch[e]}", self.sem[e], self.cnt[e])
        self._commit(tok, reads, writes)
        self.ninst += 1
        return tok

    def dma(self, q, out, in_, reads=(), writes=(), **kw):
        d = self.dq[q]
        i = d["n"] % self.NQ
        rnd = d["n"] // self.NQ
        sem = d["sems"][i]
        key = f"d_{q}{i}"
        if rnd > 0:
            self._wait(q, (key, sem, 16 * rnd))
        nd = max(_nruns(out), _nruns(in_))
        fl = d.setdefault("inflight", [])
        fl[:] = [(t, c) for (t, c) in fl if self.seen[q].get(t[0], 0) < t[2]]
        while fl and sum(c for _, c in fl) + nd > self.DBUDGET:
            t, c = fl.pop(0)
            self._wait(q, t)
        self._deps(q, reads, writes)
        ins = self.eng[q].dma_start(out=out, in_=in_, **kw)
        ins.then_inc(sem, 16)
        d["n"] += 1
        self.ndesc = getattr(self, "ndesc", 0) + nd
        tok = (key, sem, 16 * (rnd + 1))
        fl.append((tok, nd))
        self._commit(tok, reads, writes)
        self.ninst += 1
        return tok

    def all_tokens(self):
        toks = []
        for k in self.comp:
            if self.cnt[k] > 0:
                toks.append((f"{k}#{self.epoch[k]}", self.sem[k], self.cnt[k]))
        for q, d in self.dq.items():
            for i in range(self.NQ):
                n_i = (d["n"] - i + self.NQ - 1) // self.NQ
                if n_i > 0:
                    toks.append((f"d_{q}{i}", d["sems"][i], 16 * n_i))
        return toks

    def barrier(self):
        toks = self.all_tokens()
        for e in self.eng:
            for t in toks:
                if t[0].startswith(e + "#"):
                    continue
                self._wait(e, t)
        self.lastw = {}
        self.reads = {}

    def finish(self):
        for t in self.all_tokens():
            self._wait("sp", t)


class Ring:
    def __init__(self, tiles, name):
        self.tiles = tiles
        self.name = name
        self.i = -1

    def next(self):
        self.i = (self.i + 1) % len(self.tiles)
        return self.tiles[self.i], f"{self.name}{self.i}"


class Builder:
    def __init__(self, debug=False):
        self.debug = debug
        self.nc = bass.Bass("TRN2", target_bir_lowering=False)
        self.dbg_names = []
        import os as _os
        self.QS = _os.environ.get("KQS", "sp")

    def uname(self, name):
        self.uid = getattr(self, "uid", 0) + 1
        return f"{name}_u{self.uid}"

    def sb(self, es, name, shape, dt):
        return es.enter_context(self.nc.sbuf_tensor(self.uname(name), list(shape), dt))

    def ps(self, es, name, shape, dt):
        self.fw.psum_keys.add(name)
        return es.enter_context(self.nc.psum_tensor(self.uname(name), list(shape), dt))

    def ring(self, es, name, shape, dt, n, psum=False):
        f = self.ps if psum else self.sb
        return Ring([f(es, f"{name}{i}", shape, dt) for i in range(n)], name)

    def dram_in(self, name, shape, dt=F32):
        return self.nc.dram_tensor(name, list(shape), dt, kind="ExternalInput").ap()

    def scratch(self, name, shape, dt, dbg=False):
        if dbg and self.debug:
            self.dbg_names.append(name)
            return self.nc.dram_tensor(name, list(shape), dt, kind="ExternalOutput").ap()
        return self.nc.dram_tensor(name, list(shape), dt).ap()

    def build(self, stop_after=None):
        nc = self.nc
        self.stop = stop_after
        import os as _os
        self.cut1 = int(_os.environ.get("KCUT1", "0"))
        self.cut = int(_os.environ.get("KCUT", "0"))
        self.cutb = int(_os.environ.get("KCUTB", "0"))
        self.cutc = int(_os.environ.get("KCUTC", "0"))
        I = {}
        I["xs"] = self.dram_in("xs", [NB, S, D])
        I["ctxs"] = self.dram_in("ctxs", [NB, LC, D])
        I["cvec"] = self.dram_in("cvec", [3, D])
        I["mod_w"] = self.dram_in("mod_w", [2, D, 3 * D])
        I["mod_b"] = self.dram_in("mod_b", [2, 3 * D])
        I["pre_norm_w"] = self.dram_in("pre_norm_w", [2, D])
        I["post_norm_w"] = self.dram_in("post_norm_w", [2, D])
        I["e_w_in"] = self.dram_in("e_w_in", [D, E_IN])
        I["e_sink"] = self.dram_in("e_sink", [8])
        I["e_conv_w"] = self.dram_in("e_conv_w", [3, 512])
        I["e_w_out"] = self.dram_in("e_w_out", [D, D])
        I["o_w_in"] = self.dram_in("o_w_in", [D, O_IN])
        I["o_q_norm_w"] = self.dram_in("o_q_norm_w", [768])
        I["o_kv_norm_w"] = self.dram_in("o_kv_norm_w", [256])
        I["o_w_uq"] = self.dram_in("o_w_uq", [768, 768])
        I["o_w_ukv"] = self.dram_in("o_w_ukv", [256, 1024])
        I["o_i_bias"] = self.dram_in("o_i_bias", [8])
        I["o_f_bias"] = self.dram_in("o_f_bias", [8])
        I["o_head_norm_w"] = self.dram_in("o_head_norm_w", [512])
        I["o_w_out"] = self.dram_in("o_w_out", [D, D])
        I["c128"] = self.dram_in("c128", [11, 128, 128])
        I["ropeA"] = self.dram_in("ropeA", [T, 64])
        I["ropeC"] = self.dram_in("ropeC", [T, 32])
        self.I = I
        out = nc.dram_tensor("out", [NB, S, D], F32, kind="ExternalOutput").ap()
        self.out = out

        R = {}
        R["modrow"] = self.scratch("modrow", [2, 3, 3 * D], F32, dbg=True)
        R["x1c"] = self.scratch("x1c", [NB, T, D], F32, dbg=True)
        R["qT0"] = self.scratch("qT0", [NB, 4, 128, T], BF16)
        R["kT0"] = self.scratch("kT0", [NB, 128, T], BF16)
        R["v0"] = self.scratch("v0", [NB, T, 128], BF16)
        R["za0"] = self.scratch("za0", [NB, T, 512], F32)
        R["bg0T"] = self.scratch("bg0T", [NB, 4, 128, T], BF16)
        R["kT1"] = self.scratch("kT1", [NB, 8, 96, T], BF16)
        R["v1"] = self.scratch("v1", [NB, T, 512], BF16)
        R["qT1"] = self.scratch("qT1", [NB, 8, 96, S], BF16)
        R["mk1"] = self.scratch("mk1", [NB, T, 256], BF16)
        R["mv1"] = self.scratch("mv1", [NB, T, 512], BF16)
        R["mkT1"] = self.scratch("mkT1", [NB, 256, T], BF16)
        R["mqT1"] = self.scratch("mqT1", [NB, 256, S], BF16)
        R["mg1"] = self.scratch("mg1", [NB, T, 16], F32)
        R["mo1"] = self.scratch("mo1", [NB, S, 512], F32)
        R["z1"] = self.scratch("z1", [NB, S, 1024], F32)
        R["cgT1"] = self.scratch("cgT1", [NB, 4, 128, S], BF16)
        self.R = R

        with ExitStack() as es0:
            self.fw = FW(nc, es0)
            fw = self.fw
            self.cf = self.sb(es0, "c128f", [128, 11, 128], F32)
            fw.dma("sp", self.cf[:], I["c128"].rearrange("c p n -> p c n"), writes=["c128f"])
            self.identb = self.sb(es0, "identb", [128, 128], BF16)
            fw.op("dve", lambda: nc.vector.tensor_copy(out=self.identb[:], in_=self.cf[:, C_ID, :]),
                  reads=["c128f"], writes=["identb"])
            self.onesb = self.sb(es0, "onesb", [128, 128], BF16)
            fw.op("dve", lambda: nc.vector.tensor_copy(out=self.onesb[:], in_=self.cf[:, C_ONES, :]),
                  reads=["c128f"], writes=["onesb"])
            self.stage_mod()
            fw.barrier()
            if stop_after != "mod":
                if not _os.environ.get("KSKIP0"):
                    self.layer0()
                    fw.barrier()
                if stop_after in (None, "l1a", "l1b"):
                    self.layer1()
                    fw.barrier()
            fw.finish()
        return nc

    def stage_mod(self):
        nc, fw, I, R = self.nc, self.fw, self.I, self.R
        with ExitStack() as es:
            cv = self.sb(es, "m_cv", [3, D], F32)
            sv = self.sb(es, "m_sv", [3, D], F32)
            svT = self.sb(es, "m_svT", [128, 8, 3], F32)
            ones3 = self.sb(es, "m_ones3", [1, 3], F32)
            pT = self.ps(es, "m_pT", [128, 8, 3], F32)
            wring = self.ring(es, "m_w", [128, 8, 512], F32, 2)
            pring = self.ring(es, "m_p", [3, 512], F32, 2, psum=True)
            mb = self.sb(es, "m_mb", [1, 3 * D], F32)
            mr = self.sb(es, "m_mr", [3, 3 * D], F32)
            fw.dma("sp", cv[:], I["cvec"], writes=["m_cv"])
            fw.op("act", lambda: nc.scalar.activation(out=sv[:], in_=cv[:], func=AF.Silu), reads=["m_cv"], writes=["m_sv"])
            fw.op("pool", lambda: nc.gpsimd.memset(ones3[:], 1.0), writes=["m_ones3"])
            for k in range(8):
                fw.op("pe", lambda: nc.tensor.transpose(out=pT[:, k, :], in_=sv[0:3, k * 128:(k + 1) * 128],
                                                        identity=self.cf[0:3, C_ID, 0:3]),
                      reads=["m_sv", "c128f"], writes=["m_pT"])
            fw.op("dve", lambda: nc.vector.tensor_copy(out=svT[:], in_=pT[:]), reads=["m_pT"], writes=["m_svT"])
            for l in range(2):
                fw.dma("sp", mb[:], I["mod_b"][l:l + 1, :], reads=[], writes=["m_mb"])
                for n in range(6):
                    wt, wk = wring.next()
                    fw.dma("sp", wt[:], I["mod_w"][l, :, n * 512:(n + 1) * 512].rearrange("(k p) n -> p k n", p=128),
                           writes=[wk])
                    pm, pk = pring.next()
                    for k in range(8):
                        fw.op("pe", lambda: nc.tensor.matmul(pm[:], lhsT=svT[:, k, :], rhs=wt[:, k, :],
                                                             start=(k == 0), stop=False),
                              reads=["m_svT", wk], writes=[pk])
                    fw.op("pe", lambda: nc.tensor.matmul(pm[:], lhsT=ones3[0:1, :], rhs=mb[0:1, n * 512:(n + 1) * 512],
                                                         start=False, stop=True),
                          reads=["m_ones3", "m_mb"], writes=[pk])
                    fw.op("dve", lambda: nc.vector.tensor_copy(out=mr[:, n * 512:(n + 1) * 512], in_=pm[:]),
                          reads=[pk], writes=["m_mr"])
                fw.dma("sp", R["modrow"][l], mr[:], reads=["m_mr"], writes=["modrow"])

    def src_rows(self, layer, b, tok0, n):
        if layer == 0:
            if tok0 < LC:
                return self.I["ctxs"][b, tok0:tok0 + n, :]
            return self.I["xs"][b, tok0 - LC:tok0 - LC + n, :]
        return self.R["x1c"][b, tok0:tok0 + n, :]

    def load_weight_bf16(self, es, name, w_ap, ncols, nk=8):
        nc, fw = self.nc, self.fw
        wt = self.sb(es, name, [128, nk, ncols], BF16)
        with ExitStack() as es2:
            stg = self.ring(es2, name + "_stg", [128, ncols], F32, 2)
            for k in range(nk):
                st, sk = stg.next()
                fw.dma("sp", st[:], w_ap[k * 128:(k + 1) * 128, :], writes=[sk])
                e = "pool" if k % 2 == 0 else "dve"
                eng = nc.gpsimd if k % 2 == 0 else nc.vector
                fw.op(e, lambda: eng.tensor_copy(out=wt[:, k, :], in_=st[:]), reads=[sk], writes=[name])
            fw.barrier()
        return wt

    def mod_tiles(self, es, layer, b, want_pre=True, want_post=True):
        nc, fw, I, R = self.nc, self.fw, self.I, self.R
        res = {}
        tmp = self.sb(es, "mt_tmp", [128, D], F32)
        if want_pre:
            fw.dma("sp", tmp[:], I["pre_norm_w"][layer].partition_broadcast(128), writes=["mt_tmp"])
            for nm, v in (("lat", b), ("ctx", 2)):
                sc = self.sb(es, f"mt_sc_{nm}", [128, D], F32)
                sh = self.sb(es, f"mt_sh_{nm}", [128, D], F32)
                fw.dma("sp", sh[:], R["modrow"][layer, v, 0:D].partition_broadcast(128), reads=["modrow"], writes=[f"mt_sh_{nm}"])
                fw.dma("sp", sc[:], R["modrow"][layer, v, D:2 * D].partition_broadcast(128), reads=["modrow"], writes=[f"mt_sc_{nm}"])
                fw.op("dve", lambda: nc.vector.scalar_tensor_tensor(out=sc[:], in0=sc[:], scalar=1.0, in1=tmp[:],
                                                                   op0=ALU.add, op1=ALU.mult),
                      reads=[f"mt_sc_{nm}", "mt_tmp"], writes=[f"mt_sc_{nm}"])
                res[f"sc_{nm}"] = (sc, f"mt_sc_{nm}")
                res[f"sh_{nm}"] = (sh, f"mt_sh_{nm}")
        if want_post:
            tmp2 = self.sb(es, "mt_tmp2", [128, D], F32)
            fw.dma("sp", tmp2[:], I["post_norm_w"][layer].partition_broadcast(128), writes=["mt_tmp2"])
            for nm, v in (("lat", b), ("ctx", 2)):
                g = self.sb(es, f"mt_g_{nm}", [128, D], F32)
                fw.dma("sp", g[:], R["modrow"][layer, v, 2 * D:3 * D].partition_broadcast(128), reads=["modrow"], writes=[f"mt_g_{nm}"])
                fw.op("dve", lambda: nc.vector.tensor_tensor(out=g[:], in0=g[:], in1=tmp2[:], op=ALU.mult),
                      reads=[f"mt_g_{nm}", "mt_tmp2"], writes=[f"mt_g_{nm}"])
                res[f"g_{nm}"] = (g, f"mt_g_{nm}")
        return res

    def norm_tile(self, P, layer, b, t, hT, hk, j, mt):
        nc, fw = self.nc, self.fw
        nm = "ctx" if t < 2 else "lat"
        sc, sck = mt[f"sc_{nm}"]
        sh, shk = mt[f"sh_{nm}"]
        xt, xk = P["xring"].next()
        fw.dma("sp", xt[:], self.src_rows(layer, b, t * 128, 128), reads=["x1c"] if layer == 1 else [], writes=[xk])
        jk, jkk = P["junk"].next()
        st, stk = P["stat"].next()
        fw.op("act", lambda: nc.scalar.activation(out=jk[:], in_=xt[:], func=AF.Square, scale=1.0 / 32.0, accum_out=st[:, 0:1]),
              reads=[xk], writes=[jkk, stk])
        fw.op("act", lambda: nc.scalar.activation(out=st[:, 1:2], in_=st[:, 0:1], func=AF.Sqrt, bias=EPS), reads=[stk], writes=[stk])
        fw.op("dve", lambda: nc.vector.reciprocal(out=st[:, 2:3], in_=st[:, 1:2]), reads=[stk], writes=[stk])
        t1, t1k = P["t1"].next()
        fw.op("dve", lambda: nc.vector.scalar_tensor_tensor(out=t1[:], in0=xt[:], scalar=st[:, 2:3], in1=sc[:],
                                                           op0=ALU.mult, op1=ALU.mult),
              reads=[xk, stk, sck], writes=[t1k])
        hb, hbk = P["hb"].next()
        fw.op("pool", lambda: nc.gpsimd.tensor_tensor(out=hb[:], in0=t1[:], in1=sh[:], op=ALU.add),
              reads=[t1k, shk], writes=[hbk])
        pT, pTk = P["pT"].next()
        for k in range(8):
            fw.op("pe", lambda: nc.tensor.transpose(out=pT[:, k, :], in_=hb[:, k * 128:(k + 1) * 128], identity=self.identb[:]),
                  reads=[hbk, "identb"], writes=[pTk])
        fw.op("act", lambda: nc.scalar.copy(out=hT[:, :, j * 128:(j + 1) * 128], in_=pT[:]), reads=[pTk], writes=[f"{hk}#{j}"])

    def rope_tok(self, P, src, nh, hd, rt, rtk, srck, dst, dstk, dst_off=0, src_off=0):
        nc, fw = self.nc, self.fw
        r = hd // 2
        x1 = src[:, :, src_off:src_off + r]
        x2 = src[:, :, src_off + r:src_off + hd]
        cosb = rt[:, 0:r].unsqueeze(1).to_broadcast([128, nh, r])
        sinb = rt[:, r:hd].unsqueeze(1).to_broadcast([128, nh, r])
        srcks = list(srck) if isinstance(srck, (list, tuple)) else [srck]
        ta, tak = P["ropet"].next()
        tb, tbk = P["ropet"].next()
        a = ta[:, 0:nh, 0:r]
        bb = tb[:, 0:nh, 0:r]
        fw.op("dve", lambda: nc.vector.tensor_tensor(out=a, in0=x1, in1=cosb, op=ALU.mult), reads=srcks + [rtk], writes=[tak])
        fw.op("dve", lambda: nc.vector.tensor_tensor(out=bb, in0=x2, in1=sinb, op=ALU.mult), reads=srcks + [rtk], writes=[tbk])
        fw.op("pool", lambda: nc.gpsimd.tensor_tensor(out=dst[:, :, dst_off:dst_off + r], in0=a, in1=bb, op=ALU.subtract),
              reads=[tak, tbk], writes=[dstk])
        tc_, tck = P["ropet"].next()
        td, tdk = P["ropet"].next()
        c = tc_[:, 0:nh, 0:r]
        d = td[:, 0:nh, 0:r]
        fw.op("dve", lambda: nc.vector.tensor_tensor(out=c, in0=x1, in1=sinb, op=ALU.mult), reads=srcks + [rtk], writes=[tck])
        fw.op("dve", lambda: nc.vector.tensor_tensor(out=d, in0=x2, in1=cosb, op=ALU.mult), reads=srcks + [rtk], writes=[tdk])
        fw.op("pool", lambda: nc.gpsimd.tensor_tensor(out=dst[:, :, dst_off + r:dst_off + hd], in0=c, in1=d, op=ALU.add),
              reads=[tck, tdk], writes=[dstk])

    def post_tile(self, P, py, pyk, g, gk, xin_ap, xin_reads, out_ap, out_key):
        nc, fw = self.nc, self.fw
        xt, xk = P["xres"].next()
        fw.dma("sp", xt[:], xin_ap, reads=xin_reads, writes=[xk])
        jk, jkk = P["junk"].next()
        st, stk = P["stat"].next()
        fw.op("act", lambda: nc.scalar.activation(out=jk[:], in_=py[:], func=AF.Square, scale=1.0 / 32.0, accum_out=st[:, 0:1]),
              reads=[pyk], writes=[jkk, stk])
        fw.op("act", lambda: nc.scalar.activation(out=st[:, 1:2], in_=st[:, 0:1], func=AF.Sqrt, bias=EPS), reads=[stk], writes=[stk])
        fw.op("dve", lambda: nc.vector.reciprocal(out=st[:, 2:3], in_=st[:, 1:2]), reads=[stk], writes=[stk])
        t2, t2k = P["t2"].next()
        fw.op("dve", lambda: nc.vector.scalar_tensor_tensor(out=t2[:], in0=py[:], scalar=st[:, 2:3], in1=g[:],
                                                           op0=ALU.mult, op1=ALU.mult),
              reads=[pyk, stk, gk], writes=[t2k])
        fw.op("pool", lambda: nc.gpsimd.tensor_tensor(out=t2[:], in0=t2[:], in1=xt[:], op=ALU.add),
              reads=[t2k, xk], writes=[t2k])
        fw.dma(self.QS, out_ap, t2[:], reads=[t2k], writes=[out_key])

    def layer0(self):
        nc, fw, I, R = self.nc, self.fw, self.I, self.R
        stop = getattr(self, "stop", None)
        with ExitStack() as esl:
            win = self.load_weight_bf16(esl, "l0_win", I["e_w_in"], E_IN)
            if stop == "l0w":
                return
            for b in range(NB):
                self.l0_stage_a(b, win)
                fw.barrier()
                if stop == "l0a0":
                    return
        if stop == "l0a":
            return
        with ExitStack() as esl:
            wout = self.load_weight_bf16(esl, "l0_wout", I["e_w_out"], D)
            for b in range(NB):
                self.l0_stage_b(b, wout)
                fw.barrier()

    def l0_stage_a(self, b, win):
        nc, fw, I, R = self.nc, self.fw, self.I, self.R
        with ExitStack() as es:
            mt = self.mod_tiles(es, 0, b, want_pre=True, want_post=False)
            P = {}
            P["xring"] = self.ring(es, "a_x", [128, D], F32, 2)
            P["junk"] = self.ring(es, "a_junk", [128, D], BF16, 2)
            P["stat"] = self.ring(es, "a_stat", [128, 4], F32, 4)
            P["t1"] = self.ring(es, "a_t1", [128, D], F32, 2)
            P["hb"] = self.ring(es, "a_hb", [128, D], BF16, 2)
            P["pT"] = self.ring(es, "a_pT", [128, 8, 128], BF16, 2, psum=True)
            P["ropet"] = self.ring(es, "a_ropet", [128, 8, 32], F32, 8)
            hTr = self.ring(es, "a_hT", [128, 8, 512], BF16, 3)
            ptok = self.ring(es, "a_ptok", [128, 512], F32, 2, psum=True)
            pfm = self.ring(es, "a_pfm", [128, 512], F32, 3, psum=True)
            phalo = self.ps(es, "a_phalo", [128, 4, 2, 2], F32)
            halo = self.ring(es, "a_halo", [128, 8, 2], BF16, 2)
            cw = self.sb(es, "a_cw", [128, 3, 4], F32)
            for j_ in range(3):
                fw.dma("sp", cw[:, j_, :], I["e_conv_w"][j_, :].rearrange("(c p) -> p c", p=128), writes=["a_cw"],
                       allow_slow_non_contiguous=True)
            rtr = self.ring(es, "a_rt", [128, 64], F32, 3)
            qbr = self.ring(es, "a_qb", [128, 8, 64], BF16, 2)
            kbr = self.ring(es, "a_kb", [128, 2, 64], BF16, 2)
            qTst = self.ring(es, "a_qTst", [128, 4, 512], BF16, 2)
            kTst = self.ring(es, "a_kTst", [128, 512], BF16, 2)
            vst = self.ring(es, "a_vst", [128, 4, 128], BF16, 2)
            zast = self.ring(es, "a_zast", [128, 512], F32, 3)
            bcs = self.ring(es, "a_bcs", [128, 512], F32, 2)
            uext = self.ring(es, "a_uext", [128, 514], F32, 2)
            acc = self.ring(es, "a_acc", [128, 512], F32, 2)
            szr = self.ring(es, "a_sz", [128, 512], F32, 2)
            hbh = self.ring(es, "a_hbh", [128, 2], F32, 2)
            bgst = self.ring(es, "a_bgst", [128, 512], BF16, 2)

            hts = {}

            def emit_norm(ci):
                tok0, ntok = CHUNKS[ci]
                hT, hk = hTr.next()
                hts[ci] = (hT, hk)
                for j in range(ntok // 128):
                    self.norm_tile(P, 0, b, tok0 // 128 + j, hT, hk, j, mt)

            def emit_proj(ci):
                tok0, ntok = CHUNKS[ci]
                nt = ntok // 128
                hT, hk = hts[ci]
                hkeys = [f"{hk}#{j}" for j in range(nt)]
                hl, hlk = halo.next()
                first = ci in (0, 1)
                last = ci in (0, len(CHUNKS) - 1)
                if first:
                    fw.op("dve", lambda: nc.vector.memset(hl[:, :, 0:1], 0.0), writes=[hlk])
                else:
                    pT_, pk_ = hts[ci - 1]
                    pn = CHUNKS[ci - 1][1]
                    fw.op("dve", lambda: nc.vector.tensor_copy(out=hl[:, :, 0:1], in_=pT_[:, :, pn - 1:pn]),
                          reads=[f"{pk_}#{pn // 128 - 1}"], writes=[hlk])
                if last:
                    fw.op("dve", lambda: nc.vector.memset(hl[:, :, 1:2], 0.0), writes=[hlk])
                else:
                    nT_, nk_ = hts[ci + 1]
                    fw.op("dve", lambda: nc.vector.tensor_copy(out=hl[:, :, 1:2], in_=nT_[:, :, 0:1]),
                          reads=[f"{nk_}#0"], writes=[hlk])
                qs_, qsk = qTst.next()
                ks_, ksk = kTst.next()
                vs_, vsk = vst.next()
                for j in range(nt):
                    t = tok0 // 128 + j
                    hkj = f"{hk}#{j}"
                    rt, rtk = rtr.next()
                    fw.dma("sp", rt[:], I["ropeA"][t * 128:(t + 1) * 128, :], writes=[rtk])
                    pkv, pkvk = ptok.next()
                    for k in range(8):
                        fw.op("pe", lambda: nc.tensor.matmul(pkv[:, 0:256], lhsT=hT[:, k, j * 128:(j + 1) * 128], rhs=win[:, k, 0:256],
                                                             start=(k == 0), stop=(k == 7)), reads=[hkj, "l0_win"], writes=[pkvk])
                    kb_, kbk = kbr.next()
                    self.rope_tok(P, pkv[:, 0:128].rearrange("p (h d) -> p h d", h=2), 2, 64, rt, rtk, pkvk, kb_, kbk)
                    fw.op("act", lambda: nc.scalar.copy(out=vs_[:, j, :], in_=pkv[:, 128:256]), reads=[pkvk], writes=[f"{vsk}#{j}"])
                    pq, pqk = ptok.next()
                    for k in range(8):
                        fw.op("pe", lambda: nc.tensor.matmul(pq[:], lhsT=hT[:, k, j * 128:(j + 1) * 128], rhs=win[:, k, 256:768],
                                                             start=(k == 0), stop=(k == 7)), reads=[hkj, "l0_win"], writes=[pqk])
                    qb_, qbk = qbr.next()
                    self.rope_tok(P, pq[:].rearrange("p (h d) -> p h d", h=8), 8, 64, rt, rtk, pqk, qb_, qbk)
                    pT, pTk = P["pT"].next()
                    qflat = qb_[:].rearrange("p h d -> p (h d)")
                    for c in range(4):
                        fw.op("pe", lambda: nc.tensor.transpose(out=pT[:, c, :], in_=qflat[:, c * 128:(c + 1) * 128], identity=self.identb[:]),
                              reads=[qbk, "identb"], writes=[pTk])
                    fw.op("pe", lambda: nc.tensor.transpose(out=pT[:, 4, :], in_=kb_[:].rearrange("p h d -> p (h d)"), identity=self.identb[:]),
                          reads=[kbk, "identb"], writes=[pTk])
                    fw.op("act", lambda: nc.scalar.copy(out=qs_[:, :, j * 128:(j + 1) * 128], in_=pT[:, 0:4, :]), reads=[pTk], writes=[f"{qsk}#{j}"])
                    fw.op("act", lambda: nc.scalar.copy(out=ks_[:, j * 128:(j + 1) * 128], in_=pT[:, 4, :]), reads=[pTk], writes=[f"{ksk}#{j}"])
                    pz, pzk = ptok.next()
                    for k in range(8):
                        fw.op("pe", lambda: nc.tensor.matmul(pz[:], lhsT=hT[:, k, j * 128:(j + 1) * 128], rhs=win[:, k, 2304:2816],
                                                             start=(k == 0), stop=(k == 7)), reads=[hkj, "l0_win"], writes=[pzk])
                    za_, zak = zast.next()
                    fw.op("act", lambda: nc.scalar.activation(out=za_[:], in_=pz[:], func=AF.Silu), reads=[pzk], writes=[zak])
                    fw.dma(self.QS, R["za0"][b, t * 128:(t + 1) * 128, :], za_[:], reads=[zak], writes=["za0"])
                sl = slice(tok0, tok0 + ntok)
                if getattr(self, "cut", 0) == 2:
                    return
                fw.dma(self.QS, R["qT0"][b].rearrange("j p t -> p j t")[:, :, sl], qs_[:, :, 0:ntok],
                       reads=[f"{qsk}#{j}" for j in range(nt)], writes=["qT0"])
                fw.dma(self.QS, R["kT0"][b][:, sl], ks_[:, 0:ntok], reads=[f"{ksk}#{j}" for j in range(nt)], writes=["kT0"])
                fw.dma(self.QS, R["v0"][b, sl, :].rearrange("(j p) d -> p j d", p=128), vs_[:, 0:nt, :],
                       reads=[f"{vsk}#{j}" for j in range(nt)], writes=["v0"])
                if getattr(self, "cut", 0) == 3:
                    return
                for c in range(4):
                    for g2 in range(2):
                        col0 = (1280 if g2 == 0 else 1792) + c * 128
                        for k in range(8):
                            fw.op("pe", lambda: nc.tensor.matmul(phalo[:, c, g2, :], lhsT=win[:, k, col0:col0 + 128], rhs=hl[:, k, :],
                                                                 start=(k == 0), stop=(k == 7)), reads=[hlk, "l0_win"], writes=["a_phalo"])
                if getattr(self, "cut", 0) == 4:
                    return
                for c in range(4):
                    def fm(col0):
                        pt_, ptk = pfm.next()
                        for k in range(8):
                            fw.op("pe", lambda: nc.tensor.matmul(pt_[:, 0:ntok], lhsT=win[:, k, col0:col0 + 128], rhs=hT[:, k, 0:ntok],
                                                                 start=(k == 0), stop=(k == 7)), reads=hkeys + ["l0_win"], writes=[ptk])
                        return pt_, ptk
                    pbc, pbck = fm(1280 + c * 128)
                    bc_, bck = bcs.next()
                    fw.op("act", lambda: nc.scalar.copy(out=bc_[:, 0:ntok], in_=pbc[:, 0:ntok]), reads=[pbck], writes=[bck])
                    pbx, pbxk = fm(1792 + c * 128)
                    ue, uek = uext.next()
                    fw.op("dve", lambda: nc.vector.tensor_tensor(out=ue[:, 1:ntok + 1], in0=bc_[:, 0:ntok], in1=pbx[:, 0:ntok], op=ALU.mult),
                          reads=[bck, pbxk], writes=[uek])
                    hb_, hbk_ = hbh.next()
                    fw.op("act", lambda: nc.scalar.copy(out=hb_[:], in_=phalo[:, c, 0, :]), reads=["a_phalo"], writes=[hbk_])
                    fw.op("dve", lambda: nc.vector.tensor_tensor(out=ue[:, 0:1], in0=hb_[:, 0:1], in1=phalo[:, c, 1, 0:1], op=ALU.mult),
                          reads=[hbk_, "a_phalo"], writes=[uek])
                    fw.op("dve", lambda: nc.vector.tensor_tensor(out=ue[:, ntok + 1:ntok + 2], in0=hb_[:, 1:2], in1=phalo[:, c, 1, 1:2], op=ALU.mult),
                          reads=[hbk_, "a_phalo"], writes=[uek])
                    ac, ack = acc.next()
                    fw.op("dve", lambda: nc.vector.tensor_scalar(out=ac[:, 0:ntok], in0=ue[:, 0:ntok], scalar1=cw[:, 0, c:c + 1], scalar2=None, op0=ALU.mult),
                          reads=[uek, "a_cw"], writes=[ack])
                    fw.op("dve", lambda: nc.vector.scalar_tensor_tensor(out=ac[:, 0:ntok], in0=ue[:, 1:ntok + 1], scalar=cw[:, 1, c:c + 1], in1=ac[:, 0:ntok],
                                                                       op0=ALU.mult, op1=ALU.add), reads=[uek, "a_cw", ack], writes=[ack])
                    fw.op("dve", lambda: nc.vector.scalar_tensor_tensor(out=ac[:, 0:ntok], in0=ue[:, 2:ntok + 2], scalar=cw[:, 2, c:c + 1], in1=ac[:, 0:ntok],
                                                                       op0=ALU.mult, op1=ALU.add), reads=[uek, "a_cw", ack], writes=[ack])
                    pbb, pbbk = fm(768 + c * 128)
                    fw.op("dve", lambda: nc.vector.tensor_tensor(out=ac[:, 0:ntok], in0=ac[:, 0:ntok], in1=pbb[:, 0:ntok], op=ALU.mult),
                          reads=[ack, pbbk], writes=[ack])
                    pzb, pzbk = fm(2816 + c * 128)
                    sz_, szk = szr.next()
                    fw.op("act", lambda: nc.scalar.activation(out=sz_[:, 0:ntok], in_=pzb[:, 0:ntok], func=AF.Silu), reads=[pzbk], writes=[szk])
                    bg_, bgk = bgst.next()
                    fw.op("pool", lambda: nc.gpsimd.tensor_tensor(out=bg_[:, 0:ntok], in0=ac[:, 0:ntok], in1=sz_[:, 0:ntok], op=ALU.mult),
                          reads=[ack, szk], writes=[bgk])
                    fw.dma(self.QS, R["bg0T"][b, c, :, sl], bg_[:, 0:ntok], reads=[bgk], writes=["bg0T"])

            n = len(CHUNKS)
            emit_norm(0)
            cut = getattr(self, "cut", 0)
            if cut == 1:
                return
            for ci in range(n):
                if ci + 1 < n:
                    emit_norm(ci + 1)
                emit_proj(ci)
                if cut >= 2 and ci >= cut - 5:
                    return

    def l0_stage_b(self, b, wout):
        nc, fw, I, R = self.nc, self.fw, self.I, self.R
        with ExitStack() as es:
            mt = self.mod_tiles(es, 0, b, want_pre=False, want_post=True)
            P = {}
            P["xres"] = self.ring(es, "b_xres", [128, D], F32, 2)
            P["junk"] = self.ring(es, "b_junk", [128, D], BF16, 2)
            P["stat"] = self.ring(es, "b_stat", [128, 4], F32, 4)
            P["t2"] = self.ring(es, "b_t2", [128, D], F32, 2)
            kTp = [[None, None], [None, None]]
            for kv_ in range(2):
                for p_ in range(2):
                    kt_ = self.sb(es, f"b_kTp{kv_}{p_}", [128, T], BF16)
                    key_ = f"b_kTp{kv_}{p_}"
                    fw.op("pool", lambda: nc.gpsimd.memset(kt_[:], 0.0), writes=[key_])
                    fw.dma("sp", kt_[p_ * 64:(p_ + 1) * 64, :], R["kT0"][b, kv_ * 64:(kv_ + 1) * 64, :], reads=["kT0"], writes=[key_])
                    kTp[kv_][p_] = (kt_, key_)
            vall = self.sb(es, "b_vall", [128, NT, 2, 80], BF16)
            fw.op("pool", lambda: nc.gpsimd.memset(vall[:], 1.0), writes=["b_vall"])
            for t in range(NT):
                fw.dma("sp", vall[:, t, :, 0:64], R["v0"][b, t * 128:(t + 1) * 128, :].rearrange("p (h d) -> p h d", h=2),
                       reads=["v0"], writes=["b_vall"])
            mprev = self.sb(es, "b_mprev", [128, 2, 128], BF16)
            mnext = self.sb(es, "b_mnext", [128, 2, 128], BF16)
            fw.op("dve", lambda: nc.vector.tensor_copy(out=mprev[:], in_=self.cf[:, C_MPREV, :].unsqueeze(1).to_broadcast([128, 2, 128])),
                  reads=["c128f"], writes=["b_mprev"])
            fw.op("dve", lambda: nc.vector.tensor_copy(out=mnext[:], in_=self.cf[:, C_MNEXT, :].unsqueeze(1).to_broadcast([128, 2, 128])),
                  reads=["c128f"], writes=["b_mnext"])
            masks = {"P": (mprev, "b_mprev"), "N": (mnext, "b_mnext")}
            snk = self.sb(es, "b_snk", [128, 8], F32)
            esk = self.sb(es, "b_esk", [128, 8], F32)
            fw.dma("sp", snk[:], I["e_sink"].partition_broadcast(128), writes=["b_snk"])
            for kv_ in range(2):
                fw.op("act", lambda: nc.scalar.activation(out=esk[:, kv_ * 4:(kv_ + 1) * 4].rearrange("q (p i) -> q p i", p=2),
                                                          in_=snk[:, kv_ * 4:(kv_ + 1) * 4].rearrange("q (i p) -> q p i", p=2), func=AF.Exp),
                      reads=["b_snk"], writes=["b_esk"])
            psS = self.ring(es, "b_psS", [128, 512], F32, 3, psum=True)
            poR = self.ring(es, "b_po", [128, 4, 80], F32, 2, psum=True)
            pT2 = self.ps(es, "b_pT2", [128, 4, 128], BF16)
            py = self.ps(es, "b_py", [128, D], F32)
            qblk = self.ring(es, "b_qblk", [128, 4, 128], BF16, 2)
            zablk = self.ring(es, "b_zablk", [128, 512], F32, 2)
            bgblk = self.ring(es, "b_bgblk", [128, 4, 128], BF16, 2)
            ptr = self.ring(es, "b_pt", [128, 512], BF16, 6)
            asb = self.ring(es, "b_asb", [128, 8, 64], F32, 2)
            den = self.ring(es, "b_den", [128, 8], F32, 4)
            agr = self.ring(es, "b_ag", [128, 512], BF16, 2)
            agT = self.ring(es, "b_agT", [128, 4, 128], BF16, 2)
            scale = 64 ** -0.5

            loads = {}

            def emit_loads(t):
                q_, qk = qblk.next()
                fw.dma("sp", q_[:], R["qT0"][b].rearrange("j p t -> p j t")[:, :, t * 128:(t + 1) * 128], reads=["qT0"], writes=[qk])
                z_, zk = zablk.next()
                fw.dma("sp", z_[:], R["za0"][b, t * 128:(t + 1) * 128, :], reads=["za0"], writes=[zk])
                g_, gk = bgblk.next()
                fw.dma("sp", g_[:], R["bg0T"][b].rearrange("j p t -> p j t")[:, :, t * 128:(t + 1) * 128], reads=["bg0T"], writes=[gk])
                loads[t] = (q_, qk, z_, zk, g_, gk)

            def emit_block(t):
                q_, qk, z_, zk, g_, gk = loads.pop(t)
                if self.cutc == 5:
                    return
                if t < 2:
                    kbs = [(0, None), (1, None)]
                else:
                    qi = t - 2
                    kbs = [(0, None), (1, None)]
                    if qi > 0:
                        kbs.append((t - 1, "P"))
                    kbs.append((t, None))
                    if qi < 15:
                        kbs.append((t + 1, "N"))
                a_, ak = asb.next()
                for kv in range(2):
                    pts = []
                    for (kb, mk) in kbs:
                        ps_, psk = psS.next()
                        for p in range(2):
                            kt, ktk = kTp[kv][p]
                            fw.op("pe", lambda: nc.tensor.matmul(ps_[:, p * 256:(p + 1) * 256],
                                                                 lhsT=kt[:, kb * 128:(kb + 1) * 128],
                                                                 rhs=q_[:, 2 * kv:2 * kv + 2, :].rearrange("p a b -> p (a b)"),
                                                                 start=True, stop=(mk is None)),
                                  reads=[ktk, qk], writes=[psk])
                            if mk is not None:
                                m_, mkk = masks[mk]
                                fw.op("pe", lambda: nc.tensor.matmul(ps_[:, p * 256:(p + 1) * 256], lhsT=self.identb[:],
                                                                     rhs=m_[:].rearrange("p a b -> p (a b)"), start=False, stop=True),
                                      reads=["identb", mkk], writes=[psk])
                        pt_, ptk = ptr.next()
                        fw.op("act", lambda: nc.scalar.activation(out=pt_[:], in_=ps_[:], func=AF.Exp, scale=scale), reads=[psk], writes=[ptk])
                        pts.append((pt_, ptk, kb))
                        if self.cutc == 1:
                            return
                    if self.cutc == 2:
                        return
                    po, pok = poR.next()
                    for slot in range(4):
                        for n_, (pt_, ptk, kb) in enumerate(pts):
                            fw.op("pe", lambda: nc.tensor.matmul(po[:, slot, 0:65], lhsT=pt_[:, slot * 128:(slot + 1) * 128], rhs=vall[:, kb, kv, 0:65],
                                                                 start=(n_ == 0), stop=(n_ == len(pts) - 1)),
                                  reads=[ptk, "b_vall"], writes=[pok])
                    if self.cutc == 3:
                        return
                    dn, dnk = den.next()
                    fw.op("dve", lambda: nc.vector.tensor_tensor(out=dn[:, 0:4], in0=po[:, :, 64], in1=esk[:, kv * 4:(kv + 1) * 4], op=ALU.add),
                          reads=[pok, "b_esk"], writes=[dnk])
                    fw.op("dve", lambda: nc.vector.reciprocal(out=dn[:, 4:8], in_=dn[:, 0:4]), reads=[dnk], writes=[dnk])
                    for slot in range(4):
                        p, i = slot // 2, slot % 2
                        h = 4 * kv + 2 * i + p
                        fw.op("dve", lambda: nc.vector.tensor_scalar(out=a_[:, h, :], in0=po[:, slot, 0:64], scalar1=dn[:, 4 + slot:5 + slot],
                                                                    scalar2=None, op0=ALU.mult),
                              reads=[pok, dnk], writes=[ak])
                    if self.cutc == 4:
                        return
                if getattr(self, "cutb", 0) == 2:
                    return
                ag, agk = agr.next()
                fw.op("pool", lambda: nc.gpsimd.tensor_tensor(out=ag[:], in0=a_[:].rearrange("p h d -> p (h d)"), in1=z_[:], op=ALU.mult),
                      reads=[ak, zk], writes=[agk])
                for c in range(4):
                    fw.op("pe", lambda: nc.tensor.transpose(out=pT2[:, c, :], in_=ag[:, c * 128:(c + 1) * 128], identity=self.identb[:]),
                          reads=[agk, "identb"], writes=["b_pT2"])
                at, atk = agT.next()
                fw.op("act", lambda: nc.scalar.copy(out=at[:], in_=pT2[:]), reads=["b_pT2"], writes=[atk])
                if getattr(self, "cutb", 0) == 3:
                    return
                for half in range(2):
                    for k in range(8):
                        src, srk = (at, atk) if k < 4 else (g_, gk)
                        fw.op("pe", lambda: nc.tensor.matmul(py[:, half * 512:(half + 1) * 512], lhsT=src[:, k % 4, :],
                                                             rhs=wout[:, k, half * 512:(half + 1) * 512], start=(k == 0), stop=(k == 7)),
                              reads=[srk, "l0_wout"], writes=["b_py"])
                nm = "ctx" if t < 2 else "lat"
                g, gkk = mt[f"g_{nm}"]
                self.post_tile(P, py, "b_py", g, gkk, self.src_rows(0, b, t * 128, 128), [],
                               R["x1c"][b, t * 128:(t + 1) * 128, :], "x1c")

            cutb = getattr(self, "cutb", 0)
            if cutb == 1:
                return
            emit_loads(0)
            for t in range(NT):
                if t + 1 < NT:
                    emit_loads(t + 1)
                emit_block(t)
                if cutb in (2, 3, 4) or (cutb >= 5 and t >= cutb - 3):
                    return

    def layer1(self):
        nc, fw, I, R = self.nc, self.fw, self.I, self.R
        stop = getattr(self, "stop", None)
        with ExitStack() as esl:
            win = self.load_weight_bf16(esl, "l1_win", I["o_w_in"], O_IN)
            nq = self.sb(esl, "l1_nq", [128, 6], F32)
            nkv = self.sb(esl, "l1_nkv", [128, 2], F32)
            fw.dma("sp", nq[:], I["o_q_norm_w"].rearrange("(k p) -> p k", p=128), writes=["l1_nq"], allow_slow_non_contiguous=True)
            fw.dma("sp", nkv[:], I["o_kv_norm_w"].rearrange("(k p) -> p k", p=128), writes=["l1_nkv"], allow_slow_non_contiguous=True)
            wuq = self.sb(esl, "l1_wuq", [128, 6, 768], BF16)
            wukvk = self.sb(esl, "l1_wukvk", [128, 2, 512], BF16)
            wukvv = self.sb(esl, "l1_wukvv", [128, 2, 512], BF16)
            with ExitStack() as es2:
                stg = self.ring(es2, "l1_stg", [128, 1024], F32, 2)
                for c in range(6):
                    st, sk = stg.next()
                    fw.dma("sp", st[:, 0:768], I["o_w_uq"][c * 128:(c + 1) * 128, :], writes=[sk])
                    fw.op("dve", lambda: nc.vector.tensor_scalar(out=wuq[:, c, :], in0=st[:, 0:768], scalar1=nq[:, c:c + 1], scalar2=None, op0=ALU.mult),
                          reads=[sk, "l1_nq"], writes=["l1_wuq"])
                for c in range(2):
                    st, sk = stg.next()
                    fw.dma("sp", st[:], I["o_w_ukv"][c * 128:(c + 1) * 128, :], writes=[sk])
                    sv = st[:].rearrange("p (h two d) -> p h two d", two=2, d=64)
                    fw.op("dve", lambda: nc.vector.tensor_scalar(out=wukvk[:, c, :].rearrange("p (h d) -> p h d", d=64), in0=sv[:, :, 0, :],
                                                                scalar1=nkv[:, c:c + 1], scalar2=None, op0=ALU.mult),
                          reads=[sk, "l1_nkv"], writes=["l1_wukvk"])
                    fw.op("dve", lambda: nc.vector.tensor_scalar(out=wukvv[:, c, :].rearrange("p (h d) -> p h d", d=64), in0=sv[:, :, 1, :],
                                                                scalar1=nkv[:, c:c + 1], scalar2=None, op0=ALU.mult),
                          reads=[sk, "l1_nkv"], writes=["l1_wukvv"])
                fw.barrier()
            if self.cut1 == 1:
                return
            for b in range(NB):
                if _os_env("KONLYB") and int(_os_env("KONLYB")) != b:
                    continue
                self.l1_stage_a(0 if _os_env("KSAMEB") else b, win, wuq, wukvk, wukvv)
                fw.barrier()
                if self.cut1 >= 2:
                    return
        if stop == "l1a":
            return
        for b in range(NB):
            self.l1_stage_b(b)
            fw.barrier()
        if stop == "l1b":
            return
        with ExitStack() as esl:
            wout = self.load_weight_bf16(esl, "l1_wout", I["o_w_out"], D)
            for b in range(NB):
                with ExitStack() as esb:
                    hsum = self.sb(esb, "c_hsum", [128, 16, 4, 128], F32)
                    self.l1_stage_c1(b, hsum)
                    fw.barrier()
                    self.l1_stage_c2(b, hsum, wout)
                    fw.barrier()

    def l1_stage_a(self, b, win, wuq, wukvk, wukvv):
        nc, fw, I, R = self.nc, self.fw, self.I, self.R
        CH = [(0, 256)] + [(256 + 256 * i, 256) for i in range(8)]
        onesf = self.onesb[:]
        with ExitStack() as es:
            mt = self.mod_tiles(es, 1, b, want_pre=True, want_post=False)
            P = {}
            P["xring"] = self.ring(es, "a_x", [128, D], F32, 2)
            P["junk"] = self.ring(es, "a_junk", [128, D], BF16, 2)
            P["stat"] = self.ring(es, "a_stat", [128, 4], F32, 4)
            P["t1"] = self.ring(es, "a_t1", [128, D], F32, 2)
            P["hb"] = self.ring(es, "a_hb", [128, D], BF16, 2)
            P["pT"] = self.ring(es, "a_pT", [128, 8, 128], BF16, 2, psum=True)
            P["ropet"] = self.ring(es, "a_ropet", [128, 8, 32], F32, 8)
            hTr = self.ring(es, "a_hT", [128, 8, 256], BF16, 2)
            pfm = self.ring(es, "a_pfm", [128, 256], F32, 2, psum=True)
            ptok = self.ring(es, "a_ptok", [128, 512], F32, 2, psum=True)
            prep = self.ps(es, "a_prep", [128, 256], F32)
            pcol = self.ps(es, "a_pcol", [128, 8], F32)
            ckvTr = self.ring(es, "a_ckvT", [128, 2, 256], BF16, 2)
            sqkr = self.ring(es, "a_sqk", [128, 2, 256], BF16, 2)
            rrepr = self.ring(es, "a_rrep", [128, 2, 256], F32, 2)
            rcolr = self.ring(es, "a_rcol", [128, 8], F32, 2)
            knstr = self.ring(es, "a_knst", [128, 4, 256], BF16, 2)
            vstr = self.ring(es, "a_vst", [128, 512], BF16, 2)
            rtr = self.ring(es, "a_rt", [128, 32], F32, 3)
            krfr = self.ring(es, "a_krf", [128, 1, 32], BF16, 2)
            krstr = self.ring(es, "a_krst", [32, 256], BF16, 2)
            mkstr = self.ring(es, "a_mkst", [128, 256], BF16, 2)
            mkTstr = self.ring(es, "a_mkTst", [128, 256], BF16, 2)
            mvstr = self.ring(es, "a_mvst", [128, 512], BF16, 2)
            mgstr = self.ring(es, "a_mgst", [128, 16], F32, 2)
            cqTr = self.ring(es, "a_cqT", [128, 6, 256], BF16, 2)
            sqqr = self.ring(es, "a_sqq", [128, 6, 256], BF16, 1)
            qfr = self.ring(es, "a_qf", [128, 8, 96], F32, 2)
            qbfr = self.ring(es, "a_qbf", [128, 8, 96], BF16, 2)
            qstr = self.ring(es, "a_qst", [128, 8, 256], BF16, 2)
            mqTstr = self.ring(es, "a_mqTst", [128, 256], BF16, 2)
            mostr = self.ring(es, "a_most", [128, 512], F32, 2)
            zstr = self.ring(es, "a_zst", [128, 512], F32, 3)

            hts = {}

            def emit_norm(ci):
                tok0, ntok = CH[ci]
                hT, hk = hTr.next()
                hts[ci] = (hT, hk)
                for j in range(ntok // 128):
                    self.norm_tile(P, 1, b, tok0 // 128 + j, hT, hk, j, mt)

            def mm_fm(hT, hkeys, col0, ntok):
                pf, pfk = pfm.next()
                for k in range(8):
                    fw.op("pe", lambda: nc.tensor.matmul(pf[:, 0:ntok], lhsT=win[:, k, col0:col0 + 128], rhs=hT[:, k, 0:ntok],
                                                         start=(k == 0), stop=(k == 7)), reads=hkeys + ["l1_win"], writes=[pfk])
                return pf, pfk

            def mm_tok(hT, hkj, j, col0, ncol, off=0, pt_=None, ptk=None):
                if pt_ is None:
                    pt_, ptk = ptok.next()
                for k in range(8):
                    fw.op("pe", lambda: nc.tensor.matmul(pt_[:, off:off + ncol], lhsT=hT[:, k, j * 128:(j + 1) * 128], rhs=win[:, k, col0:col0 + ncol],
                                                         start=(k == 0), stop=(k == 7)), reads=[hkj, "l1_win"], writes=[ptk])
                return pt_, ptk

            def emit_proj(ci):
                tok0, ntok = CH[ci]
                nt = ntok // 128
                is_lat = tok0 >= LC
                hT, hk = hts[ci]
                hkeys = [f"{hk}#{j}" for j in range(nt)]
                sl = slice(tok0, tok0 + ntok)
                ckvT, ckvk = ckvTr.next()
                sqk, sqkk = sqkr.next()
                for c in range(2):
                    pf, pfk = mm_fm(hT, hkeys, c * 128, ntok)
                    fw.op("act", lambda: nc.scalar.copy(out=ckvT[:, c, 0:ntok], in_=pf[:, 0:ntok]), reads=[pfk], writes=[f"{ckvk}#{c}"])
                    fw.op("act", lambda: nc.scalar.activation(out=sqk[:, c, 0:ntok], in_=pf[:, 0:ntok], func=AF.Square), reads=[pfk], writes=[f"{sqkk}#{c}"])
                ckeys = [f"{ckvk}#0", f"{ckvk}#1"]
                skeys = [f"{sqkk}#0", f"{sqkk}#1"]
                for c in range(2):
                    fw.op("pe", lambda: nc.tensor.matmul(prep[:, 0:ntok], lhsT=onesf, rhs=sqk[:, c, 0:ntok], start=(c == 0), stop=(c == 1)),
                          reads=skeys + ["c128f"], writes=["a_prep"])
                rrep, rrepk = rrepr.next()
                fw.op("act", lambda: nc.scalar.activation(out=rrep[:, 0, 0:ntok], in_=prep[:, 0:ntok], func=AF.Sqrt, bias=EPS, scale=1.0 / 256.0),
                      reads=["a_prep"], writes=[rrepk])
                fw.op("dve", lambda: nc.vector.reciprocal(out=rrep[:, 1, 0:ntok], in_=rrep[:, 0, 0:ntok]), reads=[rrepk], writes=[rrepk])
                rcol, rcolk = rcolr.next()
                for j in range(nt):
                    for c in range(2):
                        fw.op("pe", lambda: nc.tensor.matmul(pcol[:, j:j + 1], lhsT=sqk[:, c, j * 128:(j + 1) * 128], rhs=onesf[:, 0:1],
                                                             start=(c == 0), stop=(c == 1)), reads=skeys + ["c128f"], writes=["a_pcol"])
                fw.op("act", lambda: nc.scalar.activation(out=rcol[:, 2:4], in_=pcol[:, 0:2], func=AF.Sqrt, bias=EPS, scale=1.0 / 256.0),
                      reads=["a_pcol"], writes=[rcolk])
                fw.op("dve", lambda: nc.vector.reciprocal(out=rcol[:, 0:2], in_=rcol[:, 2:4]), reads=[rcolk], writes=[rcolk])
                if self.cut1 == 2:
                    return
                knst, knk = knstr.next()
                for pair in range(4):
                    pf, pfk = pfm.next()
                    for c in range(2):
                        fw.op("pe", lambda: nc.tensor.matmul(pf[:, 0:ntok], lhsT=wukvk[:, c, pair * 128:(pair + 1) * 128], rhs=ckvT[:, c, 0:ntok],
                                                             start=(c == 0), stop=(c == 1)), reads=ckeys + ["l1_wukvk"], writes=[pfk])
                    fw.op("dve", lambda: nc.vector.tensor_tensor(out=knst[:, pair, 0:ntok], in0=pf[:, 0:ntok], in1=rrep[:, 1, 0:ntok], op=ALU.mult),
                          reads=[pfk, rrepk], writes=[f"{knk}#{pair}"])
                kview = R["kT1"][b].rearrange("(j two) r t -> two r j t", two=2)
                for two_ in range(2):
                    fw.dma(self.QS, kview[two_, 0:64, :, sl], knst[two_ * 64:(two_ + 1) * 64, :, 0:ntok],
                           reads=[f"{knk}#{p_}" for p_ in range(4)], writes=["kT1"])
                if self.cut1 == 3:
                    return
                krst, krk = krstr.next()
                for j in range(nt):
                    t = tok0 // 128 + j
                    hkj = f"{hk}#{j}"
                    tsl = slice(t * 128, (t + 1) * 128)
                    pt_, ptk = ptok.next()
                    for c in range(2):
                        fw.op("pe", lambda: nc.tensor.matmul(pt_[:, 0:512], lhsT=ckvT[:, c, j * 128:(j + 1) * 128], rhs=wukvv[:, c, :],
                                                             start=(c == 0), stop=(c == 1)), reads=ckeys + ["l1_wukvv"], writes=[ptk])
                    vs_, vsk = vstr.next()
                    fw.op("act", lambda: nc.scalar.mul(out=vs_[:], in_=pt_[:, 0:512], mul=rcol[:, j:j + 1]), reads=[ptk, rcolk], writes=[vsk])
                    fw.dma(self.QS, R["v1"][b, tsl, :], vs_[:], reads=[vsk], writes=["v1"])
                    pt_, ptk = mm_tok(hT, hkj, j, 256, 32)
                    mm_tok(hT, hkj, j, 1056, 16, off=64, pt_=pt_, ptk=ptk)
                    rt, rtk = rtr.next()
                    fw.dma("sp", rt[:], I["ropeC"][tsl, :], writes=[rtk])
                    krf, krfk = krfr.next()
                    self.rope_tok(P, pt_[:, 0:32].rearrange("p (h d) -> p h d", h=1), 1, 32, rt, rtk, ptk, krf, krfk)
                    pT, pTk = P["pT"].next()
                    fw.op("pe", lambda: nc.tensor.transpose(out=pT[0:32, 0, :], in_=krf[:, 0, :], identity=self.identb[:]),
                          reads=[krfk, "identb"], writes=[pTk])
                    fw.op("act", lambda: nc.scalar.copy(out=krst[0:32, j * 128:(j + 1) * 128], in_=pT[0:32, 0, :]), reads=[pTk], writes=[f"{krk}#{j}"])
                    mg_, mgk = mgstr.next()
                    fw.op("act", lambda: nc.scalar.copy(out=mg_[:], in_=pt_[:, 64:80]), reads=[ptk], writes=[mgk])
                    fw.dma(self.QS, R["mg1"][b, tsl, :], mg_[:], reads=[mgk], writes=["mg1"])
                    pt_, ptk = mm_tok(hT, hkj, j, 288, 256)
                    mk_, mkk = mkstr.next()
                    fw.op("act", lambda: nc.scalar.mul(out=mk_[:], in_=pt_[:, 0:256], mul=0.125), reads=[ptk], writes=[mkk])
                    fw.dma(self.QS, R["mk1"][b, tsl, :], mk_[:], reads=[mkk], writes=["mk1"])
                    pt_, ptk = mm_tok(hT, hkj, j, 544, 512)
                    mv_, mvk = mvstr.next()
                    fw.op("act", lambda: nc.scalar.copy(out=mv_[:], in_=pt_[:, 0:512]), reads=[ptk], writes=[mvk])
                    fw.dma(self.QS, R["mv1"][b, tsl, :], mv_[:], reads=[mvk], writes=["mv1"])
                if self.cut1 == 4:
                    return
                for h in range(8):
                    fw.dma(self.QS, R["kT1"][b, h, 64:96, sl], krst[0:32, 0:ntok], reads=[f"{krk}#{j}" for j in range(nt)], writes=["kT1"])
                for c in range(2):
                    pf, pfk = mm_fm(hT, hkeys, 288 + c * 128, ntok)
                    mkT_, mkTk = mkTstr.next()
                    fw.op("act", lambda: nc.scalar.mul(out=mkT_[:, 0:ntok], in_=pf[:, 0:ntok], mul=0.125), reads=[pfk], writes=[mkTk])
                    fw.dma(self.QS, R["mkT1"][b, c * 128:(c + 1) * 128, sl], mkT_[:, 0:ntok], reads=[mkTk], writes=["mkT1"])
                if not is_lat:
                    return
                ls = tok0 - LC
                lsl = slice(ls, ls + ntok)
                cqT, cqk = cqTr.next()
                sqq, sqqk = sqqr.next()
                for c in range(6):
                    pf, pfk = mm_fm(hT, hkeys, 1072 + c * 128, ntok)
                    fw.op("act", lambda: nc.scalar.copy(out=cqT[:, c, 0:ntok], in_=pf[:, 0:ntok]), reads=[pfk], writes=[f"{cqk}#{c}"])
                    fw.op("act", lambda: nc.scalar.activation(out=sqq[:, c, 0:ntok], in_=pf[:, 0:ntok], func=AF.Square), reads=[pfk], writes=[f"{sqqk}#{c}"])
                cqkeys = [f"{cqk}#{c}" for c in range(6)]
                sqkeys = [f"{sqqk}#{c}" for c in range(6)]
                for j in range(nt):
                    for c in range(6):
                        fw.op("pe", lambda: nc.tensor.matmul(pcol[:, 4 + j:5 + j], lhsT=sqq[:, c, j * 128:(j + 1) * 128], rhs=onesf[:, 0:1],
                                                             start=(c == 0), stop=(c == 5)), reads=sqkeys + ["c128f"], writes=["a_pcol"])
                fw.op("act", lambda: nc.scalar.activation(out=rcol[:, 6:8], in_=pcol[:, 4:6], func=AF.Sqrt, bias=EPS, scale=1.0 / 768.0),
                      reads=["a_pcol"], writes=[rcolk])
                fw.op("dve", lambda: nc.vector.reciprocal(out=rcol[:, 4:6], in_=rcol[:, 6:8]), reads=[rcolk], writes=[rcolk])
                qst, qstk = qstr.next()
                for j in range(nt):
                    t = tok0 // 128 + j
                    tsl = slice(t * 128, (t + 1) * 128)
                    qf, qfk = qfr.next()
                    qflat = qf[:].rearrange("p h d -> p (h d)")
                    for (c0, cn) in ((0, 512), (512, 256)):
                        pt_, ptk = ptok.next()
                        for c in range(6):
                            fw.op("pe", lambda: nc.tensor.matmul(pt_[:, 0:cn], lhsT=cqT[:, c, j * 128:(j + 1) * 128], rhs=wuq[:, c, c0:c0 + cn],
                                                                 start=(c == 0), stop=(c == 5)), reads=cqkeys + ["l1_wuq"], writes=[ptk])
                        fw.op("act", lambda: nc.scalar.mul(out=qflat[:, c0:c0 + cn], in_=pt_[:, 0:cn], mul=rcol[:, 4 + j:5 + j]),
                              reads=[ptk, rcolk], writes=[f"{qfk}#{c0}"])
                    qfkeys = [f"{qfk}#0", f"{qfk}#512"]
                    qbf, qbk = qbfr.next()
                    fw.op("pool", lambda: nc.gpsimd.tensor_copy(out=qbf[:, :, 0:64], in_=qf[:, :, 0:64]), reads=qfkeys, writes=[f"{qbk}#n"])
                    rt, rtk = rtr.next()
                    fw.dma("sp", rt[:], I["ropeC"][tsl, :], writes=[rtk])
                    self.rope_tok(P, qf, 8, 32, rt, rtk, qfkeys, qbf, f"{qbk}#r", dst_off=64, src_off=64)
                    pT, pTk = P["pT"].next()
                    for h in range(8):
                        fw.op("pe", lambda: nc.tensor.transpose(out=pT[0:96, h, :], in_=qbf[:, h, :], identity=self.identb[:]),
                              reads=[f"{qbk}#n", f"{qbk}#r", "identb"], writes=[pTk])
                    fw.op("act", lambda: nc.scalar.copy(out=qst[0:96, :, j * 128:(j + 1) * 128], in_=pT[0:96, :, :]), reads=[pTk], writes=[f"{qstk}#{j}"])
                    lt = t - 2
                    ltsl = slice(lt * 128, (lt + 1) * 128)
                    pt_, ptk = mm_tok(hT, f"{hk}#{j}", j, 2096, 512)
                    mo_, mok = mostr.next()
                    fw.op("act", lambda: nc.scalar.activation(out=mo_[:], in_=pt_[:, 0:512], func=AF.Sigmoid), reads=[ptk], writes=[mok])
                    fw.dma(self.QS, R["mo1"][b, ltsl, :], mo_[:], reads=[mok], writes=["mo1"])
                    for zh in range(2):
                        pt_, ptk = mm_tok(hT, f"{hk}#{j}", j, 2608 + zh * 512, 512)
                        z_, zk = zstr.next()
                        fw.op("act", lambda: nc.scalar.activation(out=z_[:], in_=pt_[:, 0:512], func=AF.Silu), reads=[ptk], writes=[zk])
                        fw.dma(self.QS, R["z1"][b, ltsl, zh * 512:(zh + 1) * 512], z_[:], reads=[zk], writes=["z1"])
                fw.dma(self.QS, R["qT1"][b].rearrange("h r t -> r h t")[:, :, lsl], qst[0:96, :, 0:ntok],
                       reads=[f"{qstk}#{j}" for j in range(nt)], writes=["qT1"])
                for c in range(2):
                    pf, pfk = mm_fm(hT, hkeys, 1840 + c * 128, ntok)
                    mq_, mqk = mqTstr.next()
                    fw.op("act", lambda: nc.scalar.copy(out=mq_[:, 0:ntok], in_=pf[:, 0:ntok]), reads=[pfk], writes=[mqk])
                    fw.dma(self.QS, R["mqT1"][b, c * 128:(c + 1) * 128, lsl], mq_[:, 0:ntok], reads=[mqk], writes=["mqT1"])

            n = len(CH)
            emit_norm(0)
            for ci in range(n):
                if ci + 1 < n:
                    emit_norm(ci + 1)
                emit_proj(ci)
                if self.cut1 in (2, 3, 4, 5) or (self.cut1 >= 6 and ci >= self.cut1 - 5):
                    return
                if b == 1 and _os_env("KCUT2") and ci >= int(_os_env("KCUT2")) - 1:
                    return

    def l1_stage_b(self, b):
        nc, fw, I, R = self.nc, self.fw, self.I, self.R
        with ExitStack() as es:
            kT = self.sb(es, "m_kT", [128, 8, T], BF16)
            vall = self.sb(es, "m_vall", [128, NT, 8, 80], BF16)
            fw.dma("sp", kT[0:96, :, :], R["kT1"][b].rearrange("h r t -> r h t"), reads=["kT1"], writes=["m_kT"])
            fw.op("pool", lambda: nc.gpsimd.memset(vall[:], 1.0), writes=["m_vall"])
            for t in range(NT):
                fw.dma("sp", vall[:, t, :, 0:64], R["v1"][b, t * 128:(t + 1) * 128, :].rearrange("p (h d) -> p h d", h=8),
                       reads=["v1"], writes=["m_vall"])
            psS = self.ring(es, "m_psS", [128, 512], F32, 4, psum=True)
            poR = self.ring(es, "m_po", [128, 4, 80], F32, 2, psum=True)
            pT = self.ps(es, "m_pT", [128, 4, 128], BF16)
            qTr = self.ring(es, "m_qT", [128, 8, 512], BF16, 2)
            ptr = self.ring(es, "m_pt", [128, 512], BF16, 40)
            coutr = self.ring(es, "m_cout", [128, 4, 8, 64], F32, 2)
            recr = self.ring(es, "m_rec", [128, 4], F32, 4)
            zr = self.ring(es, "m_z", [128, 512], F32, 2)
            cgr = self.ring(es, "m_cg", [128, 512], BF16, 2)
            cgstr = self.ring(es, "m_cgst", [128, 4, 128], BF16, 2)
            scale = 96 ** -0.5
            for qc in range(4):
                qT, qTk = qTr.next()
                fw.dma("sp", qT[0:96, :, :], R["qT1"][b].rearrange("h r t -> r h t")[:, :, qc * 512:(qc + 1) * 512], reads=["qT1"], writes=[qTk])
                cout, coutk = coutr.next()

                def emit_qk(h):
                    pts = []
                    for kb in range(NT):
                        ps_, psk = psS.next()
                        fw.op("pe", lambda: nc.tensor.matmul(ps_[:], lhsT=kT[0:96, h, kb * 128:(kb + 1) * 128], rhs=qT[0:96, h, :], start=True, stop=True),
                              reads=["m_kT", qTk], writes=[psk])
                        pt_, ptk = ptr.next()
                        fw.op("act", lambda: nc.scalar.activation(out=pt_[:], in_=ps_[:], func=AF.Exp, scale=scale), reads=[psk], writes=[ptk])
                        pts.append((pt_, ptk))
                    return pts

                def emit_pv(h, pts):
                    po, pok = poR.next()
                    for qs in range(4):
                        for kb in range(NT):
                            pt_, ptk = pts[kb]
                            fw.op("pe", lambda: nc.tensor.matmul(po[:, qs, 0:65], lhsT=pt_[:, qs * 128:(qs + 1) * 128], rhs=vall[:, kb, h, 0:65],
                                                                 start=(kb == 0), stop=(kb == NT - 1)), reads=[ptk, "m_vall"], writes=[pok])
                    rec, reck = recr.next()
                    fw.op("dve", lambda: nc.vector.reciprocal(out=rec[:], in_=po[:, :, 64]), reads=[pok], writes=[reck])
                    fw.op("dve", lambda: nc.vector.tensor_tensor(out=cout[:, :, h, :], in0=po[:, :, 0:64],
                                                                in1=rec[:].unsqueeze(2).to_broadcast([128, 4, 64]), op=ALU.mult),
                          reads=[pok, reck], writes=[f"{coutk}#{h}"])

                nxt = emit_qk(0)
                for h in range(8):
                    cur = nxt
                    if h + 1 < 8:
                        nxt = emit_qk(h + 1)
                    emit_pv(h, cur)
                for qs in range(4):
                    lt = qc * 4 + qs
                    ltsl = slice(lt * 128, (lt + 1) * 128)
                    z_, zk = zr.next()
                    fw.dma("sp", z_[:], R["z1"][b, ltsl, 0:512], reads=["z1"], writes=[zk])
                    cg, cgk = cgr.next()
                    fw.op("pool", lambda: nc.gpsimd.tensor_tensor(out=cg[:], in0=cout[:, qs, :, :].rearrange("p h d -> p (h d)"), in1=z_[:], op=ALU.mult),
                          reads=[f"{coutk}#{h}" for h in range(8)] + [zk], writes=[cgk])
                    for c in range(4):
                        fw.op("pe", lambda: nc.tensor.transpose(out=pT[:, c, :], in_=cg[:, c * 128:(c + 1) * 128], identity=self.identb[:]),
                              reads=[cgk, "identb"], writes=["m_pT"])
                    cgst, cgsk = cgstr.next()
                    fw.op("act", lambda: nc.scalar.copy(out=cgst[:], in_=pT[:]), reads=["m_pT"], writes=[cgsk])
                    fw.dma(self.QS, R["cgT1"][b].rearrange("c p t -> p c t")[:, :, ltsl], cgst[:], reads=[cgsk], writes=["cgT1"])

    def l1_stage_c1(self, b, hsum):
        nc, fw, I, R = self.nc, self.fw, self.I, self.R
        cf = self.cf
        with ExitStack() as es:
            gt = self.sb(es, "c_gt", [128, NT, 16], F32)
            mk = self.sb(es, "c_mk", [128, NT, 256], BF16)
            VO = self.sb(es, "c_VO", [128, NT, 4, 144], BF16)
            mkT = self.sb(es, "c_mkT", [64, 4, T], BF16)
            mqT = self.sb(es, "c_mqT", [64, 4, S], BF16)
            ib = self.sb(es, "c_ib", [128, 8], F32)
            fb = self.sb(es, "c_fb", [128, 8], F32)
            fw.dma("sp", ib[:], I["o_i_bias"].partition_broadcast(128), writes=["c_ib"])
            fw.dma("sp", fb[:], I["o_f_bias"].partition_broadcast(128), writes=["c_fb"])
            fw.op("pool", lambda: nc.gpsimd.memset(VO[:], 1.0), writes=["c_VO"])
            for t in range(NT):
                tsl = slice(t * 128, (t + 1) * 128)
                fw.dma("sp", gt[:, t, :], R["mg1"][b, tsl, :], reads=["mg1"], writes=["c_gt"])
                fw.dma("sp", mk[:, t, :], R["mk1"][b, tsl, :], reads=["mk1"], writes=["c_mk"])
                fw.dma("sp", VO[:, t, :, 0:128], R["mv1"][b, tsl, :].rearrange("p (h d) -> p h d", h=4), reads=["mv1"], writes=["c_VO"])
            fw.dma("sp", mkT[:], R["mkT1"][b].rearrange("(h d) t -> d h t", h=4), reads=["mkT1"], writes=["c_mkT"])
            fw.dma("sp", mqT[:], R["mqT1"][b].rearrange("(h d) t -> d h t", h=4), reads=["mqT1"], writes=["c_mqT"])
            pbig = self.ring(es, "c_pbig", [128, 512], F32, 1, psum=True)
            pC = self.ring(es, "c_pC", [64, 2, 144], F32, 2, psum=True)
            pS = self.ring(es, "c_pS", [128, 128], F32, 1, psum=True)
            pH = self.ring(es, "c_pH", [128, 2, 144], F32, 4, psum=True)
            cbr = self.ring(es, "c_cb", [64, 4, 144], BF16, 6)
            dgr = self.ring(es, "c_dg", [128, 4, 128], F32, 2)
            rmr = self.ring(es, "c_rm", [128, 4, 128], F32, 2)
            er = self.ring(es, "c_e", [64, 4, 128], F32, 2)
            qz0r = self.ring(es, "c_qz0", [64, 4, 128], BF16, 2)
            qz1r = self.ring(es, "c_qz1", [64, 4, 128], BF16, 2)
            for rr in (qz0r, qz1r):
                for i_, tl in enumerate(rr.tiles):
                    fw.op("pool", lambda: nc.gpsimd.memset(tl[:], 0.0), writes=[f"{rr.name}{i_}"])
            dr = self.ring(es, "c_d", [128, 128], F32, 8)
            scr = self.ring(es, "c_sc", [128, 128], BF16, 8)
            dnr = self.ring(es, "c_dn", [128, 8], F32, 6)
            Dd = []
            for d in range(2):
                X = {}
                for nm in ("li", "xf", "l1", "nb", "ngc", "aa", "wcol", "bcol"):
                    X[nm] = self.sb(es, f"c_{nm}{d}", [128, 72], F32)
                X["egf"] = self.sb(es, f"c_egf{d}", [128, 2, 72], F32)
                X["VW"] = self.sb(es, f"c_VW{d}", [128, NT, 4, 144], BF16)
                X["C"] = self.sb(es, f"c_C{d}", [64, 4, 144], F32)
                Dd.append(X)
            for d in range(2):
                X = Dd[d]
                li, xf, l1, nb, ngc, aa, wcol, bcol, egf, VW, C = (X[k] for k in ("li", "xf", "l1", "nb", "ngc", "aa", "wcol", "bcol", "egf", "VW", "C"))
                K_ = lambda nm: f"c_{nm}{d}"
                tri = C_TRIF if d == 0 else C_TRIR
                g3 = lambda a: a[:].rearrange("p (t h) -> p t h", h=4)
                fw.op("dve", lambda: nc.vector.tensor_tensor(out=g3(li), in0=gt[:, :, d * 8:d * 8 + 4],
                                                            in1=ib[:, d * 4:(d + 1) * 4].unsqueeze(1).to_broadcast([128, NT, 4]), op=ALU.add),
                      reads=["c_gt", "c_ib"], writes=[K_("li")])
                fw.op("dve", lambda: nc.vector.tensor_tensor(out=g3(xf), in0=gt[:, :, d * 8 + 4:d * 8 + 8],
                                                            in1=fb[:, d * 4:(d + 1) * 4].unsqueeze(1).to_broadcast([128, NT, 4]), op=ALU.add),
                      reads=["c_gt", "c_fb"], writes=[K_("xf")])
                fw.op("act", lambda: nc.scalar.activation(out=xf[:], in_=xf[:], func=AF.Exp, scale=-1.0), reads=[K_("xf")], writes=[K_("xf")])
                fw.op("act", lambda: nc.scalar.activation(out=l1[:], in_=xf[:], func=AF.Ln, bias=1.0), reads=[K_("xf")], writes=[K_("l1")])
                p1, p1k = pbig.next()
                fw.op("pe", lambda: nc.tensor.matmul(p1[:, 0:72], lhsT=cf[:, tri, :], rhs=l1[:], start=True, stop=True), reads=["c128f", K_("l1")], writes=[p1k])
                fw.op("dve", lambda: nc.vector.tensor_copy(out=nb[:], in_=p1[:, 0:72]), reads=[p1k], writes=[K_("nb")])
                p2, p2k = pbig.next()
                fw.op("pe", lambda: nc.tensor.matmul(p2[:, 0:72], lhsT=cf[:, C_BLK, :], rhs=l1[:], start=True, stop=True), reads=["c128f", K_("l1")], writes=[p2k])
                fw.op("dve", lambda: nc.vector.tensor_copy(out=ngc[:], in_=p2[:, 0:72]), reads=[p2k], writes=[K_("ngc")])
                p3, p3k = pbig.next()
                for half in range(2):
                    fw.op("pe", lambda: nc.tensor.matmul(p3[:, half * 72:(half + 1) * 72], lhsT=cf[:, C_SEL0 + half, :], rhs=l1[:], start=True, stop=True),
                          reads=["c128f", K_("l1")], writes=[p3k])
                fw.op("act", lambda: nc.scalar.activation(out=egf[:].rearrange("p a b -> p (a b)"), in_=p3[:, 0:144], func=AF.Exp, scale=-1.0),
                      reads=[p3k], writes=[K_("egf")])
                fw.op("dve", lambda: nc.vector.tensor_tensor(out=bcol[:], in0=li[:], in1=nb[:], op=ALU.add), reads=[K_("li"), K_("nb")], writes=[K_("bcol")])
                fw.op("dve", lambda: nc.vector.tensor_tensor(out=aa[:], in0=bcol[:], in1=ngc[:], op=ALU.subtract), reads=[K_("bcol"), K_("ngc")], writes=[K_("aa")])
                fw.op("act", lambda: nc.scalar.activation(out=wcol[:], in_=aa[:], func=AF.Exp), reads=[K_("aa")], writes=[K_("wcol")])
                for part in range(2):
                    e_ = "dve" if part == 0 else "pool"
                    eng = nc.vector if part == 0 else nc.gpsimd
                    tt = slice(part * 9, (part + 1) * 9)
                    fw.op(e_, lambda: eng.tensor_tensor(out=VW[:, tt, :, :].rearrange("p t h v -> p (t h) v"),
                                                        in0=VO[:, tt, :, :].rearrange("p t h v -> p (t h) v"),
                                                        in1=wcol[:, part * 36:(part + 1) * 36].unsqueeze(2).to_broadcast([128, 36, 144]), op=ALU.mult),
                          reads=["c_VO", K_("wcol")], writes=[f"c_VW{d}#{part}"])
                fw.op("dve", lambda: nc.vector.memset(C[:], 0.0), writes=[f"c_C{d}#{h}" for h in range(4)])

            written = set()

            def process_tile(d, t):
                X = Dd[d]
                nb, bcol, egf, VW, C = X["nb"], X["bcol"], X["egf"], X["VW"], X["C"]
                K_ = lambda nm: f"c_{nm}{d}"
                ckeys = [f"c_C{d}#{h}" for h in range(4)]
                vwkeys = [f"c_VW{d}#0", f"c_VW{d}#1"]
                mbk = C_MBF if d == 0 else C_MBR
                horder = (0, 1) if d == 0 else (1, 0)
                Cin = {}
                for half in horder:
                    if t >= 2:
                        cb, cbk = cbr.next()
                        fw.op("act", lambda: nc.scalar.copy(out=cb[:], in_=C[:]), reads=ckeys, writes=[cbk])
                        Cin[half] = (cb, cbk)
                    hs_ = slice(half * 64, (half + 1) * 64)
                    for hp in range(2):
                        pc, pck = pC.next()
                        for hh in range(2):
                            h = 2 * hp + hh
                            fw.op("pe", lambda: nc.tensor.matmul(pc[:, hh, 0:129], lhsT=mk[hs_, t, h * 64:(h + 1) * 64], rhs=VW[hs_, t, h, 0:129],
                                                                 start=True, stop=True), reads=["c_mk"] + vwkeys, writes=[pck])
                        for hh in range(2):
                            h = 2 * hp + hh
                            idx = t * 4 + h
                            fw.op("dve", lambda: nc.vector.scalar_tensor_tensor(out=C[:, h, 0:129], in0=C[:, h, 0:129], scalar=egf[0:64, half, idx:idx + 1],
                                                                               in1=pc[:, hh, 0:129], op0=ALU.mult, op1=ALU.add),
                                  reads=[ckeys[h], K_("egf"), pck], writes=[ckeys[h]])
                if t < 2:
                    return
                lt = t - 2
                dg, dgk = dgr.next()
                fw.op("dve", lambda: nc.vector.tensor_tensor(out=dg[:], in0=nb[:, t * 4:(t + 1) * 4].unsqueeze(2).to_broadcast([128, 4, 128]),
                                                            in1=cf[:, C_ID, :].unsqueeze(1).to_broadcast([128, 4, 128]), op=ALU.mult),
                      reads=[K_("nb"), "c128f"], writes=[dgk])
                pR, pRk = pbig.next()
                fw.op("pe", lambda: nc.tensor.matmul(pR[:], lhsT=cf[:, C_ONES, :], rhs=dg[:].rearrange("p h n -> p (h n)"), start=True, stop=True),
                      reads=["c128f", dgk], writes=[pRk])
                rm, rmk = rmr.next()
                fw.op("dve", lambda: nc.vector.tensor_tensor(out=rm[:], in0=cf[:, mbk, :].unsqueeze(1).to_broadcast([128, 4, 128]),
                                                            in1=pR[:].rearrange("p (h n) -> p h n", h=4), op=ALU.subtract),
                      reads=["c128f", pRk], writes=[rmk])
                e_, ek = er.next()
                fw.op("act", lambda: nc.scalar.activation(out=e_[:].rearrange("p h n -> p (h n)"), in_=pR[0:64, :], func=AF.Exp, scale=-1.0),
                      reads=[pRk], writes=[ek])
                qz0, qz0k = qz0r.next()
                qz1, qz1k = qz1r.next()
                fw.op("pool", lambda: nc.gpsimd.tensor_tensor(out=qz0[:, :, 0:64], in0=mqT[:, :, lt * 128:lt * 128 + 64], in1=e_[:, :, 0:64], op=ALU.mult),
                      reads=["c_mqT", ek], writes=[qz0k])
                fw.op("pool", lambda: nc.gpsimd.tensor_tensor(out=qz1[:, :, 64:128], in0=mqT[:, :, lt * 128 + 64:lt * 128 + 128], in1=e_[:, :, 64:128], op=ALU.mult),
                      reads=["c_mqT", ek], writes=[qz1k])
                scs = []
                for h in range(4):
                    idx = t * 4 + h
                    dt_, dtk = dr.next()
                    fw.op("act", lambda: nc.scalar.activation(out=dt_[:], in_=rm[:, h, :], func=AF.Exp, bias=bcol[:, idx:idx + 1], scale=1.0),
                          reads=[rmk, K_("bcol")], writes=[dtk])
                    ps_, psk = pS.next()
                    fw.op("pe", lambda: nc.tensor.matmul(ps_[:], lhsT=mkT[:, h, t * 128:(t + 1) * 128], rhs=mqT[:, h, lt * 128:(lt + 1) * 128],
                                                         start=True, stop=True), reads=["c_mkT", "c_mqT"], writes=[psk])
                    sc, sck = scr.next()
                    fw.op("dve", lambda: nc.vector.tensor_tensor(out=sc[:], in0=ps_[:], in1=dt_[:], op=ALU.mult), reads=[psk, dtk], writes=[sck])
                    scs.append((sc, sck))
                c0, c0k = Cin[0]
                c1, c1k = Cin[1]
                phs = []
                for hp in range(2):
                    ph, phk = pH.next()
                    phs.append((ph, phk))
                    for hh in range(2):
                        h = 2 * hp + hh
                        sc, sck = scs[h]
                        fw.op("pe", lambda: nc.tensor.matmul(ph[:, hh, 0:129], lhsT=sc[:], rhs=VO[:, t, h, 0:129], start=True, stop=False),
                              reads=[sck, "c_VO"], writes=[phk])
                        fw.op("pe", lambda: nc.tensor.matmul(ph[:, hh, 0:129], lhsT=qz0[:, h, :], rhs=c0[:, h, 0:129], start=False, stop=False),
                              reads=[qz0k, c0k], writes=[phk])
                        fw.op("pe", lambda: nc.tensor.matmul(ph[:, hh, 0:129], lhsT=qz1[:, h, :], rhs=c1[:, h, 0:129], start=False, stop=True),
                              reads=[qz1k, c1k], writes=[phk])
                for hp in range(2):
                    ph, phk = phs[hp]
                    dn, dnk = dnr.next()
                    fw.op("dve", lambda: nc.vector.tensor_scalar(out=dn[:, 0:2], in0=ph[:, :, 128], scalar1=-1.0, scalar2=1.0, op0=ALU.mult, op1=ALU.max),
                          reads=[phk], writes=[dnk])
                    fw.op("dve", lambda: nc.vector.tensor_tensor(out=dn[:, 2:4], in0=dn[:, 0:2], in1=ph[:, :, 128], op=ALU.max), reads=[dnk, phk], writes=[dnk])
                    fw.op("dve", lambda: nc.vector.reciprocal(out=dn[:, 4:6], in_=dn[:, 2:4]), reads=[dnk], writes=[dnk])
                    for hh in range(2):
                        h = 2 * hp + hh
                        hkey = f"c_hsum#{lt}#{h}"
                        if (lt, h) not in written:
                            written.add((lt, h))
                            fw.op("dve", lambda: nc.vector.tensor_scalar(out=hsum[:, lt, h, :], in0=ph[:, hh, 0:128], scalar1=dn[:, 4 + hh:5 + hh], scalar2=None, op0=ALU.mult),
                                  reads=[phk, dnk], writes=[hkey])
                        else:
                            fw.op("dve", lambda: nc.vector.scalar_tensor_tensor(out=hsum[:, lt, h, :], in0=ph[:, hh, 0:128], scalar=dn[:, 4 + hh:5 + hh], in1=hsum[:, lt, h, :],
                                                                               op0=ALU.mult, op1=ALU.add), reads=[phk, dnk, hkey], writes=[hkey])

            torder = [list(range(NT)), [1, 0] + list(range(NT - 1, 1, -1))]
            for step in range(NT):
                for d in range(2):
                    process_tile(d, torder[d][step])

    def l1_stage_c2(self, b, hsum, wout):
        nc, fw, I, R = self.nc, self.fw, self.I, self.R
        with ExitStack() as es:
            mt = self.mod_tiles(es, 1, b, want_pre=False, want_post=True)
            g, gk = mt["g_lat"]
            P = {}
            P["xres"] = self.ring(es, "d_xres", [128, D], F32, 2)
            P["junk"] = self.ring(es, "d_junk", [128, D], BF16, 2)
            P["stat"] = self.ring(es, "d_stat", [128, 4], F32, 4)
            P["t2"] = self.ring(es, "d_t2", [128, D], F32, 2)
            hnw = self.sb(es, "d_hnw", [128, 512], F32)
            fw.dma("sp", hnw[:], I["o_head_norm_w"].partition_broadcast(128), writes=["d_hnw"])
            pT = self.ps(es, "d_pT", [128, 4, 128], BF16)
            py = self.ps(es, "d_py", [128, D], F32)
            mor = self.ring(es, "d_mo", [128, 512], F32, 2)
            zmr = self.ring(es, "d_zm", [128, 512], F32, 2)
            cgr = self.ring(es, "d_cg", [128, 4, 128], BF16, 2)
            str_ = self.ring(es, "d_st", [128, 12], F32, 3)
            jr = self.ring(es, "d_j", [128, 128], BF16, 2)
            g1r = self.ring(es, "d_g1", [128, 512], F32, 2)
            hnr = self.ring(es, "d_hn", [128, 4, 128], F32, 2)
            mgr = self.ring(es, "d_mg", [128, 512], BF16, 2)
            mgTr = self.ring(es, "d_mgT", [128, 4, 128], BF16, 2)
            loads = {}

            def emit_loads(lt):
                ltsl = slice(lt * 128, (lt + 1) * 128)
                mo_, mok = mor.next()
                fw.dma("sp", mo_[:], R["mo1"][b, ltsl, :], reads=["mo1"], writes=[mok])
                zm_, zmk = zmr.next()
                fw.dma("sp", zm_[:], R["z1"][b, ltsl, 512:1024], reads=["z1"], writes=[zmk])
                cg_, cgk = cgr.next()
                fw.dma("sp", cg_[:], R["cgT1"][b].rearrange("c p t -> p c t")[:, :, ltsl], reads=["cgT1"], writes=[cgk])
                loads[lt] = (mo_, mok, zm_, zmk, cg_, cgk)

            def emit_tile(lt):
                mo_, mok, zm_, zmk, cg_, cgk = loads.pop(lt)
                hkeys = [f"c_hsum#{lt}#{h}" for h in range(4)]
                st, stk = str_.next()
                for h in range(4):
                    j_, jk = jr.next()
                    fw.op("act", lambda: nc.scalar.activation(out=j_[:], in_=hsum[:, lt, h, :], func=AF.Square, scale=128 ** -0.5, accum_out=st[:, h:h + 1]),
                          reads=hkeys, writes=[jk, f"{stk}#{h}"])
                fw.op("act", lambda: nc.scalar.activation(out=st[:, 4:8], in_=st[:, 0:4], func=AF.Sqrt, bias=EPS),
                      reads=[f"{stk}#{h}" for h in range(4)], writes=[f"{stk}#s"])
                fw.op("dve", lambda: nc.vector.reciprocal(out=st[:, 8:12], in_=st[:, 4:8]), reads=[f"{stk}#s"], writes=[f"{stk}#r"])
                g1, g1k = g1r.next()
                fw.op("pool", lambda: nc.gpsimd.tensor_tensor(out=g1[:], in0=mo_[:], in1=zm_[:], op=ALU.mult), reads=[mok, zmk], writes=[g1k])
                fw.op("pool", lambda: nc.gpsimd.tensor_tensor(out=g1[:], in0=g1[:], in1=hnw[:], op=ALU.mult), reads=[g1k, "d_hnw"], writes=[g1k])
                hn, hnk = hnr.next()
                fw.op("dve", lambda: nc.vector.tensor_tensor(out=hn[:], in0=hsum[:, lt, :, :], in1=st[:, 8:12].unsqueeze(2).to_broadcast([128, 4, 128]), op=ALU.mult),
                      reads=hkeys + [f"{stk}#r"], writes=[hnk])
                mg, mgk = mgr.next()
                fw.op("dve", lambda: nc.vector.tensor_tensor(out=mg[:], in0=hn[:].rearrange("p h v -> p (h v)"), in1=g1[:], op=ALU.mult),
                      reads=[hnk, g1k], writes=[mgk])
                for c in range(4):
                    fw.op("pe", lambda: nc.tensor.transpose(out=pT[:, c, :], in_=mg[:, c * 128:(c + 1) * 128], identity=self.identb[:]),
                          reads=[mgk, "identb"], writes=["d_pT"])
                mgT, mgTk = mgTr.next()
                fw.op("act", lambda: nc.scalar.copy(out=mgT[:], in_=pT[:]), reads=["d_pT"], writes=[mgTk])
                for half in range(2):
                    for k in range(8):
                        src, srk = (cg_, cgk) if k < 4 else (mgT, mgTk)
                        fw.op("pe", lambda: nc.tensor.matmul(py[:, half * 512:(half + 1) * 512], lhsT=src[:, k % 4, :],
                                                             rhs=wout[:, k, half * 512:(half + 1) * 512], start=(k == 0), stop=(k == 7)),
                              reads=[srk, "l1_wout"], writes=["d_py"])
                t = lt + 2
                self.post_tile(P, py, "d_py", g, gk, R["x1c"][b, t * 128:(t + 1) * 128, :], ["x1c"],
                               self.out[b, lt * 128:(lt + 1) * 128, :], "out")

            emit_loads(0)
            for lt in range(16):
                if lt + 1 < 16:
                    emit_loads(lt + 1)
                emit_tile(lt)


def make_consts():
    c = np.zeros((11, 128, 128), np.float32)
    p = np.arange(128)[:, None]
    n = np.arange(128)[None, :]
    same = (p // 64) == (n // 64)
    c[C_ID] = (p == n)
    c[C_ONES] = 1.0
    c[C_TRIF] = same & (p <= n)
    c[C_TRIR] = same & (p >= n)
    c[C_BLK] = same
    c[C_SEL0] = (p < 64) & (n >= 0)
    c[C_SEL1] = (p >= 64) & (n >= 0)
    c[C_MBF] = np.where(same & (p <= n), 0.0, NEG)
    c[C_MBR] = np.where(same & (p >= n), 0.0, NEG)
    c[C_MPREV] = np.where(p >= n, 0.0, NEG)
    c[C_MNEXT] = np.where(p <= n, 0.0, NEG)

    def axial(rot_dim):
        rows = S // 64
        row = np.repeat(np.arange(rows), 64).astype(np.float32)
        col = np.tile(np.arange(64), rows).astype(np.float32)
        nf = rot_dim // 4
        inv = (np.float32(10000.0) ** (-np.arange(nf, dtype=np.float32) / np.float32(nf))).astype(np.float32)
        ang = np.concatenate([row[:, None] * inv, col[:, None] * inv], axis=-1).astype(np.float32)
        tab = np.zeros((T, rot_dim), np.float32)
        tab[:LC, :rot_dim // 2] = 1.0
        tab[LC:, :rot_dim // 2] = np.cos(ang)
        tab[LC:, rot_dim // 2:] = np.sin(ang)
        return tab
    return c, axial(64), axial(32)


_CACHE = {}


def get_program(debug=False, stop_after=None):
    key = (debug, stop_after)
    if key not in _CACHE:
        bld = Builder(debug=debug)
        nc = bld.build(stop_after=stop_after)
        _CACHE[key] = (nc, bld)
    return _CACHE[key]


def make_in_maps(inputs):
    c128, ropeA, ropeC = make_consts()
    f = lambda a: np.ascontiguousarray(np.asarray(a, dtype=np.float32))
    shared = {
        "mod_w": f(inputs["mod_w"]), "mod_b": f(inputs["mod_b"]),
        "pre_norm_w": f(inputs["pre_norm_w"]), "post_norm_w": f(inputs["post_norm_w"]),
        "e_w_in": f(inputs["e_w_in"][0]), "e_sink": f(inputs["e_sink"][0]), "e_conv_w": f(inputs["e_conv_w"][0]),
        "e_w_out": f(inputs["e_w_out"][0]), "o_w_in": f(inputs["o_w_in"][0]),
        "o_q_norm_w": f(inputs["o_q_norm_w"][0]), "o_kv_norm_w": f(inputs["o_kv_norm_w"][0]),
        "o_w_uq": f(inputs["o_w_uq"][0]), "o_w_ukv": f(inputs["o_w_ukv"][0]),
        "o_i_bias": f(inputs["o_i_bias"][0]).reshape(8), "o_f_bias": f(inputs["o_f_bias"][0]).reshape(8),
        "o_head_norm_w": f(inputs["o_head_norm_w"][0]), "o_w_out": f(inputs["o_w_out"][0]),
        "c128": c128, "ropeA": ropeA, "ropeC": ropeC,
    }
    x = f(inputs["x"])
    c = f(inputs["c"])
    ctx = f(inputs["ctx"])
    cc = f(inputs["c_ctx"])
    maps = []
    for i in range(NCORES):
        m = dict(shared)
        m["xs"] = x[NB * i:NB * (i + 1)]
        m["ctxs"] = ctx[NB * i:NB * (i + 1)]
        m["cvec"] = np.ascontiguousarray(np.stack([c[NB * i], c[NB * i + 1], cc], axis=0))
        maps.append(m)
    return maps


def kernel(**inputs):
    nc, _ = get_program()
    maps = make_in_maps(inputs)
    res = run_bass_kernel_spmd(nc, maps, core_ids=list(range(NCORES)))
    return np.concatenate([r["out"] for r in res.results], axis=0)
```

```python
import numpy as np
from contextlib import ExitStack
import concourse.bass as bass
import concourse.mybir as mybir
from concourse.bass_utils import run_bass_kernel_spmd

F32 = mybir.dt.float32
BF16 = mybir.dt.bfloat16
AF = mybir.ActivationFunctionType
ALU = mybir.AluOpType

D = 1024
S = 2048
LC = 256
T = S + LC
NT = T // 128
NB = 2
NCORES = 8
EPS = 1e-6
NEG = -30000.0
E_IN = 3328
O_IN = 3632
CHUNKS = [(0, 256), (256, 512), (768, 512), (1280, 512), (1792, 512)]

C_ID, C_ONES, C_TRIF, C_TRIR, C_BLK, C_SEL0, C_SEL1, C_MBF, C_MBR, C_MPREV, C_MNEXT = range(11)


def _nruns(ap):
    pat = [list(x) for x in ap.ap]
    tot = 1
    for st, n in pat:
        tot *= n
    run = 1
    for st, n in sorted(pat, key=lambda x: abs(x[0]) if x[0] != 0 else 1 << 60):
        if st == run:
            run *= n
        elif n > 1:
            break
    return max(1, tot // run)


def _nbytes(ap):
    tot = 1
    for st, n in ap.ap:
        tot *= n
    return tot


def _os_env(k):
    import os
    return os.environ.get(k)


class FW:
    ROT = 30000

    def __init__(self, nc, es):
        self.nc = nc
        self.es = es
        self.eng = {"pe": nc.tensor, "act": nc.scalar, "dve": nc.vector, "pool": nc.gpsimd, "sp": nc.sync}
        self.comp = ["pe", "act", "dve", "pool"]
        self.epoch = {k: 0 for k in self.comp}
        self.sem = {k: es.enter_context(nc.semaphore(f"s_{k}_0")) for k in self.comp}
        self.cnt = {k: 0 for k in self.comp}
        self.seen = {e: {} for e in self.eng}
        self.NQ = int(_os_env("KNQ") or 8)
        self.DBUDGET = int(_os_env("KDB") or 1024)
        self.dq = {}
        for q in ["sp", "act", "pool"]:
            sems = [es.enter_context(nc.semaphore(f"d_{q}{i}")) for i in range(self.NQ)]
            self.dq[q] = {"sems": sems, "n": 0}
        self.lastw = {}
        self.reads = {}
        self.ninst = 0
        self.psum_keys = set()

    def _wait(self, e, tok):
        key, sem, val = tok
        if self.seen[e].get(key, 0) >= val:
            return
        self.eng[e].wait_ge(sem, val)
        self.seen[e][key] = val
        self.ninst += 1

    def _deps(self, e, reads, writes):
        toks = []
        for b in list(reads) + list(writes):
            t = self.lastw.get(b)
            if t is not None:
                toks.append(t)
        for b in writes:
            toks.extend(self.reads.get(b, []))
        for b in reads:
            if b in self.psum_keys:
                toks.extend(t for t in self.reads.get(b, []) if not t[0].startswith(e + "#"))
        for t in toks:
            if e == "pe" and t[0].startswith("pe#"):
                continue
            self._wait(e, t)

    def _commit(self, tok, reads, writes):
        for b in writes:
            self.lastw[b] = tok
            self.reads[b] = []
        for b in reads:
            lst = self.reads.setdefault(b, [])
            lst[:] = [t for t in lst if t[0] != tok[0]]
            lst.append(tok)

    def op(self, e, fn, reads=(), writes=()):
        self._deps(e, reads, writes)
        if self.cnt[e] >= self.ROT:
            self.epoch[e] += 1
            self.sem[e] = self.es.enter_context(self.nc.semaphore(f"s_{e}_{self.epoch[e]}"))
            self.cnt[e] = 0
        ins = fn()
        self.cnt[e] += 1
        ins.then_inc(self.sem[e], 1)
        tok = (f"{e}#{self.epoch[e]}", self.sem[e], self.cnt[e])
        self._commit(tok, reads, writes)
        self.ninst += 1
        return tok

    def dma(self, q, out, in_, reads=(), writes=(), **kw):
        d = self.dq[q]
        i = d["n"] % self.NQ
        rnd = d["n"] // self.NQ
        sem = d["sems"][i]
        key = f"d_{q}{i}"
        if rnd > 0:
            self._wait(q, (key, sem, 16 * rnd))
        nd = max(_nruns(out), _nruns(in_))
        fl = d.setdefault("inflight", [])
        fl[:] = [(t, c) for (t, c) in fl if self.seen[q].get(t[0], 0) < t[2]]
        while fl and sum(c for _, c in fl) + nd > self.DBUDGET:
            t, c = fl.pop(0)
            self._wait(q, t)
        self._deps(q, reads, writes)
        ins = self.eng[q].dma_start(out=out, in_=in_, **kw)
        ins.then_inc(sem, 16)
        d["n"] += 1
        self.ndesc = getattr(self, "ndesc", 0) + nd
        tok = (key, sem, 16 * (rnd + 1))
        fl.append((tok, nd))
        self._commit(tok, reads, writes)
        self.ninst += 1
        return tok

    def all_tokens(self):
        toks = []
        for k in self.comp:
            if self.cnt[k] > 0:
                toks.append((f"{k}#{self.epoch[k]}", self.sem[k], self.cnt[k]))
        for q, d in self.dq.items():
            for i in range(self.NQ):
                n_i = (d["n"] - i + self.NQ - 1) // self.NQ
                if n_i > 0:
                    toks.append((f"d_{q}{i}", d["sems"][i], 16 * n_i))
        return toks

    def barrier(self):
        toks = self.all_tokens()
        for e in self.eng:
            for t in toks:
                if t[0].startswith(e + "#"):
                    continue
                self._wait(e, t)
        self.lastw = {}
        self.reads = {}

    def finish(self):
        for t in self.all_tokens():
            self._wait("sp", t)


class Ring:
    def __init__(self, tiles, name):
        self.tiles = tiles
        self.name = name
        self.i = -1

    def next(self):
        self.i = (self.i + 1) % len(self.tiles)
        return self.tiles[self.i], f"{self.name}{self.i}"


class Builder:
    def __init__(self, debug=False):
        self.debug = debug
        self.nc = bass.Bass("TRN2", target_bir_lowering=False)
        self.dbg_names = []
        import os as _os
        self.QS = _os.environ.get("KQS", "sp")

    def uname(self, name):
        self.uid = getattr(self, "uid", 0) + 1
        return f"{name}_u{self.uid}"

    def sb(self, es, name, shape, dt):
        return es.enter_context(self.nc.sbuf_tensor(self.uname(name), list(shape), dt))

    def ps(self, es, name, shape, dt):
        self.fw.psum_keys.add(name)
        return es.enter_context(self.nc.psum_tensor(self.uname(name), list(shape), dt))

    def ring(self, es, name, shape, dt, n, psum=False):
        f = self.ps if psum else self.sb
        return Ring([f(es, f"{name}{i}", shape, dt) for i in range(n)], name)

    def dram_in(self, name, shape, dt=F32):
        return self.nc.dram_tensor(name, list(shape), dt, kind="ExternalInput").ap()

    def scratch(self, name, shape, dt, dbg=False):
        if dbg and self.debug:
            self.dbg_names.append(name)
            return self.nc.dram_tensor(name, list(shape), dt, kind="ExternalOutput").ap()
        return self.nc.dram_tensor(name, list(shape), dt).ap()

    def build(self, stop_after=None):
        nc = self.nc
        self.stop = stop_after
        import os as _os
        self.cut1 = int(_os.environ.get("KCUT1", "0"))
        self.cut = int(_os.environ.get("KCUT", "0"))
        self.cutb = int(_os.environ.get("KCUTB", "0"))
        self.cutc = int(_os.environ.get("KCUTC", "0"))
        I = {}
        I["xs"] = self.dram_in("xs", [NB, S, D])
        I["ctxs"] = self.dram_in("ctxs", [NB, LC, D])
        I["cvec"] = self.dram_in("cvec", [3, D])
        I["mod_w"] = self.dram_in("mod_w", [2, D, 3 * D])
        I["mod_b"] = self.dram_in("mod_b", [2, 3 * D])
        I["pre_norm_w"] = self.dram_in("pre_norm_w", [2, D])
        I["post_norm_w"] = self.dram_in("post_norm_w", [2, D])
        I["e_w_in"] = self.dram_in("e_w_in", [D, E_IN])
        I["e_sink"] = self.dram_in("e_sink", [8])
        I["e_conv_w"] = self.dram_in("e_conv_w", [3, 512])
        I["e_w_out"] = self.dram_in("e_w_out", [D, D])
        I["o_w_in"] = self.dram_in("o_w_in", [D, O_IN])
        I["o_q_norm_w"] = self.dram_in("o_q_norm_w", [768])
        I["o_kv_norm_w"] = self.dram_in("o_kv_norm_w", [256])
        I["o_w_uq"] = self.dram_in("o_w_uq", [768, 768])
        I["o_w_ukv"] = self.dram_in("o_w_ukv", [256, 1024])
        I["o_i_bias"] = self.dram_in("o_i_bias", [8])
        I["o_f_bias"] = self.dram_in("o_f_bias", [8])
        I["o_head_norm_w"] = self.dram_in("o_head_norm_w", [512])
        I["o_w_out"] = self.dram_in("o_w_out", [D, D])
        I["c128"] = self.dram_in("c128", [11, 128, 128])
        I["ropeA"] = self.dram_in("ropeA", [T, 64])
        I["ropeC"] = self.dram_in("ropeC", [T, 32])
        self.I = I
        out = nc.dram_tensor("out", [NB, S, D], F32, kind="ExternalOutput").ap()
        self.out = out

        R = {}
        R["modrow"] = self.scratch("modrow", [2, 3, 3 * D], F32, dbg=True)
        R["x1c"] = self.scratch("x1c", [NB, T, D], F32, dbg=True)
        R["qT0"] = self.scratch("qT0", [NB, 4, 128, T], BF16)
        R["kT0"] = self.scratch("kT0", [NB, 128, T], BF16)
        R["v0"] = self.scratch("v0", [NB, T, 128], BF16)
        R["za0"] = self.scratch("za0", [NB, T, 512], F32)
        R["bg0T"] = self.scratch("bg0T", [NB, 4, 128, T], BF16)
        R["kT1"] = self.scratch("kT1", [NB, 8, 96, T], BF16)
        R["v1"] = self.scratch("v1", [NB, T, 512], BF16)
        R["qT1"] = self.scratch("qT1", [NB, 8, 96, S], BF16)
        R["mk1"] = self.scratch("mk1", [NB, T, 256], BF16)
        R["mv1"] = self.scratch("mv1", [NB, T, 512], BF16)
        R["mkT1"] = self.scratch("mkT1", [NB, 256, T], BF16)
        R["mqT1"] = self.scratch("mqT1", [NB, 256, S], BF16)
        R["mg1"] = self.scratch("mg1", [NB, T, 16], F32)
        R["mo1"] = self.scratch("mo1", [NB, S, 512], F32)
        R["z1"] = self.scratch("z1", [NB, S, 1024], F32)
        R["cgT1"] = self.scratch("cgT1", [NB, 4, 128, S], BF16)
        self.R = R

        with ExitStack() as es0:
            self.fw = FW(nc, es0)
            fw = self.fw
            self.cf = self.sb(es0, "c128f", [128, 11, 128], F32)
            fw.dma("sp", self.cf[:], I["c128"].rearrange("c p n -> p c n"), writes=["c128f"])
            self.identb = self.sb(es0, "identb", [128, 128], BF16)
            fw.op("dve", lambda: nc.vector.tensor_copy(out=self.identb[:], in_=self.cf[:, C_ID, :]),
                  reads=["c128f"], writes=["identb"])
            self.onesb = self.sb(es0, "onesb", [128, 128], BF16)
            fw.op("dve", lambda: nc.vector.tensor_copy(out=self.onesb[:], in_=self.cf[:, C_ONES, :]),
                  reads=["c128f"], writes=["onesb"])
            self.stage_mod()
            fw.barrier()
            if stop_after != "mod":
                if not _os.environ.get("KSKIP0"):
                    self.layer0()
                    fw.barrier()
                if stop_after in (None, "l1a", "l1b"):
                    self.layer1()
                    fw.barrier()
            fw.finish()
        return nc

    def stage_mod(self):
        nc, fw, I, R = self.nc, self.fw, self.I, self.R
        with ExitStack() as es:
            cv = self.sb(es, "m_cv", [3, D], F32)
            sv = self.sb(es, "m_sv", [3, D], F32)
            svT = self.sb(es, "m_svT", [128, 8, 3], F32)
            ones3 = self.sb(es, "m_ones3", [1, 3], F32)
            pT = self.ps(es, "m_pT", [128, 8, 3], F32)
            wring = self.ring(es, "m_w", [128, 8, 512], F32, 2)
            pring = self.ring(es, "m_p", [3, 512], F32, 2, psum=True)
            mb = self.sb(es, "m_mb", [1, 3 * D], F32)
            mr = self.sb(es, "m_mr", [3, 3 * D], F32)
            fw.dma("sp", cv[:], I["cvec"], writes=["m_cv"])
            fw.op("act", lambda: nc.scalar.activation(out=sv[:], in_=cv[:], func=AF.Silu), reads=["m_cv"], writes=["m_sv"])
            fw.op("pool", lambda: nc.gpsimd.memset(ones3[:], 1.0), writes=["m_ones3"])
            for k in range(8):
                fw.op("pe", lambda: nc.tensor.transpose(out=pT[:, k, :], in_=sv[0:3, k * 128:(k + 1) * 128],
                                                        identity=self.cf[0:3, C_ID, 0:3]),
                      reads=["m_sv", "c128f"], writes=["m_pT"])
            fw.op("dve", lambda: nc.vector.tensor_copy(out=svT[:], in_=pT[:]), reads=["m_pT"], writes=["m_svT"])
            for l in range(2):
                fw.dma("sp", mb[:], I["mod_b"][l:l + 1, :], reads=[], writes=["m_mb"])
                for n in range(6):
                    wt, wk = wring.next()
                    fw.dma("sp", wt[:], I["mod_w"][l, :, n * 512:(n + 1) * 512].rearrange("(k p) n -> p k n", p=128),
                           writes=[wk])
                    pm, pk = pring.next()
                    for k in range(8):
                        fw.op("pe", lambda: nc.tensor.matmul(pm[:], lhsT=svT[:, k, :], rhs=wt[:, k, :],
                                                             start=(k == 0), stop=False),
                              reads=["m_svT", wk], writes=[pk])
                    fw.op("pe", lambda: nc.tensor.matmul(pm[:], lhsT=ones3[0:1, :], rhs=mb[0:1, n * 512:(n + 1) * 512],
                                                         start=False, stop=True),
                          reads=["m_ones3", "m_mb"], writes=[pk])
                    fw.op("dve", lambda: nc.vector.tensor_copy(out=mr[:, n * 512:(n + 1) * 512], in_=pm[:]),
                          reads=[pk], writes=["m_mr"])
                fw.dma("sp", R["modrow"][l], mr[:], reads=["m_mr"], writes=["modrow"])

    def src_rows(self, layer, b, tok0, n):
        if layer == 0:
            if tok0 < LC:
                return self.I["ctxs"][b, tok0:tok0 + n, :]
            return self.I["xs"][b, tok0 - LC:tok0 - LC + n, :]
        return self.R["x1c"][b, tok0:tok0 + n, :]

    def load_weight_bf16(self, es, name, w_ap, ncols, nk=8):
        nc, fw = self.nc, self.fw
        wt = self.sb(es, name, [128, nk, ncols], BF16)
        with ExitStack() as es2:
            stg = self.ring(es2, name + "_stg", [128, ncols], F32, 4)
            for k in range(nk):
                st, sk = stg.next()
                fw.dma("sp", st[:], w_ap[k * 128:(k + 1) * 128, :], writes=[sk])
                e = "pool" if k % 2 == 0 else "dve"
                eng = nc.gpsimd if k % 2 == 0 else nc.vector
                fw.op(e, lambda: eng.tensor_copy(out=wt[:, k, :], in_=st[:]), reads=[sk], writes=[name])
            fw.barrier()
        return wt

    def mod_tiles(self, es, layer, b, want_pre=True, want_post=True):
        nc, fw, I, R = self.nc, self.fw, self.I, self.R
        res = {}
        tmp = self.sb(es, "mt_tmp", [128, D], F32)
        if want_pre:
            fw.dma("sp", tmp[:], I["pre_norm_w"][layer].partition_broadcast(128), writes=["mt_tmp"])
            for nm, v in (("lat", b), ("ctx", 2)):
                sc = self.sb(es, f"mt_sc_{nm}", [128, D], F32)
                sh = self.sb(es, f"mt_sh_{nm}", [128, D], F32)
                fw.dma("sp", sh[:], R["modrow"][layer, v, 0:D].partition_broadcast(128), reads=["modrow"], writes=[f"mt_sh_{nm}"])
                fw.dma("sp", sc[:], R["modrow"][layer, v, D:2 * D].partition_broadcast(128), reads=["modrow"], writes=[f"mt_sc_{nm}"])
                fw.op("dve", lambda: nc.vector.scalar_tensor_tensor(out=sc[:], in0=sc[:], scalar=1.0, in1=tmp[:],
                                                                   op0=ALU.add, op1=ALU.mult),
                      reads=[f"mt_sc_{nm}", "mt_tmp"], writes=[f"mt_sc_{nm}"])
                res[f"sc_{nm}"] = (sc, f"mt_sc_{nm}")
                res[f"sh_{nm}"] = (sh, f"mt_sh_{nm}")
        if want_post:
            tmp2 = self.sb(es, "mt_tmp2", [128, D], F32)
            fw.dma("sp", tmp2[:], I["post_norm_w"][layer].partition_broadcast(128), writes=["mt_tmp2"])
            for nm, v in (("lat", b), ("ctx", 2)):
                g = self.sb(es, f"mt_g_{nm}", [128, D], F32)
                fw.dma("sp", g[:], R["modrow"][layer, v, 2 * D:3 * D].partition_broadcast(128), reads=["modrow"], writes=[f"mt_g_{nm}"])
                fw.op("dve", lambda: nc.vector.tensor_tensor(out=g[:], in0=g[:], in1=tmp2[:], op=ALU.mult),
                      reads=[f"mt_g_{nm}", "mt_tmp2"], writes=[f"mt_g_{nm}"])
                res[f"g_{nm}"] = (g, f"mt_g_{nm}")
        return res

    def norm_tile(self, P, layer, b, t, hT, hk, j, mt):
        nc, fw = self.nc, self.fw
        nm = "ctx" if t < 2 else "lat"
        sc, sck = mt[f"sc_{nm}"]
        sh, shk = mt[f"sh_{nm}"]
        xt, xk = P["xring"].next()
        fw.dma("sp", xt[:], self.src_rows(layer, b, t * 128, 128), reads=["x1c"] if layer == 1 else [], writes=[xk])
        jk, jkk = P["junk"].next()
        st, stk = P["stat"].next()
        fw.op("act", lambda: nc.scalar.activation(out=jk[:], in_=xt[:], func=AF.Square, scale=1.0 / 32.0, accum_out=st[:, 0:1]),
              reads=[xk], writes=[jkk, stk])
        fw.op("act", lambda: nc.scalar.activation(out=st[:, 1:2], in_=st[:, 0:1], func=AF.Sqrt, bias=EPS), reads=[stk], writes=[stk])
        fw.op("dve", lambda: nc.vector.reciprocal(out=st[:, 2:3], in_=st[:, 1:2]), reads=[stk], writes=[stk])
        t1, t1k = P["t1"].next()
        fw.op("dve", lambda: nc.vector.scalar_tensor_tensor(out=t1[:], in0=xt[:], scalar=st[:, 2:3], in1=sc[:],
                                                           op0=ALU.mult, op1=ALU.mult),
              reads=[xk, stk, sck], writes=[t1k])
        hb, hbk = P["hb"].next()
        fw.op("pool", lambda: nc.gpsimd.tensor_tensor(out=hb[:], in0=t1[:], in1=sh[:], op=ALU.add),
              reads=[t1k, shk], writes=[hbk])
        pT, pTk = P["pT"].next()
        for k in range(8):
            fw.op("pe", lambda: nc.tensor.transpose(out=pT[:, k, :], in_=hb[:, k * 128:(k + 1) * 128], identity=self.identb[:]),
                  reads=[hbk, "identb"], writes=[pTk])
        fw.op("act", lambda: nc.scalar.copy(out=hT[:, :, j * 128:(j + 1) * 128], in_=pT[:]), reads=[pTk], writes=[f"{hk}#{j}"])

    def rope_tok(self, P, src, nh, hd, rt, rtk, srck, dst, dstk, dst_off=0, src_off=0):
        nc, fw = self.nc, self.fw
        r = hd // 2
        x1 = src[:, :, src_off:src_off + r]
        x2 = src[:, :, src_off + r:src_off + hd]
        cosb = rt[:, 0:r].unsqueeze(1).to_broadcast([128, nh, r])
        sinb = rt[:, r:hd].unsqueeze(1).to_broadcast([128, nh, r])
        srcks = list(srck) if isinstance(srck, (list, tuple)) else [srck]
        ta, tak = P["ropet"].next()
        tb, tbk = P["ropet"].next()
        a = ta[:, 0:nh, 0:r]
        bb = tb[:, 0:nh, 0:r]
        fw.op("dve", lambda: nc.vector.tensor_tensor(out=a, in0=x1, in1=cosb, op=ALU.mult), reads=srcks + [rtk], writes=[tak])
        fw.op("dve", lambda: nc.vector.tensor_tensor(out=bb, in0=x2, in1=sinb, op=ALU.mult), reads=srcks + [rtk], writes=[tbk])
        fw.op("pool", lambda: nc.gpsimd.tensor_tensor(out=dst[:, :, dst_off:dst_off + r], in0=a, in1=bb, op=ALU.subtract),
              reads=[tak, tbk], writes=[dstk])
        tc_, tck = P["ropet"].next()
        td, tdk = P["ropet"].next()
        c = tc_[:, 0:nh, 0:r]
        d = td[:, 0:nh, 0:r]
        fw.op("dve", lambda: nc.vector.tensor_tensor(out=c, in0=x1, in1=sinb, op=ALU.mult), reads=srcks + [rtk], writes=[tck])
        fw.op("dve", lambda: nc.vector.tensor_tensor(out=d, in0=x2, in1=cosb, op=ALU.mult), reads=srcks + [rtk], writes=[tdk])
        fw.op("pool", lambda: nc.gpsimd.tensor_tensor(out=dst[:, :, dst_off + r:dst_off + hd], in0=c, in1=d, op=ALU.add),
              reads=[tck, tdk], writes=[dstk])

    def post_tile(self, P, py, pyk, g, gk, xin_ap, xin_reads, out_ap, out_key):
        nc, fw = self.nc, self.fw
        xt, xk = P["xres"].next()
        fw.dma("sp", xt[:], xin_ap, reads=xin_reads, writes=[xk])
        jk, jkk = P["junk"].next()
        st, stk = P["stat"].next()
        fw.op("act", lambda: nc.scalar.activation(out=jk[:], in_=py[:], func=AF.Square, scale=1.0 / 32.0, accum_out=st[:, 0:1]),
              reads=[pyk], writes=[jkk, stk])
        fw.op("act", lambda: nc.scalar.activation(out=st[:, 1:2], in_=st[:, 0:1], func=AF.Sqrt, bias=EPS), reads=[stk], writes=[stk])
        fw.op("dve", lambda: nc.vector.reciprocal(out=st[:, 2:3], in_=st[:, 1:2]), reads=[stk], writes=[stk])
        t2, t2k = P["t2"].next()
        fw.op("dve", lambda: nc.vector.scalar_tensor_tensor(out=t2[:], in0=py[:], scalar=st[:, 2:3], in1=g[:],
                                                           op0=ALU.mult, op1=ALU.mult),
              reads=[pyk, stk, gk], writes=[t2k])
        fw.op("pool", lambda: nc.gpsimd.tensor_tensor(out=t2[:], in0=t2[:], in1=xt[:], op=ALU.add),
              reads=[t2k, xk], writes=[t2k])
        fw.dma(self.QS, out_ap, t2[:], reads=[t2k], writes=[out_key])

    def layer0(self):
        nc, fw, I, R = self.nc, self.fw, self.I, self.R
        stop = getattr(self, "stop", None)
        with ExitStack() as esl:
            win = self.load_weight_bf16(esl, "l0_win", I["e_w_in"], E_IN)
            if stop == "l0w":
                return
            for b in range(NB):
                self.l0_stage_a(b, win)
                fw.barrier()
                if stop == "l0a0":
                    return
        if stop == "l0a":
            return
        with ExitStack() as esl:
            wout = self.load_weight_bf16(esl, "l0_wout", I["e_w_out"], D)
            for b in range(NB):
                self.l0_stage_b(b, wout)
                fw.barrier()

    def l0_stage_a(self, b, win):
        nc, fw, I, R = self.nc, self.fw, self.I, self.R
        with ExitStack() as es:
            mt = self.mod_tiles(es, 0, b, want_pre=True, want_post=False)
            P = {}
            P["xring"] = self.ring(es, "a_x", [128, D], F32, 2)
            P["junk"] = self.ring(es, "a_junk", [128, D], BF16, 2)
            P["stat"] = self.ring(es, "a_stat", [128, 4], F32, 4)
            P["t1"] = self.ring(es, "a_t1", [128, D], F32, 2)
            P["hb"] = self.ring(es, "a_hb", [128, D], BF16, 2)
            P["pT"] = self.ring(es, "a_pT", [128, 8, 128], BF16, 2, psum=True)
            P["ropet"] = self.ring(es, "a_ropet", [128, 8, 32], F32, 8)
            hTr = self.ring(es, "a_hT", [128, 8, 512], BF16, 3)
            ptok = self.ring(es, "a_ptok", [128, 512], F32, 2, psum=True)
            pfm = self.ring(es, "a_pfm", [128, 512], F32, 3, psum=True)
            phalo = self.ps(es, "a_phalo", [128, 4, 2, 2], F32)
            halo = self.ring(es, "a_halo", [128, 8, 2], BF16, 2)
            cw = self.sb(es, "a_cw", [128, 3, 4], F32)
            for j_ in range(3):
                fw.dma("sp", cw[:, j_, :], I["e_conv_w"][j_, :].rearrange("(c p) -> p c", p=128), writes=["a_cw"],
                       allow_slow_non_contiguous=True)
            rtr = self.ring(es, "a_rt", [128, 64], F32, 3)
            qbr = self.ring(es, "a_qb", [128, 8, 64], BF16, 2)
            kbr = self.ring(es, "a_kb", [128, 2, 64], BF16, 2)
            qTst = self.ring(es, "a_qTst", [128, 4, 512], BF16, 2)
            kTst = self.ring(es, "a_kTst", [128, 512], BF16, 2)
            vst = self.ring(es, "a_vst", [128, 4, 128], BF16, 2)
            zast = self.ring(es, "a_zast", [128, 512], F32, 3)
            bcs = self.ring(es, "a_bcs", [128, 512], F32, 2)
            uext = self.ring(es, "a_uext", [128, 514], F32, 2)
            acc = self.ring(es, "a_acc", [128, 512], F32, 2)
            szr = self.ring(es, "a_sz", [128, 512], F32, 2)
            hbh = self.ring(es, "a_hbh", [128, 2], F32, 2)
            bgst = self.ring(es, "a_bgst", [128, 512], BF16, 2)

            hts = {}

            def emit_norm(ci):
                tok0, ntok = CHUNKS[ci]
                hT, hk = hTr.next()
                hts[ci] = (hT, hk)
                for j in range(ntok // 128):
                    self.norm_tile(P, 0, b, tok0 // 128 + j, hT, hk, j, mt)

            def emit_proj(ci):
                tok0, ntok = CHUNKS[ci]
                nt = ntok // 128
                hT, hk = hts[ci]
                hkeys = [f"{hk}#{j}" for j in range(nt)]
                hl, hlk = halo.next()
                first = ci in (0, 1)
                last = ci in (0, len(CHUNKS) - 1)
                if first:
                    fw.op("dve", lambda: nc.vector.memset(hl[:, :, 0:1], 0.0), writes=[hlk])
                else:
                    pT_, pk_ = hts[ci - 1]
                    pn = CHUNKS[ci - 1][1]
                    fw.op("dve", lambda: nc.vector.tensor_copy(out=hl[:, :, 0:1], in_=pT_[:, :, pn - 1:pn]),
                          reads=[f"{pk_}#{pn // 128 - 1}"], writes=[hlk])
                if last:
                    fw.op("dve", lambda: nc.vector.memset(hl[:, :, 1:2], 0.0), writes=[hlk])
                else:
                    nT_, nk_ = hts[ci + 1]
                    fw.op("dve", lambda: nc.vector.tensor_copy(out=hl[:, :, 1:2], in_=nT_[:, :, 0:1]),
                          reads=[f"{nk_}#0"], writes=[hlk])
                qs_, qsk = qTst.next()
                ks_, ksk = kTst.next()
                vs_, vsk = vst.next()
                for j in range(nt):
                    t = tok0 // 128 + j
                    hkj = f"{hk}#{j}"
                    rt, rtk = rtr.next()
                    fw.dma("sp", rt[:], I["ropeA"][t * 128:(t + 1) * 128, :], writes=[rtk])
                    pkv, pkvk = ptok.next()
                    for k in range(8):
                        fw.op("pe", lambda: nc.tensor.matmul(pkv[:, 0:256], lhsT=hT[:, k, j * 128:(j + 1) * 128], rhs=win[:, k, 0:256],
                                                             start=(k == 0), stop=(k == 7)), reads=[hkj, "l0_win"], writes=[pkvk])
                    kb_, kbk = kbr.next()
                    self.rope_tok(P, pkv[:, 0:128].rearrange("p (h d) -> p h d", h=2), 2, 64, rt, rtk, pkvk, kb_, kbk)
                    fw.op("act", lambda: nc.scalar.copy(out=vs_[:, j, :], in_=pkv[:, 128:256]), reads=[pkvk], writes=[f"{vsk}#{j}"])
                    pq, pqk = ptok.next()
                    for k in range(8):
                        fw.op("pe", lambda: nc.tensor.matmul(pq[:], lhsT=hT[:, k, j * 128:(j + 1) * 128], rhs=win[:, k, 256:768],
                                                             start=(k == 0), stop=(k == 7)), reads=[hkj, "l0_win"], writes=[pqk])
                    qb_, qbk = qbr.next()
                    self.rope_tok(P, pq[:].rearrange("p (h d) -> p h d", h=8), 8, 64, rt, rtk, pqk, qb_, qbk)
                    pT, pTk = P["pT"].next()
                    qflat = qb_[:].rearrange("p h d -> p (h d)")
                    for c in range(4):
                        fw.op("pe", lambda: nc.tensor.transpose(out=pT[:, c, :], in_=qflat[:, c * 128:(c + 1) * 128], identity=self.identb[:]),
                              reads=[qbk, "identb"], writes=[pTk])
                    fw.op("pe", lambda: nc.tensor.transpose(out=pT[:, 4, :], in_=kb_[:].rearrange("p h d -> p (h d)"), identity=self.identb[:]),
                          reads=[kbk, "identb"], writes=[pTk])
                    fw.op("act", lambda: nc.scalar.copy(out=qs_[:, :, j * 128:(j + 1) * 128], in_=pT[:, 0:4, :]), reads=[pTk], writes=[f"{qsk}#{j}"])
                    fw.op("act", lambda: nc.scalar.copy(out=ks_[:, j * 128:(j + 1) * 128], in_=pT[:, 4, :]), reads=[pTk], writes=[f"{ksk}#{j}"])
                    pz, pzk = ptok.next()
                    for k in range(8):
                        fw.op("pe", lambda: nc.tensor.matmul(pz[:], lhsT=hT[:, k, j * 128:(j + 1) * 128], rhs=win[:, k, 2304:2816],
                                                             start=(k == 0), stop=(k == 7)), reads=[hkj, "l0_win"], writes=[pzk])
                    za_, zak = zast.next()
                    fw.op("act", lambda: nc.scalar.activation(out=za_[:], in_=pz[:], func=AF.Silu), reads=[pzk], writes=[zak])
                    fw.dma(self.QS, R["za0"][b, t * 128:(t + 1) * 128, :], za_[:], reads=[zak], writes=["za0"])
                sl = slice(tok0, tok0 + ntok)
                if getattr(self, "cut", 0) == 2:
                    return
                fw.dma(self.QS, R["qT0"][b].rearrange("j p t -> p j t")[:, :, sl], qs_[:, :, 0:ntok],
                       reads=[f"{qsk}#{j}" for j in range(nt)], writes=["qT0"])
                fw.dma(self.QS, R["kT0"][b][:, sl], ks_[:, 0:ntok], reads=[f"{ksk}#{j}" for j in range(nt)], writes=["kT0"])
                fw.dma(self.QS, R["v0"][b, sl, :].rearrange("(j p) d -> p j d", p=128), vs_[:, 0:nt, :],
                       reads=[f"{vsk}#{j}" for j in range(nt)], writes=["v0"])
                if getattr(self, "cut", 0) == 3:
                    return
                for c in range(4):
                    for g2 in range(2):
                        col0 = (1280 if g2 == 0 else 1792) + c * 128
                        for k in range(8):
                            fw.op("pe", lambda: nc.tensor.matmul(phalo[:, c, g2, :], lhsT=win[:, k, col0:col0 + 128], rhs=hl[:, k, :],
                                                                 start=(k == 0), stop=(k == 7)), reads=[hlk, "l0_win"], writes=["a_phalo"])
                if getattr(self, "cut", 0) == 4:
                    return
                for c in range(4):
                    def fm(col0):
                        pt_, ptk = pfm.next()
                        for k in range(8):
                            fw.op("pe", lambda: nc.tensor.matmul(pt_[:, 0:ntok], lhsT=win[:, k, col0:col0 + 128], rhs=hT[:, k, 0:ntok],
                                                                 start=(k == 0), stop=(k == 7)), reads=hkeys + ["l0_win"], writes=[ptk])
                        return pt_, ptk
                    pbc, pbck = fm(1280 + c * 128)
                    bc_, bck = bcs.next()
                    fw.op("act", lambda: nc.scalar.copy(out=bc_[:, 0:ntok], in_=pbc[:, 0:ntok]), reads=[pbck], writes=[bck])
                    pbx, pbxk = fm(1792 + c * 128)
                    ue, uek = uext.next()
                    fw.op("dve", lambda: nc.vector.tensor_tensor(out=ue[:, 1:ntok + 1], in0=bc_[:, 0:ntok], in1=pbx[:, 0:ntok], op=ALU.mult),
                          reads=[bck, pbxk], writes=[uek])
                    hb_, hbk_ = hbh.next()
                    fw.op("act", lambda: nc.scalar.copy(out=hb_[:], in_=phalo[:, c, 0, :]), reads=["a_phalo"], writes=[hbk_])
                    fw.op("dve", lambda: nc.vector.tensor_tensor(out=ue[:, 0:1], in0=hb_[:, 0:1], in1=phalo[:, c, 1, 0:1], op=ALU.mult),
                          reads=[hbk_, "a_phalo"], writes=[uek])
                    fw.op("dve", lambda: nc.vector.tensor_tensor(out=ue[:, ntok + 1:ntok + 2], in0=hb_[:, 1:2], in1=phalo[:, c, 1, 1:2], op=ALU.mult),
                          reads=[hbk_, "a_phalo"], writes=[uek])
                    ac, ack = acc.next()
                    fw.op("dve", lambda: nc.vector.tensor_scalar(out=ac[:, 0:ntok], in0=ue[:, 0:ntok], scalar1=cw[:, 0, c:c + 1], scalar2=None, op0=ALU.mult),
                          reads=[uek, "a_cw"], writes=[ack])
                    fw.op("dve", lambda: nc.vector.scalar_tensor_tensor(out=ac[:, 0:ntok], in0=ue[:, 1:ntok + 1], scalar=cw[:, 1, c:c + 1], in1=ac[:, 0:ntok],
                                                                       op0=ALU.mult, op1=ALU.add), reads=[uek, "a_cw", ack], writes=[ack])
                    fw.op("dve", lambda: nc.vector.scalar_tensor_tensor(out=ac[:, 0:ntok], in0=ue[:, 2:ntok + 2], scalar=cw[:, 2, c:c + 1], in1=ac[:, 0:ntok],
                                                                       op0=ALU.mult, op1=ALU.add), reads=[uek, "a_cw", ack], writes=[ack])
                    pbb, pbbk = fm(768 + c * 128)
                    fw.op("dve", lambda: nc.vector.tensor_tensor(out=ac[:, 0:ntok], in0=ac[:, 0:ntok], in1=pbb[:, 0:ntok], op=ALU.mult),
                          reads=[ack, pbbk], writes=[ack])
                    pzb, pzbk = fm(2816 + c * 128)
                    sz_, szk = szr.next()
                    fw.op("act", lambda: nc.scalar.activation(out=sz_[:, 0:ntok], in_=pzb[:, 0:ntok], func=AF.Silu), reads=[pzbk], writes=[szk])
                    bg_, bgk = bgst.next()
                    fw.op("pool", lambda: nc.gpsimd.tensor_tensor(out=bg_[:, 0:ntok], in0=ac[:, 0:ntok], in1=sz_[:, 0:ntok], op=ALU.mult),
                          reads=[ack, szk], writes=[bgk])
                    fw.dma(self.QS, R["bg0T"][b, c, :, sl], bg_[:, 0:ntok], reads=[bgk], writes=["bg0T"])

            n = len(CHUNKS)
            emit_norm(0)
            cut = getattr(self, "cut", 0)
            if cut == 1:
                return
            for ci in range(n):
                if ci + 1 < n:
                    emit_norm(ci + 1)
                emit_proj(ci)
                if cut >= 2 and ci >= cut - 5:
                    return

    def l0_stage_b(self, b, wout):
        nc, fw, I, R = self.nc, self.fw, self.I, self.R
        with ExitStack() as es:
            mt = self.mod_tiles(es, 0, b, want_pre=False, want_post=True)
            P = {}
            P["xres"] = self.ring(es, "b_xres", [128, D], F32, 2)
            P["junk"] = self.ring(es, "b_junk", [128, D], BF16, 2)
            P["stat"] = self.ring(es, "b_stat", [128, 4], F32, 4)
            P["t2"] = self.ring(es, "b_t2", [128, D], F32, 2)
            kTp = [[None, None], [None, None]]
            for kv_ in range(2):
                for p_ in range(2):
                    kt_ = self.sb(es, f"b_kTp{kv_}{p_}", [128, T], BF16)
                    key_ = f"b_kTp{kv_}{p_}"
                    fw.op("pool", lambda: nc.gpsimd.memset(kt_[:], 0.0), writes=[key_])
                    fw.dma("sp", kt_[p_ * 64:(p_ + 1) * 64, :], R["kT0"][b, kv_ * 64:(kv_ + 1) * 64, :], reads=["kT0"], writes=[key_])
                    kTp[kv_][p_] = (kt_, key_)
            vall = self.sb(es, "b_vall", [128, NT, 2, 80], BF16)
            fw.op("pool", lambda: nc.gpsimd.memset(vall[:], 1.0), writes=["b_vall"])
            for t in range(NT):
                fw.dma("sp", vall[:, t, :, 0:64], R["v0"][b, t * 128:(t + 1) * 128, :].rearrange("p (h d) -> p h d", h=2),
                       reads=["v0"], writes=["b_vall"])
            mprev = self.sb(es, "b_mprev", [128, 2, 128], BF16)
            mnext = self.sb(es, "b_mnext", [128, 2, 128], BF16)
            fw.op("dve", lambda: nc.vector.tensor_copy(out=mprev[:], in_=self.cf[:, C_MPREV, :].unsqueeze(1).to_broadcast([128, 2, 128])),
                  reads=["c128f"], writes=["b_mprev"])
            fw.op("dve", lambda: nc.vector.tensor_copy(out=mnext[:], in_=self.cf[:, C_MNEXT, :].unsqueeze(1).to_broadcast([128, 2, 128])),
                  reads=["c128f"], writes=["b_mnext"])
            masks = {"P": (mprev, "b_mprev"), "N": (mnext, "b_mnext")}
            snk = self.sb(es, "b_snk", [128, 8], F32)
            esk = self.sb(es, "b_esk", [128, 8], F32)
            fw.dma("sp", snk[:], I["e_sink"].partition_broadcast(128), writes=["b_snk"])
            for kv_ in range(2):
                fw.op("act", lambda: nc.scalar.activation(out=esk[:, kv_ * 4:(kv_ + 1) * 4].rearrange("q (p i) -> q p i", p=2),
                                                          in_=snk[:, kv_ * 4:(kv_ + 1) * 4].rearrange("q (i p) -> q p i", p=2), func=AF.Exp),
                      reads=["b_snk"], writes=["b_esk"])
            psS = self.ring(es, "b_psS", [128, 512], F32, 3, psum=True)
            poR = self.ring(es, "b_po", [128, 4, 80], F32, 2, psum=True)
            pT2 = self.ps(es, "b_pT2", [128, 4, 128], BF16)
            py = self.ps(es, "b_py", [128, D], F32)
            qblk = self.ring(es, "b_qblk", [128, 4, 128], BF16, 2)
            zablk = self.ring(es, "b_zablk", [128, 512], F32, 2)
            bgblk = self.ring(es, "b_bgblk", [128, 4, 128], BF16, 2)
            ptr = self.ring(es, "b_pt", [128, 512], BF16, 6)
            asb = self.ring(es, "b_asb", [128, 8, 64], F32, 2)
            den = self.ring(es, "b_den", [128, 8], F32, 4)
            agr = self.ring(es, "b_ag", [128, 512], BF16, 2)
            agT = self.ring(es, "b_agT", [128, 4, 128], BF16, 2)
            scale = 64 ** -0.5

            loads = {}

            def emit_loads(t):
                q_, qk = qblk.next()
                fw.dma("sp", q_[:], R["qT0"][b].rearrange("j p t -> p j t")[:, :, t * 128:(t + 1) * 128], reads=["qT0"], writes=[qk])
                z_, zk = zablk.next()
                fw.dma("sp", z_[:], R["za0"][b, t * 128:(t + 1) * 128, :], reads=["za0"], writes=[zk])
                g_, gk = bgblk.next()
                fw.dma("sp", g_[:], R["bg0T"][b].rearrange("j p t -> p j t")[:, :, t * 128:(t + 1) * 128], reads=["bg0T"], writes=[gk])
                loads[t] = (q_, qk, z_, zk, g_, gk)

            def emit_block(t):
                q_, qk, z_, zk, g_, gk = loads.pop(t)
                if self.cutc == 5:
                    return
                if t < 2:
                    kbs = [(0, None), (1, None)]
                else:
                    qi = t - 2
                    kbs = [(0, None), (1, None)]
                    if qi > 0:
                        kbs.append((t - 1, "P"))
                    kbs.append((t, None))
                    if qi < 15:
                        kbs.append((t + 1, "N"))
                a_, ak = asb.next()
                for kv in range(2):
                    pts = []
                    for (kb, mk) in kbs:
                        ps_, psk = psS.next()
                        for p in range(2):
                            kt, ktk = kTp[kv][p]
                            fw.op("pe", lambda: nc.tensor.matmul(ps_[:, p * 256:(p + 1) * 256],
                                                                 lhsT=kt[:, kb * 128:(kb + 1) * 128],
                                                                 rhs=q_[:, 2 * kv:2 * kv + 2, :].rearrange("p a b -> p (a b)"),
                                                                 start=True, stop=(mk is None)),
                                  reads=[ktk, qk], writes=[psk])
                            if mk is not None:
                                m_, mkk = masks[mk]
                                fw.op("pe", lambda: nc.tensor.matmul(ps_[:, p * 256:(p + 1) * 256], lhsT=self.identb[:],
                                                                     rhs=m_[:].rearrange("p a b -> p (a b)"), start=False, stop=True),
                                      reads=["identb", mkk], writes=[psk])
                        pt_, ptk = ptr.next()
                        fw.op("act", lambda: nc.scalar.activation(out=pt_[:], in_=ps_[:], func=AF.Exp, scale=scale), reads=[psk], writes=[ptk])
                        pts.append((pt_, ptk, kb))
                        if self.cutc == 1:
                            return
                    if self.cutc == 2:
                        return
                    po, pok = poR.next()
                    for slot in range(4):
                        for n_, (pt_, ptk, kb) in enumerate(pts):
                            fw.op("pe", lambda: nc.tensor.matmul(po[:, slot, 0:65], lhsT=pt_[:, slot * 128:(slot + 1) * 128], rhs=vall[:, kb, kv, 0:65],
                                                                 start=(n_ == 0), stop=(n_ == len(pts) - 1)),
                                  reads=[ptk, "b_vall"], writes=[pok])
                    if self.cutc == 3:
                        return
                    dn, dnk = den.next()
                    fw.op("dve", lambda: nc.vector.tensor_tensor(out=dn[:, 0:4], in0=po[:, :, 64], in1=esk[:, kv * 4:(kv + 1) * 4], op=ALU.add),
                          reads=[pok, "b_esk"], writes=[dnk])
                    fw.op("dve", lambda: nc.vector.reciprocal(out=dn[:, 4:8], in_=dn[:, 0:4]), reads=[dnk], writes=[dnk])
                    for slot in range(4):
                        p, i = slot // 2, slot % 2
                        h = 4 * kv + 2 * i + p
                        fw.op("dve", lambda: nc.vector.tensor_scalar(out=a_[:, h, :], in0=po[:, slot, 0:64], scalar1=dn[:, 4 + slot:5 + slot],
                                                                    scalar2=None, op0=ALU.mult),
                              reads=[pok, dnk], writes=[ak])
                    if self.cutc == 4:
                        return
                if getattr(self, "cutb", 0) == 2:
                    return
                ag, agk = agr.next()
                fw.op("pool", lambda: nc.gpsimd.tensor_tensor(out=ag[:], in0=a_[:].rearrange("p h d -> p (h d)"), in1=z_[:], op=ALU.mult),
                      reads=[ak, zk], writes=[agk])
                for c in range(4):
                    fw.op("pe", lambda: nc.tensor.transpose(out=pT2[:, c, :], in_=ag[:, c * 128:(c + 1) * 128], identity=self.identb[:]),
                          reads=[agk, "identb"], writes=["b_pT2"])
                at, atk = agT.next()
                fw.op("act", lambda: nc.scalar.copy(out=at[:], in_=pT2[:]), reads=["b_pT2"], writes=[atk])
                if getattr(self, "cutb", 0) == 3:
                    return
                for half in range(2):
                    for k in range(8):
                        src, srk = (at, atk) if k < 4 else (g_, gk)
                        fw.op("pe", lambda: nc.tensor.matmul(py[:, half * 512:(half + 1) * 512], lhsT=src[:, k % 4, :],
                                                             rhs=wout[:, k, half * 512:(half + 1) * 512], start=(k == 0), stop=(k == 7)),
                              reads=[srk, "l0_wout"], writes=["b_py"])
                nm = "ctx" if t < 2 else "lat"
                g, gkk = mt[f"g_{nm}"]
                self.post_tile(P, py, "b_py", g, gkk, self.src_rows(0, b, t * 128, 128), [],
                               R["x1c"][b, t * 128:(t + 1) * 128, :], "x1c")

            cutb = getattr(self, "cutb", 0)
            if cutb == 1:
                return
            emit_loads(0)
            for t in range(NT):
                if t + 1 < NT:
                    emit_loads(t + 1)
                emit_block(t)
                if cutb in (2, 3, 4) or (cutb >= 5 and t >= cutb - 3):
                    return

    def layer1(self):
        nc, fw, I, R = self.nc, self.fw, self.I, self.R
        stop = getattr(self, "stop", None)
        with ExitStack() as esl:
            win = self.load_weight_bf16(esl, "l1_win", I["o_w_in"], O_IN)
            nq = self.sb(esl, "l1_nq", [128, 6], F32)
            nkv = self.sb(esl, "l1_nkv", [128, 2], F32)
            fw.dma("sp", nq[:], I["o_q_norm_w"].rearrange("(k p) -> p k", p=128), writes=["l1_nq"], allow_slow_non_contiguous=True)
            fw.dma("sp", nkv[:], I["o_kv_norm_w"].rearrange("(k p) -> p k", p=128), writes=["l1_nkv"], allow_slow_non_contiguous=True)
            wuq = self.sb(esl, "l1_wuq", [128, 6, 768], BF16)
            wukvk = self.sb(esl, "l1_wukvk", [128, 2, 512], BF16)
            wukvv = self.sb(esl, "l1_wukvv", [128, 2, 512], BF16)
            with ExitStack() as es2:
                stg = self.ring(es2, "l1_stg", [128, 1024], F32, 2)
                for c in range(6):
                    st, sk = stg.next()
                    fw.dma("sp", st[:, 0:768], I["o_w_uq"][c * 128:(c + 1) * 128, :], writes=[sk])
                    fw.op("dve", lambda: nc.vector.tensor_scalar(out=wuq[:, c, :], in0=st[:, 0:768], scalar1=nq[:, c:c + 1], scalar2=None, op0=ALU.mult),
                          reads=[sk, "l1_nq"], writes=["l1_wuq"])
                for c in range(2):
                    st, sk = stg.next()
                    fw.dma("sp", st[:], I["o_w_ukv"][c * 128:(c + 1) * 128, :], writes=[sk])
                    sv = st[:].rearrange("p (h two d) -> p h two d", two=2, d=64)
                    fw.op("dve", lambda: nc.vector.tensor_scalar(out=wukvk[:, c, :].rearrange("p (h d) -> p h d", d=64), in0=sv[:, :, 0, :],
                                                                scalar1=nkv[:, c:c + 1], scalar2=None, op0=ALU.mult),
                          reads=[sk, "l1_nkv"], writes=["l1_wukvk"])
                    fw.op("dve", lambda: nc.vector.tensor_scalar(out=wukvv[:, c, :].rearrange("p (h d) -> p h d", d=64), in0=sv[:, :, 1, :],
                                                                scalar1=nkv[:, c:c + 1], scalar2=None, op0=ALU.mult),
                          reads=[sk, "l1_nkv"], writes=["l1_wukvv"])
                fw.barrier()
            if self.cut1 == 1:
                return
            for b in range(NB):
                if _os_env("KONLYB") and int(_os_env("KONLYB")) != b:
                    continue
                self.l1_stage_a(0 if _os_env("KSAMEB") else b, win, wuq, wukvk, wukvv)
                fw.barrier()
                if self.cut1 >= 2:
                    return
        if stop == "l1a":
            return
        for b in range(NB):
            self.l1_stage_b(b)
            fw.barrier()
        if stop == "l1b":
            return
        with ExitStack() as esl:
            wout = self.load_weight_bf16(esl, "l1_wout", I["o_w_out"], D)
            for b in range(NB):
                with ExitStack() as esb:
                    hsum = self.sb(esb, "c_hsum", [128, 16, 4, 128], F32)
                    self.l1_stage_c1(b, hsum)
                    fw.barrier()
                    self.l1_stage_c2(b, hsum, wout)
                    fw.barrier()

    def l1_stage_a(self, b, win, wuq, wukvk, wukvv):
        nc, fw, I, R = self.nc, self.fw, self.I, self.R
        CH = [(0, 256)] + [(256 + 512 * i, 512) for i in range(4)]
        onesf = self.onesb[:]
        with ExitStack() as es:
            mt = self.mod_tiles(es, 1, b, want_pre=True, want_post=False)
            P = {}
            P["xring"] = self.ring(es, "a_x", [128, D], F32, 2)
            P["junk"] = self.ring(es, "a_junk", [128, D], BF16, 1)
            P["stat"] = self.ring(es, "a_stat", [128, 4], F32, 4)
            P["t1"] = self.ring(es, "a_t1", [128, D], F32, 1)
            P["hb"] = self.ring(es, "a_hb", [128, D], BF16, 2)
            P["pT"] = self.ring(es, "a_pT", [128, 8, 128], BF16, 2, psum=True)
            P["ropet"] = self.ring(es, "a_ropet", [128, 8, 32], F32, 8)
            hTr = self.ring(es, "a_hT", [128, 8, 512], BF16, 2)
            pfm = self.ring(es, "a_pfm", [128, 512], F32, 2, psum=True)
            ptok = self.ring(es, "a_ptok", [128, 512], F32, 2, psum=True)
            prep = self.ps(es, "a_prep", [128, 512], F32)
            pcol = self.ps(es, "a_pcol", [128, 16], F32)
            ckvTr = self.ring(es, "a_ckvT", [128, 2, 512], BF16, 2)
            sqkr = self.ring(es, "a_sqk", [128, 2, 512], BF16, 2)
            rrepr = self.ring(es, "a_rrep", [128, 2, 512], F32, 1)
            rcolr = self.ring(es, "a_rcol", [128, 16], F32, 2)
            knstr = self.ring(es, "a_knst", [128, 4, 512], BF16, 1)
            vstr = self.ring(es, "a_vst", [128, 512], BF16, 2)
            rtr = self.ring(es, "a_rt", [128, 32], F32, 3)
            krfr = self.ring(es, "a_krf", [128, 1, 32], BF16, 2)
            krstr = self.ring(es, "a_krst", [32, 512], BF16, 2)
            mkstr = self.ring(es, "a_mkst", [128, 256], BF16, 2)
            mkTstr = self.ring(es, "a_mkTst", [128, 512], BF16, 2)
            mvstr = self.ring(es, "a_mvst", [128, 512], BF16, 2)
            mgstr = self.ring(es, "a_mgst", [128, 16], F32, 2)
            cqTr = self.ring(es, "a_cqT", [128, 6, 512], BF16, 1)
            sqqr = self.ring(es, "a_sqq", [128, 6, 512], BF16, 1)
            qfr = self.ring(es, "a_qf", [128, 8, 96], F32, 1)
            qbfr = self.ring(es, "a_qbf", [128, 8, 96], BF16, 2)
            qstr = self.ring(es, "a_qst", [128, 8, 512], BF16, 1)
            mqTstr = self.ring(es, "a_mqTst", [128, 512], BF16, 2)
            mostr = self.ring(es, "a_most", [128, 512], F32, 2)
            zstr = self.ring(es, "a_zst", [128, 512], F32, 2)

            hts = {}

            def emit_norm(ci):
                tok0, ntok = CH[ci]
                hT, hk = hTr.next()
                hts[ci] = (hT, hk)
                for j in range(ntok // 128):
                    self.norm_tile(P, 1, b, tok0 // 128 + j, hT, hk, j, mt)

            def mm_fm(hT, hkeys, col0, ntok):
                pf, pfk = pfm.next()
                for k in range(8):
                    fw.op("pe", lambda: nc.tensor.matmul(pf[:, 0:ntok], lhsT=win[:, k, col0:col0 + 128], rhs=hT[:, k, 0:ntok],
                                                         start=(k == 0), stop=(k == 7)), reads=hkeys + ["l1_win"], writes=[pfk])
                return pf, pfk

            def mm_tok(hT, hkj, j, col0, ncol, off=0, pt_=None, ptk=None):
                if pt_ is None:
                    pt_, ptk = ptok.next()
                for k in range(8):
                    fw.op("pe", lambda: nc.tensor.matmul(pt_[:, off:off + ncol], lhsT=hT[:, k, j * 128:(j + 1) * 128], rhs=win[:, k, col0:col0 + ncol],
                                                         start=(k == 0), stop=(k == 7)), reads=[hkj, "l1_win"], writes=[ptk])
                return pt_, ptk

            def emit_proj(ci):
                tok0, ntok = CH[ci]
                nt = ntok // 128
                is_lat = tok0 >= LC
                hT, hk = hts[ci]
                hkeys = [f"{hk}#{j}" for j in range(nt)]
                sl = slice(tok0, tok0 + ntok)
                ckvT, ckvk = ckvTr.next()
                sqk, sqkk = sqkr.next()
                for c in range(2):
                    pf, pfk = mm_fm(hT, hkeys, c * 128, ntok)
                    fw.op("act", lambda: nc.scalar.copy(out=ckvT[:, c, 0:ntok], in_=pf[:, 0:ntok]), reads=[pfk], writes=[f"{ckvk}#{c}"])
                    fw.op("act", lambda: nc.scalar.activation(out=sqk[:, c, 0:ntok], in_=pf[:, 0:ntok], func=AF.Square), reads=[pfk], writes=[f"{sqkk}#{c}"])
                ckeys = [f"{ckvk}#0", f"{ckvk}#1"]
                skeys = [f"{sqkk}#0", f"{sqkk}#1"]
                for c in range(2):
                    fw.op("pe", lambda: nc.tensor.matmul(prep[:, 0:ntok], lhsT=onesf, rhs=sqk[:, c, 0:ntok], start=(c == 0), stop=(c == 1)),
                          reads=skeys + ["c128f"], writes=["a_prep"])
                rrep, rrepk = rrepr.next()
                fw.op("act", lambda: nc.scalar.activation(out=rrep[:, 0, 0:ntok], in_=prep[:, 0:ntok], func=AF.Sqrt, bias=EPS, scale=1.0 / 256.0),
                      reads=["a_prep"], writes=[rrepk])
                fw.op("dve", lambda: nc.vector.reciprocal(out=rrep[:, 1, 0:ntok], in_=rrep[:, 0, 0:ntok]), reads=[rrepk], writes=[rrepk])
                rcol, rcolk = rcolr.next()
                for j in range(nt):
                    for c in range(2):
                        fw.op("pe", lambda: nc.tensor.matmul(pcol[:, j:j + 1], lhsT=sqk[:, c, j * 128:(j + 1) * 128], rhs=onesf[:, 0:1],
                                                             start=(c == 0), stop=(c == 1)), reads=skeys + ["c128f"], writes=["a_pcol"])
                fw.op("act", lambda: nc.scalar.activation(out=rcol[:, 4:4 + nt], in_=pcol[:, 0:nt], func=AF.Sqrt, bias=EPS, scale=1.0 / 256.0),
                      reads=["a_pcol"], writes=[rcolk])
                fw.op("dve", lambda: nc.vector.reciprocal(out=rcol[:, 0:nt], in_=rcol[:, 4:4 + nt]), reads=[rcolk], writes=[rcolk])
                if self.cut1 == 2:
                    return
                knst, knk = knstr.next()
                for pair in range(4):
                    pf, pfk = pfm.next()
                    for c in range(2):
                        fw.op("pe", lambda: nc.tensor.matmul(pf[:, 0:ntok], lhsT=wukvk[:, c, pair * 128:(pair + 1) * 128], rhs=ckvT[:, c, 0:ntok],
                                                             start=(c == 0), stop=(c == 1)), reads=ckeys + ["l1_wukvk"], writes=[pfk])
                    fw.op("dve", lambda: nc.vector.tensor_tensor(out=knst[:, pair, 0:ntok], in0=pf[:, 0:ntok], in1=rrep[:, 1, 0:ntok], op=ALU.mult),
                          reads=[pfk, rrepk], writes=[f"{knk}#{pair}"])
                kview = R["kT1"][b].rearrange("(j two) r t -> two r j t", two=2)
                for two_ in range(2):
                    fw.dma(self.QS, kview[two_, 0:64, :, sl], knst[two_ * 64:(two_ + 1) * 64, :, 0:ntok],
                           reads=[f"{knk}#{p_}" for p_ in range(4)], writes=["kT1"])
                if self.cut1 == 3:
                    return
                krst, krk = krstr.next()
                for j in range(nt):
                    t = tok0 // 128 + j
                    hkj = f"{hk}#{j}"
                    tsl = slice(t * 128, (t + 1) * 128)
                    pt_, ptk = ptok.next()
                    for c in range(2):
                        fw.op("pe", lambda: nc.tensor.matmul(pt_[:, 0:512], lhsT=ckvT[:, c, j * 128:(j + 1) * 128], rhs=wukvv[:, c, :],
                                                             start=(c == 0), stop=(c == 1)), reads=ckeys + ["l1_wukvv"], writes=[ptk])
                    vs_, vsk = vstr.next()
                    fw.op("act", lambda: nc.scalar.mul(out=vs_[:], in_=pt_[:, 0:512], mul=rcol[:, j:j + 1]), reads=[ptk, rcolk], writes=[vsk])
                    fw.dma(self.QS, R["v1"][b, tsl, :], vs_[:], reads=[vsk], writes=["v1"])
                    pt_, ptk = mm_tok(hT, hkj, j, 256, 32)
                    mm_tok(hT, hkj, j, 1056, 16, off=64, pt_=pt_, ptk=ptk)
                    rt, rtk = rtr.next()
                    fw.dma("sp", rt[:], I["ropeC"][tsl, :], writes=[rtk])
                    krf, krfk = krfr.next()
                    self.rope_tok(P, pt_[:, 0:32].rearrange("p (h d) -> p h d", h=1), 1, 32, rt, rtk, ptk, krf, krfk)
                    pT, pTk = P["pT"].next()
                    fw.op("pe", lambda: nc.tensor.transpose(out=pT[0:32, 0, :], in_=krf[:, 0, :], identity=self.identb[:]),
                          reads=[krfk, "identb"], writes=[pTk])
                    fw.op("act", lambda: nc.scalar.copy(out=krst[0:32, j * 128:(j + 1) * 128], in_=pT[0:32, 0, :]), reads=[pTk], writes=[f"{krk}#{j}"])
                    mg_, mgk = mgstr.next()
                    fw.op("act", lambda: nc.scalar.copy(out=mg_[:], in_=pt_[:, 64:80]), reads=[ptk], writes=[mgk])
                    fw.dma(self.QS, R["mg1"][b, tsl, :], mg_[:], reads=[mgk], writes=["mg1"])
                    pt_, ptk = mm_tok(hT, hkj, j, 288, 256)
                    mk_, mkk = mkstr.next()
                    fw.op("act", lambda: nc.scalar.mul(out=mk_[:], in_=pt_[:, 0:256], mul=0.125), reads=[ptk], writes=[mkk])
                    fw.dma(self.QS, R["mk1"][b, tsl, :], mk_[:], reads=[mkk], writes=["mk1"])
                    pt_, ptk = mm_tok(hT, hkj, j, 544, 512)
                    mv_, mvk = mvstr.next()
                    fw.op("act", lambda: nc.scalar.copy(out=mv_[:], in_=pt_[:, 0:512]), reads=[ptk], writes=[mvk])
                    fw.dma(self.QS, R["mv1"][b, tsl, :], mv_[:], reads=[mvk], writes=["mv1"])
                if self.cut1 == 4:
                    return
                for h in range(8):
                    fw.dma(self.QS, R["kT1"][b, h, 64:96, sl], krst[0:32, 0:ntok], reads=[f"{krk}#{j}" for j in range(nt)], writes=["kT1"])
                for c in range(2):
                    pf, pfk = mm_fm(hT, hkeys, 288 + c * 128, ntok)
                    mkT_, mkTk = mkTstr.next()
                    fw.op("act", lambda: nc.scalar.mul(out=mkT_[:, 0:ntok], in_=pf[:, 0:ntok], mul=0.125), reads=[pfk], writes=[mkTk])
                    fw.dma(self.QS, R["mkT1"][b, c * 128:(c + 1) * 128, sl], mkT_[:, 0:ntok], reads=[mkTk], writes=["mkT1"])
                if not is_lat:
                    return
                ls = tok0 - LC
                lsl = slice(ls, ls + ntok)
                cqT, cqk = cqTr.next()
                sqq, sqqk = sqqr.next()
                for c in range(6):
                    pf, pfk = mm_fm(hT, hkeys, 1072 + c * 128, ntok)
                    fw.op("act", lambda: nc.scalar.copy(out=cqT[:, c, 0:ntok], in_=pf[:, 0:ntok]), reads=[pfk], writes=[f"{cqk}#{c}"])
                    fw.op("act", lambda: nc.scalar.activation(out=sqq[:, c, 0:ntok], in_=pf[:, 0:ntok], func=AF.Square), reads=[pfk], writes=[f"{sqqk}#{c}"])
                cqkeys = [f"{cqk}#{c}" for c in range(6)]
                sqkeys = [f"{sqqk}#{c}" for c in range(6)]
                for j in range(nt):
                    for c in range(6):
                        fw.op("pe", lambda: nc.tensor.matmul(pcol[:, 8 + j:9 + j], lhsT=sqq[:, c, j * 128:(j + 1) * 128], rhs=onesf[:, 0:1],
                                                             start=(c == 0), stop=(c == 5)), reads=sqkeys + ["c128f"], writes=["a_pcol"])
                fw.op("act", lambda: nc.scalar.activation(out=rcol[:, 12:12 + nt], in_=pcol[:, 8:8 + nt], func=AF.Sqrt, bias=EPS, scale=1.0 / 768.0),
                      reads=["a_pcol"], writes=[rcolk])
                fw.op("dve", lambda: nc.vector.reciprocal(out=rcol[:, 8:8 + nt], in_=rcol[:, 12:12 + nt]), reads=[rcolk], writes=[rcolk])
                qst, qstk = qstr.next()
                for j in range(nt):
                    t = tok0 // 128 + j
                    tsl = slice(t * 128, (t + 1) * 128)
                    qf, qfk = qfr.next()
                    qflat = qf[:].rearrange("p h d -> p (h d)")
                    for (c0, cn) in ((0, 512), (512, 256)):
                        pt_, ptk = ptok.next()
                        for c in range(6):
                            fw.op("pe", lambda: nc.tensor.matmul(pt_[:, 0:cn], lhsT=cqT[:, c, j * 128:(j + 1) * 128], rhs=wuq[:, c, c0:c0 + cn],
                                                                 start=(c == 0), stop=(c == 5)), reads=cqkeys + ["l1_wuq"], writes=[ptk])
                        fw.op("act", lambda: nc.scalar.mul(out=qflat[:, c0:c0 + cn], in_=pt_[:, 0:cn], mul=rcol[:, 8 + j:9 + j]),
                              reads=[ptk, rcolk], writes=[f"{qfk}#{c0}"])
                    qfkeys = [f"{qfk}#0", f"{qfk}#512"]
                    qbf, qbk = qbfr.next()
                    fw.op("pool", lambda: nc.gpsimd.tensor_copy(out=qbf[:, :, 0:64], in_=qf[:, :, 0:64]), reads=qfkeys, writes=[f"{qbk}#n"])
                    rt, rtk = rtr.next()
                    fw.dma("sp", rt[:], I["ropeC"][tsl, :], writes=[rtk])
                    self.rope_tok(P, qf, 8, 32, rt, rtk, qfkeys, qbf, f"{qbk}#r", dst_off=64, src_off=64)
                    pT, pTk = P["pT"].next()
                    for h in range(8):
                        fw.op("pe", lambda: nc.tensor.transpose(out=pT[0:96, h, :], in_=qbf[:, h, :], identity=self.identb[:]),
                              reads=[f"{qbk}#n", f"{qbk}#r", "identb"], writes=[pTk])
                    fw.op("act", lambda: nc.scalar.copy(out=qst[0:96, :, j * 128:(j + 1) * 128], in_=pT[0:96, :, :]), reads=[pTk], writes=[f"{qstk}#{j}"])
                    lt = t - 2
                    ltsl = slice(lt * 128, (lt + 1) * 128)
                    pt_, ptk = mm_tok(hT, f"{hk}#{j}", j, 2096, 512)
                    mo_, mok = mostr.next()
                    fw.op("act", lambda: nc.scalar.activation(out=mo_[:], in_=pt_[:, 0:512], func=AF.Sigmoid), reads=[ptk], writes=[mok])
                    fw.dma(self.QS, R["mo1"][b, ltsl, :], mo_[:], reads=[mok], writes=["mo1"])
                    for zh in range(2):
                        pt_, ptk = mm_tok(hT, f"{hk}#{j}", j, 2608 + zh * 512, 512)
                        z_, zk = zstr.next()
                        fw.op("act", lambda: nc.scalar.activation(out=z_[:], in_=pt_[:, 0:512], func=AF.Silu), reads=[ptk], writes=[zk])
                        fw.dma(self.QS, R["z1"][b, ltsl, zh * 512:(zh + 1) * 512], z_[:], reads=[zk], writes=["z1"])
                fw.dma(self.QS, R["qT1"][b].rearrange("h r t -> r h t")[:, :, lsl], qst[0:96, :, 0:ntok],
                       reads=[f"{qstk}#{j}" for j in range(nt)], writes=["qT1"])
                for c in range(2):
                    pf, pfk = mm_fm(hT, hkeys, 1840 + c * 128, ntok)
                    mq_, mqk = mqTstr.next()
                    fw.op("act", lambda: nc.scalar.copy(out=mq_[:, 0:ntok], in_=pf[:, 0:ntok]), reads=[pfk], writes=[mqk])
                    fw.dma(self.QS, R["mqT1"][b, c * 128:(c + 1) * 128, lsl], mq_[:, 0:ntok], reads=[mqk], writes=["mqT1"])

            n = len(CH)
            emit_norm(0)
            for ci in range(n):
                if ci + 1 < n:
                    emit_norm(ci + 1)
                emit_proj(ci)
                if self.cut1 in (2, 3, 4, 5) or (self.cut1 >= 6 and ci >= self.cut1 - 5):
                    return
                if b == 1 and _os_env("KCUT2") and ci >= int(_os_env("KCUT2")) - 1:
                    return

    def l1_stage_b(self, b):
        nc, fw, I, R = self.nc, self.fw, self.I, self.R
        with ExitStack() as es:
            kT = self.sb(es, "m_kT", [128, 8, T], BF16)
            vall = self.sb(es, "m_vall", [128, NT, 8, 80], BF16)
            fw.dma("sp", kT[0:96, :, :], R["kT1"][b].rearrange("h r t -> r h t"), reads=["kT1"], writes=["m_kT"])
            fw.op("pool", lambda: nc.gpsimd.memset(vall[:], 1.0), writes=["m_vall"])
            for t in range(NT):
                fw.dma("sp", vall[:, t, :, 0:64], R["v1"][b, t * 128:(t + 1) * 128, :].rearrange("p (h d) -> p h d", h=8),
                       reads=["v1"], writes=["m_vall"])
            psS = self.ring(es, "m_psS", [128, 512], F32, 4, psum=True)
            poR = self.ring(es, "m_po", [128, 4, 80], F32, 2, psum=True)
            pT = self.ps(es, "m_pT", [128, 4, 128], BF16)
            qTr = self.ring(es, "m_qT", [128, 8, 512], BF16, 2)
            ptr = self.ring(es, "m_pt", [128, 512], BF16, 40)
            coutr = self.ring(es, "m_cout", [128, 4, 8, 64], F32, 2)
            recr = self.ring(es, "m_rec", [128, 4], F32, 4)
            zr = self.ring(es, "m_z", [128, 512], F32, 2)
            cgr = self.ring(es, "m_cg", [128, 512], BF16, 2)
            cgstr = self.ring(es, "m_cgst", [128, 4, 128], BF16, 2)
            scale = 96 ** -0.5
            for qc in range(4):
                qT, qTk = qTr.next()
                fw.dma("sp", qT[0:96, :, :], R["qT1"][b].rearrange("h r t -> r h t")[:, :, qc * 512:(qc + 1) * 512], reads=["qT1"], writes=[qTk])
                cout, coutk = coutr.next()

                def emit_qk(h):
                    pts = []
                    for kb in range(NT):
                        ps_, psk = psS.next()
                        fw.op("pe", lambda: nc.tensor.matmul(ps_[:], lhsT=kT[0:96, h, kb * 128:(kb + 1) * 128], rhs=qT[0:96, h, :], start=True, stop=True),
                              reads=["m_kT", qTk], writes=[psk])
                        pt_, ptk = ptr.next()
                        fw.op("act", lambda: nc.scalar.activation(out=pt_[:], in_=ps_[:], func=AF.Exp, scale=scale), reads=[psk], writes=[ptk])
                        pts.append((pt_, ptk))
                    return pts

                def emit_pv(h, pts):
                    po, pok = poR.next()
                    for qs in range(4):
                        for kb in range(NT):
                            pt_, ptk = pts[kb]
                            fw.op("pe", lambda: nc.tensor.matmul(po[:, qs, 0:65], lhsT=pt_[:, qs * 128:(qs + 1) * 128], rhs=vall[:, kb, h, 0:65],
                                                                 start=(kb == 0), stop=(kb == NT - 1)), reads=[ptk, "m_vall"], writes=[pok])
                    rec, reck = recr.next()
                    fw.op("dve", lambda: nc.vector.reciprocal(out=rec[:], in_=po[:, :, 64]), reads=[pok], writes=[reck])
                    fw.op("dve", lambda: nc.vector.tensor_tensor(out=cout[:, :, h, :], in0=po[:, :, 0:64],
                                                                in1=rec[:].unsqueeze(2).to_broadcast([128, 4, 64]), op=ALU.mult),
                          reads=[pok, reck], writes=[f"{coutk}#{h}"])

                nxt = emit_qk(0)
                for h in range(8):
                    cur = nxt
                    if h + 1 < 8:
                        nxt = emit_qk(h + 1)
                    emit_pv(h, cur)
                for qs in range(4):
                    lt = qc * 4 + qs
                    ltsl = slice(lt * 128, (lt + 1) * 128)
                    z_, zk = zr.next()
                    fw.dma("sp", z_[:], R["z1"][b, ltsl, 0:512], reads=["z1"], writes=[zk])
                    cg, cgk = cgr.next()
                    fw.op("pool", lambda: nc.gpsimd.tensor_tensor(out=cg[:], in0=cout[:, qs, :, :].rearrange("p h d -> p (h d)"), in1=z_[:], op=ALU.mult),
                          reads=[f"{coutk}#{h}" for h in range(8)] + [zk], writes=[cgk])
                    for c in range(4):
                        fw.op("pe", lambda: nc.tensor.transpose(out=pT[:, c, :], in_=cg[:, c * 128:(c + 1) * 128], identity=self.identb[:]),
                              reads=[cgk, "identb"], writes=["m_pT"])
                    cgst, cgsk = cgstr.next()
                    fw.op("act", lambda: nc.scalar.copy(out=cgst[:], in_=pT[:]), reads=["m_pT"], writes=[cgsk])
                    fw.dma(self.QS, R["cgT1"][b].rearrange("c p t -> p c t")[:, :, ltsl], cgst[:], reads=[cgsk], writes=["cgT1"])

    def l1_stage_c1(self, b, hsum):
        nc, fw, I, R = self.nc, self.fw, self.I, self.R
        cf = self.cf
        with ExitStack() as es:
            gt = self.sb(es, "c_gt", [128, NT, 16], F32)
            mk = self.sb(es, "c_mk", [128, NT, 256], BF16)
            VO = self.sb(es, "c_VO", [128, NT, 4, 144], BF16)
            mkT = self.sb(es, "c_mkT", [64, 4, T], BF16)
            mqT = self.sb(es, "c_mqT", [64, 4, S], BF16)
            ib = self.sb(es, "c_ib", [128, 8], F32)
            fb = self.sb(es, "c_fb", [128, 8], F32)
            fw.dma("sp", ib[:], I["o_i_bias"].partition_broadcast(128), writes=["c_ib"])
            fw.dma("sp", fb[:], I["o_f_bias"].partition_broadcast(128), writes=["c_fb"])
            fw.op("pool", lambda: nc.gpsimd.memset(VO[:], 1.0), writes=["c_VO"])
            for t in range(NT):
                tsl = slice(t * 128, (t + 1) * 128)
                fw.dma("sp", gt[:, t, :], R["mg1"][b, tsl, :], reads=["mg1"], writes=["c_gt"])
                fw.dma("sp", mk[:, t, :], R["mk1"][b, tsl, :], reads=["mk1"], writes=["c_mk"])
                fw.dma("sp", VO[:, t, :, 0:128], R["mv1"][b, tsl, :].rearrange("p (h d) -> p h d", h=4), reads=["mv1"], writes=["c_VO"])
            fw.dma("sp", mkT[:], R["mkT1"][b].rearrange("(h d) t -> d h t", h=4), reads=["mkT1"], writes=["c_mkT"])
            fw.dma("sp", mqT[:], R["mqT1"][b].rearrange("(h d) t -> d h t", h=4), reads=["mqT1"], writes=["c_mqT"])
            pbig = self.ring(es, "c_pbig", [128, 512], F32, 1, psum=True)
            pC = self.ring(es, "c_pC", [64, 2, 144], F32, 2, psum=True)
            pS = self.ring(es, "c_pS", [128, 128], F32, 1, psum=True)
            pH = self.ring(es, "c_pH", [128, 2, 144], F32, 4, psum=True)
            cbr = self.ring(es, "c_cb", [64, 4, 144], BF16, 6)
            dgr = self.ring(es, "c_dg", [128, 4, 128], F32, 2)
            rmr = self.ring(es, "c_rm", [128, 4, 128], F32, 2)
            er = self.ring(es, "c_e", [64, 4, 128], F32, 2)
            qz0r = self.ring(es, "c_qz0", [64, 4, 128], BF16, 2)
            qz1r = self.ring(es, "c_qz1", [64, 4, 128], BF16, 2)
            for rr in (qz0r, qz1r):
                for i_, tl in enumerate(rr.tiles):
                    fw.op("pool", lambda: nc.gpsimd.memset(tl[:], 0.0), writes=[f"{rr.name}{i_}"])
            dr = self.ring(es, "c_d", [128, 128], F32, 8)
            scr = self.ring(es, "c_sc", [128, 128], BF16, 8)
            dnr = self.ring(es, "c_dn", [128, 8], F32, 6)
            Dd = []
            for d in range(2):
                X = {}
                for nm in ("li", "xf", "l1", "nb", "ngc", "aa", "wcol", "bcol"):
                    X[nm] = self.sb(es, f"c_{nm}{d}", [128, 72], F32)
                X["egf"] = self.sb(es, f"c_egf{d}", [128, 2, 72], F32)
                X["VW"] = self.sb(es, f"c_VW{d}", [128, NT, 4, 144], BF16)
                X["C"] = self.sb(es, f"c_C{d}", [64, 4, 144], F32)
                Dd.append(X)
            for d in range(2):
                X = Dd[d]
                li, xf, l1, nb, ngc, aa, wcol, bcol, egf, VW, C = (X[k] for k in ("li", "xf", "l1", "nb", "ngc", "aa", "wcol", "bcol", "egf", "VW", "C"))
                K_ = lambda nm: f"c_{nm}{d}"
                tri = C_TRIF if d == 0 else C_TRIR
                g3 = lambda a: a[:].rearrange("p (t h) -> p t h", h=4)
                fw.op("dve", lambda: nc.vector.tensor_tensor(out=g3(li), in0=gt[:, :, d * 8:d * 8 + 4],
                                                            in1=ib[:, d * 4:(d + 1) * 4].unsqueeze(1).to_broadcast([128, NT, 4]), op=ALU.add),
                      reads=["c_gt", "c_ib"], writes=[K_("li")])
                fw.op("dve", lambda: nc.vector.tensor_tensor(out=g3(xf), in0=gt[:, :, d * 8 + 4:d * 8 + 8],
                                                            in1=fb[:, d * 4:(d + 1) * 4].unsqueeze(1).to_broadcast([128, NT, 4]), op=ALU.add),
                      reads=["c_gt", "c_fb"], writes=[K_("xf")])
                fw.op("act", lambda: nc.scalar.activation(out=xf[:], in_=xf[:], func=AF.Exp, scale=-1.0), reads=[K_("xf")], writes=[K_("xf")])
                fw.op("act", lambda: nc.scalar.activation(out=l1[:], in_=xf[:], func=AF.Ln, bias=1.0), reads=[K_("xf")], writes=[K_("l1")])
                p1, p1k = pbig.next()
                fw.op("pe", lambda: nc.tensor.matmul(p1[:, 0:72], lhsT=cf[:, tri, :], rhs=l1[:], start=True, stop=True), reads=["c128f", K_("l1")], writes=[p1k])
                fw.op("dve", lambda: nc.vector.tensor_copy(out=nb[:], in_=p1[:, 0:72]), reads=[p1k], writes=[K_("nb")])
                p2, p2k = pbig.next()
                fw.op("pe", lambda: nc.tensor.matmul(p2[:, 0:72], lhsT=cf[:, C_BLK, :], rhs=l1[:], start=True, stop=True), reads=["c128f", K_("l1")], writes=[p2k])
                fw.op("dve", lambda: nc.vector.tensor_copy(out=ngc[:], in_=p2[:, 0:72]), reads=[p2k], writes=[K_("ngc")])
                p3, p3k = pbig.next()
                for half in range(2):
                    fw.op("pe", lambda: nc.tensor.matmul(p3[:, half * 72:(half + 1) * 72], lhsT=cf[:, C_SEL0 + half, :], rhs=l1[:], start=True, stop=True),
                          reads=["c128f", K_("l1")], writes=[p3k])
                fw.op("act", lambda: nc.scalar.activation(out=egf[:].rearrange("p a b -> p (a b)"), in_=p3[:, 0:144], func=AF.Exp, scale=-1.0),
                      reads=[p3k], writes=[K_("egf")])
                fw.op("dve", lambda: nc.vector.tensor_tensor(out=bcol[:], in0=li[:], in1=nb[:], op=ALU.add), reads=[K_("li"), K_("nb")], writes=[K_("bcol")])
                fw.op("dve", lambda: nc.vector.tensor_tensor(out=aa[:], in0=bcol[:], in1=ngc[:], op=ALU.subtract), reads=[K_("bcol"), K_("ngc")], writes=[K_("aa")])
                fw.op("act", lambda: nc.scalar.activation(out=wcol[:], in_=aa[:], func=AF.Exp), reads=[K_("aa")], writes=[K_("wcol")])
                for part in range(2):
                    e_ = "dve" if part == 0 else "pool"
                    eng = nc.vector if part == 0 else nc.gpsimd
                    tt = slice(part * 9, (part + 1) * 9)
                    fw.op(e_, lambda: eng.tensor_tensor(out=VW[:, tt, :, :].rearrange("p t h v -> p (t h) v"),
                                                        in0=VO[:, tt, :, :].rearrange("p t h v -> p (t h) v"),
                                                        in1=wcol[:, part * 36:(part + 1) * 36].unsqueeze(2).to_broadcast([128, 36, 144]), op=ALU.mult),
                          reads=["c_VO", K_("wcol")], writes=[f"c_VW{d}#{part}"])
                fw.op("dve", lambda: nc.vector.memset(C[:], 0.0), writes=[f"c_C{d}#{h}" for h in range(4)])

            written = set()

            def process_tile(d, t):
                X = Dd[d]
                nb, bcol, egf, VW, C = X["nb"], X["bcol"], X["egf"], X["VW"], X["C"]
                K_ = lambda nm: f"c_{nm}{d}"
                ckeys = [f"c_C{d}#{h}" for h in range(4)]
                vwkeys = [f"c_VW{d}#0", f"c_VW{d}#1"]
                mbk = C_MBF if d == 0 else C_MBR
                horder = (0, 1) if d == 0 else (1, 0)
                Cin = {}
                for half in horder:
                    if t >= 2:
                        cb, cbk = cbr.next()
                        fw.op("act", lambda: nc.scalar.copy(out=cb[:], in_=C[:]), reads=ckeys, writes=[cbk])
                        Cin[half] = (cb, cbk)
                    hs_ = slice(half * 64, (half + 1) * 64)
                    for hp in range(2):
                        pc, pck = pC.next()
                        for hh in range(2):
                            h = 2 * hp + hh
                            fw.op("pe", lambda: nc.tensor.matmul(pc[:, hh, 0:129], lhsT=mk[hs_, t, h * 64:(h + 1) * 64], rhs=VW[hs_, t, h, 0:129],
                                                                 start=True, stop=True), reads=["c_mk"] + vwkeys, writes=[pck])
                        for hh in range(2):
                            h = 2 * hp + hh
                            idx = t * 4 + h
                            fw.op("dve", lambda: nc.vector.scalar_tensor_tensor(out=C[:, h, 0:129], in0=C[:, h, 0:129], scalar=egf[0:64, half, idx:idx + 1],
                                                                               in1=pc[:, hh, 0:129], op0=ALU.mult, op1=ALU.add),
                                  reads=[ckeys[h], K_("egf"), pck], writes=[ckeys[h]])
                if t < 2:
                    return
                lt = t - 2
                dg, dgk = dgr.next()
                fw.op("dve", lambda: nc.vector.tensor_tensor(out=dg[:], in0=nb[:, t * 4:(t + 1) * 4].unsqueeze(2).to_broadcast([128, 4, 128]),
                                                            in1=cf[:, C_ID, :].unsqueeze(1).to_broadcast([128, 4, 128]), op=ALU.mult),
                      reads=[K_("nb"), "c128f"], writes=[dgk])
                pR, pRk = pbig.next()
                fw.op("pe", lambda: nc.tensor.matmul(pR[:], lhsT=cf[:, C_ONES, :], rhs=dg[:].rearrange("p h n -> p (h n)"), start=True, stop=True),
                      reads=["c128f", dgk], writes=[pRk])
                rm, rmk = rmr.next()
                fw.op("dve", lambda: nc.vector.tensor_tensor(out=rm[:], in0=cf[:, mbk, :].unsqueeze(1).to_broadcast([128, 4, 128]),
                                                            in1=pR[:].rearrange("p (h n) -> p h n", h=4), op=ALU.subtract),
                      reads=["c128f", pRk], writes=[rmk])
                e_, ek = er.next()
                fw.op("act", lambda: nc.scalar.activation(out=e_[:].rearrange("p h n -> p (h n)"), in_=pR[0:64, :], func=AF.Exp, scale=-1.0),
                      reads=[pRk], writes=[ek])
                qz0, qz0k = qz0r.next()
                qz1, qz1k = qz1r.next()
                fw.op("pool", lambda: nc.gpsimd.tensor_tensor(out=qz0[:, :, 0:64], in0=mqT[:, :, lt * 128:lt * 128 + 64], in1=e_[:, :, 0:64], op=ALU.mult),
                      reads=["c_mqT", ek], writes=[qz0k])
                fw.op("pool", lambda: nc.gpsimd.tensor_tensor(out=qz1[:, :, 64:128], in0=mqT[:, :, lt * 128 + 64:lt * 128 + 128], in1=e_[:, :, 64:128], op=ALU.mult),
                      reads=["c_mqT", ek], writes=[qz1k])
                scs = []
                for h in range(4):
                    idx = t * 4 + h
                    dt_, dtk = dr.next()
                    fw.op("act", lambda: nc.scalar.activation(out=dt_[:], in_=rm[:, h, :], func=AF.Exp, bias=bcol[:, idx:idx + 1], scale=1.0),
                          reads=[rmk, K_("bcol")], writes=[dtk])
                    ps_, psk = pS.next()
                    fw.op("pe", lambda: nc.tensor.matmul(ps_[:], lhsT=mkT[:, h, t * 128:(t + 1) * 128], rhs=mqT[:, h, lt * 128:(lt + 1) * 128],
                                                         start=True, stop=True), reads=["c_mkT", "c_mqT"], writes=[psk])
                    sc, sck = scr.next()
                    fw.op("dve", lambda: nc.vector.tensor_tensor(out=sc[:], in0=ps_[:], in1=dt_[:], op=ALU.mult), reads=[psk, dtk], writes=[sck])
                    scs.append((sc, sck))
                c0, c0k = Cin[0]
                c1, c1k = Cin[1]
                phs = []
                for hp in range(2):
                    ph, phk = pH.next()
                    phs.append((ph, phk))
                    for hh in range(2):
                        h = 2 * hp + hh
                        sc, sck = scs[h]
                        fw.op("pe", lambda: nc.tensor.matmul(ph[:, hh, 0:129], lhsT=sc[:], rhs=VO[:, t, h, 0:129], start=True, stop=False),
                              reads=[sck, "c_VO"], writes=[phk])
                        fw.op("pe", lambda: nc.tensor.matmul(ph[:, hh, 0:129], lhsT=qz0[:, h, :], rhs=c0[:, h, 0:129], start=False, stop=False),
                              reads=[qz0k, c0k], writes=[phk])
                        fw.op("pe", lambda: nc.tensor.matmul(ph[:, hh, 0:129], lhsT=qz1[:, h, :], rhs=c1[:, h, 0:129], start=False, stop=True),
                              reads=[qz1k, c1k], writes=[phk])
                for hp in range(2):
                    ph, phk = phs[hp]
                    dn, dnk = dnr.next()
                    fw.op("dve", lambda: nc.vector.tensor_scalar(out=dn[:, 0:2], in0=ph[:, :, 128], scalar1=-1.0, scalar2=1.0, op0=ALU.mult, op1=ALU.max),
                          reads=[phk], writes=[dnk])
                    fw.op("dve", lambda: nc.vector.tensor_tensor(out=dn[:, 2:4], in0=dn[:, 0:2], in1=ph[:, :, 128], op=ALU.max), reads=[dnk, phk], writes=[dnk])
                    fw.op("dve", lambda: nc.vector.reciprocal(out=dn[:, 4:6], in_=dn[:, 2:4]), reads=[dnk], writes=[dnk])
                    for hh in range(2):
                        h = 2 * hp + hh
                        hkey = f"c_hsum#{lt}#{h}"
                        if (lt, h) not in written:
                            written.add((lt, h))
                            fw.op("dve", lambda: nc.vector.tensor_scalar(out=hsum[:, lt, h, :], in0=ph[:, hh, 0:128], scalar1=dn[:, 4 + hh:5 + hh], scalar2=None, op0=ALU.mult),
                                  reads=[phk, dnk], writes=[hkey])
                        else:
                            fw.op("dve", lambda: nc.vector.scalar_tensor_tensor(out=hsum[:, lt, h, :], in0=ph[:, hh, 0:128], scalar=dn[:, 4 + hh:5 + hh], in1=hsum[:, lt, h, :],
                                                                               op0=ALU.mult, op1=ALU.add), reads=[phk, dnk, hkey], writes=[hkey])

            torder = [list(range(NT)), [1, 0] + list(range(NT - 1, 1, -1))]
            for step in range(NT):
                for d in range(2):
                    process_tile(d, torder[d][step])

    def l1_stage_c2(self, b, hsum, wout):
        nc, fw, I, R = self.nc, self.fw, self.I, self.R
        with ExitStack() as es:
            mt = self.mod_tiles(es, 1, b, want_pre=False, want_post=True)
            g, gk = mt["g_lat"]
            P = {}
            P["xres"] = self.ring(es, "d_xres", [128, D], F32, 2)
            P["junk"] = self.ring(es, "d_junk", [128, D], BF16, 2)
            P["stat"] = self.ring(es, "d_stat", [128, 4], F32, 4)
            P["t2"] = self.ring(es, "d_t2", [128, D], F32, 2)
            hnw = self.sb(es, "d_hnw", [128, 512], F32)
            fw.dma("sp", hnw[:], I["o_head_norm_w"].partition_broadcast(128), writes=["d_hnw"])
            pT = self.ps(es, "d_pT", [128, 4, 128], BF16)
            py = self.ps(es, "d_py", [128, D], F32)
            mor = self.ring(es, "d_mo", [128, 512], F32, 2)
            zmr = self.ring(es, "d_zm", [128, 512], F32, 2)
            cgr = self.ring(es, "d_cg", [128, 4, 128], BF16, 2)
            str_ = self.ring(es, "d_st", [128, 12], F32, 3)
            jr = self.ring(es, "d_j", [128, 128], BF16, 2)
            g1r = self.ring(es, "d_g1", [128, 512], F32, 2)
            hnr = self.ring(es, "d_hn", [128, 4, 128], F32, 2)
            mgr = self.ring(es, "d_mg", [128, 512], BF16, 2)
            mgTr = self.ring(es, "d_mgT", [128, 4, 128], BF16, 2)
            loads = {}

            def emit_loads(lt):
                ltsl = slice(lt * 128, (lt + 1) * 128)
                mo_, mok = mor.next()
                fw.dma("sp", mo_[:], R["mo1"][b, ltsl, :], reads=["mo1"], writes=[mok])
                zm_, zmk = zmr.next()
                fw.dma("sp", zm_[:], R["z1"][b, ltsl, 512:1024], reads=["z1"], writes=[zmk])
                cg_, cgk = cgr.next()
                fw.dma("sp", cg_[:], R["cgT1"][b].rearrange("c p t -> p c t")[:, :, ltsl], reads=["cgT1"], writes=[cgk])
                loads[lt] = (mo_, mok, zm_, zmk, cg_, cgk)

            def emit_tile(lt):
                mo_, mok, zm_, zmk, cg_, cgk = loads.pop(lt)
                hkeys = [f"c_hsum#{lt}#{h}" for h in range(4)]
                st, stk = str_.next()
                for h in range(4):
                    j_, jk = jr.next()
                    fw.op("act", lambda: nc.scalar.activation(out=j_[:], in_=hsum[:, lt, h, :], func=AF.Square, scale=128 ** -0.5, accum_out=st[:, h:h + 1]),
                          reads=hkeys, writes=[jk, f"{stk}#{h}"])
                fw.op("act", lambda: nc.scalar.activation(out=st[:, 4:8], in_=st[:, 0:4], func=AF.Sqrt, bias=EPS),
                      reads=[f"{stk}#{h}" for h in range(4)], writes=[f"{stk}#s"])
                fw.op("dve", lambda: nc.vector.reciprocal(out=st[:, 8:12], in_=st[:, 4:8]), reads=[f"{stk}#s"], writes=[f"{stk}#r"])
                g1, g1k = g1r.next()
                fw.op("pool", lambda: nc.gpsimd.tensor_tensor(out=g1[:], in0=mo_[:], in1=zm_[:], op=ALU.mult), reads=[mok, zmk], writes=[g1k])
                fw.op("pool", lambda: nc.gpsimd.tensor_tensor(out=g1[:], in0=g1[:], in1=hnw[:], op=ALU.mult), reads=[g1k, "d_hnw"], writes=[g1k])
                hn, hnk = hnr.next()
                fw.op("dve", lambda: nc.vector.tensor_tensor(out=hn[:], in0=hsum[:, lt, :, :], in1=st[:, 8:12].unsqueeze(2).to_broadcast([128, 4, 128]), op=ALU.mult),
                      reads=hkeys + [f"{stk}#r"], writes=[hnk])
                mg, mgk = mgr.next()
                fw.op("dve", lambda: nc.vector.tensor_tensor(out=mg[:], in0=hn[:].rearrange("p h v -> p (h v)"), in1=g1[:], op=ALU.mult),
                      reads=[hnk, g1k], writes=[mgk])
                for c in range(4):
                    fw.op("pe", lambda: nc.tensor.transpose(out=pT[:, c, :], in_=mg[:, c * 128:(c + 1) * 128], identity=self.identb[:]),
                          reads=[mgk, "identb"], writes=["d_pT"])
                mgT, mgTk = mgTr.next()
                fw.op("act", lambda: nc.scalar.copy(out=mgT[:], in_=pT[:]), reads=["d_pT"], writes=[mgTk])
                for half in range(2):
                    for k in range(8):
                        src, srk = (cg_, cgk) if k < 4 else (mgT, mgTk)
                        fw.op("pe", lambda: nc.tensor.matmul(py[:, half * 512:(half + 1) * 512], lhsT=src[:, k % 4, :],
                                                             rhs=wout[:, k, half * 512:(half + 1) * 512], start=(k == 0), stop=(k == 7)),
                              reads=[srk, "l1_wout"], writes=["d_py"])
                t = lt + 2
                self.post_tile(P, py, "d_py", g, gk, R["x1c"][b, t * 128:(t + 1) * 128, :], ["x1c"],
                               self.out[b, lt * 128:(lt + 1) * 128, :], "out")

            emit_loads(0)
            for lt in range(16):
                if lt + 1 < 16:
                    emit_loads(lt + 1)
                emit_tile(lt)


def make_consts():
    c = np.zeros((11, 128, 128), np.float32)
    p = np.arange(128)[:, None]
    n = np.arange(128)[None, :]
    same = (p // 64) == (n // 64)
    c[C_ID] = (p == n)
    c[C_ONES] = 1.0
    c[C_TRIF] = same & (p <= n)
    c[C_TRIR] = same & (p >= n)
    c[C_BLK] = same
    c[C_SEL0] = (p < 64) & (n >= 0)
    c[C_SEL1] = (p >= 64) & (n >= 0)
    c[C_MBF] = np.where(same & (p <= n), 0.0, NEG)
    c[C_MBR] = np.where(same & (p >= n), 0.0, NEG)
    c[C_MPREV] = np.where(p >= n, 0.0, NEG)
    c[C_MNEXT] = np.where(p <= n, 0.0, NEG)

    def axial(rot_dim):
        rows = S // 64
        row = np.repeat(np.arange(rows), 64).astype(np.float32)
        col = np.tile(np.arange(64), rows).astype(np.float32)
        nf = rot_dim // 4
        inv = (np.float32(10000.0) ** (-np.arange(nf, dtype=np.float32) / np.float32(nf))).astype(np.float32)
        ang = np.concatenate([row[:, None] * inv, col[:, None] * inv], axis=-1).astype(np.float32)
        tab = np.zeros((T, rot_dim), np.float32)
        tab[:LC, :rot_dim // 2] = 1.0
        tab[LC:, :rot_dim // 2] = np.cos(ang)
        tab[LC:, rot_dim // 2:] = np.sin(ang)
        return tab
    return c, axial(64), axial(32)


_CACHE = {}


def get_program(debug=False, stop_after=None):
    key = (debug, stop_after)
    if key not in _CACHE:
        bld = Builder(debug=debug)
        nc = bld.build(stop_after=stop_after)
        _CACHE[key] = (nc, bld)
    return _CACHE[key]


def make_in_maps(inputs):
    c128, ropeA, ropeC = make_consts()
    f = lambda a: np.ascontiguousarray(np.asarray(a, dtype=np.float32))
    shared = {
        "mod_w": f(inputs["mod_w"]), "mod_b": f(inputs["mod_b"]),
        "pre_norm_w": f(inputs["pre_norm_w"]), "post_norm_w": f(inputs["post_norm_w"]),
        "e_w_in": f(inputs["e_w_in"][0]), "e_sink": f(inputs["e_sink"][0]), "e_conv_w": f(inputs["e_conv_w"][0]),
        "e_w_out": f(inputs["e_w_out"][0]), "o_w_in": f(inputs["o_w_in"][0]),
        "o_q_norm_w": f(inputs["o_q_norm_w"][0]), "o_kv_norm_w": f(inputs["o_kv_norm_w"][0]),
        "o_w_uq": f(inputs["o_w_uq"][0]), "o_w_ukv": f(inputs["o_w_ukv"][0]),
        "o_i_bias": f(inputs["o_i_bias"][0]).reshape(8), "o_f_bias": f(inputs["o_f_bias"][0]).reshape(8),
        "o_head_norm_w": f(inputs["o_head_norm_w"][0]), "o_w_out": f(inputs["o_w_out"][0]),
        "c128": c128, "ropeA": ropeA, "ropeC": ropeC,
    }
    x = f(inputs["x"])
    c = f(inputs["c"])
    ctx = f(inputs["ctx"])
    cc = f(inputs["c_ctx"])
    maps = []
    for i in range(NCORES):
        m = dict(shared)
        m["xs"] = x[NB * i:NB * (i + 1)]
        m["ctxs"] = ctx[NB * i:NB * (i + 1)]
        m["cvec"] = np.ascontiguousarray(np.stack([c[NB * i], c[NB * i + 1], cc], axis=0))
        maps.append(m)
    return maps


def kernel(**inputs):
    nc, _ = get_program()
    maps = make_in_maps(inputs)
    res = run_bass_kernel_spmd(nc, maps, core_ids=list(range(NCORES)))
    return np.concatenate([r["out"] for r in res.results], axis=0)
```

```python
import numpy as np
from contextlib import ExitStack
import concourse.bass as bass
import concourse.mybir as mybir
from concourse.bass_utils import run_bass_kernel_spmd

F32 = mybir.dt.float32
BF16 = mybir.dt.bfloat16
AF = mybir.ActivationFunctionType
ALU = mybir.AluOpType

D = 1024
S = 2048
LC = 256
T = S + LC
NT = T // 128
NB = 2
NCORES = 8
EPS = 1e-6
NEG = -30000.0
E_IN = 3328
O_IN = 3632
CHUNKS = [(0, 256), (256, 512), (768, 512), (1280, 512), (1792, 512)]

C_ID, C_ONES, C_TRIF, C_TRIR, C_BLK, C_SEL0, C_SEL1, C_MBF, C_MBR, C_MPREV, C_MNEXT = range(11)


def _nruns(ap):
    pat = [list(x) for x in ap.ap]
    tot = 1
    for st, n in pat:
        tot *= n
    run = 1
    for st, n in sorted(pat, key=lambda x: abs(x[0]) if x[0] != 0 else 1 << 60):
        if st == run:
            run *= n
        elif n > 1:
            break
    return max(1, tot // run)


def _nbytes(ap):
    tot = 1
    for st, n in ap.ap:
        tot *= n
    return tot


def _os_env(k):
    import os
    return os.environ.get(k)


class FW:
    ROT = 30000

    def __init__(self, nc, es):
        self.nc = nc
        self.es = es
        self.eng = {"pe": nc.tensor, "act": nc.scalar, "dve": nc.vector, "pool": nc.gpsimd, "sp": nc.sync}
        self.comp = ["pe", "act", "dve", "pool"]
        self.epoch = {k: 0 for k in self.comp}
        self.sem = {k: es.enter_context(nc.semaphore(f"s_{k}_0")) for k in self.comp}
        self.cnt = {k: 0 for k in self.comp}
        self.seen = {e: {} for e in self.eng}
        self.NQ = int(_os_env("KNQ") or 8)
        self.DBUDGET = int(_os_env("KDB") or 1024)
        self.dq = {}
        for q in ["sp", "act", "pool"]:
            sems = [es.enter_context(nc.semaphore(f"d_{q}{i}")) for i in range(self.NQ)]
            self.dq[q] = {"sems": sems, "n": 0}
        self.lastw = {}
        self.reads = {}
        self.ninst = 0
        self.psum_keys = set()

    def _wait(self, e, tok):
        key, sem, val = tok
        if self.seen[e].get(key, 0) >= val:
            return
        self.eng[e].wait_ge(sem, val)
        self.seen[e][key] = val
        self.ninst += 1

    def _deps(self, e, reads, writes):
        toks = []
        for b in list(reads) + list(writes):
            t = self.lastw.get(b)
            if t is not None:
                toks.append(t)
        for b in writes:
            toks.extend(self.reads.get(b, []))
        for b in reads:
            if b in self.psum_keys:
                toks.extend(t for t in self.reads.get(b, []) if not t[0].startswith(e + "#"))
        for t in toks:
            if e == "pe" and t[0].startswith("pe#"):
                continue
            self._wait(e, t)

    def _commit(self, tok, reads, writes):
        for b in writes:
            self.lastw[b] = tok
            self.reads[b] = []
        for b in reads:
            lst = self.reads.setdefault(b, [])
            lst[:] = [t for t in lst if t[0] != tok[0]]
            lst.append(tok)

    def op(self, e, fn, reads=(), writes=()):
        self._deps(e, reads, writes)
        if self.cnt[e] >= self.ROT:
            self.epoch[e] += 1
            self.sem[e] = self.es.enter_context(self.nc.semaphore(f"s_{e}_{self.epoch[e]}"))
            self.cnt[e] = 0
        ins = fn()
        self.cnt[e] += 1
        ins.then_inc(self.sem[e], 1)
        tok = (f"{e}#{self.epoch[e]}", self.sem[e], self.cnt[e])
        self._commit(tok, reads, writes)
        self.ninst += 1
        return tok

    def dma(self, q, out, in_, reads=(), writes=(), **kw):
        d = self.dq[q]
        i = d["n"] % self.NQ
        rnd = d["n"] // self.NQ
        sem = d["sems"][i]
        key = f"d_{q}{i}"
        if rnd > 0:
            self._wait(q, (key, sem, 16 * rnd))
        nd = max(_nruns(out), _nruns(in_))
        fl = d.setdefault("inflight", [])
        fl[:] = [(t, c) for (t, c) in fl if self.seen[q].get(t[0], 0) < t[2]]
        while fl and sum(c for _, c in fl) + nd > self.DBUDGET:
            t, c = fl.pop(0)
            self._wait(q, t)
        self._deps(q, reads, writes)
        ins = self.eng[q].dma_start(out=out, in_=in_, **kw)
        ins.then_inc(sem, 16)
        d["n"] += 1
        self.ndesc = getattr(self, "ndesc", 0) + nd
        tok = (key, sem, 16 * (rnd + 1))
        fl.append((tok, nd))
        self._commit(tok, reads, writes)
        self.ninst += 1
        return tok

    def all_tokens(self):
        toks = []
        for k in self.comp:
            if self.cnt[k] > 0:
                toks.append((f"{k}#{self.epoch[k]}", self.sem[k], self.cnt[k]))
        for q, d in self.dq.items():
            for i in range(self.NQ):
                n_i = (d["n"] - i + self.NQ - 1) // self.NQ
                if n_i > 0:
                    toks.append((f"d_{q}{i}", d["sems"][i], 16 * n_i))
        return toks

    def barrier(self):
        toks = self.all_tokens()
        for e in self.eng:
            for t in toks:
                if t[0].startswith(e + "#"):
                    continue
                self._wait(e, t)
        self.lastw = {}
        self.reads = {}

    def finish(self):
        for t in self.all_tokens():
            self._wait("sp", t)


class Ring:
    def __init__(self, tiles, name):
        self.tiles = tiles
        self.name = name
        self.i = -1

    def next(self):
        self.i = (self.i + 1) % len(self.tiles)
        return self.tiles[self.i], f"{self.name}{self.i}"


class Builder:
    def __init__(self, debug=False):
        self.debug = debug
        self.nc = bass.Bass("TRN2", target_bir_lowering=False)
        self.dbg_names = []
        import os as _os
        self.QS = _os.environ.get("KQS", "sp")

    def uname(self, name):
        self.uid = getattr(self, "uid", 0) + 1
        return f"{name}_u{self.uid}"

    def sb(self, es, name, shape, dt):
        return es.enter_context(self.nc.sbuf_tensor(self.uname(name), list(shape), dt))

    def ps(self, es, name, shape, dt):
        self.fw.psum_keys.add(name)
        return es.enter_context(self.nc.psum_tensor(self.uname(name), list(shape), dt))

    def ring(self, es, name, shape, dt, n, psum=False):
        f = self.ps if psum else self.sb
        return Ring([f(es, f"{name}{i}", shape, dt) for i in range(n)], name)

    def dram_in(self, name, shape, dt=F32):
        return self.nc.dram_tensor(name, list(shape), dt, kind="ExternalInput").ap()

    def scratch(self, name, shape, dt, dbg=False):
        if dbg and self.debug:
            self.dbg_names.append(name)
            return self.nc.dram_tensor(name, list(shape), dt, kind="ExternalOutput").ap()
        return self.nc.dram_tensor(name, list(shape), dt).ap()

    def build(self, stop_after=None):
        nc = self.nc
        self.stop = stop_after
        import os as _os
        self.cut1 = int(_os.environ.get("KCUT1", "0"))
        self.cut = int(_os.environ.get("KCUT", "0"))
        self.cutb = int(_os.environ.get("KCUTB", "0"))
        self.cutc = int(_os.environ.get("KCUTC", "0"))
        I = {}
        I["xs"] = self.dram_in("xs", [NB, S, D])
        I["ctxs"] = self.dram_in("ctxs", [NB, LC, D])
        I["cvec"] = self.dram_in("cvec", [3, D])
        I["mod_w"] = self.dram_in("mod_w", [2, D, 3 * D])
        I["mod_b"] = self.dram_in("mod_b", [2, 3 * D])
        I["pre_norm_w"] = self.dram_in("pre_norm_w", [2, D])
        I["post_norm_w"] = self.dram_in("post_norm_w", [2, D])
        I["e_w_in"] = self.dram_in("e_w_in", [D, E_IN])
        I["e_sink"] = self.dram_in("e_sink", [8])
        I["e_conv_w"] = self.dram_in("e_conv_w", [3, 512])
        I["e_w_out"] = self.dram_in("e_w_out", [D, D])
        I["o_w_in"] = self.dram_in("o_w_in", [D, O_IN])
        I["o_q_norm_w"] = self.dram_in("o_q_norm_w", [768])
        I["o_kv_norm_w"] = self.dram_in("o_kv_norm_w", [256])
        I["o_w_uq"] = self.dram_in("o_w_uq", [768, 768])
        I["o_w_ukv"] = self.dram_in("o_w_ukv", [256, 1024])
        I["o_i_bias"] = self.dram_in("o_i_bias", [8])
        I["o_f_bias"] = self.dram_in("o_f_bias", [8])
        I["o_head_norm_w"] = self.dram_in("o_head_norm_w", [512])
        I["o_w_out"] = self.dram_in("o_w_out", [D, D])
        I["c128"] = self.dram_in("c128", [11, 128, 128])
        I["ropeA"] = self.dram_in("ropeA", [T, 64])
        I["ropeC"] = self.dram_in("ropeC", [T, 32])
        self.I = I
        out = nc.dram_tensor("out", [NB, S, D], F32, kind="ExternalOutput").ap()
        self.out = out

        R = {}
        R["modrow"] = self.scratch("modrow", [2, 3, 3 * D], F32, dbg=True)
        R["x1c"] = self.scratch("x1c", [NB, T, D], F32, dbg=True)
        R["qT0"] = self.scratch("qT0", [NB, 4, 128, T], BF16)
        R["kT0"] = self.scratch("kT0", [NB, 128, T], BF16)
        R["v0"] = self.scratch("v0", [NB, T, 128], BF16)
        R["za0"] = self.scratch("za0", [NB, T, 512], F32)
        R["bg0T"] = self.scratch("bg0T", [NB, 4, 128, T], BF16)
        R["kT1"] = self.scratch("kT1", [NB, 8, 96, T], BF16)
        R["v1"] = self.scratch("v1", [NB, T, 512], BF16)
        R["qT1"] = self.scratch("qT1", [NB, 8, 96, S], BF16)
        R["mk1"] = self.scratch("mk1", [NB, T, 256], BF16)
        R["mv1"] = self.scratch("mv1", [NB, T, 512], BF16)
        R["mkT1"] = self.scratch("mkT1", [NB, 256, T], BF16)
        R["mqT1"] = self.scratch("mqT1", [NB, 256, S], BF16)
        R["mg1"] = self.scratch("mg1", [NB, T, 16], F32)
        R["mo1"] = self.scratch("mo1", [NB, S, 512], F32)
        R["z1"] = self.scratch("z1", [NB, S, 1024], F32)
        R["cgT1"] = self.scratch("cgT1", [NB, 4, 128, S], BF16)
        self.R = R

        with ExitStack() as es0:
            self.fw = FW(nc, es0)
            fw = self.fw
            self.cf = self.sb(es0, "c128f", [128, 11, 128], F32)
            fw.dma("sp", self.cf[:], I["c128"].rearrange("c p n -> p c n"), writes=["c128f"])
            self.identb = self.sb(es0, "identb", [128, 128], BF16)
            fw.op("dve", lambda: nc.vector.tensor_copy(out=self.identb[:], in_=self.cf[:, C_ID, :]),
                  reads=["c128f"], writes=["identb"])
            self.onesb = self.sb(es0, "onesb", [128, 128], BF16)
            fw.op("dve", lambda: nc.vector.tensor_copy(out=self.onesb[:], in_=self.cf[:, C_ONES, :]),
                  reads=["c128f"], writes=["onesb"])
            self.stage_mod()
            fw.barrier()
            if stop_after != "mod":
                if not _os.environ.get("KSKIP0"):
                    self.layer0()
                    fw.barrier()
                if stop_after in (None, "l1a", "l1b"):
                    self.layer1()
                    fw.barrier()
            fw.finish()
        return nc

    def stage_mod(self):
        nc, fw, I, R = self.nc, self.fw, self.I, self.R
        with ExitStack() as es:
            cv = self.sb(es, "m_cv", [3, D], F32)
            sv = self.sb(es, "m_sv", [3, D], F32)
            svT = self.sb(es, "m_svT", [128, 8, 3], F32)
            ones3 = self.sb(es, "m_ones3", [1, 3], F32)
            pT = self.ps(es, "m_pT", [128, 8, 3], F32)
            wring = self.ring(es, "m_w", [128, 8, 512], F32, 2)
            pring = self.ring(es, "m_p", [3, 512], F32, 2, psum=True)
            mb = self.sb(es, "m_mb", [1, 3 * D], F32)
            mr = self.sb(es, "m_mr", [3, 3 * D], F32)
            fw.dma("sp", cv[:], I["cvec"], writes=["m_cv"])
            fw.op("act", lambda: nc.scalar.activation(out=sv[:], in_=cv[:], func=AF.Silu), reads=["m_cv"], writes=["m_sv"])
            fw.op("pool", lambda: nc.gpsimd.memset(ones3[:], 1.0), writes=["m_ones3"])
            for k in range(8):
                fw.op("pe", lambda: nc.tensor.transpose(out=pT[:, k, :], in_=sv[0:3, k * 128:(k + 1) * 128],
                                                        identity=self.cf[0:3, C_ID, 0:3]),
                      reads=["m_sv", "c128f"], writes=["m_pT"])
            fw.op("dve", lambda: nc.vector.tensor_copy(out=svT[:], in_=pT[:]), reads=["m_pT"], writes=["m_svT"])
            for l in range(2):
                fw.dma("sp", mb[:], I["mod_b"][l:l + 1, :], reads=[], writes=["m_mb"])
                for n in range(6):
                    wt, wk = wring.next()
                    fw.dma("sp", wt[:], I["mod_w"][l, :, n * 512:(n + 1) * 512].rearrange("(k p) n -> p k n", p=128),
                           writes=[wk])
                    pm, pk = pring.next()
                    for k in range(8):
                        fw.op("pe", lambda: nc.tensor.matmul(pm[:], lhsT=svT[:, k, :], rhs=wt[:, k, :],
                                                             start=(k == 0), stop=False),
                              reads=["m_svT", wk], writes=[pk])
                    fw.op("pe", lambda: nc.tensor.matmul(pm[:], lhsT=ones3[0:1, :], rhs=mb[0:1, n * 512:(n + 1) * 512],
                                                         start=False, stop=True),
                          reads=["m_ones3", "m_mb"], writes=[pk])
                    fw.op("dve", lambda: nc.vector.tensor_copy(out=mr[:, n * 512:(n + 1) * 512], in_=pm[:]),
                          reads=[pk], writes=["m_mr"])
                fw.dma("sp", R["modrow"][l], mr[:], reads=["m_mr"], writes=["modrow"])

    def src_rows(self, layer, b, tok0, n):
        if layer == 0:
            if tok0 < LC:
                return self.I["ctxs"][b, tok0:tok0 + n, :]
            return self.I["xs"][b, tok0 - LC:tok0 - LC + n, :]
        return self.R["x1c"][b, tok0:tok0 + n, :]

    def load_weight_bf16(self, es, name, w_ap, ncols, nk=8):
        nc, fw = self.nc, self.fw
        wt = self.sb(es, name, [128, nk, ncols], BF16)
        with ExitStack() as es2:
            stg = self.ring(es2, name + "_stg", [128, ncols], F32, 4)
            for k in range(nk):
                st, sk = stg.next()
                fw.dma("sp", st[:], w_ap[k * 128:(k + 1) * 128, :], writes=[sk])
                e = "pool" if k % 2 == 0 else "dve"
                eng = nc.gpsimd if k % 2 == 0 else nc.vector
                fw.op(e, lambda: eng.tensor_copy(out=wt[:, k, :], in_=st[:]), reads=[sk], writes=[name])
            fw.barrier()
        return wt

    def mod_tiles(self, es, layer, b, want_pre=True, want_post=True):
        nc, fw, I, R = self.nc, self.fw, self.I, self.R
        res = {}
        tmp = self.sb(es, "mt_tmp", [128, D], F32)
        if want_pre:
            fw.dma("sp", tmp[:], I["pre_norm_w"][layer].partition_broadcast(128), writes=["mt_tmp"])
            for nm, v in (("lat", b), ("ctx", 2)):
                sc = self.sb(es, f"mt_sc_{nm}", [128, D], F32)
                sh = self.sb(es, f"mt_sh_{nm}", [128, D], F32)
                fw.dma("sp", sh[:], R["modrow"][layer, v, 0:D].partition_broadcast(128), reads=["modrow"], writes=[f"mt_sh_{nm}"])
                fw.dma("sp", sc[:], R["modrow"][layer, v, D:2 * D].partition_broadcast(128), reads=["modrow"], writes=[f"mt_sc_{nm}"])
                fw.op("dve", lambda: nc.vector.scalar_tensor_tensor(out=sc[:], in0=sc[:], scalar=1.0, in1=tmp[:],
                                                                   op0=ALU.add, op1=ALU.mult),
                      reads=[f"mt_sc_{nm}", "mt_tmp"], writes=[f"mt_sc_{nm}"])
                res[f"sc_{nm}"] = (sc, f"mt_sc_{nm}")
                res[f"sh_{nm}"] = (sh, f"mt_sh_{nm}")
        if want_post:
            tmp2 = self.sb(es, "mt_tmp2", [128, D], F32)
            fw.dma("sp", tmp2[:], I["post_norm_w"][layer].partition_broadcast(128), writes=["mt_tmp2"])
            for nm, v in (("lat", b), ("ctx", 2)):
                g = self.sb(es, f"mt_g_{nm}", [128, D], F32)
                fw.dma("sp", g[:], R["modrow"][layer, v, 2 * D:3 * D].partition_broadcast(128), reads=["modrow"], writes=[f"mt_g_{nm}"])
                fw.op("dve", lambda: nc.vector.tensor_tensor(out=g[:], in0=g[:], in1=tmp2[:], op=ALU.mult),
                      reads=[f"mt_g_{nm}", "mt_tmp2"], writes=[f"mt_g_{nm}"])
                res[f"g_{nm}"] = (g, f"mt_g_{nm}")
        return res

    def norm_tile(self, P, layer, b, t, hT, hk, j, mt):
        nc, fw = self.nc, self.fw
        nm = "ctx" if t < 2 else "lat"
        sc, sck = mt[f"sc_{nm}"]
        sh, shk = mt[f"sh_{nm}"]
        xt, xk = P["xring"].next()
        fw.dma("sp", xt[:], self.src_rows(layer, b, t * 128, 128), reads=["x1c"] if layer == 1 else [], writes=[xk])
        jk, jkk = P["junk"].next()
        st, stk = P["stat"].next()
        fw.op("act", lambda: nc.scalar.activation(out=jk[:], in_=xt[:], func=AF.Square, scale=1.0 / 32.0, accum_out=st[:, 0:1]),
              reads=[xk], writes=[jkk, stk])
        fw.op("act", lambda: nc.scalar.activation(out=st[:, 1:2], in_=st[:, 0:1], func=AF.Sqrt, bias=EPS), reads=[stk], writes=[stk])
        fw.op("dve", lambda: nc.vector.reciprocal(out=st[:, 2:3], in_=st[:, 1:2]), reads=[stk], writes=[stk])
        t1, t1k = P["t1"].next()
        fw.op("dve", lambda: nc.vector.scalar_tensor_tensor(out=t1[:], in0=xt[:], scalar=st[:, 2:3], in1=sc[:],
                                                           op0=ALU.mult, op1=ALU.mult),
              reads=[xk, stk, sck], writes=[t1k])
        hb, hbk = P["hb"].next()
        fw.op("pool", lambda: nc.gpsimd.tensor_tensor(out=hb[:], in0=t1[:], in1=sh[:], op=ALU.add),
              reads=[t1k, shk], writes=[hbk])
        pT, pTk = P["pT"].next()
        for k in range(8):
            fw.op("pe", lambda: nc.tensor.transpose(out=pT[:, k, :], in_=hb[:, k * 128:(k + 1) * 128], identity=self.identb[:]),
                  reads=[hbk, "identb"], writes=[pTk])
        fw.op("act", lambda: nc.scalar.copy(out=hT[:, :, j * 128:(j + 1) * 128], in_=pT[:]), reads=[pTk], writes=[f"{hk}#{j}"])

    def rope_tok(self, P, src, nh, hd, rt, rtk, srck, dst, dstk, dst_off=0, src_off=0):
        nc, fw = self.nc, self.fw
        r = hd // 2
        x1 = src[:, :, src_off:src_off + r]
        x2 = src[:, :, src_off + r:src_off + hd]
        cosb = rt[:, 0:r].unsqueeze(1).to_broadcast([128, nh, r])
        sinb = rt[:, r:hd].unsqueeze(1).to_broadcast([128, nh, r])
        srcks = list(srck) if isinstance(srck, (list, tuple)) else [srck]
        ta, tak = P["ropet"].next()
        tb, tbk = P["ropet"].next()
        a = ta[:, 0:nh, 0:r]
        bb = tb[:, 0:nh, 0:r]
        fw.op("dve", lambda: nc.vector.tensor_tensor(out=a, in0=x1, in1=cosb, op=ALU.mult), reads=srcks + [rtk], writes=[tak])
        fw.op("dve", lambda: nc.vector.tensor_tensor(out=bb, in0=x2, in1=sinb, op=ALU.mult), reads=srcks + [rtk], writes=[tbk])
        fw.op("pool", lambda: nc.gpsimd.tensor_tensor(out=dst[:, :, dst_off:dst_off + r], in0=a, in1=bb, op=ALU.subtract),
              reads=[tak, tbk], writes=[dstk])
        tc_, tck = P["ropet"].next()
        td, tdk = P["ropet"].next()
        c = tc_[:, 0:nh, 0:r]
        d = td[:, 0:nh, 0:r]
        fw.op("dve", lambda: nc.vector.tensor_tensor(out=c, in0=x1, in1=sinb, op=ALU.mult), reads=srcks + [rtk], writes=[tck])
        fw.op("dve", lambda: nc.vector.tensor_tensor(out=d, in0=x2, in1=cosb, op=ALU.mult), reads=srcks + [rtk], writes=[tdk])
        fw.op("pool", lambda: nc.gpsimd.tensor_tensor(out=dst[:, :, dst_off + r:dst_off + hd], in0=c, in1=d, op=ALU.add),
              reads=[tck, tdk], writes=[dstk])

    def post_tile(self, P, py, pyk, g, gk, xin_ap, xin_reads, out_ap, out_key):
        nc, fw = self.nc, self.fw
        xt, xk = P["xres"].next()
        fw.dma("sp", xt[:], xin_ap, reads=xin_reads, writes=[xk])
        jk, jkk = P["junk"].next()
        st, stk = P["stat"].next()
        fw.op("act", lambda: nc.scalar.activation(out=jk[:], in_=py[:], func=AF.Square, scale=1.0 / 32.0, accum_out=st[:, 0:1]),
              reads=[pyk], writes=[jkk, stk])
        fw.op("act", lambda: nc.scalar.activation(out=st[:, 1:2], in_=st[:, 0:1], func=AF.Sqrt, bias=EPS), reads=[stk], writes=[stk])
        fw.op("dve", lambda: nc.vector.reciprocal(out=st[:, 2:3], in_=st[:, 1:2]), reads=[stk], writes=[stk])
        t2, t2k = P["t2"].next()
        fw.op("dve", lambda: nc.vector.scalar_tensor_tensor(out=t2[:], in0=py[:], scalar=st[:, 2:3], in1=g[:],
                                                           op0=ALU.mult, op1=ALU.mult),
              reads=[pyk, stk, gk], writes=[t2k])
        fw.op("pool", lambda: nc.gpsimd.tensor_tensor(out=t2[:], in0=t2[:], in1=xt[:], op=ALU.add),
              reads=[t2k, xk], writes=[t2k])
        fw.dma(self.QS, out_ap, t2[:], reads=[t2k], writes=[out_key])

    def layer0(self):
        nc, fw, I, R = self.nc, self.fw, self.I, self.R
        stop = getattr(self, "stop", None)
        with ExitStack() as esl:
            win = self.load_weight_bf16(esl, "l0_win", I["e_w_in"], E_IN)
            if stop == "l0w":
                return
            for b in range(NB):
                self.l0_stage_a(b, win)
                fw.barrier()
                if stop == "l0a0":
                    return
        if stop == "l0a":
            return
        with ExitStack() as esl:
            wout = self.load_weight_bf16(esl, "l0_wout", I["e_w_out"], D)
            for b in range(NB):
                self.l0_stage_b(b, wout)
                fw.barrier()

    def l0_stage_a(self, b, win):
        nc, fw, I, R = self.nc, self.fw, self.I, self.R
        with ExitStack() as es:
            mt = self.mod_tiles(es, 0, b, want_pre=True, want_post=False)
            P = {}
            P["xring"] = self.ring(es, "a_x", [128, D], F32, 2)
            P["junk"] = self.ring(es, "a_junk", [128, D], BF16, 2)
            P["stat"] = self.ring(es, "a_stat", [128, 4], F32, 4)
            P["t1"] = self.ring(es, "a_t1", [128, D], F32, 2)
            P["hb"] = self.ring(es, "a_hb", [128, D], BF16, 2)
            P["pT"] = self.ring(es, "a_pT", [128, 8, 128], BF16, 2, psum=True)
            P["ropet"] = self.ring(es, "a_ropet", [128, 8, 32], F32, 8)
            hTr = self.ring(es, "a_hT", [128, 8, 512], BF16, 3)
            ptok = self.ring(es, "a_ptok", [128, 512], F32, 2, psum=True)
            pfm = self.ring(es, "a_pfm", [128, 512], F32, 3, psum=True)
            phalo = self.ps(es, "a_phalo", [128, 4, 2, 2], F32)
            halo = self.ring(es, "a_halo", [128, 8, 2], BF16, 2)
            cw = self.sb(es, "a_cw", [128, 3, 4], F32)
            for j_ in range(3):
                fw.dma("sp", cw[:, j_, :], I["e_conv_w"][j_, :].rearrange("(c p) -> p c", p=128), writes=["a_cw"],
                       allow_slow_non_contiguous=True)
            rtr = self.ring(es, "a_rt", [128, 64], F32, 3)
            qbr = self.ring(es, "a_qb", [128, 8, 64], BF16, 2)
            kbr = self.ring(es, "a_kb", [128, 2, 64], BF16, 2)
            qTst = self.ring(es, "a_qTst", [128, 4, 512], BF16, 2)
            kTst = self.ring(es, "a_kTst", [128, 512], BF16, 2)
            vst = self.ring(es, "a_vst", [128, 4, 128], BF16, 2)
            zast = self.ring(es, "a_zast", [128, 512], F32, 3)
            bcs = self.ring(es, "a_bcs", [128, 512], F32, 2)
            uext = self.ring(es, "a_uext", [128, 514], F32, 2)
            acc = self.ring(es, "a_acc", [128, 512], F32, 2)
            szr = self.ring(es, "a_sz", [128, 512], F32, 2)
            hbh = self.ring(es, "a_hbh", [128, 2], F32, 2)
            bgst = self.ring(es, "a_bgst", [128, 512], BF16, 2)

            hts = {}

            def emit_norm(ci):
                tok0, ntok = CHUNKS[ci]
                hT, hk = hTr.next()
                hts[ci] = (hT, hk)
                for j in range(ntok // 128):
                    self.norm_tile(P, 0, b, tok0 // 128 + j, hT, hk, j, mt)

            def emit_proj(ci):
                tok0, ntok = CHUNKS[ci]
                nt = ntok // 128
                hT, hk = hts[ci]
                hkeys = [f"{hk}#{j}" for j in range(nt)]
                hl, hlk = halo.next()
                first = ci in (0, 1)
                last = ci in (0, len(CHUNKS) - 1)
                if first:
                    fw.op("dve", lambda: nc.vector.memset(hl[:, :, 0:1], 0.0), writes=[hlk])
                else:
                    pT_, pk_ = hts[ci - 1]
                    pn = CHUNKS[ci - 1][1]
                    fw.op("dve", lambda: nc.vector.tensor_copy(out=hl[:, :, 0:1], in_=pT_[:, :, pn - 1:pn]),
                          reads=[f"{pk_}#{pn // 128 - 1}"], writes=[hlk])
                if last:
                    fw.op("dve", lambda: nc.vector.memset(hl[:, :, 1:2], 0.0), writes=[hlk])
                else:
                    nT_, nk_ = hts[ci + 1]
                    fw.op("dve", lambda: nc.vector.tensor_copy(out=hl[:, :, 1:2], in_=nT_[:, :, 0:1]),
                          reads=[f"{nk_}#0"], writes=[hlk])
                qs_, qsk = qTst.next()
                ks_, ksk = kTst.next()
                vs_, vsk = vst.next()
                for j in range(nt):
                    t = tok0 // 128 + j
                    hkj = f"{hk}#{j}"
                    rt, rtk = rtr.next()
                    fw.dma("sp", rt[:], I["ropeA"][t * 128:(t + 1) * 128, :], writes=[rtk])
                    pkv, pkvk = ptok.next()
                    for k in range(8):
                        fw.op("pe", lambda: nc.tensor.matmul(pkv[:, 0:256], lhsT=hT[:, k, j * 128:(j + 1) * 128], rhs=win[:, k, 0:256],
                                                             start=(k == 0), stop=(k == 7)), reads=[hkj, "l0_win"], writes=[pkvk])
                    kb_, kbk = kbr.next()
                    self.rope_tok(P, pkv[:, 0:128].rearrange("p (h d) -> p h d", h=2), 2, 64, rt, rtk, pkvk, kb_, kbk)
                    fw.op("act", lambda: nc.scalar.copy(out=vs_[:, j, :], in_=pkv[:, 128:256]), reads=[pkvk], writes=[f"{vsk}#{j}"])
                    pq, pqk = ptok.next()
                    for k in range(8):
                        fw.op("pe", lambda: nc.tensor.matmul(pq[:], lhsT=hT[:, k, j * 128:(j + 1) * 128], rhs=win[:, k, 256:768],
                                                             start=(k == 0), stop=(k == 7)), reads=[hkj, "l0_win"], writes=[pqk])
                    qb_, qbk = qbr.next()
                    self.rope_tok(P, pq[:].rearrange("p (h d) -> p h d", h=8), 8, 64, rt, rtk, pqk, qb_, qbk)
                    pT, pTk = P["pT"].next()
                    qflat = qb_[:].rearrange("p h d -> p (h d)")
                    for c in range(4):
                        fw.op("pe", lambda: nc.tensor.transpose(out=pT[:, c, :], in_=qflat[:, c * 128:(c + 1) * 128], identity=self.identb[:]),
                              reads=[qbk, "identb"], writes=[pTk])
                    fw.op("pe", lambda: nc.tensor.transpose(out=pT[:, 4, :], in_=kb_[:].rearrange("p h d -> p (h d)"), identity=self.identb[:]),
                          reads=[kbk, "identb"], writes=[pTk])
                    fw.op("act", lambda: nc.scalar.copy(out=qs_[:, :, j * 128:(j + 1) * 128], in_=pT[:, 0:4, :]), reads=[pTk], writes=[f"{qsk}#{j}"])
                    fw.op("act", lambda: nc.scalar.copy(out=ks_[:, j * 128:(j + 1) * 128], in_=pT[:, 4, :]), reads=[pTk], writes=[f"{ksk}#{j}"])
                    pz, pzk = ptok.next()
                    for k in range(8):
                        fw.op("pe", lambda: nc.tensor.matmul(pz[:], lhsT=hT[:, k, j * 128:(j + 1) * 128], rhs=win[:, k, 2304:2816],
                                                             start=(k == 0), stop=(k == 7)), reads=[hkj, "l0_win"], writes=[pzk])
                    za_, zak = zast.next()
                    fw.op("act", lambda: nc.scalar.activation(out=za_[:], in_=pz[:], func=AF.Silu), reads=[pzk], writes=[zak])
                    fw.dma(self.QS, R["za0"][b, t * 128:(t + 1) * 128, :], za_[:], reads=[zak], writes=["za0"])
                sl = slice(tok0, tok0 + ntok)
                if getattr(self, "cut", 0) == 2:
                    return
                fw.dma(self.QS, R["qT0"][b].rearrange("j p t -> p j t")[:, :, sl], qs_[:, :, 0:ntok],
                       reads=[f"{qsk}#{j}" for j in range(nt)], writes=["qT0"])
                fw.dma(self.QS, R["kT0"][b][:, sl], ks_[:, 0:ntok], reads=[f"{ksk}#{j}" for j in range(nt)], writes=["kT0"])
                fw.dma(self.QS, R["v0"][b, sl, :].rearrange("(j p) d -> p j d", p=128), vs_[:, 0:nt, :],
                       reads=[f"{vsk}#{j}" for j in range(nt)], writes=["v0"])
                if getattr(self, "cut", 0) == 3:
                    return
                for c in range(4):
                    for g2 in range(2):
                        col0 = (1280 if g2 == 0 else 1792) + c * 128
                        for k in range(8):
                            fw.op("pe", lambda: nc.tensor.matmul(phalo[:, c, g2, :], lhsT=win[:, k, col0:col0 + 128], rhs=hl[:, k, :],
                                                                 start=(k == 0), stop=(k == 7)), reads=[hlk, "l0_win"], writes=["a_phalo"])
                if getattr(self, "cut", 0) == 4:
                    return
                for c in range(4):
                    def fm(col0):
                        pt_, ptk = pfm.next()
                        for k in range(8):
                            fw.op("pe", lambda: nc.tensor.matmul(pt_[:, 0:ntok], lhsT=win[:, k, col0:col0 + 128], rhs=hT[:, k, 0:ntok],
                                                                 start=(k == 0), stop=(k == 7)), reads=hkeys + ["l0_win"], writes=[ptk])
                        return pt_, ptk
                    pbc, pbck = fm(1280 + c * 128)
                    bc_, bck = bcs.next()
                    fw.op("act", lambda: nc.scalar.copy(out=bc_[:, 0:ntok], in_=pbc[:, 0:ntok]), reads=[pbck], writes=[bck])
                    pbx, pbxk = fm(1792 + c * 128)
                    ue, uek = uext.next()
                    fw.op("dve", lambda: nc.vector.tensor_tensor(out=ue[:, 1:ntok + 1], in0=bc_[:, 0:ntok], in1=pbx[:, 0:ntok], op=ALU.mult),
                          reads=[bck, pbxk], writes=[uek])
                    hb_, hbk_ = hbh.next()
                    fw.op("act", lambda: nc.scalar.copy(out=hb_[:], in_=phalo[:, c, 0, :]), reads=["a_phalo"], writes=[hbk_])
                    fw.op("dve", lambda: nc.vector.tensor_tensor(out=ue[:, 0:1], in0=hb_[:, 0:1], in1=phalo[:, c, 1, 0:1], op=ALU.mult),
                          reads=[hbk_, "a_phalo"], writes=[uek])
                    fw.op("dve", lambda: nc.vector.tensor_tensor(out=ue[:, ntok + 1:ntok + 2], in0=hb_[:, 1:2], in1=phalo[:, c, 1, 1:2], op=ALU.mult),
                          reads=[hbk_, "a_phalo"], writes=[uek])
                    ac, ack = acc.next()
                    fw.op("dve", lambda: nc.vector.tensor_scalar(out=ac[:, 0:ntok], in0=ue[:, 0:ntok], scalar1=cw[:, 0, c:c + 1], scalar2=None, op0=ALU.mult),
                          reads=[uek, "a_cw"], writes=[ack])
                    fw.op("dve", lambda: nc.vector.scalar_tensor_tensor(out=ac[:, 0:ntok], in0=ue[:, 1:ntok + 1], scalar=cw[:, 1, c:c + 1], in1=ac[:, 0:ntok],
                                                                       op0=ALU.mult, op1=ALU.add), reads=[uek, "a_cw", ack], writes=[ack])
                    fw.op("dve", lambda: nc.vector.scalar_tensor_tensor(out=ac[:, 0:ntok], in0=ue[:, 2:ntok + 2], scalar=cw[:, 2, c:c + 1], in1=ac[:, 0:ntok],
                                                                       op0=ALU.mult, op1=ALU.add), reads=[uek, "a_cw", ack], writes=[ack])
                    pbb, pbbk = fm(768 + c * 128)
                    fw.op("dve", lambda: nc.vector.tensor_tensor(out=ac[:, 0:ntok], in0=ac[:, 0:ntok], in1=pbb[:, 0:ntok], op=ALU.mult),
                          reads=[ack, pbbk], writes=[ack])
                    pzb, pzbk = fm(2816 + c * 128)
                    sz_, szk = szr.next()
                    fw.op("act", lambda: nc.scalar.activation(out=sz_[:, 0:ntok], in_=pzb[:, 0:ntok], func=AF.Silu), reads=[pzbk], writes=[szk])
                    bg_, bgk = bgst.next()
                    fw.op("pool", lambda: nc.gpsimd.tensor_tensor(out=bg_[:, 0:ntok], in0=ac[:, 0:ntok], in1=sz_[:, 0:ntok], op=ALU.mult),
                          reads=[ack, szk], writes=[bgk])
                    fw.dma(self.QS, R["bg0T"][b, c, :, sl], bg_[:, 0:ntok], reads=[bgk], writes=["bg0T"])

            n = len(CHUNKS)
            emit_norm(0)
            cut = getattr(self, "cut", 0)
            if cut == 1:
                return
            for ci in range(n):
                if ci + 1 < n:
                    emit_norm(ci + 1)
                emit_proj(ci)
                if cut >= 2 and ci >= cut - 5:
                    return

    def l0_stage_b(self, b, wout):
        nc, fw, I, R = self.nc, self.fw, self.I, self.R
        with ExitStack() as es:
            mt = self.mod_tiles(es, 0, b, want_pre=False, want_post=True)
            P = {}
            P["xres"] = self.ring(es, "b_xres", [128, D], F32, 2)
            P["junk"] = self.ring(es, "b_junk", [128, D], BF16, 2)
            P["stat"] = self.ring(es, "b_stat", [128, 4], F32, 4)
            P["t2"] = self.ring(es, "b_t2", [128, D], F32, 2)
            kTp = [[None, None], [None, None]]
            for kv_ in range(2):
                for p_ in range(2):
                    kt_ = self.sb(es, f"b_kTp{kv_}{p_}", [128, T], BF16)
                    key_ = f"b_kTp{kv_}{p_}"
                    fw.op("pool", lambda: nc.gpsimd.memset(kt_[:], 0.0), writes=[key_])
                    fw.dma("sp", kt_[p_ * 64:(p_ + 1) * 64, :], R["kT0"][b, kv_ * 64:(kv_ + 1) * 64, :], reads=["kT0"], writes=[key_])
                    kTp[kv_][p_] = (kt_, key_)
            vall = self.sb(es, "b_vall", [128, NT, 2, 80], BF16)
            fw.op("pool", lambda: nc.gpsimd.memset(vall[:], 1.0), writes=["b_vall"])
            for t in range(NT):
                fw.dma("sp", vall[:, t, :, 0:64], R["v0"][b, t * 128:(t + 1) * 128, :].rearrange("p (h d) -> p h d", h=2),
                       reads=["v0"], writes=["b_vall"])
            mprev = self.sb(es, "b_mprev", [128, 2, 128], BF16)
            mnext = self.sb(es, "b_mnext", [128, 2, 128], BF16)
            fw.op("dve", lambda: nc.vector.tensor_copy(out=mprev[:], in_=self.cf[:, C_MPREV, :].unsqueeze(1).to_broadcast([128, 2, 128])),
                  reads=["c128f"], writes=["b_mprev"])
            fw.op("dve", lambda: nc.vector.tensor_copy(out=mnext[:], in_=self.cf[:, C_MNEXT, :].unsqueeze(1).to_broadcast([128, 2, 128])),
                  reads=["c128f"], writes=["b_mnext"])
            masks = {"P": (mprev, "b_mprev"), "N": (mnext, "b_mnext")}
            snk = self.sb(es, "b_snk", [128, 8], F32)
            esk = self.sb(es, "b_esk", [128, 8], F32)
            fw.dma("sp", snk[:], I["e_sink"].partition_broadcast(128), writes=["b_snk"])
            for kv_ in range(2):
                fw.op("act", lambda: nc.scalar.activation(out=esk[:, kv_ * 4:(kv_ + 1) * 4].rearrange("q (p i) -> q p i", p=2),
                                                          in_=snk[:, kv_ * 4:(kv_ + 1) * 4].rearrange("q (i p) -> q p i", p=2), func=AF.Exp),
                      reads=["b_snk"], writes=["b_esk"])
            psS = self.ring(es, "b_psS", [128, 512], F32, 3, psum=True)
            poR = self.ring(es, "b_po", [128, 4, 80], F32, 2, psum=True)
            pT2 = self.ps(es, "b_pT2", [128, 4, 128], BF16)
            py = self.ps(es, "b_py", [128, D], F32)
            qblk = self.ring(es, "b_qblk", [128, 4, 128], BF16, 3)
            zablk = self.ring(es, "b_zablk", [128, 512], F32, 3)
            bgblk = self.ring(es, "b_bgblk", [128, 4, 128], BF16, 3)
            ptr = self.ring(es, "b_pt", [128, 512], BF16, 12)
            asb = self.ring(es, "b_asb", [128, 8, 64], F32, 3)
            den = self.ring(es, "b_den", [128, 8], F32, 4)
            agr = self.ring(es, "b_ag", [128, 512], BF16, 2)
            agT = self.ring(es, "b_agT", [128, 4, 128], BF16, 2)
            scale = 64 ** -0.5

            loads = {}
            tails = {}

            def emit_loads(t):
                q_, qk = qblk.next()
                fw.dma("sp", q_[:], R["qT0"][b].rearrange("j p t -> p j t")[:, :, t * 128:(t + 1) * 128], reads=["qT0"], writes=[qk])
                z_, zk = zablk.next()
                fw.dma("sp", z_[:], R["za0"][b, t * 128:(t + 1) * 128, :], reads=["za0"], writes=[zk])
                g_, gk = bgblk.next()
                fw.dma("sp", g_[:], R["bg0T"][b].rearrange("j p t -> p j t")[:, :, t * 128:(t + 1) * 128], reads=["bg0T"], writes=[gk])
                loads[t] = (q_, qk, z_, zk, g_, gk)

            def emit_block(t):
                q_, qk, z_, zk, g_, gk = loads.pop(t)
                if self.cutc == 5:
                    return
                if t < 2:
                    kbs = [(0, None), (1, None)]
                else:
                    qi = t - 2
                    kbs = [(0, None), (1, None)]
                    if qi > 0:
                        kbs.append((t - 1, "P"))
                    kbs.append((t, None))
                    if qi < 15:
                        kbs.append((t + 1, "N"))
                a_, ak = asb.next()
                for kv in range(2):
                    pts = []
                    for (kb, mk) in kbs:
                        ps_, psk = psS.next()
                        for p in range(2):
                            kt, ktk = kTp[kv][p]
                            fw.op("pe", lambda: nc.tensor.matmul(ps_[:, p * 256:(p + 1) * 256],
                                                                 lhsT=kt[:, kb * 128:(kb + 1) * 128],
                                                                 rhs=q_[:, 2 * kv:2 * kv + 2, :].rearrange("p a b -> p (a b)"),
                                                                 start=True, stop=(mk is None)),
                                  reads=[ktk, qk], writes=[psk])
                            if mk is not None:
                                m_, mkk = masks[mk]
                                fw.op("pe", lambda: nc.tensor.matmul(ps_[:, p * 256:(p + 1) * 256], lhsT=self.identb[:],
                                                                     rhs=m_[:].rearrange("p a b -> p (a b)"), start=False, stop=True),
                                      reads=["identb", mkk], writes=[psk])
                        pt_, ptk = ptr.next()
                        fw.op("act", lambda: nc.scalar.activation(out=pt_[:], in_=ps_[:], func=AF.Exp, scale=scale), reads=[psk], writes=[ptk])
                        pts.append((pt_, ptk, kb))
                        if self.cutc == 1:
                            return
                    if self.cutc == 2:
                        return
                    po, pok = poR.next()
                    for slot in range(4):
                        for n_, (pt_, ptk, kb) in enumerate(pts):
                            fw.op("pe", lambda: nc.tensor.matmul(po[:, slot, 0:65], lhsT=pt_[:, slot * 128:(slot + 1) * 128], rhs=vall[:, kb, kv, 0:65],
                                                                 start=(n_ == 0), stop=(n_ == len(pts) - 1)),
                                  reads=[ptk, "b_vall"], writes=[pok])
                    if self.cutc == 3:
                        return
                    dn, dnk = den.next()
                    fw.op("dve", lambda: nc.vector.tensor_tensor(out=dn[:, 0:4], in0=po[:, :, 64], in1=esk[:, kv * 4:(kv + 1) * 4], op=ALU.add),
                          reads=[pok, "b_esk"], writes=[dnk])
                    fw.op("dve", lambda: nc.vector.reciprocal(out=dn[:, 4:8], in_=dn[:, 0:4]), reads=[dnk], writes=[dnk])
                    for slot in range(4):
                        p, i = slot // 2, slot % 2
                        h = 4 * kv + 2 * i + p
                        fw.op("dve", lambda: nc.vector.tensor_scalar(out=a_[:, h, :], in0=po[:, slot, 0:64], scalar1=dn[:, 4 + slot:5 + slot],
                                                                    scalar2=None, op0=ALU.mult),
                              reads=[pok, dnk], writes=[ak])
                    if self.cutc == 4:
                        return
                tails[t] = (a_, ak, z_, zk, g_, gk)

            def emit_tail(t):
                a_, ak, z_, zk, g_, gk = tails.pop(t)
                ag, agk = agr.next()
                fw.op("pool", lambda: nc.gpsimd.tensor_tensor(out=ag[:], in0=a_[:].rearrange("p h d -> p (h d)"), in1=z_[:], op=ALU.mult),
                      reads=[ak, zk], writes=[agk])
                for c in range(4):
                    fw.op("pe", lambda: nc.tensor.transpose(out=pT2[:, c, :], in_=ag[:, c * 128:(c + 1) * 128], identity=self.identb[:]),
                          reads=[agk, "identb"], writes=["b_pT2"])
                at, atk = agT.next()
                fw.op("act", lambda: nc.scalar.copy(out=at[:], in_=pT2[:]), reads=["b_pT2"], writes=[atk])
                for half in range(2):
                    for k in range(8):
                        src, srk = (at, atk) if k < 4 else (g_, gk)
                        fw.op("pe", lambda: nc.tensor.matmul(py[:, half * 512:(half + 1) * 512], lhsT=src[:, k % 4, :],
                                                             rhs=wout[:, k, half * 512:(half + 1) * 512], start=(k == 0), stop=(k == 7)),
                              reads=[srk, "l0_wout"], writes=["b_py"])
                nm = "ctx" if t < 2 else "lat"
                g, gkk = mt[f"g_{nm}"]
                self.post_tile(P, py, "b_py", g, gkk, self.src_rows(0, b, t * 128, 128), [],
                               R["x1c"][b, t * 128:(t + 1) * 128, :], "x1c")

            emit_loads(0)
            emit_loads(1)
            emit_block(0)
            for t in range(NT):
                if t + 2 < NT:
                    emit_loads(t + 2)
                if t + 1 < NT:
                    emit_block(t + 1)
                emit_tail(t)

    def layer1(self):
        nc, fw, I, R = self.nc, self.fw, self.I, self.R
        stop = getattr(self, "stop", None)
        with ExitStack() as esl:
            win = self.load_weight_bf16(esl, "l1_win", I["o_w_in"], O_IN)
            nq = self.sb(esl, "l1_nq", [128, 6], F32)
            nkv = self.sb(esl, "l1_nkv", [128, 2], F32)
            fw.dma("sp", nq[:], I["o_q_norm_w"].rearrange("(k p) -> p k", p=128), writes=["l1_nq"], allow_slow_non_contiguous=True)
            fw.dma("sp", nkv[:], I["o_kv_norm_w"].rearrange("(k p) -> p k", p=128), writes=["l1_nkv"], allow_slow_non_contiguous=True)
            wuq = self.sb(esl, "l1_wuq", [128, 6, 768], BF16)
            wukvk = self.sb(esl, "l1_wukvk", [128, 2, 512], BF16)
            wukvv = self.sb(esl, "l1_wukvv", [128, 2, 512], BF16)
            with ExitStack() as es2:
                stg = self.ring(es2, "l1_stg", [128, 1024], F32, 2)
                for c in range(6):
                    st, sk = stg.next()
                    fw.dma("sp", st[:, 0:768], I["o_w_uq"][c * 128:(c + 1) * 128, :], writes=[sk])
                    fw.op("dve", lambda: nc.vector.tensor_scalar(out=wuq[:, c, :], in0=st[:, 0:768], scalar1=nq[:, c:c + 1], scalar2=None, op0=ALU.mult),
                          reads=[sk, "l1_nq"], writes=["l1_wuq"])
                for c in range(2):
                    st, sk = stg.next()
                    fw.dma("sp", st[:], I["o_w_ukv"][c * 128:(c + 1) * 128, :], writes=[sk])
                    sv = st[:].rearrange("p (h two d) -> p h two d", two=2, d=64)
                    fw.op("dve", lambda: nc.vector.tensor_scalar(out=wukvk[:, c, :].rearrange("p (h d) -> p h d", d=64), in0=sv[:, :, 0, :],
                                                                scalar1=nkv[:, c:c + 1], scalar2=None, op0=ALU.mult),
                          reads=[sk, "l1_nkv"], writes=["l1_wukvk"])
                    fw.op("dve", lambda: nc.vector.tensor_scalar(out=wukvv[:, c, :].rearrange("p (h d) -> p h d", d=64), in0=sv[:, :, 1, :],
                                                                scalar1=nkv[:, c:c + 1], scalar2=None, op0=ALU.mult),
                          reads=[sk, "l1_nkv"], writes=["l1_wukvv"])
                fw.barrier()
            if self.cut1 == 1:
                return
            for b in range(NB):
                if _os_env("KONLYB") and int(_os_env("KONLYB")) != b:
                    continue
                self.l1_stage_a(0 if _os_env("KSAMEB") else b, win, wuq, wukvk, wukvv)
                fw.barrier()
                if self.cut1 >= 2:
                    return
        if stop == "l1a":
            return
        for b in range(NB):
            self.l1_stage_b(b)
            fw.barrier()
        if stop == "l1b":
            return
        with ExitStack() as esl:
            wout = self.load_weight_bf16(esl, "l1_wout", I["o_w_out"], D)
            for b in range(NB):
                with ExitStack() as esb:
                    hsum = self.sb(esb, "c_hsum", [128, 16, 4, 128], F32)
                    self.l1_stage_c1(b, hsum)
                    fw.barrier()
                    self.l1_stage_c2(b, hsum, wout)
                    fw.barrier()

    def l1_stage_a(self, b, win, wuq, wukvk, wukvv):
        nc, fw, I, R = self.nc, self.fw, self.I, self.R
        CH = [(0, 256)] + [(256 + 512 * i, 512) for i in range(4)]
        onesf = self.onesb[:]
        with ExitStack() as es:
            mt = self.mod_tiles(es, 1, b, want_pre=True, want_post=False)
            P = {}
            P["xring"] = self.ring(es, "a_x", [128, D], F32, 2)
            P["junk"] = self.ring(es, "a_junk", [128, D], BF16, 1)
            P["stat"] = self.ring(es, "a_stat", [128, 4], F32, 4)
            P["t1"] = self.ring(es, "a_t1", [128, D], F32, 1)
            P["hb"] = self.ring(es, "a_hb", [128, D], BF16, 2)
            P["pT"] = self.ring(es, "a_pT", [128, 8, 128], BF16, 2, psum=True)
            P["ropet"] = self.ring(es, "a_ropet", [128, 8, 32], F32, 8)
            hTr = self.ring(es, "a_hT", [128, 8, 512], BF16, 2)
            pfm = self.ring(es, "a_pfm", [128, 512], F32, 2, psum=True)
            ptok = self.ring(es, "a_ptok", [128, 512], F32, 2, psum=True)
            prep = self.ps(es, "a_prep", [128, 512], F32)
            pcol = self.ps(es, "a_pcol", [128, 16], F32)
            ckvTr = self.ring(es, "a_ckvT", [128, 2, 512], BF16, 2)
            sqkr = self.ring(es, "a_sqk", [128, 2, 512], BF16, 2)
            rrepr = self.ring(es, "a_rrep", [128, 2, 512], F32, 1)
            rcolr = self.ring(es, "a_rcol", [128, 16], F32, 2)
            knstr = self.ring(es, "a_knst", [128, 4, 512], BF16, 1)
            vstr = self.ring(es, "a_vst", [128, 512], BF16, 2)
            rtr = self.ring(es, "a_rt", [128, 32], F32, 3)
            krfr = self.ring(es, "a_krf", [128, 1, 32], BF16, 2)
            krstr = self.ring(es, "a_krst", [32, 512], BF16, 2)
            mkstr = self.ring(es, "a_mkst", [128, 256], BF16, 2)
            mkTstr = self.ring(es, "a_mkTst", [128, 512], BF16, 2)
            mvstr = self.ring(es, "a_mvst", [128, 512], BF16, 2)
            mgstr = self.ring(es, "a_mgst", [128, 16], F32, 2)
            cqTr = self.ring(es, "a_cqT", [128, 6, 512], BF16, 1)
            sqqr = self.ring(es, "a_sqq", [128, 6, 512], BF16, 1)
            qfr = self.ring(es, "a_qf", [128, 8, 96], F32, 1)
            qbfr = self.ring(es, "a_qbf", [128, 8, 96], BF16, 2)
            qstr = self.ring(es, "a_qst", [128, 8, 512], BF16, 1)
            mqTstr = self.ring(es, "a_mqTst", [128, 512], BF16, 2)
            mostr = self.ring(es, "a_most", [128, 512], F32, 2)
            zstr = self.ring(es, "a_zst", [128, 512], F32, 2)

            hts = {}

            def emit_norm(ci):
                tok0, ntok = CH[ci]
                hT, hk = hTr.next()
                hts[ci] = (hT, hk)
                for j in range(ntok // 128):
                    self.norm_tile(P, 1, b, tok0 // 128 + j, hT, hk, j, mt)

            def mm_fm(hT, hkeys, col0, ntok):
                pf, pfk = pfm.next()
                for k in range(8):
                    fw.op("pe", lambda: nc.tensor.matmul(pf[:, 0:ntok], lhsT=win[:, k, col0:col0 + 128], rhs=hT[:, k, 0:ntok],
                                                         start=(k == 0), stop=(k == 7)), reads=hkeys + ["l1_win"], writes=[pfk])
                return pf, pfk

            def mm_tok(hT, hkj, j, col0, ncol, off=0, pt_=None, ptk=None):
                if pt_ is None:
                    pt_, ptk = ptok.next()
                for k in range(8):
                    fw.op("pe", lambda: nc.tensor.matmul(pt_[:, off:off + ncol], lhsT=hT[:, k, j * 128:(j + 1) * 128], rhs=win[:, k, col0:col0 + ncol],
                                                         start=(k == 0), stop=(k == 7)), reads=[hkj, "l1_win"], writes=[ptk])
                return pt_, ptk

            def emit_proj(ci):
                tok0, ntok = CH[ci]
                nt = ntok // 128
                is_lat = tok0 >= LC
                hT, hk = hts[ci]
                hkeys = [f"{hk}#{j}" for j in range(nt)]
                sl = slice(tok0, tok0 + ntok)
                ckvT, ckvk = ckvTr.next()
                sqk, sqkk = sqkr.next()
                for c in range(2):
                    pf, pfk = mm_fm(hT, hkeys, c * 128, ntok)
                    fw.op("act", lambda: nc.scalar.copy(out=ckvT[:, c, 0:ntok], in_=pf[:, 0:ntok]), reads=[pfk], writes=[f"{ckvk}#{c}"])
                    fw.op("act", lambda: nc.scalar.activation(out=sqk[:, c, 0:ntok], in_=pf[:, 0:ntok], func=AF.Square), reads=[pfk], writes=[f"{sqkk}#{c}"])
                ckeys = [f"{ckvk}#0", f"{ckvk}#1"]
                skeys = [f"{sqkk}#0", f"{sqkk}#1"]
                for c in range(2):
                    fw.op("pe", lambda: nc.tensor.matmul(prep[:, 0:ntok], lhsT=onesf, rhs=sqk[:, c, 0:ntok], start=(c == 0), stop=(c == 1)),
                          reads=skeys + ["c128f"], writes=["a_prep"])
                rrep, rrepk = rrepr.next()
                fw.op("act", lambda: nc.scalar.activation(out=rrep[:, 0, 0:ntok], in_=prep[:, 0:ntok], func=AF.Sqrt, bias=EPS, scale=1.0 / 256.0),
                      reads=["a_prep"], writes=[rrepk])
                fw.op("dve", lambda: nc.vector.reciprocal(out=rrep[:, 1, 0:ntok], in_=rrep[:, 0, 0:ntok]), reads=[rrepk], writes=[rrepk])
                rcol, rcolk = rcolr.next()
                for j in range(nt):
                    for c in range(2):
                        fw.op("pe", lambda: nc.tensor.matmul(pcol[:, j:j + 1], lhsT=sqk[:, c, j * 128:(j + 1) * 128], rhs=onesf[:, 0:1],
                                                             start=(c == 0), stop=(c == 1)), reads=skeys + ["c128f"], writes=["a_pcol"])
                fw.op("act", lambda: nc.scalar.activation(out=rcol[:, 4:4 + nt], in_=pcol[:, 0:nt], func=AF.Sqrt, bias=EPS, scale=1.0 / 256.0),
                      reads=["a_pcol"], writes=[rcolk])
                fw.op("dve", lambda: nc.vector.reciprocal(out=rcol[:, 0:nt], in_=rcol[:, 4:4 + nt]), reads=[rcolk], writes=[rcolk])
                if self.cut1 == 2:
                    return
                knst, knk = knstr.next()
                for pair in range(4):
                    pf, pfk = pfm.next()
                    for c in range(2):
                        fw.op("pe", lambda: nc.tensor.matmul(pf[:, 0:ntok], lhsT=wukvk[:, c, pair * 128:(pair + 1) * 128], rhs=ckvT[:, c, 0:ntok],
                                                             start=(c == 0), stop=(c == 1)), reads=ckeys + ["l1_wukvk"], writes=[pfk])
                    fw.op("dve", lambda: nc.vector.tensor_tensor(out=knst[:, pair, 0:ntok], in0=pf[:, 0:ntok], in1=rrep[:, 1, 0:ntok], op=ALU.mult),
                          reads=[pfk, rrepk], writes=[f"{knk}#{pair}"])
                kview = R["kT1"][b].rearrange("(j two) r t -> two r j t", two=2)
                for two_ in range(2):
                    fw.dma(self.QS, kview[two_, 0:64, :, sl], knst[two_ * 64:(two_ + 1) * 64, :, 0:ntok],
                           reads=[f"{knk}#{p_}" for p_ in range(4)], writes=["kT1"])
                if self.cut1 == 3:
                    return
                krst, krk = krstr.next()
                for j in range(nt):
                    t = tok0 // 128 + j
                    hkj = f"{hk}#{j}"
                    tsl = slice(t * 128, (t + 1) * 128)
                    pt_, ptk = ptok.next()
                    for c in range(2):
                        fw.op("pe", lambda: nc.tensor.matmul(pt_[:, 0:512], lhsT=ckvT[:, c, j * 128:(j + 1) * 128], rhs=wukvv[:, c, :],
                                                             start=(c == 0), stop=(c == 1)), reads=ckeys + ["l1_wukvv"], writes=[ptk])
                    vs_, vsk = vstr.next()
                    fw.op("act", lambda: nc.scalar.mul(out=vs_[:], in_=pt_[:, 0:512], mul=rcol[:, j:j + 1]), reads=[ptk, rcolk], writes=[vsk])
                    fw.dma(self.QS, R["v1"][b, tsl, :], vs_[:], reads=[vsk], writes=["v1"])
                    pt_, ptk = mm_tok(hT, hkj, j, 256, 32)
                    mm_tok(hT, hkj, j, 1056, 16, off=64, pt_=pt_, ptk=ptk)
                    rt, rtk = rtr.next()
                    fw.dma("sp", rt[:], I["ropeC"][tsl, :], writes=[rtk])
                    krf, krfk = krfr.next()
                    self.rope_tok(P, pt_[:, 0:32].rearrange("p (h d) -> p h d", h=1), 1, 32, rt, rtk, ptk, krf, krfk)
                    pT, pTk = P["pT"].next()
                    fw.op("pe", lambda: nc.tensor.transpose(out=pT[0:32, 0, :], in_=krf[:, 0, :], identity=self.identb[:]),
                          reads=[krfk, "identb"], writes=[pTk])
                    fw.op("act", lambda: nc.scalar.copy(out=krst[0:32, j * 128:(j + 1) * 128], in_=pT[0:32, 0, :]), reads=[pTk], writes=[f"{krk}#{j}"])
                    mg_, mgk = mgstr.next()
                    fw.op("act", lambda: nc.scalar.copy(out=mg_[:], in_=pt_[:, 64:80]), reads=[ptk], writes=[mgk])
                    fw.dma(self.QS, R["mg1"][b, tsl, :], mg_[:], reads=[mgk], writes=["mg1"])
                    pt_, ptk = mm_tok(hT, hkj, j, 288, 256)
                    mk_, mkk = mkstr.next()
                    fw.op("act", lambda: nc.scalar.mul(out=mk_[:], in_=pt_[:, 0:256], mul=0.125), reads=[ptk], writes=[mkk])
                    fw.dma(self.QS, R["mk1"][b, tsl, :], mk_[:], reads=[mkk], writes=["mk1"])
                    pt_, ptk = mm_tok(hT, hkj, j, 544, 512)
                    mv_, mvk = mvstr.next()
                    fw.op("act", lambda: nc.scalar.copy(out=mv_[:], in_=pt_[:, 0:512]), reads=[ptk], writes=[mvk])
                    fw.dma(self.QS, R["mv1"][b, tsl, :], mv_[:], reads=[mvk], writes=["mv1"])
                if self.cut1 == 4:
                    return
                for h in range(8):
                    fw.dma(self.QS, R["kT1"][b, h, 64:96, sl], krst[0:32, 0:ntok], reads=[f"{krk}#{j}" for j in range(nt)], writes=["kT1"])
                for c in range(2):
                    pf, pfk = mm_fm(hT, hkeys, 288 + c * 128, ntok)
                    mkT_, mkTk = mkTstr.next()
                    fw.op("act", lambda: nc.scalar.mul(out=mkT_[:, 0:ntok], in_=pf[:, 0:ntok], mul=0.125), reads=[pfk], writes=[mkTk])
                    fw.dma(self.QS, R["mkT1"][b, c * 128:(c + 1) * 128, sl], mkT_[:, 0:ntok], reads=[mkTk], writes=["mkT1"])
                if not is_lat:
                    return
                ls = tok0 - LC
                lsl = slice(ls, ls + ntok)
                cqT, cqk = cqTr.next()
                sqq, sqqk = sqqr.next()
                for c in range(6):
                    pf, pfk = mm_fm(hT, hkeys, 1072 + c * 128, ntok)
                    fw.op("act", lambda: nc.scalar.copy(out=cqT[:, c, 0:ntok], in_=pf[:, 0:ntok]), reads=[pfk], writes=[f"{cqk}#{c}"])
                    fw.op("act", lambda: nc.scalar.activation(out=sqq[:, c, 0:ntok], in_=pf[:, 0:ntok], func=AF.Square), reads=[pfk], writes=[f"{sqqk}#{c}"])
                cqkeys = [f"{cqk}#{c}" for c in range(6)]
                sqkeys = [f"{sqqk}#{c}" for c in range(6)]
                for j in range(nt):
                    for c in range(6):
                        fw.op("pe", lambda: nc.tensor.matmul(pcol[:, 8 + j:9 + j], lhsT=sqq[:, c, j * 128:(j + 1) * 128], rhs=onesf[:, 0:1],
                                                             start=(c == 0), stop=(c == 5)), reads=sqkeys + ["c128f"], writes=["a_pcol"])
                fw.op("act", lambda: nc.scalar.activation(out=rcol[:, 12:12 + nt], in_=pcol[:, 8:8 + nt], func=AF.Sqrt, bias=EPS, scale=1.0 / 768.0),
                      reads=["a_pcol"], writes=[rcolk])
                fw.op("dve", lambda: nc.vector.reciprocal(out=rcol[:, 8:8 + nt], in_=rcol[:, 12:12 + nt]), reads=[rcolk], writes=[rcolk])
                qst, qstk = qstr.next()
                for j in range(nt):
                    t = tok0 // 128 + j
                    tsl = slice(t * 128, (t + 1) * 128)
                    qf, qfk = qfr.next()
                    qflat = qf[:].rearrange("p h d -> p (h d)")
                    for (c0, cn) in ((0, 512), (512, 256)):
                        pt_, ptk = ptok.next()
                        for c in range(6):
                            fw.op("pe", lambda: nc.tensor.matmul(pt_[:, 0:cn], lhsT=cqT[:, c, j * 128:(j + 1) * 128], rhs=wuq[:, c, c0:c0 + cn],
                                                                 start=(c == 0), stop=(c == 5)), reads=cqkeys + ["l1_wuq"], writes=[ptk])
                        fw.op("act", lambda: nc.scalar.mul(out=qflat[:, c0:c0 + cn], in_=pt_[:, 0:cn], mul=rcol[:, 8 + j:9 + j]),
                              reads=[ptk, rcolk], writes=[f"{qfk}#{c0}"])
                    qfkeys = [f"{qfk}#0", f"{qfk}#512"]
                    qbf, qbk = qbfr.next()
                    fw.op("pool", lambda: nc.gpsimd.tensor_copy(out=qbf[:, :, 0:64], in_=qf[:, :, 0:64]), reads=qfkeys, writes=[f"{qbk}#n"])
                    rt, rtk = rtr.next()
                    fw.dma("sp", rt[:], I["ropeC"][tsl, :], writes=[rtk])
                    self.rope_tok(P, qf, 8, 32, rt, rtk, qfkeys, qbf, f"{qbk}#r", dst_off=64, src_off=64)
                    pT, pTk = P["pT"].next()
                    for h in range(8):
                        fw.op("pe", lambda: nc.tensor.transpose(out=pT[0:96, h, :], in_=qbf[:, h, :], identity=self.identb[:]),
                              reads=[f"{qbk}#n", f"{qbk}#r", "identb"], writes=[pTk])
                    fw.op("act", lambda: nc.scalar.copy(out=qst[0:96, :, j * 128:(j + 1) * 128], in_=pT[0:96, :, :]), reads=[pTk], writes=[f"{qstk}#{j}"])
                    lt = t - 2
                    ltsl = slice(lt * 128, (lt + 1) * 128)
                    pt_, ptk = mm_tok(hT, f"{hk}#{j}", j, 2096, 512)
                    mo_, mok = mostr.next()
                    fw.op("act", lambda: nc.scalar.activation(out=mo_[:], in_=pt_[:, 0:512], func=AF.Sigmoid), reads=[ptk], writes=[mok])
                    fw.dma(self.QS, R["mo1"][b, ltsl, :], mo_[:], reads=[mok], writes=["mo1"])
                    for zh in range(2):
                        pt_, ptk = mm_tok(hT, f"{hk}#{j}", j, 2608 + zh * 512, 512)
                        z_, zk = zstr.next()
                        fw.op("act", lambda: nc.scalar.activation(out=z_[:], in_=pt_[:, 0:512], func=AF.Silu), reads=[ptk], writes=[zk])
                        fw.dma(self.QS, R["z1"][b, ltsl, zh * 512:(zh + 1) * 512], z_[:], reads=[zk], writes=["z1"])
                fw.dma(self.QS, R["qT1"][b].rearrange("h r t -> r h t")[:, :, lsl], qst[0:96, :, 0:ntok],
                       reads=[f"{qstk}#{j}" for j in range(nt)], writes=["qT1"])
                for c in range(2):
                    pf, pfk = mm_fm(hT, hkeys, 1840 + c * 128, ntok)
                    mq_, mqk = mqTstr.next()
                    fw.op("act", lambda: nc.scalar.copy(out=mq_[:, 0:ntok], in_=pf[:, 0:ntok]), reads=[pfk], writes=[mqk])
                    fw.dma(self.QS, R["mqT1"][b, c * 128:(c + 1) * 128, lsl], mq_[:, 0:ntok], reads=[mqk], writes=["mqT1"])

            n = len(CH)
            emit_norm(0)
            for ci in range(n):
                if ci + 1 < n:
                    emit_norm(ci + 1)
                emit_proj(ci)
                if self.cut1 in (2, 3, 4, 5) or (self.cut1 >= 6 and ci >= self.cut1 - 5):
                    return
                if b == 1 and _os_env("KCUT2") and ci >= int(_os_env("KCUT2")) - 1:
                    return

    def l1_stage_b(self, b):
        nc, fw, I, R = self.nc, self.fw, self.I, self.R
        with ExitStack() as es:
            kT = self.sb(es, "m_kT", [128, 8, T], BF16)
            vall = self.sb(es, "m_vall", [128, NT, 8, 80], BF16)
            fw.dma("sp", kT[0:96, :, :], R["kT1"][b].rearrange("h r t -> r h t"), reads=["kT1"], writes=["m_kT"])
            fw.op("pool", lambda: nc.gpsimd.memset(vall[:], 1.0), writes=["m_vall"])
            for t in range(NT):
                fw.dma("sp", vall[:, t, :, 0:64], R["v1"][b, t * 128:(t + 1) * 128, :].rearrange("p (h d) -> p h d", h=8),
                       reads=["v1"], writes=["m_vall"])
            psS = self.ring(es, "m_psS", [128, 512], F32, 4, psum=True)
            poR = self.ring(es, "m_po", [128, 4, 80], F32, 2, psum=True)
            pT = self.ps(es, "m_pT", [128, 4, 128], BF16)
            qTr = self.ring(es, "m_qT", [128, 8, 512], BF16, 2)
            ptr = self.ring(es, "m_pt", [128, 512], BF16, 40)
            coutr = self.ring(es, "m_cout", [128, 4, 8, 64], F32, 2)
            recr = self.ring(es, "m_rec", [128, 4], F32, 4)
            zr = self.ring(es, "m_z", [128, 512], F32, 2)
            cgr = self.ring(es, "m_cg", [128, 512], BF16, 2)
            cgstr = self.ring(es, "m_cgst", [128, 4, 128], BF16, 2)
            scale = 96 ** -0.5
            for qc in range(4):
                qT, qTk = qTr.next()
                fw.dma("sp", qT[0:96, :, :], R["qT1"][b].rearrange("h r t -> r h t")[:, :, qc * 512:(qc + 1) * 512], reads=["qT1"], writes=[qTk])
                cout, coutk = coutr.next()

                def emit_qk(h):
                    pts = []
                    for kb in range(NT):
                        ps_, psk = psS.next()
                        fw.op("pe", lambda: nc.tensor.matmul(ps_[:], lhsT=kT[0:96, h, kb * 128:(kb + 1) * 128], rhs=qT[0:96, h, :], start=True, stop=True),
                              reads=["m_kT", qTk], writes=[psk])
                        pt_, ptk = ptr.next()
                        fw.op("act", lambda: nc.scalar.activation(out=pt_[:], in_=ps_[:], func=AF.Exp, scale=scale), reads=[psk], writes=[ptk])
                        pts.append((pt_, ptk))
                    return pts

                def emit_pv(h, pts):
                    po, pok = poR.next()
                    for qs in range(4):
                        for kb in range(NT):
                            pt_, ptk = pts[kb]
                            fw.op("pe", lambda: nc.tensor.matmul(po[:, qs, 0:65], lhsT=pt_[:, qs * 128:(qs + 1) * 128], rhs=vall[:, kb, h, 0:65],
                                                                 start=(kb == 0), stop=(kb == NT - 1)), reads=[ptk, "m_vall"], writes=[pok])
                    rec, reck = recr.next()
                    fw.op("dve", lambda: nc.vector.reciprocal(out=rec[:], in_=po[:, :, 64]), reads=[pok], writes=[reck])
                    fw.op("dve", lambda: nc.vector.tensor_tensor(out=cout[:, :, h, :], in0=po[:, :, 0:64],
                                                                in1=rec[:].unsqueeze(2).to_broadcast([128, 4, 64]), op=ALU.mult),
                          reads=[pok, reck], writes=[f"{coutk}#{h}"])

                nxt = emit_qk(0)
                for h in range(8):
                    cur = nxt
                    if h + 1 < 8:
                        nxt = emit_qk(h + 1)
                    emit_pv(h, cur)
                for qs in range(4):
                    lt = qc * 4 + qs
                    ltsl = slice(lt * 128, (lt + 1) * 128)
                    z_, zk = zr.next()
                    fw.dma("sp", z_[:], R["z1"][b, ltsl, 0:512], reads=["z1"], writes=[zk])
                    cg, cgk = cgr.next()
                    fw.op("pool", lambda: nc.gpsimd.tensor_tensor(out=cg[:], in0=cout[:, qs, :, :].rearrange("p h d -> p (h d)"), in1=z_[:], op=ALU.mult),
                          reads=[f"{coutk}#{h}" for h in range(8)] + [zk], writes=[cgk])
                    for c in range(4):
                        fw.op("pe", lambda: nc.tensor.transpose(out=pT[:, c, :], in_=cg[:, c * 128:(c + 1) * 128], identity=self.identb[:]),
                              reads=[cgk, "identb"], writes=["m_pT"])
                    cgst, cgsk = cgstr.next()
                    fw.op("act", lambda: nc.scalar.copy(out=cgst[:], in_=pT[:]), reads=["m_pT"], writes=[cgsk])
                    fw.dma(self.QS, R["cgT1"][b].rearrange("c p t -> p c t")[:, :, ltsl], cgst[:], reads=[cgsk], writes=["cgT1"])

    def l1_stage_c1(self, b, hsum):
        nc, fw, I, R = self.nc, self.fw, self.I, self.R
        cf = self.cf
        with ExitStack() as es:
            gt = self.sb(es, "c_gt", [128, NT, 16], F32)
            mk = self.sb(es, "c_mk", [128, NT, 256], BF16)
            VO = self.sb(es, "c_VO", [128, NT, 4, 144], BF16)
            mkT = self.sb(es, "c_mkT", [64, 4, T], BF16)
            mqT = self.sb(es, "c_mqT", [64, 4, S], BF16)
            ib = self.sb(es, "c_ib", [128, 8], F32)
            fb = self.sb(es, "c_fb", [128, 8], F32)
            fw.dma("sp", ib[:], I["o_i_bias"].partition_broadcast(128), writes=["c_ib"])
            fw.dma("sp", fb[:], I["o_f_bias"].partition_broadcast(128), writes=["c_fb"])
            fw.op("pool", lambda: nc.gpsimd.memset(VO[:], 1.0), writes=["c_VO"])
            for t in range(NT):
                tsl = slice(t * 128, (t + 1) * 128)
                fw.dma("sp", gt[:, t, :], R["mg1"][b, tsl, :], reads=["mg1"], writes=["c_gt"])
                fw.dma("sp", mk[:, t, :], R["mk1"][b, tsl, :], reads=["mk1"], writes=["c_mk"])
                fw.dma("sp", VO[:, t, :, 0:128], R["mv1"][b, tsl, :].rearrange("p (h d) -> p h d", h=4), reads=["mv1"], writes=["c_VO"])
            fw.dma("sp", mkT[:], R["mkT1"][b].rearrange("(h d) t -> d h t", h=4), reads=["mkT1"], writes=["c_mkT"])
            fw.dma("sp", mqT[:], R["mqT1"][b].rearrange("(h d) t -> d h t", h=4), reads=["mqT1"], writes=["c_mqT"])
            pbig = self.ring(es, "c_pbig", [128, 512], F32, 1, psum=True)
            pC = self.ring(es, "c_pC", [64, 2, 144], F32, 2, psum=True)
            pS = self.ring(es, "c_pS", [128, 128], F32, 1, psum=True)
            pH = self.ring(es, "c_pH", [128, 2, 144], F32, 4, psum=True)
            cbr = self.ring(es, "c_cb", [64, 4, 144], BF16, 6)
            dgr = self.ring(es, "c_dg", [128, 4, 128], F32, 2)
            rmr = self.ring(es, "c_rm", [128, 4, 128], F32, 2)
            er = self.ring(es, "c_e", [64, 4, 128], F32, 2)
            qz0r = self.ring(es, "c_qz0", [64, 4, 128], BF16, 2)
            qz1r = self.ring(es, "c_qz1", [64, 4, 128], BF16, 2)
            for rr in (qz0r, qz1r):
                for i_, tl in enumerate(rr.tiles):
                    fw.op("pool", lambda: nc.gpsimd.memset(tl[:], 0.0), writes=[f"{rr.name}{i_}"])
            dr = self.ring(es, "c_d", [128, 128], F32, 8)
            scr = self.ring(es, "c_sc", [128, 128], BF16, 8)
            dnr = self.ring(es, "c_dn", [128, 8], F32, 6)
            Dd = []
            for d in range(2):
                X = {}
                for nm in ("li", "xf", "l1", "nb", "ngc", "aa", "wcol", "bcol"):
                    X[nm] = self.sb(es, f"c_{nm}{d}", [128, 72], F32)
                X["egf"] = self.sb(es, f"c_egf{d}", [128, 2, 72], F32)
                X["VW"] = self.sb(es, f"c_VW{d}", [128, NT, 4, 144], BF16)
                X["C"] = self.sb(es, f"c_C{d}", [64, 4, 144], F32)
                Dd.append(X)
            for d in range(2):
                X = Dd[d]
                li, xf, l1, nb, ngc, aa, wcol, bcol, egf, VW, C = (X[k] for k in ("li", "xf", "l1", "nb", "ngc", "aa", "wcol", "bcol", "egf", "VW", "C"))
                K_ = lambda nm: f"c_{nm}{d}"
                tri = C_TRIF if d == 0 else C_TRIR
                g3 = lambda a: a[:].rearrange("p (t h) -> p t h", h=4)
                fw.op("dve", lambda: nc.vector.tensor_tensor(out=g3(li), in0=gt[:, :, d * 8:d * 8 + 4],
                                                            in1=ib[:, d * 4:(d + 1) * 4].unsqueeze(1).to_broadcast([128, NT, 4]), op=ALU.add),
                      reads=["c_gt", "c_ib"], writes=[K_("li")])
                fw.op("dve", lambda: nc.vector.tensor_tensor(out=g3(xf), in0=gt[:, :, d * 8 + 4:d * 8 + 8],
                                                            in1=fb[:, d * 4:(d + 1) * 4].unsqueeze(1).to_broadcast([128, NT, 4]), op=ALU.add),
                      reads=["c_gt", "c_fb"], writes=[K_("xf")])
                fw.op("act", lambda: nc.scalar.activation(out=xf[:], in_=xf[:], func=AF.Exp, scale=-1.0), reads=[K_("xf")], writes=[K_("xf")])
                fw.op("act", lambda: nc.scalar.activation(out=l1[:], in_=xf[:], func=AF.Ln, bias=1.0), reads=[K_("xf")], writes=[K_("l1")])
                p1, p1k = pbig.next()
                fw.op("pe", lambda: nc.tensor.matmul(p1[:, 0:72], lhsT=cf[:, tri, :], rhs=l1[:], start=True, stop=True), reads=["c128f", K_("l1")], writes=[p1k])
                fw.op("dve", lambda: nc.vector.tensor_copy(out=nb[:], in_=p1[:, 0:72]), reads=[p1k], writes=[K_("nb")])
                p2, p2k = pbig.next()
                fw.op("pe", lambda: nc.tensor.matmul(p2[:, 0:72], lhsT=cf[:, C_BLK, :], rhs=l1[:], start=True, stop=True), reads=["c128f", K_("l1")], writes=[p2k])
                fw.op("dve", lambda: nc.vector.tensor_copy(out=ngc[:], in_=p2[:, 0:72]), reads=[p2k], writes=[K_("ngc")])
                p3, p3k = pbig.next()
                for half in range(2):
                    fw.op("pe", lambda: nc.tensor.matmul(p3[:, half * 72:(half + 1) * 72], lhsT=cf[:, C_SEL0 + half, :], rhs=l1[:], start=True, stop=True),
                          reads=["c128f", K_("l1")], writes=[p3k])
                fw.op("act", lambda: nc.scalar.activation(out=egf[:].rearrange("p a b -> p (a b)"), in_=p3[:, 0:144], func=AF.Exp, scale=-1.0),
                      reads=[p3k], writes=[K_("egf")])
                fw.op("dve", lambda: nc.vector.tensor_tensor(out=bcol[:], in0=li[:], in1=nb[:], op=ALU.add), reads=[K_("li"), K_("nb")], writes=[K_("bcol")])
                fw.op("dve", lambda: nc.vector.tensor_tensor(out=aa[:], in0=bcol[:], in1=ngc[:], op=ALU.subtract), reads=[K_("bcol"), K_("ngc")], writes=[K_("aa")])
                fw.op("act", lambda: nc.scalar.activation(out=wcol[:], in_=aa[:], func=AF.Exp), reads=[K_("aa")], writes=[K_("wcol")])
                for part in range(2):
                    e_ = "dve" if part == 0 else "pool"
                    eng = nc.vector if part == 0 else nc.gpsimd
                    tt = slice(part * 9, (part + 1) * 9)
                    fw.op(e_, lambda: eng.tensor_tensor(out=VW[:, tt, :, :].rearrange("p t h v -> p (t h) v"),
                                                        in0=VO[:, tt, :, :].rearrange("p t h v -> p (t h) v"),
                                                        in1=wcol[:, part * 36:(part + 1) * 36].unsqueeze(2).to_broadcast([128, 36, 144]), op=ALU.mult),
                          reads=["c_VO", K_("wcol")], writes=[f"c_VW{d}#{part}"])
                fw.op("dve", lambda: nc.vector.memset(C[:], 0.0), writes=[f"c_C{d}#{h}" for h in range(4)])

            written = set()

            def process_tile(d, t):
                X = Dd[d]
                nb, bcol, egf, VW, C = X["nb"], X["bcol"], X["egf"], X["VW"], X["C"]
                K_ = lambda nm: f"c_{nm}{d}"
                ckeys = [f"c_C{d}#{h}" for h in range(4)]
                vwkeys = [f"c_VW{d}#0", f"c_VW{d}#1"]
                mbk = C_MBF if d == 0 else C_MBR
                horder = (0, 1) if d == 0 else (1, 0)
                Cin = {}
                for half in horder:
                    if t >= 2:
                        cb, cbk = cbr.next()
                        fw.op("act", lambda: nc.scalar.copy(out=cb[:], in_=C[:]), reads=ckeys, writes=[cbk])
                        Cin[half] = (cb, cbk)
                    hs_ = slice(half * 64, (half + 1) * 64)
                    for hp in range(2):
                        pc, pck = pC.next()
                        for hh in range(2):
                            h = 2 * hp + hh
                            fw.op("pe", lambda: nc.tensor.matmul(pc[:, hh, 0:129], lhsT=mk[hs_, t, h * 64:(h + 1) * 64], rhs=VW[hs_, t, h, 0:129],
                                                                 start=True, stop=True), reads=["c_mk"] + vwkeys, writes=[pck])
                        for hh in range(2):
                            h = 2 * hp + hh
                            idx = t * 4 + h
                            fw.op("dve", lambda: nc.vector.scalar_tensor_tensor(out=C[:, h, 0:129], in0=C[:, h, 0:129], scalar=egf[0:64, half, idx:idx + 1],
                                                                               in1=pc[:, hh, 0:129], op0=ALU.mult, op1=ALU.add),
                                  reads=[ckeys[h], K_("egf"), pck], writes=[ckeys[h]])
                if t < 2:
                    return
                lt = t - 2
                dg, dgk = dgr.next()
                fw.op("dve", lambda: nc.vector.tensor_tensor(out=dg[:], in0=nb[:, t * 4:(t + 1) * 4].unsqueeze(2).to_broadcast([128, 4, 128]),
                                                            in1=cf[:, C_ID, :].unsqueeze(1).to_broadcast([128, 4, 128]), op=ALU.mult),
                      reads=[K_("nb"), "c128f"], writes=[dgk])
                pR, pRk = pbig.next()
                fw.op("pe", lambda: nc.tensor.matmul(pR[:], lhsT=cf[:, C_ONES, :], rhs=dg[:].rearrange("p h n -> p (h n)"), start=True, stop=True),
                      reads=["c128f", dgk], writes=[pRk])
                rm, rmk = rmr.next()
                fw.op("dve", lambda: nc.vector.tensor_tensor(out=rm[:], in0=cf[:, mbk, :].unsqueeze(1).to_broadcast([128, 4, 128]),
                                                            in1=pR[:].rearrange("p (h n) -> p h n", h=4), op=ALU.subtract),
                      reads=["c128f", pRk], writes=[rmk])
                e_, ek = er.next()
                fw.op("act", lambda: nc.scalar.activation(out=e_[:].rearrange("p h n -> p (h n)"), in_=pR[0:64, :], func=AF.Exp, scale=-1.0),
                      reads=[pRk], writes=[ek])
                qz0, qz0k = qz0r.next()
                qz1, qz1k = qz1r.next()
                fw.op("pool", lambda: nc.gpsimd.tensor_tensor(out=qz0[:, :, 0:64], in0=mqT[:, :, lt * 128:lt * 128 + 64], in1=e_[:, :, 0:64], op=ALU.mult),
                      reads=["c_mqT", ek], writes=[qz0k])
                fw.op("pool", lambda: nc.gpsimd.tensor_tensor(out=qz1[:, :, 64:128], in0=mqT[:, :, lt * 128 + 64:lt * 128 + 128], in1=e_[:, :, 64:128], op=ALU.mult),
                      reads=["c_mqT", ek], writes=[qz1k])
                scs = []
                for h in range(4):
                    idx = t * 4 + h
                    dt_, dtk = dr.next()
                    fw.op("act", lambda: nc.scalar.activation(out=dt_[:], in_=rm[:, h, :], func=AF.Exp, bias=bcol[:, idx:idx + 1], scale=1.0),
                          reads=[rmk, K_("bcol")], writes=[dtk])
                    ps_, psk = pS.next()
                    fw.op("pe", lambda: nc.tensor.matmul(ps_[:], lhsT=mkT[:, h, t * 128:(t + 1) * 128], rhs=mqT[:, h, lt * 128:(lt + 1) * 128],
                                                         start=True, stop=True), reads=["c_mkT", "c_mqT"], writes=[psk])
                    sc, sck = scr.next()
                    fw.op("dve", lambda: nc.vector.tensor_tensor(out=sc[:], in0=ps_[:], in1=dt_[:], op=ALU.mult), reads=[psk, dtk], writes=[sck])
                    scs.append((sc, sck))
                c0, c0k = Cin[0]
                c1, c1k = Cin[1]
                phs = []
                for hp in range(2):
                    ph, phk = pH.next()
                    phs.append((ph, phk))
                    for hh in range(2):
                        h = 2 * hp + hh
                        sc, sck = scs[h]
                        fw.op("pe", lambda: nc.tensor.matmul(ph[:, hh, 0:129], lhsT=sc[:], rhs=VO[:, t, h, 0:129], start=True, stop=False),
                              reads=[sck, "c_VO"], writes=[phk])
                        fw.op("pe", lambda: nc.tensor.matmul(ph[:, hh, 0:129], lhsT=qz0[:, h, :], rhs=c0[:, h, 0:129], start=False, stop=False),
                              reads=[qz0k, c0k], writes=[phk])
                        fw.op("pe", lambda: nc.tensor.matmul(ph[:, hh, 0:129], lhsT=qz1[:, h, :], rhs=c1[:, h, 0:129], start=False, stop=True),
                              reads=[qz1k, c1k], writes=[phk])
                for hp in range(2):
                    ph, phk = phs[hp]
                    dn, dnk = dnr.next()
                    fw.op("dve", lambda: nc.vector.tensor_scalar(out=dn[:, 0:2], in0=ph[:, :, 128], scalar1=-1.0, scalar2=1.0, op0=ALU.mult, op1=ALU.max),
                          reads=[phk], writes=[dnk])
                    fw.op("dve", lambda: nc.vector.tensor_tensor(out=dn[:, 2:4], in0=dn[:, 0:2], in1=ph[:, :, 128], op=ALU.max), reads=[dnk, phk], writes=[dnk])
                    fw.op("dve", lambda: nc.vector.reciprocal(out=dn[:, 4:6], in_=dn[:, 2:4]), reads=[dnk], writes=[dnk])
                    for hh in range(2):
                        h = 2 * hp + hh
                        hkey = f"c_hsum#{lt}#{h}"
                        if (lt, h) not in written:
                            written.add((lt, h))
                            fw.op("dve", lambda: nc.vector.tensor_scalar(out=hsum[:, lt, h, :], in0=ph[:, hh, 0:128], scalar1=dn[:, 4 + hh:5 + hh], scalar2=None, op0=ALU.mult),
                                  reads=[phk, dnk], writes=[hkey])
                        else:
                            fw.op("dve", lambda: nc.vector.scalar_tensor_tensor(out=hsum[:, lt, h, :], in0=ph[:, hh, 0:128], scalar=dn[:, 4 + hh:5 + hh], in1=hsum[:, lt, h, :],
                                                                               op0=ALU.mult, op1=ALU.add), reads=[phk, dnk, hkey], writes=[hkey])

            torder = [list(range(NT)), [1, 0] + list(range(NT - 1, 1, -1))]
            for step in range(NT):
                for d in range(2):
                    process_tile(d, torder[d][step])

    def l1_stage_c2(self, b, hsum, wout):
        nc, fw, I, R = self.nc, self.fw, self.I, self.R
        with ExitStack() as es:
            mt = self.mod_tiles(es, 1, b, want_pre=False, want_post=True)
            g, gk = mt["g_lat"]
            P = {}
            P["xres"] = self.ring(es, "d_xres", [128, D], F32, 2)
            P["junk"] = self.ring(es, "d_junk", [128, D], BF16, 2)
            P["stat"] = self.ring(es, "d_stat", [128, 4], F32, 4)
            P["t2"] = self.ring(es, "d_t2", [128, D], F32, 2)
            hnw = self.sb(es, "d_hnw", [128, 512], F32)
            fw.dma("sp", hnw[:], I["o_head_norm_w"].partition_broadcast(128), writes=["d_hnw"])
            pT = self.ps(es, "d_pT", [128, 4, 128], BF16)
            py = self.ps(es, "d_py", [128, D], F32)
            mor = self.ring(es, "d_mo", [128, 512], F32, 3)
            zmr = self.ring(es, "d_zm", [128, 512], F32, 3)
            cgr = self.ring(es, "d_cg", [128, 4, 128], BF16, 3)
            str_ = self.ring(es, "d_st", [128, 12], F32, 3)
            jr = self.ring(es, "d_j", [128, 128], BF16, 2)
            g1r = self.ring(es, "d_g1", [128, 512], F32, 2)
            hnr = self.ring(es, "d_hn", [128, 4, 128], F32, 2)
            mgr = self.ring(es, "d_mg", [128, 512], BF16, 2)
            mgTr = self.ring(es, "d_mgT", [128, 4, 128], BF16, 3)
            loads = {}
            tails = {}

            def emit_loads(lt):
                ltsl = slice(lt * 128, (lt + 1) * 128)
                mo_, mok = mor.next()
                fw.dma("sp", mo_[:], R["mo1"][b, ltsl, :], reads=["mo1"], writes=[mok])
                zm_, zmk = zmr.next()
                fw.dma("sp", zm_[:], R["z1"][b, ltsl, 512:1024], reads=["z1"], writes=[zmk])
                cg_, cgk = cgr.next()
                fw.dma("sp", cg_[:], R["cgT1"][b].rearrange("c p t -> p c t")[:, :, ltsl], reads=["cgT1"], writes=[cgk])
                loads[lt] = (mo_, mok, zm_, zmk, cg_, cgk)

            def emit_tile(lt):
                mo_, mok, zm_, zmk, cg_, cgk = loads.pop(lt)
                hkeys = [f"c_hsum#{lt}#{h}" for h in range(4)]
                st, stk = str_.next()
                for h in range(4):
                    j_, jk = jr.next()
                    fw.op("act", lambda: nc.scalar.activation(out=j_[:], in_=hsum[:, lt, h, :], func=AF.Square, scale=128 ** -0.5, accum_out=st[:, h:h + 1]),
                          reads=hkeys, writes=[jk, f"{stk}#{h}"])
                fw.op("act", lambda: nc.scalar.activation(out=st[:, 4:8], in_=st[:, 0:4], func=AF.Sqrt, bias=EPS),
                      reads=[f"{stk}#{h}" for h in range(4)], writes=[f"{stk}#s"])
                fw.op("dve", lambda: nc.vector.reciprocal(out=st[:, 8:12], in_=st[:, 4:8]), reads=[f"{stk}#s"], writes=[f"{stk}#r"])
                g1, g1k = g1r.next()
                fw.op("pool", lambda: nc.gpsimd.tensor_tensor(out=g1[:], in0=mo_[:], in1=zm_[:], op=ALU.mult), reads=[mok, zmk], writes=[g1k])
                fw.op("pool", lambda: nc.gpsimd.tensor_tensor(out=g1[:], in0=g1[:], in1=hnw[:], op=ALU.mult), reads=[g1k, "d_hnw"], writes=[g1k])
                hn, hnk = hnr.next()
                fw.op("dve", lambda: nc.vector.tensor_tensor(out=hn[:], in0=hsum[:, lt, :, :], in1=st[:, 8:12].unsqueeze(2).to_broadcast([128, 4, 128]), op=ALU.mult),
                      reads=hkeys + [f"{stk}#r"], writes=[hnk])
                mg, mgk = mgr.next()
                fw.op("dve", lambda: nc.vector.tensor_tensor(out=mg[:], in0=hn[:].rearrange("p h v -> p (h v)"), in1=g1[:], op=ALU.mult),
                      reads=[hnk, g1k], writes=[mgk])
                for c in range(4):
                    fw.op("pe", lambda: nc.tensor.transpose(out=pT[:, c, :], in_=mg[:, c * 128:(c + 1) * 128], identity=self.identb[:]),
                          reads=[mgk, "identb"], writes=["d_pT"])
                mgT, mgTk = mgTr.next()
                fw.op("act", lambda: nc.scalar.copy(out=mgT[:], in_=pT[:]), reads=["d_pT"], writes=[mgTk])
                tails[lt] = (cg_, cgk, mgT, mgTk)

            def emit_tail(lt):
                cg_, cgk, mgT, mgTk = tails.pop(lt)
                for half in range(2):
                    for k in range(8):
                        src, srk = (cg_, cgk) if k < 4 else (mgT, mgTk)
                        fw.op("pe", lambda: nc.tensor.matmul(py[:, half * 512:(half + 1) * 512], lhsT=src[:, k % 4, :],
                                                             rhs=wout[:, k, half * 512:(half + 1) * 512], start=(k == 0), stop=(k == 7)),
                              reads=[srk, "l1_wout"], writes=["d_py"])
                t = lt + 2
                self.post_tile(P, py, "d_py", g, gk, R["x1c"][b, t * 128:(t + 1) * 128, :], ["x1c"],
                               self.out[b, lt * 128:(lt + 1) * 128, :], "out")

            emit_loads(0)
            emit_loads(1)
            emit_tile(0)
            for lt in range(16):
                if lt + 2 < 16:
                    emit_loads(lt + 2)
                if lt + 1 < 16:
                    emit_tile(lt + 1)
                emit_tail(lt)


def make_consts():
    c = np.zeros((11, 128, 128), np.float32)
    p = np.arange(128)[:, None]
    n = np.arange(128)[None, :]
    same = (p // 64) == (n // 64)
    c[C_ID] = (p == n)
    c[C_ONES] = 1.0
    c[C_TRIF] = same & (p <= n)
    c[C_TRIR] = same & (p >= n)
    c[C_BLK] = same
    c[C_SEL0] = (p < 64) & (n >= 0)
    c[C_SEL1] = (p >= 64) & (n >= 0)
    c[C_MBF] = np.where(same & (p <= n), 0.0, NEG)
    c[C_MBR] = np.where(same & (p >= n), 0.0, NEG)
    c[C_MPREV] = np.where(p >= n, 0.0, NEG)
    c[C_MNEXT] = np.where(p <= n, 0.0, NEG)

    def axial(rot_dim):
        rows = S // 64
        row = np.repeat(np.arange(rows), 64).astype(np.float32)
        col = np.tile(np.arange(64), rows).astype(np.float32)
        nf = rot_dim // 4
        inv = (np.float32(10000.0) ** (-np.arange(nf, dtype=np.float32) / np.float32(nf))).astype(np.float32)
        ang = np.concatenate([row[:, None] * inv, col[:, None] * inv], axis=-1).astype(np.float32)
        tab = np.zeros((T, rot_dim), np.float32)
        tab[:LC, :rot_dim // 2] = 1.0
        tab[LC:, :rot_dim // 2] = np.cos(ang)
        tab[LC:, rot_dim // 2:] = np.sin(ang)
        return tab
    return c, axial(64), axial(32)


_CACHE = {}


def get_program(debug=False, stop_after=None):
    key = (debug, stop_after)
    if key not in _CACHE:
        bld = Builder(debug=debug)
        nc = bld.build(stop_after=stop_after)
        _CACHE[key] = (nc, bld)
    return _CACHE[key]


def make_in_maps(inputs):
    c128, ropeA, ropeC = make_consts()
    f = lambda a: np.ascontiguousarray(np.asarray(a, dtype=np.float32))
    shared = {
        "mod_w": f(inputs["mod_w"]), "mod_b": f(inputs["mod_b"]),
        "pre_norm_w": f(inputs["pre_norm_w"]), "post_norm_w": f(inputs["post_norm_w"]),
        "e_w_in": f(inputs["e_w_in"][0]), "e_sink": f(inputs["e_sink"][0]), "e_conv_w": f(inputs["e_conv_w"][0]),
        "e_w_out": f(inputs["e_w_out"][0]), "o_w_in": f(inputs["o_w_in"][0]),
        "o_q_norm_w": f(inputs["o_q_norm_w"][0]), "o_kv_norm_w": f(inputs["o_kv_norm_w"][0]),
        "o_w_uq": f(inputs["o_w_uq"][0]), "o_w_ukv": f(inputs["o_w_ukv"][0]),
        "o_i_bias": f(inputs["o_i_bias"][0]).reshape(8), "o_f_bias": f(inputs["o_f_bias"][0]).reshape(8),
        "o_head_norm_w": f(inputs["o_head_norm_w"][0]), "o_w_out": f(inputs["o_w_out"][0]),
        "c128": c128, "ropeA": ropeA, "ropeC": ropeC,
    }
    x = f(inputs["x"])
    c = f(inputs["c"])
    ctx = f(inputs["ctx"])
    cc = f(inputs["c_ctx"])
    maps = []
    for i in range(NCORES):
        m = dict(shared)
        m["xs"] = x[NB * i:NB * (i + 1)]
        m["ctxs"] = ctx[NB * i:NB * (i + 1)]
        m["cvec"] = np.ascontiguousarray(np.stack([c[NB * i], c[NB * i + 1], cc], axis=0))
        maps.append(m)
    return maps


def kernel(**inputs):
    nc, _ = get_program()
    maps = make_in_maps(inputs)
    res = run_bass_kernel_spmd(nc, maps, core_ids=list(range(NCORES)))
    return np.concatenate([r["out"] for r in res.results], axis=0)
```

```python
import numpy as np
from contextlib import ExitStack
import concourse.bass as bass
import concourse.mybir as mybir
from concourse.bass_utils import run_bass_kernel_spmd

F32 = mybir.dt.float32
BF16 = mybir.dt.bfloat16
AF = mybir.ActivationFunctionType
ALU = mybir.AluOpType

D = 1024
S = 2048
LC = 256
T = S + LC
NT = T // 128
NB = 2
NCORES = 8
EPS = 1e-6
NEG = -30000.0
E_IN = 3328
O_IN = 3632
CHUNKS = [(0, 256), (256, 512), (768, 512), (1280, 512), (1792, 512)]

C_ID, C_ONES, C_TRIF, C_TRIR, C_BLK, C_SEL0, C_SEL1, C_MBF, C_MBR, C_MPREV, C_MNEXT = range(11)


def _nruns(ap):
    pat = [list(x) for x in ap.ap]
    tot = 1
    for st, n in pat:
        tot *= n
    run = 1
    for st, n in sorted(pat, key=lambda x: abs(x[0]) if x[0] != 0 else 1 << 60):
        if st == run:
            run *= n
        elif n > 1:
            break
    return max(1, tot // run)


def _nbytes(ap):
    tot = 1
    for st, n in ap.ap:
        tot *= n
    return tot


def _os_env(k):
    import os
    return os.environ.get(k)


class FW:
    ROT = 30000

    def __init__(self, nc, es):
        self.nc = nc
        self.es = es
        self.eng = {"pe": nc.tensor, "act": nc.scalar, "dve": nc.vector, "pool": nc.gpsimd, "sp": nc.sync}
        self.comp = ["pe", "act", "dve", "pool"]
        self.epoch = {k: 0 for k in self.comp}
        self.sem = {k: es.enter_context(nc.semaphore(f"s_{k}_0")) for k in self.comp}
        self.cnt = {k: 0 for k in self.comp}
        self.seen = {e: {} for e in self.eng}
        self.NQ = int(_os_env("KNQ") or 8)
        self.DBUDGET = int(_os_env("KDB") or 1024)
        self.dq = {}
        for q in ["sp", "act", "pool"]:
            sems = [es.enter_context(nc.semaphore(f"d_{q}{i}")) for i in range(self.NQ)]
            self.dq[q] = {"sems": sems, "n": 0}
        self.lastw = {}
        self.reads = {}
        self.ninst = 0
        self.psum_keys = set()

    def _wait(self, e, tok):
        key, sem, val = tok
        if self.seen[e].get(key, 0) >= val:
            return
        self.eng[e].wait_ge(sem, val)
        self.seen[e][key] = val
        self.ninst += 1

    def _deps(self, e, reads, writes):
        toks = []
        for b in list(reads) + list(writes):
            t = self.lastw.get(b)
            if t is not None:
                toks.append(t)
        for b in writes:
            toks.extend(self.reads.get(b, []))
        for b in reads:
            if b in self.psum_keys:
                toks.extend(t for t in self.reads.get(b, []) if not t[0].startswith(e + "#"))
        for t in toks:
            if e == "pe" and t[0].startswith("pe#"):
                continue
            self._wait(e, t)

    def _commit(self, tok, reads, writes):
        for b in writes:
            self.lastw[b] = tok
            self.reads[b] = []
        for b in reads:
            lst = self.reads.setdefault(b, [])
            lst[:] = [t for t in lst if t[0] != tok[0]]
            lst.append(tok)

    def op(self, e, fn, reads=(), writes=()):
        self._deps(e, reads, writes)
        if self.cnt[e] >= self.ROT:
            self.epoch[e] += 1
            self.sem[e] = self.es.enter_context(self.nc.semaphore(f"s_{e}_{self.epoch[e]}"))
            self.cnt[e] = 0
        ins = fn()
        self.cnt[e] += 1
        ins.then_inc(self.sem[e], 1)
        tok = (f"{e}#{self.epoch[e]}", self.sem[e], self.cnt[e])
        self._commit(tok, reads, writes)
        self.ninst += 1
        return tok

    def dma(self, q, out, in_, reads=(), writes=(), **kw):
        d = self.dq[q]
        i = d["n"] % self.NQ
        rnd = d["n"] // self.NQ
        sem = d["sems"][i]
        key = f"d_{q}{i}"
        if rnd > 0:
            self._wait(q, (key, sem, 16 * rnd))
        nd = max(_nruns(out), _nruns(in_))
        fl = d.setdefault("inflight", [])
        fl[:] = [(t, c) for (t, c) in fl if self.seen[q].get(t[0], 0) < t[2]]
        while fl and sum(c for _, c in fl) + nd > self.DBUDGET:
            t, c = fl.pop(0)
            self._wait(q, t)
        self._deps(q, reads, writes)
        ins = self.eng[q].dma_start(out=out, in_=in_, **kw)
        ins.then_inc(sem, 16)
        d["n"] += 1
        self.ndesc = getattr(self, "ndesc", 0) + nd
        tok = (key, sem, 16 * (rnd + 1))
        fl.append((tok, nd))
        self._commit(tok, reads, writes)
        self.ninst += 1
        return tok

    def all_tokens(self):
        toks = []
        for k in self.comp:
            if self.cnt[k] > 0:
                toks.append((f"{k}#{self.epoch[k]}", self.sem[k], self.cnt[k]))
        for q, d in self.dq.items():
            for i in range(self.NQ):
                n_i = (d["n"] - i + self.NQ - 1) // self.NQ
                if n_i > 0:
                    toks.append((f"d_{q}{i}", d["sems"][i], 16 * n_i))
        return toks

    def barrier(self):
        toks = self.all_tokens()
        for e in self.eng:
            for t in toks:
                if t[0].startswith(e + "#"):
                    continue
                self._wait(e, t)
        self.lastw = {}
        self.reads = {}

    def finish(self):
        for t in self.all_tokens():
            self._wait("sp", t)


class Ring:
    def __init__(self, tiles, name):
        self.tiles = tiles
        self.name = name
        self.i = -1

    def next(self):
        self.i = (self.i + 1) % len(self.tiles)
        return self.tiles[self.i], f"{self.name}{self.i}"


class Builder:
    def __init__(self, debug=False):
        self.debug = debug
        self.nc = bass.Bass("TRN2", target_bir_lowering=False)
        self.dbg_names = []
        import os as _os
        self.QS = _os.environ.get("KQS", "sp")

    def uname(self, name):
        self.uid = getattr(self, "uid", 0) + 1
        return f"{name}_u{self.uid}"

    def sb(self, es, name, shape, dt):
        return es.enter_context(self.nc.sbuf_tensor(self.uname(name), list(shape), dt))

    def ps(self, es, name, shape, dt):
        self.fw.psum_keys.add(name)
        return es.enter_context(self.nc.psum_tensor(self.uname(name), list(shape), dt))

    def ring(self, es, name, shape, dt, n, psum=False):
        f = self.ps if psum else self.sb
        return Ring([f(es, f"{name}{i}", shape, dt) for i in range(n)], name)

    def dram_in(self, name, shape, dt=F32):
        return self.nc.dram_tensor(name, list(shape), dt, kind="ExternalInput").ap()

    def scratch(self, name, shape, dt, dbg=False):
        if dbg and self.debug:
            self.dbg_names.append(name)
            return self.nc.dram_tensor(name, list(shape), dt, kind="ExternalOutput").ap()
        return self.nc.dram_tensor(name, list(shape), dt).ap()

    def build(self, stop_after=None):
        nc = self.nc
        self.stop = stop_after
        import os as _os
        self.cut1 = int(_os.environ.get("KCUT1", "0"))
        self.cut = int(_os.environ.get("KCUT", "0"))
        self.cutb = int(_os.environ.get("KCUTB", "0"))
        self.cutc = int(_os.environ.get("KCUTC", "0"))
        I = {}
        I["xs"] = self.dram_in("xs", [NB, S, D])
        I["ctxs"] = self.dram_in("ctxs", [NB, LC, D])
        I["cvec"] = self.dram_in("cvec", [3, D])
        I["mod_w"] = self.dram_in("mod_w", [2, D, 3 * D])
        I["mod_b"] = self.dram_in("mod_b", [2, 3 * D])
        I["pre_norm_w"] = self.dram_in("pre_norm_w", [2, D])
        I["post_norm_w"] = self.dram_in("post_norm_w", [2, D])
        I["e_w_in"] = self.dram_in("e_w_in", [D, E_IN])
        I["e_sink"] = self.dram_in("e_sink", [8])
        I["e_conv_w"] = self.dram_in("e_conv_w", [3, 512])
        I["e_w_out"] = self.dram_in("e_w_out", [D, D])
        I["o_w_in"] = self.dram_in("o_w_in", [D, O_IN])
        I["o_q_norm_w"] = self.dram_in("o_q_norm_w", [768])
        I["o_kv_norm_w"] = self.dram_in("o_kv_norm_w", [256])
        I["o_w_uq"] = self.dram_in("o_w_uq", [768, 768])
        I["o_w_ukv"] = self.dram_in("o_w_ukv", [256, 1024])
        I["o_i_bias"] = self.dram_in("o_i_bias", [8])
        I["o_f_bias"] = self.dram_in("o_f_bias", [8])
        I["o_head_norm_w"] = self.dram_in("o_head_norm_w", [512])
        I["o_w_out"] = self.dram_in("o_w_out", [D, D])
        I["c128"] = self.dram_in("c128", [11, 128, 128])
        I["ropeA"] = self.dram_in("ropeA", [T, 64])
        I["ropeC"] = self.dram_in("ropeC", [T, 32])
        self.I = I
        out = nc.dram_tensor("out", [NB, S, D], F32, kind="ExternalOutput").ap()
        self.out = out

        R = {}
        R["modrow"] = self.scratch("modrow", [2, 3, 3 * D], F32, dbg=True)
        R["x1c"] = self.scratch("x1c", [NB, T, D], F32, dbg=True)
        R["qT0"] = self.scratch("qT0", [NB, 4, 128, T], BF16)
        R["kT0"] = self.scratch("kT0", [NB, 128, T], BF16)
        R["v0"] = self.scratch("v0", [NB, T, 160], BF16)
        R["za0"] = self.scratch("za0", [NB, T, 512], F32)
        R["bg0T"] = self.scratch("bg0T", [NB, 4, 128, T], BF16)
        R["kT1"] = self.scratch("kT1", [NB, 8, 96, T], BF16)
        R["v1"] = self.scratch("v1", [NB, T, 640], BF16)
        R["qT1"] = self.scratch("qT1", [NB, 8, 96, S], BF16)
        R["mk1"] = self.scratch("mk1", [NB, T, 256], BF16)
        R["mv1"] = self.scratch("mv1", [NB, T, 576], BF16)
        R["mkT1"] = self.scratch("mkT1", [NB, 256, T], BF16)
        R["mqT1"] = self.scratch("mqT1", [NB, 256, S], BF16)
        R["mg1"] = self.scratch("mg1", [NB, T, 16], F32)
        R["mo1"] = self.scratch("mo1", [NB, S, 512], F32)
        R["z1"] = self.scratch("z1", [NB, S, 1024], F32)
        R["cgT1"] = self.scratch("cgT1", [NB, 4, 128, S], BF16)
        self.R = R

        with ExitStack() as es0:
            self.fw = FW(nc, es0)
            fw = self.fw
            self.cf = self.sb(es0, "c128f", [128, 11, 128], F32)
            fw.dma("sp", self.cf[:], I["c128"].rearrange("c p n -> p c n"), writes=["c128f"])
            self.identb = self.sb(es0, "identb", [128, 128], BF16)
            fw.op("dve", lambda: nc.vector.tensor_copy(out=self.identb[:], in_=self.cf[:, C_ID, :]),
                  reads=["c128f"], writes=["identb"])
            self.onesb = self.sb(es0, "onesb", [128, 128], BF16)
            fw.op("dve", lambda: nc.vector.tensor_copy(out=self.onesb[:], in_=self.cf[:, C_ONES, :]),
                  reads=["c128f"], writes=["onesb"])
            self.stage_mod()
            fw.barrier()
            if stop_after != "mod":
                if not _os.environ.get("KSKIP0"):
                    self.layer0()
                    fw.barrier()
                if stop_after in (None, "l1a", "l1b"):
                    self.layer1()
                    fw.barrier()
            fw.finish()
        return nc

    def stage_mod(self):
        nc, fw, I, R = self.nc, self.fw, self.I, self.R
        with ExitStack() as es:
            cv = self.sb(es, "m_cv", [3, D], F32)
            sv = self.sb(es, "m_sv", [3, D], F32)
            svT = self.sb(es, "m_svT", [128, 8, 3], F32)
            ones3 = self.sb(es, "m_ones3", [1, 3], F32)
            pT = self.ps(es, "m_pT", [128, 8, 3], F32)
            wring = self.ring(es, "m_w", [128, 8, 512], F32, 2)
            pring = self.ring(es, "m_p", [3, 512], F32, 2, psum=True)
            mb = self.sb(es, "m_mb", [1, 3 * D], F32)
            mr = self.sb(es, "m_mr", [3, 3 * D], F32)
            fw.dma("sp", cv[:], I["cvec"], writes=["m_cv"])
            fw.op("act", lambda: nc.scalar.activation(out=sv[:], in_=cv[:], func=AF.Silu), reads=["m_cv"], writes=["m_sv"])
            fw.op("pool", lambda: nc.gpsimd.memset(ones3[:], 1.0), writes=["m_ones3"])
            for k in range(8):
                fw.op("pe", lambda: nc.tensor.transpose(out=pT[:, k, :], in_=sv[0:3, k * 128:(k + 1) * 128],
                                                        identity=self.cf[0:3, C_ID, 0:3]),
                      reads=["m_sv", "c128f"], writes=["m_pT"])
            fw.op("dve", lambda: nc.vector.tensor_copy(out=svT[:], in_=pT[:]), reads=["m_pT"], writes=["m_svT"])
            for l in range(2):
                fw.dma("sp", mb[:], I["mod_b"][l:l + 1, :], reads=[], writes=["m_mb"])
                for n in range(6):
                    wt, wk = wring.next()
                    fw.dma("sp", wt[:], I["mod_w"][l, :, n * 512:(n + 1) * 512].rearrange("(k p) n -> p k n", p=128),
                           writes=[wk])
                    pm, pk = pring.next()
                    for k in range(8):
                        fw.op("pe", lambda: nc.tensor.matmul(pm[:], lhsT=svT[:, k, :], rhs=wt[:, k, :],
                                                             start=(k == 0), stop=False),
                              reads=["m_svT", wk], writes=[pk])
                    fw.op("pe", lambda: nc.tensor.matmul(pm[:], lhsT=ones3[0:1, :], rhs=mb[0:1, n * 512:(n + 1) * 512],
                                                         start=False, stop=True),
                          reads=["m_ones3", "m_mb"], writes=[pk])
                    fw.op("dve", lambda: nc.vector.tensor_copy(out=mr[:, n * 512:(n + 1) * 512], in_=pm[:]),
                          reads=[pk], writes=["m_mr"])
                fw.dma("sp", R["modrow"][l], mr[:], reads=["m_mr"], writes=["modrow"])

    def src_rows(self, layer, b, tok0, n):
        if layer == 0:
            if tok0 < LC:
                return self.I["ctxs"][b, tok0:tok0 + n, :]
            return self.I["xs"][b, tok0 - LC:tok0 - LC + n, :]
        return self.R["x1c"][b, tok0:tok0 + n, :]

    def load_weight_bf16(self, es, name, w_ap, ncols, nk=8):
        nc, fw = self.nc, self.fw
        wt = self.sb(es, name, [128, nk, ncols], BF16)
        with ExitStack() as es2:
            stg = self.ring(es2, name + "_stg", [128, ncols], F32, 4)
            for k in range(nk):
                st, sk = stg.next()
                fw.dma("sp", st[:], w_ap[k * 128:(k + 1) * 128, :], writes=[sk])
                e = "pool" if k % 2 == 0 else "dve"
                eng = nc.gpsimd if k % 2 == 0 else nc.vector
                fw.op(e, lambda: eng.tensor_copy(out=wt[:, k, :], in_=st[:]), reads=[sk], writes=[name])
            fw.barrier()
        return wt

    def mod_tiles(self, es, layer, b, want_pre=True, want_post=True):
        nc, fw, I, R = self.nc, self.fw, self.I, self.R
        res = {}
        tmp = self.sb(es, "mt_tmp", [128, D], F32)
        if want_pre:
            fw.dma("sp", tmp[:], I["pre_norm_w"][layer].partition_broadcast(128), writes=["mt_tmp"])
            for nm, v in (("lat", b), ("ctx", 2)):
                sc = self.sb(es, f"mt_sc_{nm}", [128, D], F32)
                sh = self.sb(es, f"mt_sh_{nm}", [128, D], F32)
                fw.dma("sp", sh[:], R["modrow"][layer, v, 0:D].partition_broadcast(128), reads=["modrow"], writes=[f"mt_sh_{nm}"])
                fw.dma("sp", sc[:], R["modrow"][layer, v, D:2 * D].partition_broadcast(128), reads=["modrow"], writes=[f"mt_sc_{nm}"])
                fw.op("dve", lambda: nc.vector.scalar_tensor_tensor(out=sc[:], in0=sc[:], scalar=1.0, in1=tmp[:],
                                                                   op0=ALU.add, op1=ALU.mult),
                      reads=[f"mt_sc_{nm}", "mt_tmp"], writes=[f"mt_sc_{nm}"])
                res[f"sc_{nm}"] = (sc, f"mt_sc_{nm}")
                res[f"sh_{nm}"] = (sh, f"mt_sh_{nm}")
        if want_post:
            tmp2 = self.sb(es, "mt_tmp2", [128, D], F32)
            fw.dma("sp", tmp2[:], I["post_norm_w"][layer].partition_broadcast(128), writes=["mt_tmp2"])
            for nm, v in (("lat", b), ("ctx", 2)):
                g = self.sb(es, f"mt_g_{nm}", [128, D], F32)
                fw.dma("sp", g[:], R["modrow"][layer, v, 2 * D:3 * D].partition_broadcast(128), reads=["modrow"], writes=[f"mt_g_{nm}"])
                fw.op("dve", lambda: nc.vector.tensor_tensor(out=g[:], in0=g[:], in1=tmp2[:], op=ALU.mult),
                      reads=[f"mt_g_{nm}", "mt_tmp2"], writes=[f"mt_g_{nm}"])
                res[f"g_{nm}"] = (g, f"mt_g_{nm}")
        return res

    def norm_tile(self, P, layer, b, t, hT, hk, j, mt):
        nc, fw = self.nc, self.fw
        nm = "ctx" if t < 2 else "lat"
        sc, sck = mt[f"sc_{nm}"]
        sh, shk = mt[f"sh_{nm}"]
        xt, xk = P["xring"].next()
        fw.dma("sp", xt[:], self.src_rows(layer, b, t * 128, 128), reads=["x1c"] if layer == 1 else [], writes=[xk])
        jk, jkk = P["junk"].next()
        st, stk = P["stat"].next()
        fw.op("act", lambda: nc.scalar.activation(out=jk[:], in_=xt[:], func=AF.Square, scale=1.0 / 32.0, accum_out=st[:, 0:1]),
              reads=[xk], writes=[jkk, stk])
        fw.op("act", lambda: nc.scalar.activation(out=st[:, 1:2], in_=st[:, 0:1], func=AF.Sqrt, bias=EPS), reads=[stk], writes=[stk])
        fw.op("dve", lambda: nc.vector.reciprocal(out=st[:, 2:3], in_=st[:, 1:2]), reads=[stk], writes=[stk])
        t1, t1k = P["t1"].next()
        fw.op("dve", lambda: nc.vector.scalar_tensor_tensor(out=t1[:], in0=xt[:], scalar=st[:, 2:3], in1=sc[:],
                                                           op0=ALU.mult, op1=ALU.mult),
              reads=[xk, stk, sck], writes=[t1k])
        hb, hbk = P["hb"].next()
        fw.op("pool", lambda: nc.gpsimd.tensor_tensor(out=hb[:], in0=t1[:], in1=sh[:], op=ALU.add),
              reads=[t1k, shk], writes=[hbk])
        pT, pTk = P["pT"].next()
        for k in range(8):
            fw.op("pe", lambda: nc.tensor.transpose(out=pT[:, k, :], in_=hb[:, k * 128:(k + 1) * 128], identity=self.identb[:]),
                  reads=[hbk, "identb"], writes=[pTk])
        fw.op("act", lambda: nc.scalar.copy(out=hT[:, :, j * 128:(j + 1) * 128], in_=pT[:]), reads=[pTk], writes=[f"{hk}#{j}"])

    def rope_tok(self, P, src, nh, hd, rt, rtk, srck, dst, dstk, dst_off=0, src_off=0):
        nc, fw = self.nc, self.fw
        r = hd // 2
        x1 = src[:, :, src_off:src_off + r]
        x2 = src[:, :, src_off + r:src_off + hd]
        cosb = rt[:, 0:r].unsqueeze(1).to_broadcast([128, nh, r])
        sinb = rt[:, r:hd].unsqueeze(1).to_broadcast([128, nh, r])
        srcks = list(srck) if isinstance(srck, (list, tuple)) else [srck]
        ta, tak = P["ropet"].next()
        tb, tbk = P["ropet"].next()
        a = ta[:, 0:nh, 0:r]
        bb = tb[:, 0:nh, 0:r]
        fw.op("dve", lambda: nc.vector.tensor_tensor(out=a, in0=x1, in1=cosb, op=ALU.mult), reads=srcks + [rtk], writes=[tak])
        fw.op("dve", lambda: nc.vector.tensor_tensor(out=bb, in0=x2, in1=sinb, op=ALU.mult), reads=srcks + [rtk], writes=[tbk])
        fw.op("pool", lambda: nc.gpsimd.tensor_tensor(out=dst[:, :, dst_off:dst_off + r], in0=a, in1=bb, op=ALU.subtract),
              reads=[tak, tbk], writes=[dstk])
        tc_, tck = P["ropet"].next()
        td, tdk = P["ropet"].next()
        c = tc_[:, 0:nh, 0:r]
        d = td[:, 0:nh, 0:r]
        fw.op("dve", lambda: nc.vector.tensor_tensor(out=c, in0=x1, in1=sinb, op=ALU.mult), reads=srcks + [rtk], writes=[tck])
        fw.op("dve", lambda: nc.vector.tensor_tensor(out=d, in0=x2, in1=cosb, op=ALU.mult), reads=srcks + [rtk], writes=[tdk])
        fw.op("pool", lambda: nc.gpsimd.tensor_tensor(out=dst[:, :, dst_off + r:dst_off + hd], in0=c, in1=d, op=ALU.add),
              reads=[tck, tdk], writes=[dstk])

    def post_tile(self, P, py, pyk, g, gk, xin_ap, xin_reads, out_ap, out_key):
        nc, fw = self.nc, self.fw
        xt, xk = P["xres"].next()
        fw.dma("sp", xt[:], xin_ap, reads=xin_reads, writes=[xk])
        jk, jkk = P["junk"].next()
        st, stk = P["stat"].next()
        fw.op("act", lambda: nc.scalar.activation(out=jk[:], in_=py[:], func=AF.Square, scale=1.0 / 32.0, accum_out=st[:, 0:1]),
              reads=[pyk], writes=[jkk, stk])
        fw.op("act", lambda: nc.scalar.activation(out=st[:, 1:2], in_=st[:, 0:1], func=AF.Sqrt, bias=EPS), reads=[stk], writes=[stk])
        fw.op("dve", lambda: nc.vector.reciprocal(out=st[:, 2:3], in_=st[:, 1:2]), reads=[stk], writes=[stk])
        t2, t2k = P["t2"].next()
        fw.op("dve", lambda: nc.vector.scalar_tensor_tensor(out=t2[:], in0=py[:], scalar=st[:, 2:3], in1=g[:],
                                                           op0=ALU.mult, op1=ALU.mult),
              reads=[pyk, stk, gk], writes=[t2k])
        fw.op("pool", lambda: nc.gpsimd.tensor_tensor(out=t2[:], in0=t2[:], in1=xt[:], op=ALU.add),
              reads=[t2k, xk], writes=[t2k])
        fw.dma(self.QS, out_ap, t2[:], reads=[t2k], writes=[out_key])

    def layer0(self):
        nc, fw, I, R = self.nc, self.fw, self.I, self.R
        stop = getattr(self, "stop", None)
        with ExitStack() as esl:
            win = self.load_weight_bf16(esl, "l0_win", I["e_w_in"], E_IN)
            if stop == "l0w":
                return
            for b in range(NB):
                self.l0_stage_a(b, win)
                fw.barrier()
                if stop == "l0a0":
                    return
        if stop == "l0a":
            return
        with ExitStack() as esl:
            wout = self.load_weight_bf16(esl, "l0_wout", I["e_w_out"], D)
            for b in range(NB):
                self.l0_stage_b(b, wout)
                fw.barrier()

    def l0_stage_a(self, b, win):
        nc, fw, I, R = self.nc, self.fw, self.I, self.R
        with ExitStack() as es:
            mt = self.mod_tiles(es, 0, b, want_pre=True, want_post=False)
            P = {}
            P["xring"] = self.ring(es, "a_x", [128, D], F32, 2)
            P["junk"] = self.ring(es, "a_junk", [128, D], BF16, 2)
            P["stat"] = self.ring(es, "a_stat", [128, 4], F32, 4)
            P["t1"] = self.ring(es, "a_t1", [128, D], F32, 2)
            P["hb"] = self.ring(es, "a_hb", [128, D], BF16, 2)
            P["pT"] = self.ring(es, "a_pT", [128, 8, 128], BF16, 2, psum=True)
            P["ropet"] = self.ring(es, "a_ropet", [128, 8, 32], F32, 8)
            hTr = self.ring(es, "a_hT", [128, 8, 512], BF16, 3)
            ptok = self.ring(es, "a_ptok", [128, 512], F32, 2, psum=True)
            pfm = self.ring(es, "a_pfm", [128, 512], F32, 3, psum=True)
            phalo = self.ps(es, "a_phalo", [128, 4, 2, 2], F32)
            halo = self.ring(es, "a_halo", [128, 8, 2], BF16, 2)
            cw = self.sb(es, "a_cw", [128, 3, 4], F32)
            for j_ in range(3):
                fw.dma("sp", cw[:, j_, :], I["e_conv_w"][j_, :].rearrange("(c p) -> p c", p=128), writes=["a_cw"],
                       allow_slow_non_contiguous=True)
            rtr = self.ring(es, "a_rt", [128, 64], F32, 3)
            qbr = self.ring(es, "a_qb", [128, 8, 64], BF16, 2)
            kbr = self.ring(es, "a_kb", [128, 2, 64], BF16, 2)
            qTst = self.ring(es, "a_qTst", [128, 4, 512], BF16, 2)
            kTst = self.ring(es, "a_kTst", [128, 512], BF16, 2)
            vst = self.ring(es, "a_vst", [128, 4, 2, 80], BF16, 2)
            for i_, tl in enumerate(vst.tiles):
                fw.op("pool", lambda: nc.gpsimd.memset(tl[:], 1.0), writes=[f"a_vst{i_}#{j_}" for j_ in range(4)])
            zast = self.ring(es, "a_zast", [128, 512], F32, 3)
            bcs = self.ring(es, "a_bcs", [128, 512], F32, 2)
            uext = self.ring(es, "a_uext", [128, 514], F32, 2)
            acc = self.ring(es, "a_acc", [128, 512], F32, 2)
            szr = self.ring(es, "a_sz", [128, 512], F32, 2)
            hbh = self.ring(es, "a_hbh", [128, 2], F32, 2)
            bgst = self.ring(es, "a_bgst", [128, 512], BF16, 2)

            hts = {}

            def emit_norm(ci):
                tok0, ntok = CHUNKS[ci]
                hT, hk = hTr.next()
                hts[ci] = (hT, hk)
                for j in range(ntok // 128):
                    self.norm_tile(P, 0, b, tok0 // 128 + j, hT, hk, j, mt)

            def emit_proj(ci):
                tok0, ntok = CHUNKS[ci]
                nt = ntok // 128
                hT, hk = hts[ci]
                hkeys = [f"{hk}#{j}" for j in range(nt)]
                hl, hlk = halo.next()
                first = ci in (0, 1)
                last = ci in (0, len(CHUNKS) - 1)
                if first:
                    fw.op("dve", lambda: nc.vector.memset(hl[:, :, 0:1], 0.0), writes=[hlk])
                else:
                    pT_, pk_ = hts[ci - 1]
                    pn = CHUNKS[ci - 1][1]
                    fw.op("dve", lambda: nc.vector.tensor_copy(out=hl[:, :, 0:1], in_=pT_[:, :, pn - 1:pn]),
                          reads=[f"{pk_}#{pn // 128 - 1}"], writes=[hlk])
                if last:
                    fw.op("dve", lambda: nc.vector.memset(hl[:, :, 1:2], 0.0), writes=[hlk])
                else:
                    nT_, nk_ = hts[ci + 1]
                    fw.op("dve", lambda: nc.vector.tensor_copy(out=hl[:, :, 1:2], in_=nT_[:, :, 0:1]),
                          reads=[f"{nk_}#0"], writes=[hlk])
                qs_, qsk = qTst.next()
                ks_, ksk = kTst.next()
                vs_, vsk = vst.next()
                for j in range(nt):
                    t = tok0 // 128 + j
                    hkj = f"{hk}#{j}"
                    rt, rtk = rtr.next()
                    fw.dma("sp", rt[:], I["ropeA"][t * 128:(t + 1) * 128, :], writes=[rtk])
                    pkv, pkvk = ptok.next()
                    for k in range(8):
                        fw.op("pe", lambda: nc.tensor.matmul(pkv[:, 0:256], lhsT=hT[:, k, j * 128:(j + 1) * 128], rhs=win[:, k, 0:256],
                                                             start=(k == 0), stop=(k == 7)), reads=[hkj, "l0_win"], writes=[pkvk])
                    kb_, kbk = kbr.next()
                    self.rope_tok(P, pkv[:, 0:128].rearrange("p (h d) -> p h d", h=2), 2, 64, rt, rtk, pkvk, kb_, kbk)
                    fw.op("act", lambda: nc.scalar.copy(out=vs_[:, j, :, 0:64], in_=pkv[:, 128:256].rearrange("p (h d) -> p h d", h=2)), reads=[pkvk], writes=[f"{vsk}#{j}"])
                    pq, pqk = ptok.next()
                    for k in range(8):
                        fw.op("pe", lambda: nc.tensor.matmul(pq[:], lhsT=hT[:, k, j * 128:(j + 1) * 128], rhs=win[:, k, 256:768],
                                                             start=(k == 0), stop=(k == 7)), reads=[hkj, "l0_win"], writes=[pqk])
                    qb_, qbk = qbr.next()
                    self.rope_tok(P, pq[:].rearrange("p (h d) -> p h d", h=8), 8, 64, rt, rtk, pqk, qb_, qbk)
                    pT, pTk = P["pT"].next()
                    qflat = qb_[:].rearrange("p h d -> p (h d)")
                    for c in range(4):
                        fw.op("pe", lambda: nc.tensor.transpose(out=pT[:, c, :], in_=qflat[:, c * 128:(c + 1) * 128], identity=self.identb[:]),
                              reads=[qbk, "identb"], writes=[pTk])
                    fw.op("pe", lambda: nc.tensor.transpose(out=pT[:, 4, :], in_=kb_[:].rearrange("p h d -> p (h d)"), identity=self.identb[:]),
                          reads=[kbk, "identb"], writes=[pTk])
                    fw.op("act", lambda: nc.scalar.copy(out=qs_[:, :, j * 128:(j + 1) * 128], in_=pT[:, 0:4, :]), reads=[pTk], writes=[f"{qsk}#{j}"])
                    fw.op("act", lambda: nc.scalar.copy(out=ks_[:, j * 128:(j + 1) * 128], in_=pT[:, 4, :]), reads=[pTk], writes=[f"{ksk}#{j}"])
                    pz, pzk = ptok.next()
                    for k in range(8):
                        fw.op("pe", lambda: nc.tensor.matmul(pz[:], lhsT=hT[:, k, j * 128:(j + 1) * 128], rhs=win[:, k, 2304:2816],
                                                             start=(k == 0), stop=(k == 7)), reads=[hkj, "l0_win"], writes=[pzk])
                    za_, zak = zast.next()
                    fw.op("act", lambda: nc.scalar.activation(out=za_[:], in_=pz[:], func=AF.Silu), reads=[pzk], writes=[zak])
                    fw.dma(self.QS, R["za0"][b, t * 128:(t + 1) * 128, :], za_[:], reads=[zak], writes=["za0"])
                sl = slice(tok0, tok0 + ntok)
                if getattr(self, "cut", 0) == 2:
                    return
                fw.dma(self.QS, R["qT0"][b].rearrange("j p t -> p j t")[:, :, sl], qs_[:, :, 0:ntok],
                       reads=[f"{qsk}#{j}" for j in range(nt)], writes=["qT0"])
                fw.dma(self.QS, R["kT0"][b][:, sl], ks_[:, 0:ntok], reads=[f"{ksk}#{j}" for j in range(nt)], writes=["kT0"])
                fw.dma(self.QS, R["v0"][b, sl, :].rearrange("(j p) d -> p j d", p=128), vs_[:, 0:nt, :, :].rearrange("p j h d -> p j (h d)"),
                       reads=[f"{vsk}#{j}" for j in range(nt)], writes=["v0"])
                if getattr(self, "cut", 0) == 3:
                    return
                for c in range(4):
                    for g2 in range(2):
                        col0 = (1280 if g2 == 0 else 1792) + c * 128
                        for k in range(8):
                            fw.op("pe", lambda: nc.tensor.matmul(phalo[:, c, g2, :], lhsT=win[:, k, col0:col0 + 128], rhs=hl[:, k, :],
                                                                 start=(k == 0), stop=(k == 7)), reads=[hlk, "l0_win"], writes=["a_phalo"])
                if getattr(self, "cut", 0) == 4:
                    return
                for c in range(4):
                    def fm(col0):
                        pt_, ptk = pfm.next()
                        for k in range(8):
                            fw.op("pe", lambda: nc.tensor.matmul(pt_[:, 0:ntok], lhsT=win[:, k, col0:col0 + 128], rhs=hT[:, k, 0:ntok],
                                                                 start=(k == 0), stop=(k == 7)), reads=hkeys + ["l0_win"], writes=[ptk])
                        return pt_, ptk
                    pbc, pbck = fm(1280 + c * 128)
                    bc_, bck = bcs.next()
                    fw.op("act", lambda: nc.scalar.copy(out=bc_[:, 0:ntok], in_=pbc[:, 0:ntok]), reads=[pbck], writes=[bck])
                    pbx, pbxk = fm(1792 + c * 128)
                    ue, uek = uext.next()
                    fw.op("dve", lambda: nc.vector.tensor_tensor(out=ue[:, 1:ntok + 1], in0=bc_[:, 0:ntok], in1=pbx[:, 0:ntok], op=ALU.mult),
                          reads=[bck, pbxk], writes=[uek])
                    hb_, hbk_ = hbh.next()
                    fw.op("act", lambda: nc.scalar.copy(out=hb_[:], in_=phalo[:, c, 0, :]), reads=["a_phalo"], writes=[hbk_])
                    fw.op("dve", lambda: nc.vector.tensor_tensor(out=ue[:, 0:1], in0=hb_[:, 0:1], in1=phalo[:, c, 1, 0:1], op=ALU.mult),
                          reads=[hbk_, "a_phalo"], writes=[uek])
                    fw.op("dve", lambda: nc.vector.tensor_tensor(out=ue[:, ntok + 1:ntok + 2], in0=hb_[:, 1:2], in1=phalo[:, c, 1, 1:2], op=ALU.mult),
                          reads=[hbk_, "a_phalo"], writes=[uek])
                    ac, ack = acc.next()
                    fw.op("dve", lambda: nc.vector.tensor_scalar(out=ac[:, 0:ntok], in0=ue[:, 0:ntok], scalar1=cw[:, 0, c:c + 1], scalar2=None, op0=ALU.mult),
                          reads=[uek, "a_cw"], writes=[ack])
                    fw.op("dve", lambda: nc.vector.scalar_tensor_tensor(out=ac[:, 0:ntok], in0=ue[:, 1:ntok + 1], scalar=cw[:, 1, c:c + 1], in1=ac[:, 0:ntok],
                                                                       op0=ALU.mult, op1=ALU.add), reads=[uek, "a_cw", ack], writes=[ack])
                    fw.op("dve", lambda: nc.vector.scalar_tensor_tensor(out=ac[:, 0:ntok], in0=ue[:, 2:ntok + 2], scalar=cw[:, 2, c:c + 1], in1=ac[:, 0:ntok],
                                                                       op0=ALU.mult, op1=ALU.add), reads=[uek, "a_cw", ack], writes=[ack])
                    pbb, pbbk = fm(768 + c * 128)
                    fw.op("dve", lambda: nc.vector.tensor_tensor(out=ac[:, 0:ntok], in0=ac[:, 0:ntok], in1=pbb[:, 0:ntok], op=ALU.mult),
                          reads=[ack, pbbk], writes=[ack])
                    pzb, pzbk = fm(2816 + c * 128)
                    sz_, szk = szr.next()
                    fw.op("act", lambda: nc.scalar.activation(out=sz_[:, 0:ntok], in_=pzb[:, 0:ntok], func=AF.Silu), reads=[pzbk], writes=[szk])
                    bg_, bgk = bgst.next()
                    fw.op("pool", lambda: nc.gpsimd.tensor_tensor(out=bg_[:, 0:ntok], in0=ac[:, 0:ntok], in1=sz_[:, 0:ntok], op=ALU.mult),
                          reads=[ack, szk], writes=[bgk])
                    fw.dma(self.QS, R["bg0T"][b, c, :, sl], bg_[:, 0:ntok], reads=[bgk], writes=["bg0T"])

            n = len(CHUNKS)
            emit_norm(0)
            cut = getattr(self, "cut", 0)
            if cut == 1:
                return
            for ci in range(n):
                if ci + 1 < n:
                    emit_norm(ci + 1)
                emit_proj(ci)
                if cut >= 2 and ci >= cut - 5:
                    return

    def l0_stage_b(self, b, wout):
        nc, fw, I, R = self.nc, self.fw, self.I, self.R
        with ExitStack() as es:
            mt = self.mod_tiles(es, 0, b, want_pre=False, want_post=True)
            P = {}
            P["xres"] = self.ring(es, "b_xres", [128, D], F32, 2)
            P["junk"] = self.ring(es, "b_junk", [128, D], BF16, 2)
            P["stat"] = self.ring(es, "b_stat", [128, 4], F32, 4)
            P["t2"] = self.ring(es, "b_t2", [128, D], F32, 2)
            kTp = [[None, None], [None, None]]
            for kv_ in range(2):
                for p_ in range(2):
                    kt_ = self.sb(es, f"b_kTp{kv_}{p_}", [128, T], BF16)
                    key_ = f"b_kTp{kv_}{p_}"
                    fw.op("pool", lambda: nc.gpsimd.memset(kt_[:], 0.0), writes=[key_])
                    fw.dma("sp", kt_[p_ * 64:(p_ + 1) * 64, :], R["kT0"][b, kv_ * 64:(kv_ + 1) * 64, :], reads=["kT0"], writes=[key_])
                    kTp[kv_][p_] = (kt_, key_)
            vall = self.sb(es, "b_vall", [128, NT, 2, 80], BF16)
            for t0_ in range(0, NT, 6):
                fw.dma("sp", vall[:, t0_:t0_ + 6, :, :].rearrange("p t h d -> p t (h d)"),
                       R["v0"][b, t0_ * 128:(t0_ + 6) * 128, :].rearrange("(t p) d -> p t d", p=128), reads=["v0"], writes=["b_vall"])
            mprev = self.sb(es, "b_mprev", [128, 2, 128], BF16)
            mnext = self.sb(es, "b_mnext", [128, 2, 128], BF16)
            fw.op("dve", lambda: nc.vector.tensor_copy(out=mprev[:], in_=self.cf[:, C_MPREV, :].unsqueeze(1).to_broadcast([128, 2, 128])),
                  reads=["c128f"], writes=["b_mprev"])
            fw.op("dve", lambda: nc.vector.tensor_copy(out=mnext[:], in_=self.cf[:, C_MNEXT, :].unsqueeze(1).to_broadcast([128, 2, 128])),
                  reads=["c128f"], writes=["b_mnext"])
            masks = {"P": (mprev, "b_mprev"), "N": (mnext, "b_mnext")}
            snk = self.sb(es, "b_snk", [128, 8], F32)
            esk = self.sb(es, "b_esk", [128, 8], F32)
            fw.dma("sp", snk[:], I["e_sink"].partition_broadcast(128), writes=["b_snk"])
            for kv_ in range(2):
                fw.op("act", lambda: nc.scalar.activation(out=esk[:, kv_ * 4:(kv_ + 1) * 4].rearrange("q (p i) -> q p i", p=2),
                                                          in_=snk[:, kv_ * 4:(kv_ + 1) * 4].rearrange("q (i p) -> q p i", p=2), func=AF.Exp),
                      reads=["b_snk"], writes=["b_esk"])
            psS = self.ring(es, "b_psS", [128, 512], F32, 3, psum=True)
            poR = self.ring(es, "b_po", [128, 4, 80], F32, 2, psum=True)
            pT2 = self.ps(es, "b_pT2", [128, 4, 128], BF16)
            py = self.ps(es, "b_py", [128, D], F32)
            qblk = self.ring(es, "b_qblk", [128, 4, 128], BF16, 3)
            zablk = self.ring(es, "b_zablk", [128, 512], F32, 3)
            bgblk = self.ring(es, "b_bgblk", [128, 4, 128], BF16, 3)
            ptr = self.ring(es, "b_pt", [128, 512], BF16, 12)
            asb = self.ring(es, "b_asb", [128, 8, 64], F32, 3)
            den = self.ring(es, "b_den", [128, 8], F32, 4)
            agr = self.ring(es, "b_ag", [128, 512], BF16, 2)
            agT = self.ring(es, "b_agT", [128, 4, 128], BF16, 2)
            scale = 64 ** -0.5

            loads = {}
            tails = {}

            def emit_loads(t):
                q_, qk = qblk.next()
                fw.dma("sp", q_[:], R["qT0"][b].rearrange("j p t -> p j t")[:, :, t * 128:(t + 1) * 128], reads=["qT0"], writes=[qk])
                z_, zk = zablk.next()
                fw.dma("sp", z_[:], R["za0"][b, t * 128:(t + 1) * 128, :], reads=["za0"], writes=[zk])
                g_, gk = bgblk.next()
                fw.dma("sp", g_[:], R["bg0T"][b].rearrange("j p t -> p j t")[:, :, t * 128:(t + 1) * 128], reads=["bg0T"], writes=[gk])
                loads[t] = (q_, qk, z_, zk, g_, gk)

            def emit_block(t):
                q_, qk, z_, zk, g_, gk = loads.pop(t)
                if self.cutc == 5:
                    return
                if t < 2:
                    kbs = [(0, None), (1, None)]
                else:
                    qi = t - 2
                    kbs = [(0, None), (1, None)]
                    if qi > 0:
                        kbs.append((t - 1, "P"))
                    kbs.append((t, None))
                    if qi < 15:
                        kbs.append((t + 1, "N"))
                a_, ak = asb.next()
                for kv in range(2):
                    pts = []
                    for (kb, mk) in kbs:
                        ps_, psk = psS.next()
                        for p in range(2):
                            kt, ktk = kTp[kv][p]
                            fw.op("pe", lambda: nc.tensor.matmul(ps_[:, p * 256:(p + 1) * 256],
                                                                 lhsT=kt[:, kb * 128:(kb + 1) * 128],
                                                                 rhs=q_[:, 2 * kv:2 * kv + 2, :].rearrange("p a b -> p (a b)"),
                                                                 start=True, stop=(mk is None)),
                                  reads=[ktk, qk], writes=[psk])
                            if mk is not None:
                                m_, mkk = masks[mk]
                                fw.op("pe", lambda: nc.tensor.matmul(ps_[:, p * 256:(p + 1) * 256], lhsT=self.identb[:],
                                                                     rhs=m_[:].rearrange("p a b -> p (a b)"), start=False, stop=True),
                                      reads=["identb", mkk], writes=[psk])
                        pt_, ptk = ptr.next()
                        fw.op("act", lambda: nc.scalar.activation(out=pt_[:], in_=ps_[:], func=AF.Exp, scale=scale), reads=[psk], writes=[ptk])
                        pts.append((pt_, ptk, kb))
                        if self.cutc == 1:
                            return
                    if self.cutc == 2:
                        return
                    po, pok = poR.next()
                    for slot in range(4):
                        for n_, (pt_, ptk, kb) in enumerate(pts):
                            fw.op("pe", lambda: nc.tensor.matmul(po[:, slot, 0:65], lhsT=pt_[:, slot * 128:(slot + 1) * 128], rhs=vall[:, kb, kv, 0:65],
                                                                 start=(n_ == 0), stop=(n_ == len(pts) - 1)),
                                  reads=[ptk, "b_vall"], writes=[pok])
                    if self.cutc == 3:
                        return
                    dn, dnk = den.next()
                    fw.op("dve", lambda: nc.vector.tensor_tensor(out=dn[:, 0:4], in0=po[:, :, 64], in1=esk[:, kv * 4:(kv + 1) * 4], op=ALU.add),
                          reads=[pok, "b_esk"], writes=[dnk])
                    fw.op("dve", lambda: nc.vector.reciprocal(out=dn[:, 4:8], in_=dn[:, 0:4]), reads=[dnk], writes=[dnk])
                    for slot in range(4):
                        p, i = slot // 2, slot % 2
                        h = 4 * kv + 2 * i + p
                        fw.op("dve", lambda: nc.vector.tensor_scalar(out=a_[:, h, :], in0=po[:, slot, 0:64], scalar1=dn[:, 4 + slot:5 + slot],
                                                                    scalar2=None, op0=ALU.mult),
                              reads=[pok, dnk], writes=[ak])
                    if self.cutc == 4:
                        return
                tails[t] = (a_, ak, z_, zk, g_, gk)

            def emit_tail(t):
                a_, ak, z_, zk, g_, gk = tails.pop(t)
                ag, agk = agr.next()
                fw.op("pool", lambda: nc.gpsimd.tensor_tensor(out=ag[:], in0=a_[:].rearrange("p h d -> p (h d)"), in1=z_[:], op=ALU.mult),
                      reads=[ak, zk], writes=[agk])
                for c in range(4):
                    fw.op("pe", lambda: nc.tensor.transpose(out=pT2[:, c, :], in_=ag[:, c * 128:(c + 1) * 128], identity=self.identb[:]),
                          reads=[agk, "identb"], writes=["b_pT2"])
                at, atk = agT.next()
                fw.op("act", lambda: nc.scalar.copy(out=at[:], in_=pT2[:]), reads=["b_pT2"], writes=[atk])
                for half in range(2):
                    for k in range(8):
                        src, srk = (at, atk) if k < 4 else (g_, gk)
                        fw.op("pe", lambda: nc.tensor.matmul(py[:, half * 512:(half + 1) * 512], lhsT=src[:, k % 4, :],
                                                             rhs=wout[:, k, half * 512:(half + 1) * 512], start=(k == 0), stop=(k == 7)),
                              reads=[srk, "l0_wout"], writes=["b_py"])
                nm = "ctx" if t < 2 else "lat"
                g, gkk = mt[f"g_{nm}"]
                self.post_tile(P, py, "b_py", g, gkk, self.src_rows(0, b, t * 128, 128), [],
                               R["x1c"][b, t * 128:(t + 1) * 128, :], "x1c")

            emit_loads(0)
            emit_loads(1)
            emit_block(0)
            for t in range(NT):
                if t + 2 < NT:
                    emit_loads(t + 2)
                if t + 1 < NT:
                    emit_block(t + 1)
                emit_tail(t)

    def layer1(self):
        nc, fw, I, R = self.nc, self.fw, self.I, self.R
        stop = getattr(self, "stop", None)
        with ExitStack() as esl:
            win = self.load_weight_bf16(esl, "l1_win", I["o_w_in"], O_IN)
            nq = self.sb(esl, "l1_nq", [128, 6], F32)
            nkv = self.sb(esl, "l1_nkv", [128, 2], F32)
            fw.dma("sp", nq[:], I["o_q_norm_w"].rearrange("(k p) -> p k", p=128), writes=["l1_nq"], allow_slow_non_contiguous=True)
            fw.dma("sp", nkv[:], I["o_kv_norm_w"].rearrange("(k p) -> p k", p=128), writes=["l1_nkv"], allow_slow_non_contiguous=True)
            wuq = self.sb(esl, "l1_wuq", [128, 6, 768], BF16)
            wukvk = self.sb(esl, "l1_wukvk", [128, 2, 512], BF16)
            wukvv = self.sb(esl, "l1_wukvv", [128, 2, 512], BF16)
            with ExitStack() as es2:
                stg = self.ring(es2, "l1_stg", [128, 1024], F32, 2)
                for c in range(6):
                    st, sk = stg.next()
                    fw.dma("sp", st[:, 0:768], I["o_w_uq"][c * 128:(c + 1) * 128, :], writes=[sk])
                    fw.op("dve", lambda: nc.vector.tensor_scalar(out=wuq[:, c, :], in0=st[:, 0:768], scalar1=nq[:, c:c + 1], scalar2=None, op0=ALU.mult),
                          reads=[sk, "l1_nq"], writes=["l1_wuq"])
                for c in range(2):
                    st, sk = stg.next()
                    fw.dma("sp", st[:], I["o_w_ukv"][c * 128:(c + 1) * 128, :], writes=[sk])
                    sv = st[:].rearrange("p (h two d) -> p h two d", two=2, d=64)
                    fw.op("dve", lambda: nc.vector.tensor_scalar(out=wukvk[:, c, :].rearrange("p (h d) -> p h d", d=64), in0=sv[:, :, 0, :],
                                                                scalar1=nkv[:, c:c + 1], scalar2=None, op0=ALU.mult),
                          reads=[sk, "l1_nkv"], writes=["l1_wukvk"])
                    fw.op("dve", lambda: nc.vector.tensor_scalar(out=wukvv[:, c, :].rearrange("p (h d) -> p h d", d=64), in0=sv[:, :, 1, :],
                                                                scalar1=nkv[:, c:c + 1], scalar2=None, op0=ALU.mult),
                          reads=[sk, "l1_nkv"], writes=["l1_wukvv"])
                fw.barrier()
            if self.cut1 == 1:
                return
            for b in range(NB):
                if _os_env("KONLYB") and int(_os_env("KONLYB")) != b:
                    continue
                self.l1_stage_a(0 if _os_env("KSAMEB") else b, win, wuq, wukvk, wukvv)
                fw.barrier()
                if self.cut1 >= 2:
                    return
        if stop == "l1a":
            return
        for b in range(NB):
            self.l1_stage_b(b)
            fw.barrier()
        if stop == "l1b":
            return
        with ExitStack() as esl:
            wout = self.load_weight_bf16(esl, "l1_wout", I["o_w_out"], D)
            for b in range(NB):
                with ExitStack() as esb:
                    hsum = self.sb(esb, "c_hsum", [128, 16, 4, 128], F32)
                    self.l1_stage_c1(b, hsum)
                    fw.barrier()
                    self.l1_stage_c2(b, hsum, wout)
                    fw.barrier()

    def l1_stage_a(self, b, win, wuq, wukvk, wukvv):
        nc, fw, I, R = self.nc, self.fw, self.I, self.R
        CH = [(0, 256)] + [(256 + 512 * i, 512) for i in range(4)]
        onesf = self.onesb[:]
        with ExitStack() as es:
            mt = self.mod_tiles(es, 1, b, want_pre=True, want_post=False)
            P = {}
            P["xring"] = self.ring(es, "a_x", [128, D], F32, 2)
            P["junk"] = self.ring(es, "a_junk", [128, D], BF16, 1)
            P["stat"] = self.ring(es, "a_stat", [128, 4], F32, 4)
            P["t1"] = self.ring(es, "a_t1", [128, D], F32, 1)
            P["hb"] = self.ring(es, "a_hb", [128, D], BF16, 2)
            P["pT"] = self.ring(es, "a_pT", [128, 8, 128], BF16, 2, psum=True)
            P["ropet"] = self.ring(es, "a_ropet", [128, 8, 32], F32, 8)
            hTr = self.ring(es, "a_hT", [128, 8, 512], BF16, 2)
            pfm = self.ring(es, "a_pfm", [128, 512], F32, 2, psum=True)
            ptok = self.ring(es, "a_ptok", [128, 512], F32, 2, psum=True)
            prep = self.ps(es, "a_prep", [128, 512], F32)
            pcol = self.ps(es, "a_pcol", [128, 16], F32)
            ckvTr = self.ring(es, "a_ckvT", [128, 2, 512], BF16, 2)
            sqkr = self.ring(es, "a_sqk", [128, 2, 512], BF16, 2)
            rrepr = self.ring(es, "a_rrep", [128, 2, 512], F32, 1)
            rcolr = self.ring(es, "a_rcol", [128, 16], F32, 2)
            knstr = self.ring(es, "a_knst", [128, 4, 512], BF16, 1)
            vstr = self.ring(es, "a_vst", [128, 8, 80], BF16, 2)
            for i_, tl in enumerate(vstr.tiles):
                fw.op("pool", lambda: nc.gpsimd.memset(tl[:], 1.0), writes=[f"a_vst{i_}"])
            rtr = self.ring(es, "a_rt", [128, 32], F32, 3)
            krfr = self.ring(es, "a_krf", [128, 1, 32], BF16, 2)
            krstr = self.ring(es, "a_krst", [32, 512], BF16, 2)
            mkstr = self.ring(es, "a_mkst", [128, 256], BF16, 2)
            mkTstr = self.ring(es, "a_mkTst", [128, 512], BF16, 2)
            mvstr = self.ring(es, "a_mvst", [128, 4, 144], BF16, 2)
            for i_, tl in enumerate(mvstr.tiles):
                fw.op("pool", lambda: nc.gpsimd.memset(tl[:], 1.0), writes=[f"a_mvst{i_}"])
            mgstr = self.ring(es, "a_mgst", [128, 16], F32, 2)
            cqTr = self.ring(es, "a_cqT", [128, 6, 512], BF16, 1)
            sqqr = self.ring(es, "a_sqq", [128, 6, 512], BF16, 1)
            qfr = self.ring(es, "a_qf", [128, 8, 96], F32, 1)
            qbfr = self.ring(es, "a_qbf", [128, 8, 96], BF16, 2)
            qstr = self.ring(es, "a_qst", [128, 8, 512], BF16, 1)
            mqTstr = self.ring(es, "a_mqTst", [128, 512], BF16, 2)
            mostr = self.ring(es, "a_most", [128, 512], F32, 2)
            zstr = self.ring(es, "a_zst", [128, 512], F32, 2)

            hts = {}

            def emit_norm(ci):
                tok0, ntok = CH[ci]
                hT, hk = hTr.next()
                hts[ci] = (hT, hk)
                for j in range(ntok // 128):
                    self.norm_tile(P, 1, b, tok0 // 128 + j, hT, hk, j, mt)

            def mm_fm(hT, hkeys, col0, ntok):
                pf, pfk = pfm.next()
                for k in range(8):
                    fw.op("pe", lambda: nc.tensor.matmul(pf[:, 0:ntok], lhsT=win[:, k, col0:col0 + 128], rhs=hT[:, k, 0:ntok],
                                                         start=(k == 0), stop=(k == 7)), reads=hkeys + ["l1_win"], writes=[pfk])
                return pf, pfk

            def mm_tok(hT, hkj, j, col0, ncol, off=0, pt_=None, ptk=None):
                if pt_ is None:
                    pt_, ptk = ptok.next()
                for k in range(8):
                    fw.op("pe", lambda: nc.tensor.matmul(pt_[:, off:off + ncol], lhsT=hT[:, k, j * 128:(j + 1) * 128], rhs=win[:, k, col0:col0 + ncol],
                                                         start=(k == 0), stop=(k == 7)), reads=[hkj, "l1_win"], writes=[ptk])
                return pt_, ptk

            def emit_proj(ci):
                tok0, ntok = CH[ci]
                nt = ntok // 128
                is_lat = tok0 >= LC
                hT, hk = hts[ci]
                hkeys = [f"{hk}#{j}" for j in range(nt)]
                sl = slice(tok0, tok0 + ntok)
                ckvT, ckvk = ckvTr.next()
                sqk, sqkk = sqkr.next()
                for c in range(2):
                    pf, pfk = mm_fm(hT, hkeys, c * 128, ntok)
                    fw.op("act", lambda: nc.scalar.copy(out=ckvT[:, c, 0:ntok], in_=pf[:, 0:ntok]), reads=[pfk], writes=[f"{ckvk}#{c}"])
                    fw.op("act", lambda: nc.scalar.activation(out=sqk[:, c, 0:ntok], in_=pf[:, 0:ntok], func=AF.Square), reads=[pfk], writes=[f"{sqkk}#{c}"])
                ckeys = [f"{ckvk}#0", f"{ckvk}#1"]
                skeys = [f"{sqkk}#0", f"{sqkk}#1"]
                for c in range(2):
                    fw.op("pe", lambda: nc.tensor.matmul(prep[:, 0:ntok], lhsT=onesf, rhs=sqk[:, c, 0:ntok], start=(c == 0), stop=(c == 1)),
                          reads=skeys + ["c128f"], writes=["a_prep"])
                rrep, rrepk = rrepr.next()
                fw.op("act", lambda: nc.scalar.activation(out=rrep[:, 0, 0:ntok], in_=prep[:, 0:ntok], func=AF.Sqrt, bias=EPS, scale=1.0 / 256.0),
                      reads=["a_prep"], writes=[rrepk])
                fw.op("dve", lambda: nc.vector.reciprocal(out=rrep[:, 1, 0:ntok], in_=rrep[:, 0, 0:ntok]), reads=[rrepk], writes=[rrepk])
                rcol, rcolk = rcolr.next()
                for j in range(nt):
                    for c in range(2):
                        fw.op("pe", lambda: nc.tensor.matmul(pcol[:, j:j + 1], lhsT=sqk[:, c, j * 128:(j + 1) * 128], rhs=onesf[:, 0:1],
                                                             start=(c == 0), stop=(c == 1)), reads=skeys + ["c128f"], writes=["a_pcol"])
                fw.op("act", lambda: nc.scalar.activation(out=rcol[:, 4:4 + nt], in_=pcol[:, 0:nt], func=AF.Sqrt, bias=EPS, scale=1.0 / 256.0),
                      reads=["a_pcol"], writes=[rcolk])
                fw.op("dve", lambda: nc.vector.reciprocal(out=rcol[:, 0:nt], in_=rcol[:, 4:4 + nt]), reads=[rcolk], writes=[rcolk])
                if self.cut1 == 2:
                    return
                knst, knk = knstr.next()
                for pair in range(4):
                    pf, pfk = pfm.next()
                    for c in range(2):
                        fw.op("pe", lambda: nc.tensor.matmul(pf[:, 0:ntok], lhsT=wukvk[:, c, pair * 128:(pair + 1) * 128], rhs=ckvT[:, c, 0:ntok],
                                                             start=(c == 0), stop=(c == 1)), reads=ckeys + ["l1_wukvk"], writes=[pfk])
                    fw.op("dve", lambda: nc.vector.tensor_tensor(out=knst[:, pair, 0:ntok], in0=pf[:, 0:ntok], in1=rrep[:, 1, 0:ntok], op=ALU.mult),
                          reads=[pfk, rrepk], writes=[f"{knk}#{pair}"])
                kview = R["kT1"][b].rearrange("(j two) r t -> two r j t", two=2)
                for two_ in range(2):
                    fw.dma(self.QS, kview[two_, 0:64, :, sl], knst[two_ * 64:(two_ + 1) * 64, :, 0:ntok],
                           reads=[f"{knk}#{p_}" for p_ in range(4)], writes=["kT1"])
                if self.cut1 == 3:
                    return
                krst, krk = krstr.next()
                for j in range(nt):
                    t = tok0 // 128 + j
                    hkj = f"{hk}#{j}"
                    tsl = slice(t * 128, (t + 1) * 128)
                    pt_, ptk = ptok.next()
                    for c in range(2):
                        fw.op("pe", lambda: nc.tensor.matmul(pt_[:, 0:512], lhsT=ckvT[:, c, j * 128:(j + 1) * 128], rhs=wukvv[:, c, :],
                                                             start=(c == 0), stop=(c == 1)), reads=ckeys + ["l1_wukvv"], writes=[ptk])
                    vs_, vsk = vstr.next()
                    fw.op("act", lambda: nc.scalar.mul(out=vs_[:, :, 0:64], in_=pt_[:, 0:512].rearrange("p (h d) -> p h d", h=8), mul=rcol[:, j:j + 1]), reads=[ptk, rcolk], writes=[vsk])
                    fw.dma(self.QS, R["v1"][b, tsl, :], vs_[:].rearrange("p h d -> p (h d)"), reads=[vsk], writes=["v1"])
                    pt_, ptk = mm_tok(hT, hkj, j, 256, 32)
                    mm_tok(hT, hkj, j, 1056, 16, off=64, pt_=pt_, ptk=ptk)
                    rt, rtk = rtr.next()
                    fw.dma("sp", rt[:], I["ropeC"][tsl, :], writes=[rtk])
                    krf, krfk = krfr.next()
                    self.rope_tok(P, pt_[:, 0:32].rearrange("p (h d) -> p h d", h=1), 1, 32, rt, rtk, ptk, krf, krfk)
                    pT, pTk = P["pT"].next()
                    fw.op("pe", lambda: nc.tensor.transpose(out=pT[0:32, 0, :], in_=krf[:, 0, :], identity=self.identb[:]),
                          reads=[krfk, "identb"], writes=[pTk])
                    fw.op("act", lambda: nc.scalar.copy(out=krst[0:32, j * 128:(j + 1) * 128], in_=pT[0:32, 0, :]), reads=[pTk], writes=[f"{krk}#{j}"])
                    mg_, mgk = mgstr.next()
                    fw.op("act", lambda: nc.scalar.copy(out=mg_[:], in_=pt_[:, 64:80]), reads=[ptk], writes=[mgk])
                    fw.dma(self.QS, R["mg1"][b, tsl, :], mg_[:], reads=[mgk], writes=["mg1"])
                    pt_, ptk = mm_tok(hT, hkj, j, 288, 256)
                    mk_, mkk = mkstr.next()
                    fw.op("act", lambda: nc.scalar.mul(out=mk_[:], in_=pt_[:, 0:256], mul=0.125), reads=[ptk], writes=[mkk])
                    fw.dma(self.QS, R["mk1"][b, tsl, :], mk_[:], reads=[mkk], writes=["mk1"])
                    pt_, ptk = mm_tok(hT, hkj, j, 544, 512)
                    mv_, mvk = mvstr.next()
                    fw.op("act", lambda: nc.scalar.copy(out=mv_[:, :, 0:128], in_=pt_[:, 0:512].rearrange("p (h d) -> p h d", h=4)), reads=[ptk], writes=[mvk])
                    fw.dma(self.QS, R["mv1"][b, tsl, :], mv_[:].rearrange("p h d -> p (h d)"), reads=[mvk], writes=["mv1"])
                if self.cut1 == 4:
                    return
                for h in range(8):
                    fw.dma(self.QS, R["kT1"][b, h, 64:96, sl], krst[0:32, 0:ntok], reads=[f"{krk}#{j}" for j in range(nt)], writes=["kT1"])
                for c in range(2):
                    pf, pfk = mm_fm(hT, hkeys, 288 + c * 128, ntok)
                    mkT_, mkTk = mkTstr.next()
                    fw.op("act", lambda: nc.scalar.mul(out=mkT_[:, 0:ntok], in_=pf[:, 0:ntok], mul=0.125), reads=[pfk], writes=[mkTk])
                    fw.dma(self.QS, R["mkT1"][b, c * 128:(c + 1) * 128, sl], mkT_[:, 0:ntok], reads=[mkTk], writes=["mkT1"])
                if not is_lat:
                    return
                ls = tok0 - LC
                lsl = slice(ls, ls + ntok)
                cqT, cqk = cqTr.next()
                sqq, sqqk = sqqr.next()
                for c in range(6):
                    pf, pfk = mm_fm(hT, hkeys, 1072 + c * 128, ntok)
                    fw.op("act", lambda: nc.scalar.copy(out=cqT[:, c, 0:ntok], in_=pf[:, 0:ntok]), reads=[pfk], writes=[f"{cqk}#{c}"])
                    fw.op("act", lambda: nc.scalar.activation(out=sqq[:, c, 0:ntok], in_=pf[:, 0:ntok], func=AF.Square), reads=[pfk], writes=[f"{sqqk}#{c}"])
                cqkeys = [f"{cqk}#{c}" for c in range(6)]
                sqkeys = [f"{sqqk}#{c}" for c in range(6)]
                for j in range(nt):
                    for c in range(6):
                        fw.op("pe", lambda: nc.tensor.matmul(pcol[:, 8 + j:9 + j], lhsT=sqq[:, c, j * 128:(j + 1) * 128], rhs=onesf[:, 0:1],
                                                             start=(c == 0), stop=(c == 5)), reads=sqkeys + ["c128f"], writes=["a_pcol"])
                fw.op("act", lambda: nc.scalar.activation(out=rcol[:, 12:12 + nt], in_=pcol[:, 8:8 + nt], func=AF.Sqrt, bias=EPS, scale=1.0 / 768.0),
                      reads=["a_pcol"], writes=[rcolk])
                fw.op("dve", lambda: nc.vector.reciprocal(out=rcol[:, 8:8 + nt], in_=rcol[:, 12:12 + nt]), reads=[rcolk], writes=[rcolk])
                qst, qstk = qstr.next()
                for j in range(nt):
                    t = tok0 // 128 + j
                    tsl = slice(t * 128, (t + 1) * 128)
                    qf, qfk = qfr.next()
                    qflat = qf[:].rearrange("p h d -> p (h d)")
                    for (c0, cn) in ((0, 512), (512, 256)):
                        pt_, ptk = ptok.next()
                        for c in range(6):
                            fw.op("pe", lambda: nc.tensor.matmul(pt_[:, 0:cn], lhsT=cqT[:, c, j * 128:(j + 1) * 128], rhs=wuq[:, c, c0:c0 + cn],
                                                                 start=(c == 0), stop=(c == 5)), reads=cqkeys + ["l1_wuq"], writes=[ptk])
                        fw.op("act", lambda: nc.scalar.mul(out=qflat[:, c0:c0 + cn], in_=pt_[:, 0:cn], mul=rcol[:, 8 + j:9 + j]),
                              reads=[ptk, rcolk], writes=[f"{qfk}#{c0}"])
                    qfkeys = [f"{qfk}#0", f"{qfk}#512"]
                    qbf, qbk = qbfr.next()
                    fw.op("pool", lambda: nc.gpsimd.tensor_copy(out=qbf[:, :, 0:64], in_=qf[:, :, 0:64]), reads=qfkeys, writes=[f"{qbk}#n"])
                    rt, rtk = rtr.next()
                    fw.dma("sp", rt[:], I["ropeC"][tsl, :], writes=[rtk])
                    self.rope_tok(P, qf, 8, 32, rt, rtk, qfkeys, qbf, f"{qbk}#r", dst_off=64, src_off=64)
                    pT, pTk = P["pT"].next()
                    for h in range(8):
                        fw.op("pe", lambda: nc.tensor.transpose(out=pT[0:96, h, :], in_=qbf[:, h, :], identity=self.identb[:]),
                              reads=[f"{qbk}#n", f"{qbk}#r", "identb"], writes=[pTk])
                    fw.op("act", lambda: nc.scalar.copy(out=qst[0:96, :, j * 128:(j + 1) * 128], in_=pT[0:96, :, :]), reads=[pTk], writes=[f"{qstk}#{j}"])
                    lt = t - 2
                    ltsl = slice(lt * 128, (lt + 1) * 128)
                    pt_, ptk = mm_tok(hT, f"{hk}#{j}", j, 2096, 512)
                    mo_, mok = mostr.next()
                    fw.op("act", lambda: nc.scalar.activation(out=mo_[:], in_=pt_[:, 0:512], func=AF.Sigmoid), reads=[ptk], writes=[mok])
                    fw.dma(self.QS, R["mo1"][b, ltsl, :], mo_[:], reads=[mok], writes=["mo1"])
                    for zh in range(2):
                        pt_, ptk = mm_tok(hT, f"{hk}#{j}", j, 2608 + zh * 512, 512)
                        z_, zk = zstr.next()
                        fw.op("act", lambda: nc.scalar.activation(out=z_[:], in_=pt_[:, 0:512], func=AF.Silu), reads=[ptk], writes=[zk])
                        fw.dma(self.QS, R["z1"][b, ltsl, zh * 512:(zh + 1) * 512], z_[:], reads=[zk], writes=["z1"])
                fw.dma(self.QS, R["qT1"][b].rearrange("h r t -> r h t")[:, :, lsl], qst[0:96, :, 0:ntok],
                       reads=[f"{qstk}#{j}" for j in range(nt)], writes=["qT1"])
                for c in range(2):
                    pf, pfk = mm_fm(hT, hkeys, 1840 + c * 128, ntok)
                    mq_, mqk = mqTstr.next()
                    fw.op("act", lambda: nc.scalar.copy(out=mq_[:, 0:ntok], in_=pf[:, 0:ntok]), reads=[pfk], writes=[mqk])
                    fw.dma(self.QS, R["mqT1"][b, c * 128:(c + 1) * 128, lsl], mq_[:, 0:ntok], reads=[mqk], writes=["mqT1"])

            n = len(CH)
            emit_norm(0)
            for ci in range(n):
                if ci + 1 < n:
                    emit_norm(ci + 1)
                emit_proj(ci)
                if self.cut1 in (2, 3, 4, 5) or (self.cut1 >= 6 and ci >= self.cut1 - 5):
                    return
                if b == 1 and _os_env("KCUT2") and ci >= int(_os_env("KCUT2")) - 1:
                    return

    def l1_stage_b(self, b):
        nc, fw, I, R = self.nc, self.fw, self.I, self.R
        with ExitStack() as es:
            kT = self.sb(es, "m_kT", [128, 8, T], BF16)
            vall = self.sb(es, "m_vall", [128, NT, 8, 80], BF16)
            fw.dma("sp", kT[0:96, :, :], R["kT1"][b].rearrange("h r t -> r h t"), reads=["kT1"], writes=["m_kT"])
            for t0_ in range(0, NT, 3):
                fw.dma("sp", vall[:, t0_:t0_ + 3, :, :].rearrange("p t h d -> p t (h d)"),
                       R["v1"][b, t0_ * 128:(t0_ + 3) * 128, :].rearrange("(t p) d -> p t d", p=128), reads=["v1"], writes=["m_vall"])
            psS = self.ring(es, "m_psS", [128, 512], F32, 4, psum=True)
            poR = self.ring(es, "m_po", [128, 4, 80], F32, 2, psum=True)
            pT = self.ps(es, "m_pT", [128, 4, 128], BF16)
            qTr = self.ring(es, "m_qT", [128, 8, 512], BF16, 2)
            ptr = self.ring(es, "m_pt", [128, 512], BF16, 40)
            coutr = self.ring(es, "m_cout", [128, 4, 8, 64], F32, 2)
            recr = self.ring(es, "m_rec", [128, 4], F32, 4)
            zr = self.ring(es, "m_z", [128, 512], F32, 2)
            cgr = self.ring(es, "m_cg", [128, 512], BF16, 2)
            cgstr = self.ring(es, "m_cgst", [128, 4, 128], BF16, 2)
            scale = 96 ** -0.5
            for qc in range(4):
                qT, qTk = qTr.next()
                fw.dma("sp", qT[0:96, :, :], R["qT1"][b].rearrange("h r t -> r h t")[:, :, qc * 512:(qc + 1) * 512], reads=["qT1"], writes=[qTk])
                cout, coutk = coutr.next()

                def emit_qk(h):
                    pts = []
                    for kb in range(NT):
                        ps_, psk = psS.next()
                        fw.op("pe", lambda: nc.tensor.matmul(ps_[:], lhsT=kT[0:96, h, kb * 128:(kb + 1) * 128], rhs=qT[0:96, h, :], start=True, stop=True),
                              reads=["m_kT", qTk], writes=[psk])
                        pt_, ptk = ptr.next()
                        fw.op("act", lambda: nc.scalar.activation(out=pt_[:], in_=ps_[:], func=AF.Exp, scale=scale), reads=[psk], writes=[ptk])
                        pts.append((pt_, ptk))
                    return pts

                def emit_pv(h, pts):
                    po, pok = poR.next()
                    for qs in range(4):
                        for kb in range(NT):
                            pt_, ptk = pts[kb]
                            fw.op("pe", lambda: nc.tensor.matmul(po[:, qs, 0:65], lhsT=pt_[:, qs * 128:(qs + 1) * 128], rhs=vall[:, kb, h, 0:65],
                                                                 start=(kb == 0), stop=(kb == NT - 1)), reads=[ptk, "m_vall"], writes=[pok])
                    rec, reck = recr.next()
                    fw.op("dve", lambda: nc.vector.reciprocal(out=rec[:], in_=po[:, :, 64]), reads=[pok], writes=[reck])
                    fw.op("dve", lambda: nc.vector.tensor_tensor(out=cout[:, :, h, :], in0=po[:, :, 0:64],
                                                                in1=rec[:].unsqueeze(2).to_broadcast([128, 4, 64]), op=ALU.mult),
                          reads=[pok, reck], writes=[f"{coutk}#{h}"])

                nxt = emit_qk(0)
                for h in range(8):
                    cur = nxt
                    if h + 1 < 8:
                        nxt = emit_qk(h + 1)
                    emit_pv(h, cur)
                for qs in range(4):
                    lt = qc * 4 + qs
                    ltsl = slice(lt * 128, (lt + 1) * 128)
                    z_, zk = zr.next()
                    fw.dma("sp", z_[:], R["z1"][b, ltsl, 0:512], reads=["z1"], writes=[zk])
                    cg, cgk = cgr.next()
                    fw.op("pool", lambda: nc.gpsimd.tensor_tensor(out=cg[:], in0=cout[:, qs, :, :].rearrange("p h d -> p (h d)"), in1=z_[:], op=ALU.mult),
                          reads=[f"{coutk}#{h}" for h in range(8)] + [zk], writes=[cgk])
                    for c in range(4):
                        fw.op("pe", lambda: nc.tensor.transpose(out=pT[:, c, :], in_=cg[:, c * 128:(c + 1) * 128], identity=self.identb[:]),
                              reads=[cgk, "identb"], writes=["m_pT"])
                    cgst, cgsk = cgstr.next()
                    fw.op("act", lambda: nc.scalar.copy(out=cgst[:], in_=pT[:]), reads=["m_pT"], writes=[cgsk])
                    fw.dma(self.QS, R["cgT1"][b].rearrange("c p t -> p c t")[:, :, ltsl], cgst[:], reads=[cgsk], writes=["cgT1"])

    def l1_stage_c1(self, b, hsum):
        nc, fw, I, R = self.nc, self.fw, self.I, self.R
        cf = self.cf
        with ExitStack() as es:
            gt = self.sb(es, "c_gt", [128, NT, 16], F32)
            mk = self.sb(es, "c_mk", [128, NT, 256], BF16)
            VO = self.sb(es, "c_VO", [128, NT, 4, 144], BF16)
            mkT = self.sb(es, "c_mkT", [64, 4, T], BF16)
            mqT = self.sb(es, "c_mqT", [64, 4, S], BF16)
            ib = self.sb(es, "c_ib", [128, 8], F32)
            fb = self.sb(es, "c_fb", [128, 8], F32)
            fw.dma("sp", ib[:], I["o_i_bias"].partition_broadcast(128), writes=["c_ib"])
            fw.dma("sp", fb[:], I["o_f_bias"].partition_broadcast(128), writes=["c_fb"])
            for t0_ in range(0, NT, 3):
                fw.dma("sp", VO[:, t0_:t0_ + 3, :, :].rearrange("p t h d -> p t (h d)"),
                       R["mv1"][b, t0_ * 128:(t0_ + 3) * 128, :].rearrange("(t p) d -> p t d", p=128), reads=["mv1"], writes=["c_VO"])
            for t in range(NT):
                tsl = slice(t * 128, (t + 1) * 128)
                fw.dma("sp", gt[:, t, :], R["mg1"][b, tsl, :], reads=["mg1"], writes=["c_gt"])
                fw.dma("sp", mk[:, t, :], R["mk1"][b, tsl, :], reads=["mk1"], writes=["c_mk"])
            fw.dma("sp", mkT[:], R["mkT1"][b].rearrange("(h d) t -> d h t", h=4), reads=["mkT1"], writes=["c_mkT"])
            fw.dma("sp", mqT[:], R["mqT1"][b].rearrange("(h d) t -> d h t", h=4), reads=["mqT1"], writes=["c_mqT"])
            pbig = self.ring(es, "c_pbig", [128, 512], F32, 1, psum=True)
            pC = self.ring(es, "c_pC", [64, 2, 144], F32, 2, psum=True)
            pS = self.ring(es, "c_pS", [128, 128], F32, 1, psum=True)
            pH = self.ring(es, "c_pH", [128, 2, 144], F32, 4, psum=True)
            cbr = self.ring(es, "c_cb", [64, 4, 144], BF16, 6)
            dgr = self.ring(es, "c_dg", [128, 4, 128], F32, 2)
            rmr = self.ring(es, "c_rm", [128, 4, 128], F32, 2)
            er = self.ring(es, "c_e", [64, 4, 128], F32, 2)
            qz0r = self.ring(es, "c_qz0", [64, 4, 128], BF16, 2)
            qz1r = self.ring(es, "c_qz1", [64, 4, 128], BF16, 2)
            for rr in (qz0r, qz1r):
                for i_, tl in enumerate(rr.tiles):
                    fw.op("pool", lambda: nc.gpsimd.memset(tl[:], 0.0), writes=[f"{rr.name}{i_}"])
            dr = self.ring(es, "c_d", [128, 128], F32, 8)
            scr = self.ring(es, "c_sc", [128, 128], BF16, 8)
            dnr = self.ring(es, "c_dn", [128, 8], F32, 6)
            Dd = []
            for d in range(2):
                X = {}
                for nm in ("li", "xf", "l1", "nb", "ngc", "aa", "wcol", "bcol"):
                    X[nm] = self.sb(es, f"c_{nm}{d}", [128, 72], F32)
                X["egf"] = self.sb(es, f"c_egf{d}", [128, 2, 72], F32)
                X["VW"] = self.sb(es, f"c_VW{d}", [128, NT, 4, 144], BF16)
                X["C"] = self.sb(es, f"c_C{d}", [64, 4, 144], F32)
                Dd.append(X)
            for d in range(2):
                X = Dd[d]
                li, xf, l1, nb, ngc, aa, wcol, bcol, egf, VW, C = (X[k] for k in ("li", "xf", "l1", "nb", "ngc", "aa", "wcol", "bcol", "egf", "VW", "C"))
                K_ = lambda nm: f"c_{nm}{d}"
                tri = C_TRIF if d == 0 else C_TRIR
                g3 = lambda a: a[:].rearrange("p (t h) -> p t h", h=4)
                fw.op("dve", lambda: nc.vector.tensor_tensor(out=g3(li), in0=gt[:, :, d * 8:d * 8 + 4],
                                                            in1=ib[:, d * 4:(d + 1) * 4].unsqueeze(1).to_broadcast([128, NT, 4]), op=ALU.add),
                      reads=["c_gt", "c_ib"], writes=[K_("li")])
                fw.op("dve", lambda: nc.vector.tensor_tensor(out=g3(xf), in0=gt[:, :, d * 8 + 4:d * 8 + 8],
                                                            in1=fb[:, d * 4:(d + 1) * 4].unsqueeze(1).to_broadcast([128, NT, 4]), op=ALU.add),
                      reads=["c_gt", "c_fb"], writes=[K_("xf")])
                fw.op("act", lambda: nc.scalar.activation(out=xf[:], in_=xf[:], func=AF.Exp, scale=-1.0), reads=[K_("xf")], writes=[K_("xf")])
                fw.op("act", lambda: nc.scalar.activation(out=l1[:], in_=xf[:], func=AF.Ln, bias=1.0), reads=[K_("xf")], writes=[K_("l1")])
                p1, p1k = pbig.next()
                fw.op("pe", lambda: nc.tensor.matmul(p1[:, 0:72], lhsT=cf[:, tri, :], rhs=l1[:], start=True, stop=True), reads=["c128f", K_("l1")], writes=[p1k])
                fw.op("dve", lambda: nc.vector.tensor_copy(out=nb[:], in_=p1[:, 0:72]), reads=[p1k], writes=[K_("nb")])
                p2, p2k = pbig.next()
                fw.op("pe", lambda: nc.tensor.matmul(p2[:, 0:72], lhsT=cf[:, C_BLK, :], rhs=l1[:], start=True, stop=True), reads=["c128f", K_("l1")], writes=[p2k])
                fw.op("dve", lambda: nc.vector.tensor_copy(out=ngc[:], in_=p2[:, 0:72]), reads=[p2k], writes=[K_("ngc")])
                p3, p3k = pbig.next()
                for half in range(2):
                    fw.op("pe", lambda: nc.tensor.matmul(p3[:, half * 72:(half + 1) * 72], lhsT=cf[:, C_SEL0 + half, :], rhs=l1[:], start=True, stop=True),
                          reads=["c128f", K_("l1")], writes=[p3k])
                fw.op("act", lambda: nc.scalar.activation(out=egf[:].rearrange("p a b -> p (a b)"), in_=p3[:, 0:144], func=AF.Exp, scale=-1.0),
                      reads=[p3k], writes=[K_("egf")])
                fw.op("dve", lambda: nc.vector.tensor_tensor(out=bcol[:], in0=li[:], in1=nb[:], op=ALU.add), reads=[K_("li"), K_("nb")], writes=[K_("bcol")])
                fw.op("dve", lambda: nc.vector.tensor_tensor(out=aa[:], in0=bcol[:], in1=ngc[:], op=ALU.subtract), reads=[K_("bcol"), K_("ngc")], writes=[K_("aa")])
                fw.op("act", lambda: nc.scalar.activation(out=wcol[:], in_=aa[:], func=AF.Exp), reads=[K_("aa")], writes=[K_("wcol")])
                for part in range(2):
                    e_ = "dve" if part == 0 else "pool"
                    eng = nc.vector if part == 0 else nc.gpsimd
                    tt = slice(part * 9, (part + 1) * 9)
                    fw.op(e_, lambda: eng.tensor_tensor(out=VW[:, tt, :, :].rearrange("p t h v -> p (t h) v"),
                                                        in0=VO[:, tt, :, :].rearrange("p t h v -> p (t h) v"),
                                                        in1=wcol[:, part * 36:(part + 1) * 36].unsqueeze(2).to_broadcast([128, 36, 144]), op=ALU.mult),
                          reads=["c_VO", K_("wcol")], writes=[f"c_VW{d}#{part}"])
                fw.op("dve", lambda: nc.vector.memset(C[:], 0.0), writes=[f"c_C{d}#{h}" for h in range(4)])

            written = set()

            def process_tile(d, t):
                X = Dd[d]
                nb, bcol, egf, VW, C = X["nb"], X["bcol"], X["egf"], X["VW"], X["C"]
                K_ = lambda nm: f"c_{nm}{d}"
                ckeys = [f"c_C{d}#{h}" for h in range(4)]
                vwkeys = [f"c_VW{d}#0", f"c_VW{d}#1"]
                mbk = C_MBF if d == 0 else C_MBR
                horder = (0, 1) if d == 0 else (1, 0)
                Cin = {}
                for half in horder:
                    if t >= 2:
                        cb, cbk = cbr.next()
                        fw.op("act", lambda: nc.scalar.copy(out=cb[:], in_=C[:]), reads=ckeys, writes=[cbk])
                        Cin[half] = (cb, cbk)
                    hs_ = slice(half * 64, (half + 1) * 64)
                    for hp in range(2):
                        pc, pck = pC.next()
                        for hh in range(2):
                            h = 2 * hp + hh
                            fw.op("pe", lambda: nc.tensor.matmul(pc[:, hh, 0:129], lhsT=mk[hs_, t, h * 64:(h + 1) * 64], rhs=VW[hs_, t, h, 0:129],
                                                                 start=True, stop=True), reads=["c_mk"] + vwkeys, writes=[pck])
                        for hh in range(2):
                            h = 2 * hp + hh
                            idx = t * 4 + h
                            fw.op("dve", lambda: nc.vector.scalar_tensor_tensor(out=C[:, h, 0:129], in0=C[:, h, 0:129], scalar=egf[0:64, half, idx:idx + 1],
                                                                               in1=pc[:, hh, 0:129], op0=ALU.mult, op1=ALU.add),
                                  reads=[ckeys[h], K_("egf"), pck], writes=[ckeys[h]])
                if t < 2:
                    return
                lt = t - 2
                dg, dgk = dgr.next()
                fw.op("dve", lambda: nc.vector.tensor_tensor(out=dg[:], in0=nb[:, t * 4:(t + 1) * 4].unsqueeze(2).to_broadcast([128, 4, 128]),
                                                            in1=cf[:, C_ID, :].unsqueeze(1).to_broadcast([128, 4, 128]), op=ALU.mult),
                      reads=[K_("nb"), "c128f"], writes=[dgk])
                pR, pRk = pbig.next()
                fw.op("pe", lambda: nc.tensor.matmul(pR[:], lhsT=cf[:, C_ONES, :], rhs=dg[:].rearrange("p h n -> p (h n)"), start=True, stop=True),
                      reads=["c128f", dgk], writes=[pRk])
                rm, rmk = rmr.next()
                fw.op("dve", lambda: nc.vector.tensor_tensor(out=rm[:], in0=cf[:, mbk, :].unsqueeze(1).to_broadcast([128, 4, 128]),
                                                            in1=pR[:].rearrange("p (h n) -> p h n", h=4), op=ALU.subtract),
                      reads=["c128f", pRk], writes=[rmk])
                e_, ek = er.next()
                fw.op("act", lambda: nc.scalar.activation(out=e_[:].rearrange("p h n -> p (h n)"), in_=pR[0:64, :], func=AF.Exp, scale=-1.0),
                      reads=[pRk], writes=[ek])
                qz0, qz0k = qz0r.next()
                qz1, qz1k = qz1r.next()
                fw.op("pool", lambda: nc.gpsimd.tensor_tensor(out=qz0[:, :, 0:64], in0=mqT[:, :, lt * 128:lt * 128 + 64], in1=e_[:, :, 0:64], op=ALU.mult),
                      reads=["c_mqT", ek], writes=[qz0k])
                fw.op("pool", lambda: nc.gpsimd.tensor_tensor(out=qz1[:, :, 64:128], in0=mqT[:, :, lt * 128 + 64:lt * 128 + 128], in1=e_[:, :, 64:128], op=ALU.mult),
                      reads=["c_mqT", ek], writes=[qz1k])
                scs = []
                for h in range(4):
                    idx = t * 4 + h
                    dt_, dtk = dr.next()
                    fw.op("act", lambda: nc.scalar.activation(out=dt_[:], in_=rm[:, h, :], func=AF.Exp, bias=bcol[:, idx:idx + 1], scale=1.0),
                          reads=[rmk, K_("bcol")], writes=[dtk])
                    ps_, psk = pS.next()
                    fw.op("pe", lambda: nc.tensor.matmul(ps_[:], lhsT=mkT[:, h, t * 128:(t + 1) * 128], rhs=mqT[:, h, lt * 128:(lt + 1) * 128],
                                                         start=True, stop=True), reads=["c_mkT", "c_mqT"], writes=[psk])
                    sc, sck = scr.next()
                    fw.op("dve", lambda: nc.vector.tensor_tensor(out=sc[:], in0=ps_[:], in1=dt_[:], op=ALU.mult), reads=[psk, dtk], writes=[sck])
                    scs.append((sc, sck))
                c0, c0k = Cin[0]
                c1, c1k = Cin[1]
                phs = []
                for hp in range(2):
                    ph, phk = pH.next()
                    phs.append((ph, phk))
                    for hh in range(2):
                        h = 2 * hp + hh
                        sc, sck = scs[h]
                        fw.op("pe", lambda: nc.tensor.matmul(ph[:, hh, 0:129], lhsT=sc[:], rhs=VO[:, t, h, 0:129], start=True, stop=False),
                              reads=[sck, "c_VO"], writes=[phk])
                        fw.op("pe", lambda: nc.tensor.matmul(ph[:, hh, 0:129], lhsT=qz0[:, h, :], rhs=c0[:, h, 0:129], start=False, stop=False),
                              reads=[qz0k, c0k], writes=[phk])
                        fw.op("pe", lambda: nc.tensor.matmul(ph[:, hh, 0:129], lhsT=qz1[:, h, :], rhs=c1[:, h, 0:129], start=False, stop=True),
                              reads=[qz1k, c1k], writes=[phk])
                for hp in range(2):
                    ph, phk = phs[hp]
                    dn, dnk = dnr.next()
                    fw.op("dve", lambda: nc.vector.tensor_scalar(out=dn[:, 0:2], in0=ph[:, :, 128], scalar1=-1.0, scalar2=1.0, op0=ALU.mult, op1=ALU.max),
                          reads=[phk], writes=[dnk])
                    fw.op("dve", lambda: nc.vector.tensor_tensor(out=dn[:, 2:4], in0=dn[:, 0:2], in1=ph[:, :, 128], op=ALU.max), reads=[dnk, phk], writes=[dnk])
                    fw.op("dve", lambda: nc.vector.reciprocal(out=dn[:, 4:6], in_=dn[:, 2:4]), reads=[dnk], writes=[dnk])
                    for hh in range(2):
                        h = 2 * hp + hh
                        hkey = f"c_hsum#{lt}#{h}"
                        if (lt, h) not in written:
                            written.add((lt, h))
                            fw.op("dve", lambda: nc.vector.tensor_scalar(out=hsum[:, lt, h, :], in0=ph[:, hh, 0:128], scalar1=dn[:, 4 + hh:5 + hh], scalar2=None, op0=ALU.mult),
                                  reads=[phk, dnk], writes=[hkey])
                        else:
                            fw.op("dve", lambda: nc.vector.scalar_tensor_tensor(out=hsum[:, lt, h, :], in0=ph[:, hh, 0:128], scalar=dn[:, 4 + hh:5 + hh], in1=hsum[:, lt, h, :],
                                                                               op0=ALU.mult, op1=ALU.add), reads=[phk, dnk, hkey], writes=[hkey])

            torder = [list(range(NT)), [1, 0] + list(range(NT - 1, 1, -1))]
            for step in range(NT):
                for d in range(2):
                    process_tile(d, torder[d][step])

    def l1_stage_c2(self, b, hsum, wout):
        nc, fw, I, R = self.nc, self.fw, self.I, self.R
        with ExitStack() as es:
            mt = self.mod_tiles(es, 1, b, want_pre=False, want_post=True)
            g, gk = mt["g_lat"]
            P = {}
            P["xres"] = self.ring(es, "d_xres", [128, D], F32, 2)
            P["junk"] = self.ring(es, "d_junk", [128, D], BF16, 2)
            P["stat"] = self.ring(es, "d_stat", [128, 4], F32, 4)
            P["t2"] = self.ring(es, "d_t2", [128, D], F32, 2)
            hnw = self.sb(es, "d_hnw", [128, 512], F32)
            fw.dma("sp", hnw[:], I["o_head_norm_w"].partition_broadcast(128), writes=["d_hnw"])
            pT = self.ps(es, "d_pT", [128, 4, 128], BF16)
            py = self.ps(es, "d_py", [128, D], F32)
            mor = self.ring(es, "d_mo", [128, 512], F32, 3)
            zmr = self.ring(es, "d_zm", [128, 512], F32, 3)
            cgr = self.ring(es, "d_cg", [128, 4, 128], BF16, 3)
            str_ = self.ring(es, "d_st", [128, 12], F32, 3)
            jr = self.ring(es, "d_j", [128, 128], BF16, 2)
            g1r = self.ring(es, "d_g1", [128, 512], F32, 2)
            hnr = self.ring(es, "d_hn", [128, 4, 128], F32, 2)
            mgr = self.ring(es, "d_mg", [128, 512], BF16, 2)
            mgTr = self.ring(es, "d_mgT", [128, 4, 128], BF16, 3)
            loads = {}
            tails = {}

            def emit_loads(lt):
                ltsl = slice(lt * 128, (lt + 1) * 128)
                mo_, mok = mor.next()
                fw.dma("sp", mo_[:], R["mo1"][b, ltsl, :], reads=["mo1"], writes=[mok])
                zm_, zmk = zmr.next()
                fw.dma("sp", zm_[:], R["z1"][b, ltsl, 512:1024], reads=["z1"], writes=[zmk])
                cg_, cgk = cgr.next()
                fw.dma("sp", cg_[:], R["cgT1"][b].rearrange("c p t -> p c t")[:, :, ltsl], reads=["cgT1"], writes=[cgk])
                loads[lt] = (mo_, mok, zm_, zmk, cg_, cgk)

            def emit_tile(lt):
                mo_, mok, zm_, zmk, cg_, cgk = loads.pop(lt)
                hkeys = [f"c_hsum#{lt}#{h}" for h in range(4)]
                st, stk = str_.next()
                for h in range(4):
                    j_, jk = jr.next()
                    fw.op("act", lambda: nc.scalar.activation(out=j_[:], in_=hsum[:, lt, h, :], func=AF.Square, scale=128 ** -0.5, accum_out=st[:, h:h + 1]),
                          reads=hkeys, writes=[jk, f"{stk}#{h}"])
                fw.op("act", lambda: nc.scalar.activation(out=st[:, 4:8], in_=st[:, 0:4], func=AF.Sqrt, bias=EPS),
                      reads=[f"{stk}#{h}" for h in range(4)], writes=[f"{stk}#s"])
                fw.op("dve", lambda: nc.vector.reciprocal(out=st[:, 8:12], in_=st[:, 4:8]), reads=[f"{stk}#s"], writes=[f"{stk}#r"])
                g1, g1k = g1r.next()
                fw.op("pool", lambda: nc.gpsimd.tensor_tensor(out=g1[:], in0=mo_[:], in1=zm_[:], op=ALU.mult), reads=[mok, zmk], writes=[g1k])
                fw.op("pool", lambda: nc.gpsimd.tensor_tensor(out=g1[:], in0=g1[:], in1=hnw[:], op=ALU.mult), reads=[g1k, "d_hnw"], writes=[g1k])
                hn, hnk = hnr.next()
                fw.op("dve", lambda: nc.vector.tensor_tensor(out=hn[:], in0=hsum[:, lt, :, :], in1=st[:, 8:12].unsqueeze(2).to_broadcast([128, 4, 128]), op=ALU.mult),
                      reads=hkeys + [f"{stk}#r"], writes=[hnk])
                mg, mgk = mgr.next()
                fw.op("dve", lambda: nc.vector.tensor_tensor(out=mg[:], in0=hn[:].rearrange("p h v -> p (h v)"), in1=g1[:], op=ALU.mult),
                      reads=[hnk, g1k], writes=[mgk])
                for c in range(4):
                    fw.op("pe", lambda: nc.tensor.transpose(out=pT[:, c, :], in_=mg[:, c * 128:(c + 1) * 128], identity=self.identb[:]),
                          reads=[mgk, "identb"], writes=["d_pT"])
                mgT, mgTk = mgTr.next()
                fw.op("act", lambda: nc.scalar.copy(out=mgT[:], in_=pT[:]), reads=["d_pT"], writes=[mgTk])
                tails[lt] = (cg_, cgk, mgT, mgTk)

            def emit_tail(lt):
                cg_, cgk, mgT, mgTk = tails.pop(lt)
                for half in range(2):
                    for k in range(8):
                        src, srk = (cg_, cgk) if k < 4 else (mgT, mgTk)
                        fw.op("pe", lambda: nc.tensor.matmul(py[:, half * 512:(half + 1) * 512], lhsT=src[:, k % 4, :],
                                                             rhs=wout[:, k, half * 512:(half + 1) * 512], start=(k == 0), stop=(k == 7)),
                              reads=[srk, "l1_wout"], writes=["d_py"])
                t = lt + 2
                self.post_tile(P, py, "d_py", g, gk, R["x1c"][b, t * 128:(t + 1) * 128, :], ["x1c"],
                               self.out[b, lt * 128:(lt + 1) * 128, :], "out")

            emit_loads(0)
            emit_loads(1)
            emit_tile(0)
            for lt in range(16):
                if lt + 2 < 16:
                    emit_loads(lt + 2)
                if lt + 1 < 16:
                    emit_tile(lt + 1)
                emit_tail(lt)


def make_consts():
    c = np.zeros((11, 128, 128), np.float32)
    p = np.arange(128)[:, None]
    n = np.arange(128)[None, :]
    same = (p // 64) == (n // 64)
    c[C_ID] = (p == n)
    c[C_ONES] = 1.0
    c[C_TRIF] = same & (p <= n)
    c[C_TRIR] = same & (p >= n)
    c[C_BLK] = same
    c[C_SEL0] = (p < 64) & (n >= 0)
    c[C_SEL1] = (p >= 64) & (n >= 0)
    c[C_MBF] = np.where(same & (p <= n), 0.0, NEG)
    c[C_MBR] = np.where(same & (p >= n), 0.0, NEG)
    c[C_MPREV] = np.where(p >= n, 0.0, NEG)
    c[C_MNEXT] = np.where(p <= n, 0.0, NEG)

    def axial(rot_dim):
        rows = S // 64
        row = np.repeat(np.arange(rows), 64).astype(np.float32)
        col = np.tile(np.arange(64), rows).astype(np.float32)
        nf = rot_dim // 4
        inv = (np.float32(10000.0) ** (-np.arange(nf, dtype=np.float32) / np.float32(nf))).astype(np.float32)
        ang = np.concatenate([row[:, None] * inv, col[:, None] * inv], axis=-1).astype(np.float32)
        tab = np.zeros((T, rot_dim), np.float32)
        tab[:LC, :rot_dim // 2] = 1.0
        tab[LC:, :rot_dim // 2] = np.cos(ang)
        tab[LC:, rot_dim // 2:] = np.sin(ang)
        return tab
    return c, axial(64), axial(32)


_CACHE = {}


def get_program(debug=False, stop_after=None):
    key = (debug, stop_after)
    if key not in _CACHE:
        bld = Builder(debug=debug)
        nc = bld.build(stop_after=stop_after)
        _CACHE[key] = (nc, bld)
    return _CACHE[key]


def make_in_maps(inputs):
    c128, ropeA, ropeC = make_consts()
    f = lambda a: np.ascontiguousarray(np.asarray(a, dtype=np.float32))
    shared = {
        "mod_w": f(inputs["mod_w"]), "mod_b": f(inputs["mod_b"]),
        "pre_norm_w": f(inputs["pre_norm_w"]), "post_norm_w": f(inputs["post_norm_w"]),
        "e_w_in": f(inputs["e_w_in"][0]), "e_sink": f(inputs["e_sink"][0]), "e_conv_w": f(inputs["e_conv_w"][0]),
        "e_w_out": f(inputs["e_w_out"][0]), "o_w_in": f(inputs["o_w_in"][0]),
        "o_q_norm_w": f(inputs["o_q_norm_w"][0]), "o_kv_norm_w": f(inputs["o_kv_norm_w"][0]),
        "o_w_uq": f(inputs["o_w_uq"][0]), "o_w_ukv": f(inputs["o_w_ukv"][0]),
        "o_i_bias": f(inputs["o_i_bias"][0]).reshape(8), "o_f_bias": f(inputs["o_f_bias"][0]).reshape(8),
        "o_head_norm_w": f(inputs["o_head_norm_w"][0]), "o_w_out": f(inputs["o_w_out"][0]),
        "c128": c128, "ropeA": ropeA, "ropeC": ropeC,
    }
    x = f(inputs["x"])
    c = f(inputs["c"])
    ctx = f(inputs["ctx"])
    cc = f(inputs["c_ctx"])
    maps = []
    for i in range(NCORES):
        m = dict(shared)
        m["xs"] = x[NB * i:NB * (i + 1)]
        m["ctxs"] = ctx[NB * i:NB * (i + 1)]
        m["cvec"] = np.ascontiguousarray(np.stack([c[NB * i], c[NB * i + 1], cc], axis=0))
        maps.append(m)
    return maps


def kernel(**inputs):
    nc, _ = get_program()
    maps = make_in_maps(inputs)
    res = run_bass_kernel_spmd(nc, maps, core_ids=list(range(NCORES)))
    return np.concatenate([r["out"] for r in res.results], axis=0)
```
